# Optimizing a Trainium2 kernel written in Bass

```python
import jax, jax.numpy as jnp
from jax import lax
import numpy as np

D_MODEL = 1024
BATCH = 8
SEQ = 2048
DEPTH = 4

CHUNK = 64
D_MIX = 2 * D_MODEL

LRU_WIDTH = D_MIX // 4
LRU_BLOCKS = 8
LRU_BLOCK = LRU_WIDTH // LRU_BLOCKS
LRU_C = 8.0
CONV_W = 4

GLA_WIDTH = D_MIX // 4
GLA_HEADS = 4
GLA_DV = GLA_WIDTH // GLA_HEADS
GLA_DK = GLA_DV // 2
GLA_QK = GLA_HEADS * GLA_DK
GLA_RANK = 16
GLA_TAU = 16.0

SSD_WIDTH = D_MIX // 2
SSD_HEADDIM = 64
SSD_HEADS = SSD_WIDTH // SSD_HEADDIM
SSD_GROUPS = 2
SSD_HPG = SSD_HEADS // SSD_GROUPS
SSD_STATE = 128
SSD_CONV_DIM = SSD_WIDTH + 2 * SSD_GROUPS * SSD_STATE

N_EXPERTS = 64
TOP_K = 8
N_EXPERT_GROUPS = 8
EXPERTS_PER_GROUP = N_EXPERTS // N_EXPERT_GROUPS
TOPK_GROUPS = 4
D_EXPERT = 256
ROUTE_SCALE = 2.5

IN_SIZES = (LRU_WIDTH, LRU_WIDTH,
            GLA_QK, GLA_QK, GLA_WIDTH, GLA_WIDTH, GLA_RANK,
            SSD_WIDTH, SSD_CONV_DIM, SSD_HEADS)
D_IN_PROJ = sum(IN_SIZES)

DEEPNORM_ALPHA = (2 * DEPTH) ** 0.25
DEEPNORM_BETA = (8 * DEPTH) ** -0.25
LN_EPS = 1e-5
RMS_EPS = 1e-6

kernel_name = 'hybrid_lru_gla_ssd_moe_deepnorm_adaln'


def layer_norm(x, g, b):
    xf = x.astype(jnp.float32)
    mu = jnp.mean(xf, -1, keepdims=True)
    var = jnp.mean(jnp.square(xf - mu), -1, keepdims=True)
    return ((xf - mu) * lax.rsqrt(var + LN_EPS) * g + b).astype(x.dtype)


def rms_norm(x, g):
    xf = x.astype(jnp.float32)
    return (xf * lax.rsqrt(jnp.mean(jnp.square(xf), -1, keepdims=True) + RMS_EPS) * g).astype(x.dtype)


def causal_depthwise_conv(x, w, b):
    k, ch = w.shape
    y = lax.conv_general_dilated(x, w[:, None, :].astype(x.dtype), window_strides=(1,),
                                 padding=[(k - 1, 0)], dimension_numbers=('NWC', 'WIO', 'NWC'),
                                 feature_group_count=ch)
    return y + b.astype(x.dtype)


def _linear_recurrence_op(earlier, later):
    a1, b1 = earlier
    a2, b2 = later
    return a1 * a2, a2 * b1 + b2


def rglru_group(xb, gate, conv_w, conv_b, w_a, b_a, w_x, b_x, lam):
    bsz, seq, width = xb.shape
    xc = causal_depthwise_conv(xb, conv_w, conv_b)
    blocks = xc.reshape(bsz, seq, LRU_BLOCKS, LRU_BLOCK)
    r = jax.nn.sigmoid(jnp.einsum('bsni,nij->bsnj', blocks, w_a).reshape(bsz, seq, width) + b_a)
    i = jax.nn.sigmoid(jnp.einsum('bsni,nij->bsnj', blocks, w_x).reshape(bsz, seq, width) + b_x)
    log_a = -LRU_C * r.astype(jnp.float32) * jax.nn.softplus(-lam.astype(jnp.float32))
    a = jnp.exp(log_a)
    u = jnp.sqrt(-jnp.expm1(2.0 * log_a)) * (i * xc)
    _, h = lax.associative_scan(_linear_recurrence_op, (a, u), axis=1)
    return (h * jax.nn.gelu(gate)).astype(gate.dtype)


def gla_group(q, k, v, g, r, w_alpha, b_alpha, norm_g):
    bsz, seq, _ = q.shape
    n = seq // CHUNK

    def heads(t, d):
        return t.reshape(bsz, n, CHUNK, GLA_HEADS, d).transpose(0, 3, 1, 2, 4)

    log_alpha = jax.nn.log_sigmoid((r @ w_alpha + b_alpha).astype(jnp.float32)) / GLA_TAU
    q = heads(q, GLA_DK) * (GLA_DK ** -0.5)
    k = heads(k, GLA_DK)
    v = heads(v, GLA_DV)
    cum = jnp.cumsum(heads(log_alpha, GLA_DK), axis=3)
    q_dec = q * jnp.exp(cum)
    k_dec = k * jnp.exp(-cum)
    causal = jnp.tril(jnp.ones((CHUNK, CHUNK), dtype=bool))
    att = jnp.where(causal, jnp.einsum('bhnid,bhnjd->bhnij', q_dec, k_dec), 0.0)
    o_intra = jnp.einsum('bhnij,bhnjv->bhniv', att, v)
    cum_last = cum[:, :, :, -1]
    k_to_end = k * jnp.exp(cum_last[:, :, :, None, :] - cum)
    chunk_kv = jnp.einsum('bhncd,bhncv->bhndv', k_to_end, v)

    def step(state, inp):
        decay, kv = inp
        return decay[..., None] * state + kv, state

    _, s_prev = lax.scan(step, jnp.zeros_like(chunk_kv[:, :, 0]),
                         (jnp.moveaxis(jnp.exp(cum_last), 2, 0), jnp.moveaxis(chunk_kv, 2, 0)))
    o_inter = jnp.einsum('bhnid,bhndv->bhniv', q_dec, jnp.moveaxis(s_prev, 0, 2))
    o = rms_norm(o_intra + o_inter, norm_g)
    o = o.transpose(0, 2, 3, 1, 4).reshape(bsz, seq, GLA_WIDTH)
    return (o * jax.nn.silu(g)).astype(g.dtype)


def ssd_group(z, xbc, dt_raw, conv_w, conv_b, dt_bias, a_log, d_skip, norm_g):
    bsz, seq, _ = z.shape
    n = seq // CHUNK
    xbc = jax.nn.silu(causal_depthwise_conv(xbc, conv_w, conv_b))
    xs, bm, cm = jnp.split(xbc, [SSD_WIDTH, SSD_WIDTH + SSD_GROUPS * SSD_STATE], axis=-1)
    xs = xs.reshape(bsz, n, CHUNK, SSD_GROUPS, SSD_HPG, SSD_HEADDIM)
    bm = bm.reshape(bsz, n, CHUNK, SSD_GROUPS, SSD_STATE)
    cm = cm.reshape(bsz, n, CHUNK, SSD_GROUPS, SSD_STATE)
    dt = jax.nn.softplus((dt_raw + dt_bias).astype(jnp.float32)).reshape(bsz, n, CHUNK, SSD_GROUPS, SSD_HPG)
    a_neg = -jnp.exp(a_log.astype(jnp.float32)).reshape(SSD_GROUPS, SSD_HPG)
    a_cs = jnp.cumsum(dt * a_neg, axis=2)
    a_t = jnp.moveaxis(a_cs, 2, -1)
    causal = jnp.tril(jnp.ones((CHUNK, CHUNK), dtype=bool))
    decay_in = jnp.exp(jnp.where(causal, a_t[..., :, None] - a_t[..., None, :], -jnp.inf))
    cb = jnp.einsum('bnigs,bnjgs->bngij', cm, bm)
    xdt = xs * dt[..., None]
    y_diag = jnp.einsum('bnghij,bnjghp->bnighp', cb[:, :, :, None] * decay_in, xdt)
    decay_to_end = jnp.exp(a_cs[:, :, -1:] - a_cs)
    states = jnp.einsum('bnjgs,bnjghp->bnghps', bm, xdt * decay_to_end[..., None])

    def step(state, inp):
        log_decay, st = inp
        return jnp.exp(log_decay)[..., None, None] * state + st, state

    _, s_prev = lax.scan(step, jnp.zeros_like(states[:, 0]),
                         (jnp.moveaxis(a_cs[:, :, -1], 1, 0), jnp.moveaxis(states, 1, 0)))
    y_off = jnp.einsum('bnigs,bnghps->bnighp', cm, jnp.moveaxis(s_prev, 0, 1)) * jnp.exp(a_cs)[..., None]
    y = y_diag + y_off + xs * d_skip.reshape(SSD_GROUPS, SSD_HPG, 1)
    y = y.reshape(bsz, seq, SSD_WIDTH) * jax.nn.silu(z)
    y = rms_norm(y.reshape(bsz, seq, SSD_GROUPS, SSD_WIDTH // SSD_GROUPS),
                 norm_g.reshape(SSD_GROUPS, SSD_WIDTH // SSD_GROUPS)).reshape(bsz, seq, SSD_WIDTH)
    return y.astype(z.dtype)


def token_mixer(h, w_in, lru_conv_w, lru_conv_b, lru_w_a, lru_b_a, lru_w_x, lru_b_x, lru_lambda,
                gla_w_alpha, gla_b_alpha, gla_norm, ssd_conv_w, ssd_conv_b, ssd_dt_bias, ssd_a_log,
                ssd_d, ssd_norm, w_out):
    proj = h @ w_in
    splits = np.cumsum(IN_SIZES)[:-1].tolist()
    (lru_x, lru_gate, gla_q, gla_k, gla_v, gla_g, gla_r,
     ssd_z, ssd_xbc, ssd_dt) = jnp.split(proj, splits, axis=-1)
    y_lru = rglru_group(lru_x, lru_gate, lru_conv_w, lru_conv_b, lru_w_a, lru_b_a, lru_w_x, lru_b_x, lru_lambda)
    y_gla = gla_group(gla_q, gla_k, gla_v, gla_g, gla_r, gla_w_alpha, gla_b_alpha, gla_norm)
    y_ssd = ssd_group(ssd_z, ssd_xbc, ssd_dt, ssd_conv_w, ssd_conv_b, ssd_dt_bias, ssd_a_log, ssd_d, ssd_norm)
    return jnp.concatenate([y_lru, y_gla, y_ssd], axis=-1) @ w_out


def swiglu(t, w1, w3, w2):
    return (jax.nn.silu(t @ w1) * (t @ w3)) @ w2


def moe_ffn(h, router_w, router_bias, w1, w3, w2, sw1, sw3, sw2):
    bsz, seq, d = h.shape
    t = h.reshape(bsz * seq, d)
    n_tok = t.shape[0]
    rows = jnp.arange(n_tok)[:, None]
    scores = jax.nn.sigmoid((t @ router_w).astype(jnp.float32))
    biased = scores + router_bias.astype(jnp.float32)
    grouped = biased.reshape(n_tok, N_EXPERT_GROUPS, EXPERTS_PER_GROUP)
    group_score = lax.top_k(grouped, 2)[0].sum(-1)
    _, top_groups = lax.top_k(group_score, TOPK_GROUPS)
    group_mask = jnp.zeros((n_tok, N_EXPERT_GROUPS), dtype=bool).at[rows, top_groups].set(True)
    allowed = jnp.repeat(group_mask, EXPERTS_PER_GROUP, axis=1)
    _, top_idx = lax.top_k(jnp.where(allowed, biased, -jnp.inf), TOP_K)
    w = jnp.take_along_axis(scores, top_idx, axis=1)
    w = ROUTE_SCALE * w / jnp.sum(w, -1, keepdims=True)
    gates = jnp.zeros((n_tok, N_EXPERTS), jnp.float32).at[rows, top_idx].set(w).astype(t.dtype)
    y = swiglu(t, sw1, sw3, sw2)
    for gi in range(N_EXPERT_GROUPS):
        sl = slice(gi * EXPERTS_PER_GROUP, (gi + 1) * EXPERTS_PER_GROUP)
        hid = jax.nn.silu(jnp.einsum('td,edf->tef', t, w1[sl])) * jnp.einsum('td,edf->tef', t, w3[sl])
        y = y + jnp.einsum('tef,efd->td', hid * gates[:, sl, None], w2[sl])
    return y.reshape(bsz, seq, d)


def setup_inputs(seed: int = 0) -> dict:
    key = jax.random.key(seed)
    ks = iter(jax.random.split(key, 48))

    def nrm(shape, scale):
        return jax.random.normal(next(ks), shape, jnp.float32) * scale

    def unif(shape, lo, hi):
        return jax.random.uniform(next(ks), shape, jnp.float32, minval=lo, maxval=hi)

    L, D = DEPTH, D_MODEL
    x = nrm((BATCH, SEQ, D), 1.0)
    c = nrm((BATCH, D), 1.0)
    w_ada = nrm((L, D, 6 * D), 0.5 * D ** -0.5)
    b_ada = nrm((L, 6 * D), 0.01)
    w_in = nrm((L, D, D_IN_PROJ), D ** -0.5)
    lru_conv_w = nrm((L, CONV_W, LRU_WIDTH), CONV_W ** -0.5)
    lru_conv_b = nrm((L, LRU_WIDTH), 0.01)
    lru_w_a = nrm((L, LRU_BLOCKS, LRU_BLOCK, LRU_BLOCK), LRU_BLOCK ** -0.5)
    lru_b_a = nrm((L, LRU_WIDTH), 0.01)
    lru_w_x = nrm((L, LRU_BLOCKS, LRU_BLOCK, LRU_BLOCK), LRU_BLOCK ** -0.5)
    lru_b_x = nrm((L, LRU_WIDTH), 0.01)
    p = unif((L, LRU_WIDTH), 0.9, 0.999) ** (1.0 / LRU_C)
    lru_lambda = jnp.log(p) - jnp.log1p(-p)
    gla_w_alpha = nrm((L, GLA_RANK, GLA_QK), GLA_RANK ** -0.5)
    gla_b_alpha = nrm((L, GLA_QK), 0.1)
    gla_norm = 1.0 + nrm((L, GLA_DV), 0.01)
    ssd_conv_w = nrm((L, CONV_W, SSD_CONV_DIM), CONV_W ** -0.5)
    ssd_conv_b = nrm((L, SSD_CONV_DIM), 0.01)
    dt0 = jnp.exp(unif((L, SSD_HEADS), float(np.log(1e-3)), float(np.log(1e-1))))
    ssd_dt_bias = dt0 + jnp.log(-jnp.expm1(-dt0))
    ssd_a_log = jnp.log(unif((L, SSD_HEADS), 1.0, 16.0))
    ssd_d = 1.0 + nrm((L, SSD_HEADS), 0.01)
    ssd_norm = 1.0 + nrm((L, SSD_WIDTH), 0.01)
    w_out = nrm((L, D_MIX, D), DEEPNORM_BETA * D_MIX ** -0.5)
    ln1_g = 1.0 + nrm((L, D), 0.01)
    ln1_b = nrm((L, D), 0.01)
    router_w = nrm((L, D, N_EXPERTS), D ** -0.5)
    router_bias = nrm((L, N_EXPERTS), 0.01)
    exp_w1 = nrm((L, N_EXPERTS, D, D_EXPERT), D ** -0.5)
    exp_w3 = nrm((L, N_EXPERTS, D, D_EXPERT), D ** -0.5)
    exp_w2 = nrm((L, N_EXPERTS, D_EXPERT, D), DEEPNORM_BETA * D_EXPERT ** -0.5)
    shared_w1 = nrm((L, D, D_EXPERT), D ** -0.5)
    shared_w3 = nrm((L, D, D_EXPERT), D ** -0.5)
    shared_w2 = nrm((L, D_EXPERT, D), DEEPNORM_BETA * D_EXPERT ** -0.5)
    ln2_g = 1.0 + nrm((L, D), 0.01)
    ln2_b = nrm((L, D), 0.01)
    return {'x': x, 'c': c, 'w_ada': w_ada, 'b_ada': b_ada, 'w_in': w_in,
            'lru_conv_w': lru_conv_w, 'lru_conv_b': lru_conv_b, 'lru_w_a': lru_w_a, 'lru_b_a': lru_b_a,
            'lru_w_x': lru_w_x, 'lru_b_x': lru_b_x, 'lru_lambda': lru_lambda,
            'gla_w_alpha': gla_w_alpha, 'gla_b_alpha': gla_b_alpha, 'gla_norm': gla_norm,
            'ssd_conv_w': ssd_conv_w, 'ssd_conv_b': ssd_conv_b, 'ssd_dt_bias': ssd_dt_bias,
            'ssd_a_log': ssd_a_log, 'ssd_d': ssd_d, 'ssd_norm': ssd_norm, 'w_out': w_out,
            'ln1_g': ln1_g, 'ln1_b': ln1_b, 'router_w': router_w, 'router_bias': router_bias,
            'exp_w1': exp_w1, 'exp_w3': exp_w3, 'exp_w2': exp_w2,
            'shared_w1': shared_w1, 'shared_w3': shared_w3, 'shared_w2': shared_w2,
            'ln2_g': ln2_g, 'ln2_b': ln2_b}


def reference(x, c, w_ada, b_ada, w_in, lru_conv_w, lru_conv_b, lru_w_a, lru_b_a, lru_w_x, lru_b_x,
              lru_lambda, gla_w_alpha, gla_b_alpha, gla_norm, ssd_conv_w, ssd_conv_b, ssd_dt_bias,
              ssd_a_log, ssd_d, ssd_norm, w_out, ln1_g, ln1_b, router_w, router_bias,
              exp_w1, exp_w3, exp_w2, shared_w1, shared_w3, shared_w2, ln2_g, ln2_b):
    cond = jax.nn.silu(c)
    for l in range(DEPTH):
        mod = cond @ w_ada[l] + b_ada[l]
        sh1, sc1, g1, sh2, sc2, g2 = jnp.split(mod[:, None, :], 6, axis=-1)
        h = x * (1.0 + sc1) + sh1
        mix = token_mixer(h, w_in[l], lru_conv_w[l], lru_conv_b[l], lru_w_a[l], lru_b_a[l], lru_w_x[l],
                          lru_b_x[l], lru_lambda[l], gla_w_alpha[l], gla_b_alpha[l], gla_norm[l],
                          ssd_conv_w[l], ssd_conv_b[l], ssd_dt_bias[l], ssd_a_log[l], ssd_d[l],
                          ssd_norm[l], w_out[l])
        x = layer_norm(DEEPNORM_ALPHA * x + g1 * mix, ln1_g[l], ln1_b[l])
        h = x * (1.0 + sc2) + sh2
        ffn = moe_ffn(h, router_w[l], router_bias[l], exp_w1[l], exp_w3[l], exp_w2[l],
                      shared_w1[l], shared_w3[l], shared_w2[l])
        x = layer_norm(DEEPNORM_ALPHA * x + g2 * ffn, ln2_g[l], ln2_b[l])
    return x
```

```python
from contextlib import ExitStack
import numpy as np
import concourse.bass as bass
import concourse.mybir as mybir
from concourse.bass_utils import run_bass_kernel_spmd

F32 = mybir.dt.float32
BF16 = mybir.dt.bfloat16
AF = mybir.ActivationFunctionType
ALU = mybir.AluOpType
AX = mybir.AxisListType

D = 1024
S = 2048
DEPTH = 4
NE = 64
ALPHA = (2 * DEPTH) ** 0.25
DIN = 5152
NPP = 144
TB = 512
NTB = S // TB


class Prog:
    def __init__(self, nc, stack):
        self.nc = nc
        self.stack = stack
        self.ops = []
        self.last_write = {}
        self.readers = {}
        self.epoch = 0
        self.op_epoch = []
        self.group_open = {}
        self.group_of = {}
        self.nsem = 0
        self.bar = None
        self.xeng = []

    def barrier(self, fn):
        allk = list(set(self.last_write.keys()) | set(k for k, v in self.readers.items() if v))
        oid = self._add("dve", fn, allk, allk)
        self.bar = oid
        self.last_write = {}
        self.readers = {}

    def new_sem(self, name):
        self.nsem += 1
        return self.stack.enter_context(self.nc.semaphore(f"{name}_{self.nsem}"))

    def new_epoch(self):
        self.epoch += 1

    def _add(self, eng, fn, reads, writes, chan=None):
        oid = len(self.ops)
        raw = set()
        oth = set()
        xe = set()
        for k in reads:
            w = self.last_write.get(k)
            if w is not None:
                raw.add(w)
            if isinstance(k, tuple) and k[0] in ("ps", "ps2o"):
                for r in self.readers.get(k, ()):
                    xe.add(r)
        for k in writes:
            w = self.last_write.get(k)
            if w is not None:
                oth.add(w)
            for r in self.readers.get(k, ()):
                oth.add(r)
        if self.bar is not None:
            oth.add(self.bar)
        oth -= raw
        raw.discard(oid)
        oth.discard(oid)
        xe -= raw
        xe -= oth
        xe.discard(oid)
        self.xeng.append(sorted(xe))
        self.ops.append([eng, fn, sorted(raw), sorted(oth), chan])
        self.op_epoch.append(self.epoch)
        for k in reads:
            self.readers.setdefault(k, []).append(oid)
        for k in writes:
            self.last_write[k] = oid
            self.readers[k] = []
        return oid

    def op(self, eng, fn, reads=(), writes=()):
        return self._add(eng, fn, reads, writes)

    def dma(self, chan, out, in_, reads=(), writes=(), eng="sp", more=False, **kw):
        def fn(e, out=out, in_=in_, kw=kw):
            return e.dma_start(out=out, in_=in_, **kw)
        oid = self._add(eng, fn, reads, writes, chan=chan)
        g = self.group_open.get(chan)
        if g is None:
            g = []
            self.group_open[chan] = g
        g.append(oid)
        self.group_of[oid] = g
        if not more:
            self.group_open[chan] = None
        return oid

    def emit(self):
        nc = self.nc
        n = len(self.ops)
        is_dma = [o[4] is not None for o in self.ops]
        need_sig = [False] * n
        deps_of = []
        for i, (eng, fn, raw, oth, chan) in enumerate(self.ops):
            deps = []
            for d in raw:
                if is_dma[d] or is_dma[i] or self.ops[d][0] != eng or eng != "pe":
                    deps.append(d)
            for d in oth:
                if is_dma[d] or is_dma[i] or self.ops[d][0] != eng or eng != "pe":
                    deps.append(d)
            for d in self.xeng[i]:
                if self.ops[d][0] != eng:
                    deps.append(d)
            deps_of.append(deps)
            for d in deps:
                if not is_dma[d]:
                    need_sig[d] = True
        sig = [None] * n
        cur = {}
        chan_sem = {}
        chan_cnt = {}
        for i, (eng, fn, raw, oth, chan) in enumerate(self.ops):
            if is_dma[i]:
                if chan not in chan_sem:
                    chan_sem[chan] = self.new_sem("d")
                    chan_cnt[chan] = 0
                chan_cnt[chan] += 16
                sig[i] = (chan_sem[chan], chan_cnt[chan])
            elif need_sig[i]:
                key = (eng, self.op_epoch[i])
                if key not in cur:
                    cur[key] = [self.new_sem(eng), 0]
                cur[key][1] += 1
                sig[i] = (cur[key][0], cur[key][1])
        for i in range(n):
            if is_dma[i]:
                last = self.group_of[i][-1]
                if last != i:
                    sig[i] = (sig[i][0], sig[last][1])
        streams = {}
        for i, (eng, fn, raw, oth, chan) in enumerate(self.ops):
            streams.setdefault(eng, []).append((i, fn, [sig[d] for d in deps_of[i]]))
        final_dma = [(chan_sem[c], chan_cnt[c]) for c in chan_sem]
        self.n_waits = 0

        def run_stream(e, items, tail):
            waited = {}
            for (i, fn, waits) in items:
                best = {}
                for (s, v) in waits:
                    k = s.num
                    if waited.get(k, 0) >= v:
                        continue
                    if k not in best or best[k][1] < v:
                        best[k] = (s, v)
                for k, (s, v) in best.items():
                    e.wait_ge(s, v)
                    waited[k] = v
                    self.n_waits += 1
                ins = fn(e)
                if is_dma[i]:
                    ins.then_inc(chan_sem[self.ops[i][4]], 16)
                elif sig[i] is not None:
                    ins.then_inc(sig[i][0], 1)
            for (s, v) in tail:
                e.wait_ge(s, v)

        with nc.Block() as block:
            names = {"pe": "tensor", "act": "scalar", "dve": "vector", "pool": "gpsimd", "sp": "sync"}
            for en, attr in names.items():
                items = streams.get(en, [])
                tail = final_dma if en == "sp" else []
                if not items and not tail:
                    continue

                def body(e, items=items, tail=tail):
                    run_stream(e, items, tail)
                getattr(block, attr)(body)


C_ID = 0
C_MASK = 128
C_SUF = 256
C_CH0 = 384
C_CH1 = 512
C_M64 = 640
C_ONESD = 704
C_ONE = 832
C_SEL = 960
C_MASKG = 1984
NCONST = 2240


def build_consts():
    c = np.zeros((128, NCONST), np.float32)
    idx = np.arange(128)
    same = (idx[:, None] // 64) == (idx[None, :] // 64)
    c[:, C_ID:C_ID + 128] = np.eye(128)
    c[:, C_MASK:C_MASK + 128] = same & (idx[:, None] <= idx[None, :])
    c[:, C_SUF:C_SUF + 128] = same & (idx[:, None] > idx[None, :])
    c[:64, C_CH0:C_CH0 + 128] = 1.0
    c[64:, C_CH1:C_CH1 + 128] = 1.0
    j = idx % 64
    c[:, C_M64:C_M64 + 64] = j[:, None] <= np.arange(64)[None, :]
    c[:, C_ONESD:C_ONESD + 128] = 1.0 / 1024.0
    c[:, C_ONE:C_ONE + 128] = 1.0
    for h in range(8):
        c[h, C_SEL + h * 128:C_SEL + (h + 1) * 128] = 1.0
    c[:, C_MASKG:C_MASKG + 128] = c[:, C_MASK:C_MASK + 128] * (-1.0 / 16.0)
    c[:, C_MASKG + 128:C_MASKG + 256] = c[:, C_SUF:C_SUF + 128] * (-1.0 / 16.0)
    return c


PP_LN1G, PP_LN1B, PP_LN2G, PP_LN2B = 0, 8, 16, 24
PP_LCW, PP_LCB, PP_LBA, PP_LBX, PP_LLAM = 32, 48, 52, 56, 60
PP_SCW, PP_SCB, PP_SNORM, PP_GNORM, PP_SD = 64, 112, 124, 132, 133
PR_DTB, PR_ALOG, PR_RB = 0, 16, 32
NPR = 96


def build_program(depth=DEPTH, do_lru=True, do_gla=True, do_ssd=True, do_moe=True, n_exp=NE + 1, debug=False):
    nc = bass.Bass("TRN2", target_bir_lowering=False)
    dr = {}

    def din(name, shape):
        dr[name] = nc.dram_tensor(name, list(shape), F32, kind="ExternalInput").ap()
        return dr[name]

    xT_d = din("xT", [D, S])
    cpc_d = din("cpc", [128, 8])
    consts_d = din("consts", [128, NCONST])
    pp_d = din("pp", [DEPTH, 128, NPP])
    prow_d = din("prow", [DEPTH, NPR])
    wada_d = din("w_ada", [DEPTH, D, 6 * D])
    bada_d = din("b_ada", [DEPTH, 6 * D])
    win_d = din("w_in", [DEPTH, D, DIN])
    wout_d = din("w_out", [DEPTH, 2 * D, D])
    wabd_d = din("lru_wa_bd", [DEPTH, 4, 128, 128])
    wxbd_d = din("lru_wx_bd", [DEPTH, 4, 128, 128])
    walpha_d = din("walpha_ext", [DEPTH, 32, 256])
    rw_d = din("router_w", [DEPTH, D, NE])
    ew1_d = din("exp_w1", [DEPTH, NE, D, 256])
    ew3_d = din("exp_w3", [DEPTH, NE, D, 256])
    ew2_d = din("exp_w2", [DEPTH, NE, 256, D])
    sw1_d = din("shared_w1", [DEPTH, D, 256])
    sw3_d = din("shared_w3", [DEPTH, D, 256])
    sw2_d = din("shared_w2", [DEPTH, 256, D])
    yT_d = nc.dram_tensor("yT", [D, S], F32, kind="ExternalOutput").ap()
    gscr_d = nc.dram_tensor("gscr", [2, NE, S], F32, kind="Internal").ap()
    dbg = {}

    with ExitStack() as st:
        p = Prog(nc, st)

        def sb(name, shape, dt=F32):
            return st.enter_context(nc.sbuf_tensor(name, list(shape), dt))

        xT = sb("xT_sb", [128, 8, S])
        hT = sb("hT_sb", [128, 8, S], BF16)
        consts = sb("consts_sb", [128, NCONST])
        pp = sb("pp_sb", [128, DEPTH, NPP])
        mod = sb("mod_sb", [128, DEPTH, 64])
        cond = sb("cond_sb", [128, 8])
        SCRW = 25300
        scr = sb("scr_sb", [128, SCRW])
        banks = [st.enter_context(nc.psum_tensor(f"bank{i}", [128, 512], F32)) for i in range(8)]

        def PS(i):
            return ("ps", i)

        class Carve:
            def __init__(self):
                self.off = 0

            def get(self, shape, dt=F32):
                n = int(np.prod(shape[1:]))
                words = n if dt == F32 else (n + 1) // 2
                a = scr[:, self.off:self.off + words]
                self.off += words
                assert self.off <= SCRW - 1, self.off
                if dt != F32:
                    a = a.bitcast(dt)
                    if n % 2:
                        a = a[:, 0:n]
                if len(shape) == 3:
                    a = a.rearrange("p (a b) -> p a b", a=shape[1])
                elif len(shape) == 4:
                    a = a.rearrange("p (a b c) -> p a b c", a=shape[1], b=shape[2])
                if shape[0] != 128:
                    a = a[0:shape[0]]
                return a

        ident = consts[:, C_ID:C_ID + 128]
        onesD = consts[:, C_ONESD:C_ONESD + 128]

        def barrier():
            tok = scr[:, SCRW - 1:SCRW]
            p.barrier(lambda e: e.memset(tok, 0.0))

        p.dma("ld0", consts[:], consts_d[:, :], writes=["consts"])
        p.dma("ld1", pp[:], pp_d.rearrange("l p n -> p l n"), writes=["pp"])
        p.dma("ld2", cond[:], cpc_d[:, :], writes=["cond"])
        p.dma("ldx", xT[:], xT_d.rearrange("(c p) t -> p c t", p=128), writes=[("x", c, tb) for c in range(8) for tb in range(NTB)])
        p.op("act", lambda e: e.activation(out=cond[:], in_=cond[:], func=AF.Silu), reads=["cond"], writes=["cond"])

        ADA_BLK = 256
        N_ADA = 6 * D // ADA_BLK
        ada_stg = [None, None]

        def adaln_block(l, blk, cv_bufs, bank):
            stg, brow, mrow = cv_bufs[blk % 2]
            key = ("adastg", blk % 2)
            c0 = blk * ADA_BLK
            nj = ADA_BLK // 128
            p.dma(("adab", blk % 2), brow, bada_d[l:l + 1, c0:c0 + ADA_BLK], writes=[("adabrow", blk % 2)])
            p.dma(("ada", blk % 2), stg, wada_d[l].rearrange("(kc p) f -> p kc f", p=128)[:, :, c0:c0 + ADA_BLK], writes=[key])

            def mm(e, stg=stg):
                ins = None
                for kc in range(8):
                    ins = e.matmul(banks[bank][0:1, 0:ADA_BLK], lhsT=cond[:, kc:kc + 1], rhs=stg[:, kc, :], start=(kc == 0), stop=(kc == 7))
                return ins
            p.op("pe", mm, reads=[key, "cond"], writes=[PS(bank)])
            p.op("dve", lambda e: e.tensor_tensor(out=mrow, in0=banks[bank][0:1, 0:ADA_BLK], in1=brow, op=ALU.add),
                 reads=[PS(bank), ("adabrow", blk % 2)], writes=[("adamrow", blk % 2)])

            def mm2(e):
                ins = None
                for j in range(nj):
                    ins = e.matmul(banks[bank][:, 256 + j:257 + j], lhsT=mrow[0:1, j * 128:(j + 1) * 128], rhs=consts[0:1, C_ONE:C_ONE + 1], start=True, stop=True)
                return ins
            p.op("pe", mm2, reads=[("adamrow", blk % 2), "consts"], writes=[PS(bank)])
            p.op("act", lambda e: e.copy(out=mod[:, l, blk * nj:(blk + 1) * nj], in_=banks[bank][:, 256:256 + nj]), reads=[PS(bank)], writes=[("mod", l)])

        def adaln_finish(l, bank):
            p.op("dve", lambda e: e.tensor_scalar(out=mod[:, l, 48:56], in0=mod[:, l, 8:16], scalar1=1.0, scalar2=1.0 / float(ALPHA), op0=ALU.add, op1=ALU.mult), reads=[("mod", l)], writes=[("modd", l)])
            p.op("dve", lambda e: e.tensor_scalar(out=mod[:, l, 56:64], in0=mod[:, l, 32:40], scalar1=1.0, scalar2=1.0 / float(ALPHA), op0=ALU.add, op1=ALU.mult), reads=[("mod", l)], writes=[("modd2", l)])

        def ada_bufs_alloc(cv):
            return [(cv.get([128, 8, ADA_BLK]), cv.get([1, ADA_BLK]), cv.get([1, ADA_BLK])) for _ in range(2)]

        def MOD(l, j, c):
            if j < 6:
                return mod[:, l, j * 8 + c:j * 8 + c + 1]
            return mod[:, l, 48 + (j - 6) * 8 + c:48 + (j - 6) * 8 + c + 1]

        def PPc(l, col):
            return pp[:, l, col:col + 1]

        modkeys = lambda l: [("mod", l), ("modd", l), ("modd2", l)]

        def modulate(l, which, tb, engs=("dve", "pool")):
            jsc, jsh = (6, 0) if which == 1 else (7, 3)
            for c in range(8):
                eng = engs[c % len(engs)]
                p.op(eng, lambda e, c=c: e.tensor_scalar(out=hT[:, c, tb * TB:(tb + 1) * TB], in0=xT[:, c, tb * TB:(tb + 1) * TB],
                                                         scalar1=MOD(l, jsc, c), scalar2=MOD(l, jsh, c), op0=ALU.mult, op1=ALU.add),
                     reads=[("x", c, tb)] + modkeys(l), writes=[("h", c, tb)])

        def scale_alpha(tb, engs=("pool",)):
            for c in range(8):
                eng = engs[c % len(engs)]
                if eng == "act":
                    p.op(eng, lambda e, c=c: e.mul(out=xT[:, c, tb * TB:(tb + 1) * TB], in_=xT[:, c, tb * TB:(tb + 1) * TB], mul=float(ALPHA)),
                         reads=[("x", c, tb)], writes=[("x", c, tb)])
                else:
                    p.op(eng, lambda e, c=c: e.tensor_scalar_mul(out=xT[:, c, tb * TB:(tb + 1) * TB], in0=xT[:, c, tb * TB:(tb + 1) * TB], scalar1=float(ALPHA)),
                         reads=[("x", c, tb)], writes=[("x", c, tb)])

        def layernorm(l, gcol, bcol, cv, bank_m, bank_q, final=False):
            def GB(col):
                return pp[:, l, col:col + 1] if final else ppA[:, l, col:col + 1]
            sq = [cv.get([128, TB]) for _ in range(2)]
            mean_sbs = [cv.get([128, TB]) for _ in range(2)]
            rstds = [cv.get([128, TB]) for _ in range(2)]
            tmp = [cv.get([128, TB]) for _ in range(2)]
            tmp2 = [cv.get([128, TB]) for _ in range(2)]
            bank_m0, bank_q0 = bank_m, bank_q
            for tb in range(NTB):
                sl = slice(tb * TB, (tb + 1) * TB)
                mean_sb = mean_sbs[tb % 2]
                rstd = rstds[tb % 2]
                bank_m = bank_m0 + 2 * (tb % 2)
                bank_q = bank_q0 + 2 * (tb % 2)
                KM = ("lnmean", tb % 2)
                KR = ("lnrstd", tb % 2)

                def mm_mean(e, sl=sl, bank_m=bank_m):
                    ins = None
                    for c in range(8):
                        ins = e.matmul(banks[bank_m][:, :], lhsT=onesD, rhs=xT[:, c, sl], start=(c == 0), stop=(c == 7))
                    return ins
                p.op("pe", mm_mean, reads=[("x", c, tb) for c in range(8)] + ["consts"], writes=[PS(bank_m)])
                for c in range(8):
                    p.op("act", lambda e, c=c, sl=sl: e.activation(out=sq[c % 2], in_=xT[:, c, sl], func=AF.Square), reads=[("x", c, tb)], writes=[("lnsq", c % 2)])
                    p.op("pe", lambda e, c=c, bank_q=bank_q: e.matmul(banks[bank_q][:, :], lhsT=onesD, rhs=sq[c % 2], start=(c == 0), stop=(c == 7)),
                         reads=[("lnsq", c % 2), "consts"], writes=[PS(bank_q)])
                p.op("act", lambda e, mean_sb=mean_sb, bank_m=bank_m: e.copy(out=mean_sb, in_=banks[bank_m][:, :]), reads=[PS(bank_m)], writes=[KM])
                p.op("dve", lambda e, rstd=rstd, mean_sb=mean_sb: e.tensor_tensor(out=rstd, in0=mean_sb, in1=mean_sb, op=ALU.mult), reads=[KM], writes=[KR])
                p.op("dve", lambda e, rstd=rstd, bank_q=bank_q: e.tensor_tensor(out=rstd, in0=banks[bank_q][:, :], in1=rstd, op=ALU.subtract), reads=[PS(bank_q), KR], writes=[KR])
                p.op("act", lambda e, rstd=rstd: e.activation(out=rstd, in_=rstd, func=AF.Sqrt, bias=LNEPS[:, 0:1]), reads=[KR, "lneps"], writes=[KR])
                p.op("dve", lambda e, rstd=rstd: e.reciprocal(out=rstd, in_=rstd), reads=[KR], writes=[KR])
                for c in range(8):
                    k = c % 2
                    p.op("dve", lambda e, c=c, k=k, sl=sl, mean_sb=mean_sb: e.tensor_tensor(out=tmp[k], in0=xT[:, c, sl], in1=mean_sb, op=ALU.subtract),
                         reads=[("x", c, tb), KM], writes=[("lnt", k)])
                    p.op("pool", lambda e, k=k, rstd=rstd: e.tensor_tensor(out=tmp2[k], in0=tmp[k], in1=rstd, op=ALU.mult), reads=[("lnt", k), KR], writes=[("lnt2", k)])
                    p.op("act", lambda e, c=c, k=k, sl=sl: e.activation(out=xT[:, c, sl], in_=tmp2[k], func=AF.Identity, scale=GB(gcol + c), bias=GB(bcol + c)),
                         reads=[("lnt2", k), "pp", "ppA"], writes=[("x", c, tb)])

        ppA = sb("ppA_sb", [128, DEPTH, 32])
        p.op("dve", lambda e: e.tensor_scalar_mul(out=ppA[:], in0=pp[:, :, 0:32], scalar1=float(ALPHA)), reads=["pp"], writes=["ppA"])
        LNEPS = sb("lneps_sb", [128, 4])
        p.op("pool", lambda e: e.memset(LNEPS[:, 0:1], 1e-5), writes=["lneps"])
        p.op("pool", lambda e: e.memset(LNEPS[:, 1:2], 1e-6), reads=[], writes=["lneps"])
        p.op("pool", lambda e: e.memset(LNEPS[:, 2:3], 1.0), reads=[], writes=["lneps"])

        def router(l, cv, bank_l, bank_t):
            NI = 4
            rw = cv.get([128, 8, NE])
            rb = cv.get([128, NE])
            gT = cv.get([64, S])
            h32s = [cv.get([128, 8, 128]) for _ in range(NI)]
            Ws = [{n: cv.get([128, 64]) for n in ("sc", "bi", "eq", "b2", "mk", "sel", "gw", "gates")} for _ in range(NI)]
            Sms = [{n: cv.get([128, 8]) for n in ("m1", "m2", "gs", "t8", "gsel", "goff", "t8e")} for _ in range(NI)]
            s1s = [cv.get([128, 2]) for _ in range(NI)]
            p.dma("rw", rw, rw_d[l].rearrange("(kc p) e -> p kc e", p=128), writes=["rw"])
            p.dma("rb", rb, prow_d[l:l + 1, PR_RB:PR_RB + NE].partition_broadcast(128), writes=["rb"])
            g3 = lambda a: a.rearrange("p (g k) -> p g k", k=8)
            b3 = lambda a: a.unsqueeze(2).to_broadcast([128, 8, 8])

            def tile_ops(tt):
                j = tt % NI
                tb = tt // 4
                tsl = slice(tt * 128, (tt + 1) * 128)
                h32, W, Sm, s1 = h32s[j], Ws[j], Sms[j], s1s[j]
                bl, bt = j, 4 + j
                K = lambda n: (n, j)
                ops = []
                A = lambda eng, fn, r, w: ops.append((eng, fn, r, w))
                for c in range(8):
                    eng = ("dve", "pool")[c % 2]
                    A(eng, lambda e, c=c: e.tensor_scalar(out=h32[:, c, :], in0=xT[:, c, tsl], scalar1=MOD(l, 7, c), scalar2=MOD(l, 3, c), op0=ALU.mult, op1=ALU.add),
                      [("x", c, tb)] + modkeys(l), [("h32", j, c)])

                def mm(e):
                    ins = None
                    for c in range(8):
                        ins = e.matmul(banks[bl][:, 0:NE], lhsT=h32[:, c, :], rhs=rw[:, c, :], start=(c == 0), stop=(c == 7))
                    return ins
                A("pe", mm, [("h32", j, c) for c in range(8)] + ["rw"], [PS(bl)])
                A("act", lambda e: e.activation(out=W["sc"], in_=banks[bl][:, 0:NE], func=AF.Sigmoid), [PS(bl)], [K("r_sc")])
                V = lambda fn, r, w: A("dve", fn, r, w)
                V(lambda e: e.tensor_tensor(out=W["bi"], in0=W["sc"], in1=rb, op=ALU.add), [K("r_sc"), "rb"], [K("r_bi")])
                V(lambda e: e.tensor_reduce(out=Sm["m1"], in_=g3(W["bi"]), axis=AX.X, op=ALU.max), [K("r_bi")], [K("r_m1")])
                V(lambda e: e.tensor_tensor(out=g3(W["eq"]), in0=g3(W["bi"]), in1=b3(Sm["m1"]), op=ALU.is_equal), [K("r_bi"), K("r_m1")], [K("r_eq")])
                V(lambda e: e.scalar_tensor_tensor(out=W["b2"], in0=W["eq"], scalar=-10.0, in1=W["bi"], op0=ALU.mult, op1=ALU.add), [K("r_eq"), K("r_bi")], [K("r_b2")])
                V(lambda e: e.tensor_reduce(out=Sm["m2"], in_=g3(W["b2"]), axis=AX.X, op=ALU.max), [K("r_b2")], [K("r_m2")])
                V(lambda e: e.tensor_tensor(out=Sm["gs"], in0=Sm["m1"], in1=Sm["m2"], op=ALU.add), [K("r_m1"), K("r_m2")], [K("r_gs")])
                V(lambda e: e.max(out=Sm["t8"], in_=Sm["gs"]), [K("r_gs")], [K("r_t8")])
                V(lambda e: e.tensor_scalar(out=Sm["gsel"], in0=Sm["gs"], scalar1=Sm["t8"][:, 3:4], scalar2=None, op0=ALU.is_ge), [K("r_gs"), K("r_t8")], [K("r_gsel")])
                V(lambda e: e.tensor_scalar(out=Sm["goff"], in0=Sm["gsel"], scalar1=10.0, scalar2=-10.0, op0=ALU.mult, op1=ALU.add), [K("r_gsel")], [K("r_goff")])
                V(lambda e: e.tensor_tensor(out=g3(W["mk"]), in0=g3(W["bi"]), in1=b3(Sm["gsel"]), op=ALU.mult), [K("r_bi"), K("r_gsel")], [K("r_mk")])
                V(lambda e: e.tensor_tensor(out=g3(W["mk"]), in0=g3(W["mk"]), in1=b3(Sm["goff"]), op=ALU.add), [K("r_mk"), K("r_goff")], [K("r_mk")])
                V(lambda e: e.max(out=Sm["t8e"], in_=W["mk"]), [K("r_mk")], [K("r_t8e")])
                V(lambda e: e.tensor_scalar(out=W["sel"], in0=W["mk"], scalar1=Sm["t8e"][:, 7:8], scalar2=None, op0=ALU.is_ge), [K("r_mk"), K("r_t8e")], [K("r_sel")])
                V(lambda e: e.tensor_tensor(out=W["gw"], in0=W["sel"], in1=W["sc"], op=ALU.mult), [K("r_sel"), K("r_sc")], [K("r_gw")])
                V(lambda e: e.tensor_reduce(out=s1[:, 0:1], in_=W["gw"], axis=AX.X, op=ALU.add), [K("r_gw")], [K("r_s1")])
                V(lambda e: e.reciprocal(out=s1[:, 1:2], in_=s1[:, 0:1]), [K("r_s1")], [K("r_s2")])
                V(lambda e: e.tensor_scalar(out=W["gates"], in0=W["gw"], scalar1=s1[:, 1:2], scalar2=2.5, op0=ALU.mult, op1=ALU.mult), [K("r_gw"), K("r_s2")], [K("r_gates")])
                A("pe", lambda e: e.transpose(banks[bt][0:64, 0:128], W["gates"], ident), [K("r_gates"), "consts"], [PS(bt)])
                A("act", lambda e: e.copy(out=gT[:, tsl], in_=banks[bt][0:64, 0:128]), [PS(bt)], [("gT", tt)])
                return ops

            for g0 in range(0, S // 128, NI):
                lists = [tile_ops(tt) for tt in range(g0, g0 + NI)]
                for k in range(len(lists[0])):
                    for ol in lists:
                        eng, fn, r, w = ol[k]
                        p.op(eng, fn, reads=r, writes=w)
            p.dma("gst", gscr_d[l % 2], gT, reads=[("gT", tt) for tt in range(S // 128)], writes=[("gscr", l % 2)])

        def moe(l, cv, hooks):
            stg = {n: cv.get([128, 8, 256]) for n in ("w1", "w3")}
            stg["w2"] = cv.get([128, 2, D])
            wbf = [{"w1": cv.get([128, 8, 256], BF16), "w3": cv.get([128, 8, 256], BF16), "w2": cv.get([128, 2, D], BF16)} for _ in range(2)]
            gbc = [cv.get([128, S]) for _ in range(2)]
            sS = [[cv.get([128, TB], BF16) for f in range(2)] for _ in range(2)]
            tS = [[cv.get([128, TB], BF16) for f in range(2)] for _ in range(2)]
            hid = [[cv.get([128, TB], BF16) for f in range(2)] for _ in range(2)]
            steps = [(e, tb) for e in range(n_exp) for tb in range(NTB)]

            def load(e):
                sl = e % 2
                if e < NE:
                    srcs = {"w1": ew1_d[l, e], "w3": ew3_d[l, e], "w2": ew2_d[l, e]}
                else:
                    srcs = {"w1": sw1_d[l], "w3": sw3_d[l], "w2": sw2_d[l]}
                for n in ("w1", "w3", "w2"):
                    pat = "(kc p) f -> p kc f"
                    p.dma(("wst", n), stg[n], srcs[n].rearrange(pat, p=128), writes=[("stg", n)])
                if e < NE:
                    p.dma(("gbc", sl), gbc[sl], gscr_d[l % 2, e:e + 1, :].partition_broadcast(128), reads=[("gscr", l % 2)], writes=[("gbc", sl)])

            def cast(e):
                sl = e % 2
                for n in ("w1", "w3", "w2"):
                    if n == "w2":
                        parts = [(slice(0, 1), "act"), (slice(1, 2), "pool")]
                    else:
                        parts = [(slice(0, 3), "act"), (slice(3, 8), "pool")]
                    for (ps_, ce) in parts:
                        if ce == "act":
                            p.op("act", lambda e_, n=n, sl=sl, ps_=ps_: e_.copy(out=wbf[sl][n][:, ps_, :], in_=stg[n][:, ps_, :]), reads=[("stg", n)], writes=[("wbf", sl, n, ce)])
                        else:
                            p.op("pool", lambda e_, n=n, sl=sl, ps_=ps_: e_.tensor_copy(out=wbf[sl][n][:, ps_, :], in_=stg[n][:, ps_, :]), reads=[("stg", n)], writes=[("wbf", sl, n, ce)])

            def up(i, f):
                e, tb = steps[i]
                sl = e % 2
                for wi, n in enumerate(("w1", "w3")):
                    bk = f * 2 + wi

                    def mm(e_, n=n, bk=bk, sl=sl, tb=tb, f=f):
                        ins = None
                        for kc in range(8):
                            ins = e_.matmul(banks[bk][:, :], lhsT=wbf[sl][n][:, kc, f * 128:(f + 1) * 128], rhs=hT[:, kc, tb * TB:(tb + 1) * TB], start=(kc == 0), stop=(kc == 7))
                        return ins
                    p.op("pe", mm, reads=[("wbf", sl, n, "act"), ("wbf", sl, n, "pool")] + [("h", kc, tb) for kc in range(8)], writes=[PS(bk)])

            def gating(i, f):
                e, tb = steps[i]
                sl = e % 2
                par = i % 2
                p.op("act", lambda e_: e_.activation(out=sS[par][f], in_=banks[f * 2][:, :], func=AF.Silu), reads=[PS(f * 2)], writes=[("sS", par, f)])
                if e < NE:
                    p.op("dve", lambda e_: e_.tensor_tensor(out=tS[par][f], in0=banks[f * 2 + 1][:, :], in1=gbc[sl][:, tb * TB:(tb + 1) * TB], op=ALU.mult),
                         reads=[PS(f * 2 + 1), ("gbc", sl)], writes=[("tS", par, f)])
                    p.op("dve", lambda e_: e_.tensor_tensor(out=hid[par][f], in0=sS[par][f], in1=tS[par][f], op=ALU.mult),
                         reads=[("sS", par, f), ("tS", par, f)], writes=[("hid", par, f)])
                else:
                    p.op("dve", lambda e_: e_.tensor_tensor(out=hid[par][f], in0=banks[f * 2 + 1][:, :], in1=sS[par][f], op=ALU.mult),
                         reads=[PS(f * 2 + 1), ("sS", par, f)], writes=[("hid", par, f)])

            def down(i, dh):
                e, tb = steps[i]
                sl = e % 2
                par = i % 2
                for dq in range(4):
                    d = dh * 4 + dq
                    bk = 4 + dq

                    def mm(e_, d=d, bk=bk):
                        ins = None
                        for f in range(2):
                            ins = e_.matmul(banks[bk][:, :], lhsT=wbf[sl]["w2"][:, f, d * 128:(d + 1) * 128], rhs=hid[par][f], start=(f == 0), stop=(f == 1))
                        return ins
                    p.op("pe", mm, reads=[("wbf", sl, "w2", "act"), ("wbf", sl, "w2", "pool"), ("hid", par, 0), ("hid", par, 1)], writes=[PS(bk)])
                    p.op("dve", lambda e_, d=d, bk=bk: e_.scalar_tensor_tensor(out=xT[:, d, tb * TB:(tb + 1) * TB], in0=banks[bk][:, :], scalar=MOD(l, 5, d), in1=xT[:, d, tb * TB:(tb + 1) * TB], op0=ALU.mult, op1=ALU.add),
                         reads=[PS(bk), ("x", d, tb)] + modkeys(l), writes=[("x", d, tb)])

            load(0)
            cast(0)
            if n_exp > 1:
                load(1)
                cast(1)
            up(0, 0)
            up(0, 1)
            gating(0, 0)
            gating(0, 1)
            for i in range(len(steps)):
                e, tb = steps[i]
                if tb == 0 and i > 0 and e + 1 < n_exp:
                    load(e + 1)
                if tb == 2 and e > 0 and e + 1 < n_exp:
                    cast(e + 1)
                if tb == 1 and e in hooks:
                    hooks[e]()
                nxt = i + 1 < len(steps)
                if nxt:
                    up(i + 1, 0)
                down(i, 0)
                if nxt:
                    gating(i + 1, 0)
                    up(i + 1, 1)
                down(i, 1)
                if nxt:
                    gating(i + 1, 1)


        def ssd_units(l, cv, mark, load_win, load_wout, proj_fm, proj_tm, out_proj, conv_silu, yblk, identb, ones512b):
            cv.off = mark
            dtb = cv.get([128, 16]); alog = cv.get([128, 16]); aneg = cv.get([128, 16])
            cbuf = [cv.get([128, 3 + TB]) for _ in range(6)]
            ctmp = cv.get([128, TB])
            xs = [cv.get([128, TB], BF16) for _ in range(4)]
            BT = cv.get([128, TB], BF16); CT = cv.get([128, TB], BF16)
            sz = [cv.get([128, TB], BF16) for _ in range(4)]
            yg = cv.get([128, 4, TB])
            dt_tm = cv.get([128, 8]); dA = cv.get([128, 8]); acs = cv.get([128, 8]); dte = cv.get([128, 8])
            w2 = cv.get([128, 8]); draw = cv.get([128, 8]); ex = cv.get([128, 8])
            dAb = cv.get([128, 8, 128])
            L = dAb
            Btmz = [cv.get([128, 128], BF16) for _ in range(2)]
            decbc = cv.get([128, 2, 8])
            eD = cv.get([128, 8, 128], BF16)
            cbm = cv.get([128, 128])
            MT = cv.get([128, 8, 128], BF16)
            CTs = cv.get([128, 8, 128], BF16)
            xdt = cv.get([128, 8, 64], BF16); xw = cv.get([128, 8, 64], BF16)
            Btm = cv.get([128, 128], BF16)
            S32 = cv.get([128, 8, 64])
            Sbf = [cv.get([128, 8, 64], BF16) for _ in range(2)]
            sqb = cv.get([128, TB], BF16); rs = ctmp
            b7 = banks[7][:, :].bitcast(BF16)
            MASK = consts[:, C_MASK:C_MASK + 128]
            SUF = consts[:, C_SUF:C_SUF + 128]
            p.dma("dtb", dtb, prow_d[l:l + 1, PR_DTB:PR_DTB + 16].partition_broadcast(128), writes=["dtb"])
            p.dma("alog", alog, prow_d[l:l + 1, PR_ALOG:PR_ALOG + 16].partition_broadcast(128), writes=["alog"])
            p.op("act", lambda e: e.activation(out=aneg, in_=alog, func=AF.Exp), reads=["alog"], writes=["aneg"])
            p.op("dve", lambda e: e.tensor_scalar_mul(out=aneg, in0=aneg, scalar1=-1.0), reads=["aneg"], writes=["aneg"])
            for g in range(2):
                load_win(3600 + g * 512, 512, 0)
                load_win(2576 + g * 512, 512, 512)
                load_win(4624 + g * 128, 128, 1024)
                load_win(4880 + g * 128, 128, 1152)
                load_win(5136 + g * 8, 8, 1280)
                load_wout(8 + g * 4, 4)
                for j6 in range(6):
                    p.op("pool", lambda e, j6=j6: e.memset(cbuf[j6][:, 0:3], 0.0), writes=[("cbuf", j6)])
                p.op("pool", lambda e: e.memset(S32, 0.0), writes=["s_S32"])
                p.op("pool", lambda e: e.memset(Sbf[0], 0.0), writes=[("s_Sbf", 0)])
                par = 0
                gs = slice(g * 8, g * 8 + 8)
                for tb in range(NTB):
                    for j6 in range(6):
                        off = j6 * 128 if j6 < 4 else (1024 if j6 == 4 else 1152)
                        jc = g * 4 + j6 if j6 < 4 else (8 + g if j6 == 4 else 10 + g)
                        bank = j6 % 2
                        proj_fm(off, 128, tb, bank)
                        dst = xs[j6] if j6 < 4 else (BT if j6 == 4 else CT)
                        conv_silu(cbuf[j6], ("cbuf", j6), tb, bank, PP_SCW + jc * 4, PP_SCB + jc, ctmp, "s_ctmp", dst, ("s_fm", j6), AF.Silu, eng="dve")
                    for q in range(4):
                        proj_fm(512 + q * 128, 128, tb, q % 2)
                        p.op("act", lambda e, q=q: e.activation(out=sz[q], in_=banks[q % 2][:, :], func=AF.Silu), reads=[PS(q % 2)], writes=[("s_sz", q)])
                    for tt in range(4):
                        tsl = slice(tt * 128, (tt + 1) * 128)
                        proj_tm(1280, 8, tb, tt, 5)
                        p.op("dve", lambda e, gs=gs: e.tensor_tensor(out=draw, in0=banks[5][:, 0:8], in1=dtb[:, gs], op=ALU.add), reads=[PS(5), "dtb"], writes=["s_draw"])
                        p.op("act", lambda e: e.activation(out=ex, in_=draw, func=AF.Exp), reads=["s_draw"], writes=["s_ex"])
                        p.op("act", lambda e: e.activation(out=dt_tm, in_=ex, func=AF.Ln, bias=LNEPS[:, 2:3]), reads=["s_ex", "lneps"], writes=["s_dt"])
                        p.op("dve", lambda e, gs=gs: e.tensor_tensor(out=dA, in0=dt_tm, in1=aneg[:, gs], op=ALU.mult), reads=["s_dt", "aneg"], writes=["s_dA"])

                        def mm5(e):
                            e.matmul(banks[5][:, 8:16], lhsT=MASK, rhs=dA, start=True, stop=True)
                            e.matmul(banks[5][:, 16:24], lhsT=SUF, rhs=dA, start=True, stop=True)
                            e.matmul(banks[5][:, 160:168], lhsT=consts[:, C_CH0:C_CH0 + 128], rhs=dA, start=True, stop=True)
                            return e.matmul(banks[5][:, 168:176], lhsT=consts[:, C_CH1:C_CH1 + 128], rhs=dA, start=True, stop=True)
                        p.op("pe", mm5, reads=["s_dA", "consts"], writes=[PS(5)])
                        p.op("act", lambda e: e.copy(out=acs, in_=banks[5][:, 8:16]), reads=[PS(5)], writes=["s_acs"])
                        p.op("act", lambda e: e.activation(out=dte, in_=banks[5][:, 16:24], func=AF.Exp), reads=[PS(5)], writes=["s_dte"])
                        p.op("pool", lambda e: e.tensor_copy(out=dAb, in_=dA.unsqueeze(2).to_broadcast([128, 8, 128])), reads=["s_dA"], writes=["s_dAb", ("s_L", 0), ("s_L", 1), "s_Lm", "s_Le"])
                        p.op("act", lambda e: e.activation(out=decbc, in_=banks[5][:, 160:176].rearrange("p (a b) -> p a b", a=2), func=AF.Exp), reads=[PS(5)], writes=["s_dec"])
                        p.op("dve", lambda e: e.tensor_tensor(out=w2, in0=dt_tm, in1=dte, op=ALU.mult), reads=["s_dt", "s_dte"], writes=["s_w2"])

                        def mmD(e):
                            ins = None
                            for h in range(8):
                                ins = e.matmul(banks[2 + h // 4][:, (h % 4) * 128:(h % 4 + 1) * 128], lhsT=dAb[:, h, :], rhs=MASK, start=True, stop=True)
                            return ins
                        p.op("pe", mmD, reads=["s_dAb", "consts"], writes=[PS(2), PS(3)])
                        for k in range(2):
                            p.op("dve", lambda e, k=k: e.tensor_tensor(out=L[:, 4 * k:4 * k + 4, :], in0=banks[2 + k][:, :].rearrange("p (a b) -> p a b", a=4),
                                                                       in1=acs[:, 4 * k:4 * k + 4].unsqueeze(2).to_broadcast([128, 4, 128]), op=ALU.subtract),
                                 reads=[PS(2 + k), "s_acs"], writes=[("s_L", k)])
                            p.op("act", lambda e, k=k: e.activation(out=eD[:, 4 * k:4 * k + 4, :], in_=banks[2 + k][:, :].rearrange("p (a b) -> p a b", a=4), func=AF.Exp), reads=[PS(2 + k)], writes=[("s_eD", k)])
                        p.op("pool", lambda e: e.tensor_scalar_min(out=L, in0=L, scalar1=0.0), reads=[("s_L", 0), ("s_L", 1)], writes=["s_Lm"])
                        p.op("act", lambda e: e.activation(out=L, in_=L, func=AF.Exp), reads=["s_Lm"], writes=["s_Le"])
                        p.op("pe", lambda e, tsl=tsl: e.matmul(banks[5][:, 256:384], lhsT=BT[:, tsl], rhs=CT[:, tsl], start=True, stop=True), reads=[("s_fm", 4), ("s_fm", 5)], writes=[PS(5)])
                        p.op("dve", lambda e: e.tensor_tensor(out=cbm, in0=banks[5][:, 256:384], in1=MASK, op=ALU.mult), reads=[PS(5), "consts"], writes=["s_cbm"])
                        p.op("dve", lambda e: e.tensor_tensor(out=MT, in0=L, in1=cbm.unsqueeze(1).to_broadcast([128, 8, 128]), op=ALU.mult), reads=["s_Le", "s_cbm"], writes=["s_MT"])
                        p.op("pool", lambda e, tsl=tsl: e.tensor_tensor(out=CTs, in0=eD, in1=CT[:, tsl].unsqueeze(1).to_broadcast([128, 8, 128]), op=ALU.mult), reads=[("s_eD", 0), ("s_eD", 1), ("s_fm", 5)], writes=["s_CTs"])

                        def mmT(e, tsl=tsl):
                            for q in range(4):
                                e.transpose(b7[:, q * 128:(q + 1) * 128], xs[q][:, tsl], identb)
                            return e.transpose(b7[:, 512:640], BT[:, tsl], identb)
                        p.op("pe", mmT, reads=[("s_fm", j) for j in range(5)] + ["identb"], writes=[PS(7)])
                        xtm = b7[:, 0:512].rearrange("p (h k) -> p h k", h=8)
                        p.op("dve", lambda e: e.tensor_tensor(out=xdt, in0=xtm, in1=dt_tm.unsqueeze(2).to_broadcast([128, 8, 64]), op=ALU.mult), reads=[PS(7), "s_dt"], writes=["s_xdt"])
                        p.op("dve", lambda e: e.tensor_tensor(out=xw, in0=xtm, in1=w2.unsqueeze(2).to_broadcast([128, 8, 64]), op=ALU.mult), reads=[PS(7), "s_w2"], writes=["s_xw"])
                        p.op("act", lambda e: e.copy(out=Btm, in_=b7[:, 512:640]), reads=[PS(7)], writes=["s_Btm"])
                        for half in range(2):
                            p.op("pool", lambda e, half=half: e.tensor_scalar_mul(out=Btmz[half], in0=Btm, scalar1=consts[:, (C_CH0, C_CH1)[half]:(C_CH0, C_CH1)[half] + 1]), reads=["s_Btm", "consts"], writes=[("s_Btmz", half)])

                        def mmY(e):
                            ins = None
                            for h in range(8):
                                q, hq = h // 2, h % 2
                                ins = e.matmul(banks[4][hq * 64:(hq + 1) * 64, q * 128:(q + 1) * 128], lhsT=xdt[:, h, :], rhs=MT[:, h, :], start=True, stop=True)
                            return ins
                        p.op("pe", mmY, reads=["s_xdt", "s_MT"], writes=[PS(4)])
                        for half in range(2):
                            hs = slice(half * 64, (half + 1) * 64)

                            def mmO(e, half=half, par=par):
                                ins = None
                                for h in range(8):
                                    q, hq = h // 2, h % 2
                                    ins = e.matmul(banks[2][hq * 64:(hq + 1) * 64, q * 128 + half * 64:q * 128 + half * 64 + 64], lhsT=Sbf[par][:, h, :], rhs=CTs[:, h, half * 64:(half + 1) * 64], start=True, stop=True)
                                return ins
                            p.op("pe", mmO, reads=[("s_Sbf", par), "s_CTs"], writes=[("ps2o", half)] + ([PS(2)] if half == 0 else []))
                            p.op("pe", lambda e, half=half: e.matmul(banks[6][:, :], lhsT=Btmz[half], rhs=xw.rearrange("p h k -> p (h k)"), start=True, stop=True), reads=[("s_Btmz", half), "s_xw"], writes=[PS(6)])
                            p.op("dve", lambda e, half=half: e.tensor_tensor(out=S32, in0=S32, in1=decbc[:, half, :].unsqueeze(2).to_broadcast([128, 8, 64]), op=ALU.mult), reads=["s_S32", "s_dec"], writes=["s_S32"])
                            p.op("dve", lambda e: e.tensor_tensor(out=S32, in0=S32, in1=banks[6][:, :].rearrange("p (h k) -> p h k", h=8), op=ALU.add), reads=["s_S32", PS(6)], writes=["s_S32"])
                            p.op("act", lambda e, par=par: e.copy(out=Sbf[1 - par], in_=S32), reads=["s_S32"], writes=[("s_Sbf", 1 - par)])
                            par = 1 - par
                        for q in range(4):
                            p.op("dve", lambda e, q=q, tsl=tsl, g=g: e.scalar_tensor_tensor(out=yg[:, q, tsl], in0=xs[q][:, tsl], scalar=PPc(l, PP_SD + g * 4 + q), in1=banks[4][:, q * 128:(q + 1) * 128], op0=ALU.mult, op1=ALU.add),
                                 reads=[("s_fm", q), PS(4), "pp"], writes=[("s_yg", q)])
                            p.op("dve", lambda e, q=q, tsl=tsl: e.tensor_tensor(out=yg[:, q, tsl], in0=yg[:, q, tsl], in1=banks[2][:, q * 128:(q + 1) * 128], op=ALU.add),
                                 reads=[("s_yg", q), PS(2), ("ps2o", 0), ("ps2o", 1)], writes=[("s_yg", q)])
                    for q in range(4):
                        p.op("pool", lambda e, q=q: e.tensor_tensor(out=yg[:, q, :], in0=yg[:, q, :], in1=sz[q], op=ALU.mult), reads=[("s_yg", q), ("s_sz", q)], writes=[("s_yg", q)])
                    for q in range(4):
                        p.op("act", lambda e, q=q: e.activation(out=sqb, in_=yg[:, q, :], func=AF.Square), reads=[("s_yg", q)], writes=["s_sqb"])
                        p.op("pe", lambda e, q=q: e.matmul(banks[5][:, :], lhsT=ones512b, rhs=sqb, start=(q == 0), stop=(q == 3)), reads=["s_sqb", "ones512b"], writes=[PS(5)])
                    p.op("act", lambda e: e.activation(out=rs, in_=banks[5][:, :], func=AF.Sqrt, bias=LNEPS[:, 1:2]), reads=[PS(5), "lneps"], writes=["s_ctmp"])
                    p.op("dve", lambda e: e.reciprocal(out=rs, in_=rs), reads=["s_ctmp"], writes=["s_ctmp"])
                    for q in range(4):
                        p.op("dve", lambda e, q=q, g=g: e.scalar_tensor_tensor(out=yblk[:, q, :], in0=yg[:, q, :], scalar=PPc(l, PP_SNORM + g * 4 + q), in1=rs, op0=ALU.mult, op1=ALU.mult),
                             reads=[("s_yg", q), "s_ctmp", "pp"], writes=[("yblk", q)])
                    out_proj(tb, 4, [0, 1])

        def mixer(l):
            cv = Carve()
            wst = [cv.get([128, 8, 128]) for _ in range(2)]
            wunit = cv.get([128, 8, 1408], BF16)
            woutst = [cv.get([128, D])] * 2
            wout = cv.get([128, 4, D], BF16)
            yblk = cv.get([128, 4, TB], BF16)
            identb = cv.get([128, 128], BF16)
            ones128b = cv.get([128, 128], BF16)
            ones512b = cv.get([128, 128], BF16)
            p.op("act", lambda e: e.copy(out=identb, in_=ident), reads=["consts"], writes=["identb"])
            p.op("pool", lambda e: e.memset(ones128b, 1.0 / 128.0), writes=["ones128b"])
            p.op("pool", lambda e: e.memset(ones512b, 1.0 / 512.0), writes=["ones512b"])
            wcnt = [0]

            def load_win(col0, ncols, dst):
                c = 0
                while c < ncols:
                    n = min(128, ncols - c)
                    k = wcnt[0] % 2
                    wcnt[0] += 1
                    p.dma(("wst", k), wst[k][:, :, 0:n], win_d[l].rearrange("(kc p) f -> p kc f", p=128)[:, :, col0 + c:col0 + c + n], writes=[("wst", k)])
                    eng = ("act", "pool")[k]
                    if eng == "act":
                        p.op("act", lambda e, k=k, n=n, c=c: e.copy(out=wunit[:, :, dst + c:dst + c + n], in_=wst[k][:, :, 0:n]), reads=[("wst", k)], writes=["wunit"])
                    else:
                        p.op("pool", lambda e, k=k, n=n, c=c: e.tensor_copy(out=wunit[:, :, dst + c:dst + c + n], in_=wst[k][:, :, 0:n]), reads=[("wst", k)], writes=["wunit"])
                    c += n

            def load_wout(ych0, n):
                for j in range(n):
                    k = 0
                    p.dma(("wost", k), woutst[k], wout_d[l, (ych0 + j) * 128:(ych0 + j + 1) * 128, :], writes=[("wost", k)])
                    p.op("pool", lambda e, k=k, j=j: e.tensor_copy(out=wout[:, j, :], in_=woutst[k]), reads=[("wost", k)], writes=["wout"])

            def proj_fm(off, ncols, tb, bank):
                def mm(e):
                    ins = None
                    for kc in range(8):
                        ins = e.matmul(banks[bank][0:ncols, :], lhsT=wunit[:, kc, off:off + ncols], rhs=hT[:, kc, tb * TB:(tb + 1) * TB], start=(kc == 0), stop=(kc == 7))
                    return ins
                p.op("pe", mm, reads=["wunit"] + [("h", kc, tb) for kc in range(8)], writes=[PS(bank)])

            def proj_tm(off, ncols, tb, tt, bank, col0=0):
                t0 = tb * TB + tt * 128

                def mm(e):
                    ins = None
                    for kc in range(8):
                        ins = e.matmul(banks[bank][:, col0:col0 + ncols], lhsT=hT[:, kc, t0:t0 + 128], rhs=wunit[:, kc, off:off + ncols], start=(kc == 0), stop=(kc == 7))
                    return ins
                p.op("pe", mm, reads=["wunit"] + [("h", kc, tb) for kc in range(8)], writes=[PS(bank)])

            def out_proj(tb, nych, bks):
                for d in range(8):
                    bk = bks[d % len(bks)]

                    def mm(e, d=d, bk=bk):
                        ins = None
                        for j in range(nych):
                            ins = e.matmul(banks[bk][:, :], lhsT=wout[:, j, d * 128:(d + 1) * 128], rhs=yblk[:, j, :], start=(j == 0), stop=(j == nych - 1))
                        return ins
                    p.op("pe", mm, reads=["wout"] + [("yblk", j) for j in range(nych)], writes=[PS(bk)])
                    p.op("dve", lambda e, d=d, bk=bk: e.scalar_tensor_tensor(out=xT[:, d, tb * TB:(tb + 1) * TB], in0=banks[bk][:, :], scalar=MOD(l, 2, d), in1=xT[:, d, tb * TB:(tb + 1) * TB], op0=ALU.mult, op1=ALU.add),
                         reads=[PS(bk), ("x", d, tb)] + modkeys(l), writes=[("x", d, tb)])

            def conv_silu(buf, key, tb, bank, wcol, bcol, tmp, tmpkey, dst, dstkey, act_func, eng="dve"):
                p.op("act", lambda e: e.copy(out=buf[:, 3:3 + TB], in_=banks[bank][:, :]), reads=[PS(bank)], writes=[key])
                p.op(eng, lambda e: e.tensor_scalar(out=tmp, in0=buf[:, 0:TB], scalar1=PPc(l, wcol), scalar2=PPc(l, bcol), op0=ALU.mult, op1=ALU.add), reads=[key, "pp"], writes=[tmpkey])
                for k in range(1, 4):
                    p.op(eng, lambda e, k=k: e.scalar_tensor_tensor(out=tmp, in0=buf[:, k:k + TB], scalar=PPc(l, wcol + k), in1=tmp, op0=ALU.mult, op1=ALU.add), reads=[key, tmpkey, "pp"], writes=[tmpkey])
                p.op("pool", lambda e: e.tensor_copy(out=buf[:, 0:3], in_=buf[:, TB:TB + 3]), reads=[key], writes=[key])
                if dst is not None:
                    p.op("act", lambda e: e.activation(out=dst, in_=tmp, func=act_func), reads=[tmpkey], writes=[dstkey])

            mark = cv.off

            if do_lru:
                cv.off = mark
                wabd = cv.get([128, 4, 128], BF16)
                wxbd = cv.get([128, 4, 128], BF16)
                nsp8 = cv.get([128, 4])
                hcar = cv.get([128, 4])
                xbuf = [cv.get([128, 3 + TB]) for _ in range(4)]
                Ts = [{n: cv.get([128, TB]) for n in ("xc", "r", "i", "a", "om", "ix", "u", "h", "gg")} for _ in range(2)]
                xcbs = [cv.get([128, TB], BF16) for _ in range(2)]
                for nm, src, dst in (("wa", wabd_d, wabd), ("wx", wxbd_d, wxbd)):
                    p.dma(("wost", 0), woutst[0][:, 0:512].rearrange("p (m j) -> p m j", m=4), src[l].rearrange("m i j -> i m j"), writes=[("wost", 0)])
                    p.op("pool", lambda e, dst=dst: e.tensor_copy(out=dst, in_=woutst[0][:, 0:512].rearrange("p (m j) -> p m j", m=4)), reads=[("wost", 0)], writes=[nm])
                p.op("act", lambda e: e.activation(out=nsp8, in_=pp[:, l, PP_LLAM:PP_LLAM + 4], func=AF.Exp, scale=-1.0), reads=["pp"], writes=["nsp8"])
                p.op("act", lambda e: e.activation(out=nsp8, in_=nsp8, func=AF.Ln, bias=LNEPS[:, 2:3]), reads=["nsp8", "lneps"], writes=["nsp8"])
                p.op("dve", lambda e: e.tensor_scalar_mul(out=nsp8, in0=nsp8, scalar1=-8.0), reads=["nsp8"], writes=["nsp8"])
                for m in range(4):
                    p.op("pool", lambda e, m=m: e.memset(xbuf[m][:, 0:3], 0.0), writes=[("xbuf", m)])
                load_win(0, 1024, 0)
                load_wout(0, 4)
                for tb in range(NTB):
                    for m in range(4):
                        par = m % 2
                        T = Ts[par]
                        xcb = xcbs[par]
                        B0, B1, B2, B3 = (0, 1, 2, 3) if par == 0 else (4, 5, 6, 7)
                        KK = lambda n, par=par: (n, par)
                        proj_fm(m * 128, 128, tb, B0)
                        proj_fm(512 + m * 128, 128, tb, B1)
                        conv_silu(xbuf[m], ("xbuf", m), tb, B0, PP_LCW + m * 4, PP_LCB + m, T["xc"], KK("l_xc"), None, None, None)
                        p.op("act", lambda e, T=T, xcb=xcb: e.copy(out=xcb, in_=T["xc"]), reads=[KK("l_xc")], writes=[KK("l_xcb")])
                        p.op("pe", lambda e, m=m, xcb=xcb, B2=B2: e.matmul(banks[B2][:, :], lhsT=wabd[:, m, :], rhs=xcb, start=True, stop=True), reads=["wa", KK("l_xcb")], writes=[PS(B2)])
                        p.op("pe", lambda e, m=m, xcb=xcb, B3=B3: e.matmul(banks[B3][:, :], lhsT=wxbd[:, m, :], rhs=xcb, start=True, stop=True), reads=["wx", KK("l_xcb")], writes=[PS(B3)])
                        p.op("act", lambda e, m=m, T=T, B2=B2: e.activation(out=T["r"], in_=banks[B2][:, :], func=AF.Sigmoid, bias=PPc(l, PP_LBA + m)), reads=[PS(B2), "pp"], writes=[KK("l_r")])
                        p.op("act", lambda e, m=m, T=T, B3=B3: e.activation(out=T["i"], in_=banks[B3][:, :], func=AF.Sigmoid, bias=PPc(l, PP_LBX + m)), reads=[PS(B3), "pp"], writes=[KK("l_i")])
                        p.op("act", lambda e, m=m, T=T: e.activation(out=T["a"], in_=T["r"], func=AF.Exp, scale=nsp8[:, m:m + 1]), reads=[KK("l_r"), "nsp8"], writes=[KK("l_a")])
                        p.op("pool", lambda e, T=T: e.tensor_tensor(out=T["om"], in0=T["a"], in1=T["a"], op=ALU.mult), reads=[KK("l_a")], writes=[KK("l_om")])
                        p.op("pool", lambda e, T=T: e.tensor_scalar(out=T["om"], in0=T["om"], scalar1=-1.0, scalar2=1.0, op0=ALU.mult, op1=ALU.add), reads=[KK("l_om")], writes=[KK("l_om")])
                        p.op("act", lambda e, T=T: e.activation(out=T["om"], in_=T["om"], func=AF.Sqrt), reads=[KK("l_om")], writes=[KK("l_om")])
                        p.op("pool", lambda e, T=T: e.tensor_tensor(out=T["ix"], in0=T["i"], in1=T["xc"], op=ALU.mult), reads=[KK("l_i"), KK("l_xc")], writes=[KK("l_ix")])
                        p.op("dve", lambda e, T=T: e.tensor_tensor(out=T["u"], in0=T["om"], in1=T["ix"], op=ALU.mult), reads=[KK("l_om"), KK("l_ix")], writes=[KK("l_u")])
                        if tb == 0:
                            p.op("dve", lambda e, T=T: e.tensor_tensor_scan(out=T["h"], data0=T["a"], data1=T["u"], initial=0.0, op0=ALU.mult, op1=ALU.add), reads=[KK("l_a"), KK("l_u")], writes=[KK("l_h")])
                        else:
                            p.op("dve", lambda e, m=m, T=T: e.tensor_tensor_scan(out=T["h"], data0=T["a"], data1=T["u"], initial=hcar[:, m:m + 1], op0=ALU.mult, op1=ALU.add), reads=[KK("l_a"), KK("l_u"), ("hcar", m)], writes=[KK("l_h")])
                        p.op("pool", lambda e, m=m, T=T: e.tensor_copy(out=hcar[:, m:m + 1], in_=T["h"][:, TB - 1:TB]), reads=[KK("l_h")], writes=[("hcar", m)])
                        p.op("act", lambda e, T=T, B1=B1: e.activation(out=T["gg"], in_=banks[B1][:, :], func=AF.Gelu_apprx_tanh), reads=[PS(B1)], writes=[KK("l_gg")])
                        p.op("dve", lambda e, m=m, T=T: e.tensor_tensor(out=yblk[:, m, :], in0=T["h"], in1=T["gg"], op=ALU.mult), reads=[KK("l_h"), KK("l_gg")], writes=[("yblk", m)])
                    out_proj(tb, 4, [0, 1, 2, 3, 4, 5, 6, 7])

            if do_gla:
                cv.off = mark
                walb = cv.get([32, 256], BF16)
                rT = cv.get([32, TB], BF16)
                e1 = cv.get([128, 128])
                l1 = cv.get([128, 128])
                ecp = cv.get([128, TB])
                ecn = cv.get([128, TB])
                esuf = [cv.get([128, 128]) for _ in range(4)]
                qd = cv.get([128, TB], BF16)
                kd = cv.get([128, TB], BF16)
                kend = [cv.get([128, 128], BF16) for _ in range(4)]
                kendz = [[cv.get([128, 128], BF16) for _ in range(2)] for _ in range(4)]
                qdz = [cv.get([128, TB], BF16) for _ in range(2)]
                CHM = [consts[:, C_CH0:C_CH0 + 128], consts[:, C_CH1:C_CH1 + 128]]
                vtm = [cv.get([128, 256], BF16) for _ in range(4)]
                sg = [cv.get([128, TB]) for _ in range(2)]
                attm = cv.get([128, 4, 128], BF16)
                S32 = cv.get([128, 128])
                Sbf = [cv.get([128, 128], BF16) for _ in range(2)]
                sqb = cv.get([128, TB], BF16)
                rs = cv.get([128, TB])
                t1 = cv.get([128, TB])
                p.dma(("wost", 0), woutst[0][0:32, 0:256], walpha_d[l], writes=[("wost", 0)])
                p.op("pool", lambda e: e.tensor_copy(out=walb, in_=woutst[0][0:32, 0:256]), reads=[("wost", 0)], writes=["walb"])
                p.op("pool", lambda e: e.memset(rT, 1.0), writes=["rT"])
                m64b = consts[:, C_MASK:C_MASK + 128].unsqueeze(1).to_broadcast([128, 4, 128])
                for hp in range(2):
                    load_win(1024 + hp * 128, 128, 0)
                    load_win(1280 + hp * 128, 128, 128)
                    load_win(1536 + hp * 256, 256, 256)
                    load_win(2048 + hp * 256, 256, 512)
                    load_win(2560, 16, 768)
                    load_wout(4 + hp * 2, 2)
                    p.op("pool", lambda e: e.memset(S32, 0.0), writes=["S32"])
                    p.op("pool", lambda e: e.memset(Sbf[0], 0.0), writes=[("Sbf", 0)])
                    par_state = {0: 0, 1: 0}
                    for tb in range(NTB):
                        proj_fm(768, 16, tb, 0)
                        p.op("act", lambda e: e.copy(out=rT[0:16, :], in_=banks[0][0:16, :]), reads=[PS(0)], writes=["rT"])
                        for tt in range(4):
                            tsl = slice(tt * 128, (tt + 1) * 128)
                            p.op("pe", lambda e, tsl=tsl, hp=hp: e.matmul(banks[5][:, 0:128], lhsT=rT[:, tsl], rhs=walb[:, hp * 128:(hp + 1) * 128], start=True, stop=True), reads=["rT", "walb"], writes=[PS(5)])
                            p.op("act", lambda e: e.activation(out=e1, in_=banks[5][:, 0:128], func=AF.Exp, scale=-1.0), reads=[PS(5)], writes=["g_e1"])
                            p.op("act", lambda e: e.activation(out=l1, in_=e1, func=AF.Ln, bias=LNEPS[:, 2:3]), reads=["g_e1", "lneps"], writes=["g_l1"])
                            p.op("pe", lambda e: e.matmul(banks[5][:, 128:256], lhsT=l1, rhs=consts[:, C_MASKG:C_MASKG + 128], start=True, stop=True), reads=["g_l1", "consts"], writes=[PS(5)])
                            p.op("pe", lambda e: e.matmul(banks[5][:, 256:384], lhsT=consts[:, C_MASKG + 128:C_MASKG + 256], rhs=l1, start=True, stop=True), reads=["g_l1", "consts"], writes=[PS(5)])
                            p.op("act", lambda e, tsl=tsl: e.activation(out=ecp[:, tsl], in_=banks[5][:, 128:256], func=AF.Exp), reads=[PS(5)], writes=["g_ecp"])
                            p.op("act", lambda e, tsl=tsl: e.activation(out=ecn[:, tsl], in_=banks[5][:, 128:256], func=AF.Exp, scale=-1.0), reads=[PS(5)], writes=["g_ecn"])
                            p.op("act", lambda e, tt=tt: e.activation(out=esuf[tt], in_=banks[5][:, 256:384], func=AF.Exp), reads=[PS(5)], writes=[("g_esuf", tt)])
                        proj_fm(0, 128, tb, 0)
                        p.op("dve", lambda e: e.scalar_tensor_tensor(out=qd, in0=banks[0][:, :], scalar=0.125, in1=ecp, op0=ALU.mult, op1=ALU.mult), reads=[PS(0), "g_ecp"], writes=["g_qd"])
                        for hh in range(2):
                            p.op("pool", lambda e, hh=hh: e.tensor_scalar_mul(out=qdz[hh], in0=qd, scalar1=consts[:, (C_CH0, C_CH1)[hh]:(C_CH0, C_CH1)[hh] + 1]), reads=["g_qd", "consts"], writes=[("g_qdz", hh)])
                        proj_fm(128, 128, tb, 1)
                        p.op("dve", lambda e: e.tensor_tensor(out=kd, in0=banks[1][:, :], in1=ecn, op=ALU.mult), reads=[PS(1), "g_ecn"], writes=["g_kd"])
                        for tt in range(4):
                            proj_tm(128, 128, tb, tt, 0)
                            p.op("dve", lambda e, tt=tt: e.tensor_tensor(out=kend[tt], in0=banks[0][:, 0:128], in1=esuf[tt], op=ALU.mult), reads=[PS(0), ("g_esuf", tt)], writes=[("g_kend", tt)])
                            for half in range(2):
                                p.op("pool", lambda e, tt=tt, half=half: e.tensor_tensor(out=kendz[tt][half], in0=kend[tt], in1=CHM[half], op=ALU.mult), reads=[("g_kend", tt), "consts"], writes=[("g_kendz", tt, half)])
                            proj_tm(256, 256, tb, tt, 1)
                            p.op("act", lambda e, tt=tt: e.copy(out=vtm[tt], in_=banks[1][:, 0:256]), reads=[PS(1)], writes=[("g_vtm", tt)])
                        for hh in range(2):
                            proj_fm(512 + hh * 128, 128, tb, hh)
                            p.op("act", lambda e, hh=hh: e.activation(out=sg[hh], in_=banks[hh][:, :], func=AF.Silu), reads=[PS(hh)], writes=[("g_sg", hh)])
                        for hh in range(2):
                            b0 = hh * 64

                            def att(e, hh=hh):
                                ins = None
                                for tt in range(4):
                                    tsl = slice(tt * 128, (tt + 1) * 128)
                                    ins = e.matmul(banks[2][:, tsl], lhsT=kd[:, tsl], rhs=qdz[hh][:, tsl], start=True, stop=True)
                                return ins
                            p.op("pe", att, reads=["g_kd", ("g_qdz", hh)], writes=[PS(2)])
                            p.op("dve", lambda e: e.tensor_tensor(out=attm, in0=banks[2][:, :].rearrange("p (a b) -> p a b", a=4), in1=m64b, op=ALU.mult), reads=[PS(2), "consts"], writes=["g_attm"])
                            for c in range(8):
                                tt, half = c // 2, c % 2
                                csl = slice(c * 64, (c + 1) * 64)
                                tsl = slice(tt * 128, (tt + 1) * 128)
                                cur_par = par_state[hh]
                                if half == 0:
                                    p.op("pe", lambda e, tt=tt, tsl=tsl, hh=hh: e.matmul(banks[3][:, tsl], lhsT=vtm[tt][:, hh * 128:(hh + 1) * 128], rhs=attm[:, tt, :], start=True, stop=False),
                                         reads=[("g_vtm", tt), "g_attm"], writes=[PS(3)])
                                p.op("pe", lambda e, csl=csl, hh=hh, cur_par=cur_par, half=half: e.matmul(banks[3][:, csl], lhsT=Sbf[cur_par], rhs=qdz[hh][:, csl], start=False, stop=(half == 1)),
                                     reads=[("Sbf", cur_par), ("g_qdz", hh)], writes=[PS(3)])
                                p.op("pe", lambda e, tt=tt, half=half, hh=hh: e.matmul(banks[4][:, 0:128], lhsT=kendz[tt][half], rhs=vtm[tt][:, hh * 128:(hh + 1) * 128], start=True, stop=True),
                                     reads=[("g_kendz", tt, half), ("g_vtm", tt)], writes=[PS(4)])
                                col = c * 64 + 63
                                p.op("dve", lambda e, b0=b0, col=col: e.scalar_tensor_tensor(out=S32[b0:b0 + 64, :], in0=S32[b0:b0 + 64, :], scalar=ecp[b0:b0 + 64, col:col + 1], in1=banks[4][b0:b0 + 64, 0:128], op0=ALU.mult, op1=ALU.add),
                                     reads=["S32", "g_ecp", PS(4)], writes=["S32"])
                                nxt = 1 - cur_par
                                p.op("act", lambda e, nxt=nxt: e.copy(out=Sbf[nxt], in_=S32), reads=["S32"], writes=[("Sbf", nxt)])
                                par_state[hh] = nxt
                            p.op("act", lambda e: e.activation(out=sqb, in_=banks[3][:, :], func=AF.Square), reads=[PS(3)], writes=["g_sqb"])
                            p.op("pe", lambda e: e.matmul(banks[5][:, :], lhsT=ones128b, rhs=sqb, start=True, stop=True), reads=["ones128b", "g_sqb"], writes=[PS(5)])
                            p.op("act", lambda e: e.activation(out=rs, in_=banks[5][:, :], func=AF.Sqrt, bias=LNEPS[:, 1:2]), reads=[PS(5), "lneps"], writes=["g_rs"])
                            p.op("dve", lambda e: e.reciprocal(out=rs, in_=rs), reads=["g_rs"], writes=["g_rs"])
                            p.op("dve", lambda e: e.tensor_tensor(out=t1, in0=banks[3][:, :], in1=rs, op=ALU.mult), reads=[PS(3), "g_rs"], writes=["g_t1"])
                            p.op("dve", lambda e, hh=hh: e.scalar_tensor_tensor(out=yblk[:, hh, :], in0=t1, scalar=PPc(l, PP_GNORM), in1=sg[hh], op0=ALU.mult, op1=ALU.mult), reads=["g_t1", ("g_sg", hh), "pp"], writes=[("yblk", hh)])
                        out_proj(tb, 2, [6, 7])

            if do_ssd:
                ssd_units(l, cv, mark, load_win, load_wout, proj_fm, proj_tm, out_proj, conv_silu, yblk, identb, ones512b)

        cv = Carve()
        ada_bufs = ada_bufs_alloc(cv)
        for blk in range(N_ADA):
            adaln_block(0, blk, ada_bufs, blk % 4)
        adaln_finish(0, 1)

        for tb in range(NTB):
            scale_alpha(tb, engs=("pool", "dve", "act"))
        for l in range(depth):
            p.new_epoch()
            for tb in range(NTB):
                modulate(l, 1, tb)
            barrier()
            if do_lru or do_gla or do_ssd:
                mixer(l)
            barrier()
            cv = Carve()
            layernorm(l, PP_LN1G, PP_LN1B, cv, 0, 1)
            for tb in range(NTB):
                modulate(l, 2, tb)
            barrier()
            cv = Carve()
            if do_moe:
                router(l, cv, 2, 3)
            barrier()
            cv = Carve()
            hooks = {}
            if l + 1 < depth:
                ada_bufs2 = ada_bufs_alloc(cv)
                for blk in range(N_ADA):
                    hooks[2 + blk] = (lambda blk=blk: adaln_block(l + 1, blk, ada_bufs2, 4 + blk % 4))
                hooks[2 + N_ADA] = (lambda: adaln_finish(l + 1, 4))
            if do_moe:
                moe(l, cv, hooks)
            else:
                for k in sorted(hooks):
                    hooks[k]()
            barrier()
            cv = Carve()
            layernorm(l, PP_LN2G, PP_LN2B, cv, 0, 1, final=(l == depth - 1))
            barrier()

        p.dma("out", yT_d.rearrange("(c p) t -> p c t", p=128), xT[:], reads=[("x", c, tb) for c in range(8) for tb in range(NTB)])
        p.emit()
    return nc


def prep_inputs(inputs):
    f = lambda a: np.ascontiguousarray(np.asarray(a, dtype=np.float32))
    L = DEPTH
    shared = {}
    shared["consts"] = build_consts()
    pc = lambda v, n: np.asarray(v, np.float32).reshape(n, 128).T
    pp = np.zeros((L, 128, NPP), np.float32)
    prow = np.zeros((L, NPR), np.float32)
    wabd = np.zeros((L, 4, 128, 128), np.float32)
    wxbd = np.zeros((L, 4, 128, 128), np.float32)
    wal = np.zeros((L, 32, 256), np.float32)
    for l in range(L):
        pp[l, :, PP_LN1G:PP_LN1G + 8] = pc(inputs["ln1_g"][l], 8)
        pp[l, :, PP_LN1B:PP_LN1B + 8] = pc(inputs["ln1_b"][l], 8)
        pp[l, :, PP_LN2G:PP_LN2G + 8] = pc(inputs["ln2_g"][l], 8)
        pp[l, :, PP_LN2B:PP_LN2B + 8] = pc(inputs["ln2_b"][l], 8)
        for m in range(4):
            for k in range(4):
                pp[l, :, PP_LCW + m * 4 + k] = inputs["lru_conv_w"][l, k, m * 128:(m + 1) * 128]
        pp[l, :, PP_LCB:PP_LCB + 4] = pc(inputs["lru_conv_b"][l], 4)
        pp[l, :, PP_LBA:PP_LBA + 4] = pc(inputs["lru_b_a"][l], 4)
        pp[l, :, PP_LBX:PP_LBX + 4] = pc(inputs["lru_b_x"][l], 4)
        pp[l, :, PP_LLAM:PP_LLAM + 4] = pc(inputs["lru_lambda"][l], 4)
        for j in range(12):
            for k in range(4):
                pp[l, :, PP_SCW + j * 4 + k] = inputs["ssd_conv_w"][l, k, j * 128:(j + 1) * 128]
        pp[l, :, PP_SCB:PP_SCB + 12] = pc(inputs["ssd_conv_b"][l], 12)
        pp[l, :, PP_SNORM:PP_SNORM + 8] = pc(inputs["ssd_norm"][l], 8)
        pp[l, :, PP_GNORM] = inputs["gla_norm"][l]
        pp[l, :, PP_SD:PP_SD + 8] = pc(np.repeat(np.asarray(inputs["ssd_d"][l]), 64), 8)
        prow[l, PR_DTB:PR_DTB + 16] = inputs["ssd_dt_bias"][l]
        prow[l, PR_ALOG:PR_ALOG + 16] = inputs["ssd_a_log"][l]
        prow[l, PR_RB:PR_RB + NE] = inputs["router_bias"][l]
        for m in range(4):
            for q in range(2):
                wabd[l, m, q * 64:(q + 1) * 64, q * 64:(q + 1) * 64] = inputs["lru_w_a"][l, 2 * m + q]
                wxbd[l, m, q * 64:(q + 1) * 64, q * 64:(q + 1) * 64] = inputs["lru_w_x"][l, 2 * m + q]
        wal[l, 0:16] = inputs["gla_w_alpha"][l]
        wal[l, 16] = inputs["gla_b_alpha"][l]
    shared.update(pp=pp, prow=prow, lru_wa_bd=wabd, lru_wx_bd=wxbd, walpha_ext=wal)
    for k in ("w_ada", "b_ada", "w_in", "w_out", "router_w", "exp_w1", "exp_w3", "exp_w2", "shared_w1", "shared_w3", "shared_w2"):
        shared[k] = f(inputs[k])
    x = np.asarray(inputs["x"], np.float32)
    c = np.asarray(inputs["c"], np.float32)
    maps = []
    for b in range(x.shape[0]):
        m = dict(shared)
        m["xT"] = np.ascontiguousarray(x[b].T)
        m["cpc"] = np.ascontiguousarray(c[b].reshape(8, 128).T)
        maps.append(m)
    return maps


_NC_CACHE = {}


def kernel(**inputs):
    maps = prep_inputs(inputs)
    if "nc" not in _NC_CACHE:
        _NC_CACHE["nc"] = build_program()
    nc = _NC_CACHE["nc"]
    res = run_bass_kernel_spmd(nc, maps, core_ids=list(range(len(maps))))
    out = np.stack([np.ascontiguousarray(r["yT"].T) for r in res.results], axis=0)
    return out.astype(np.float32)
```

```python
from contextlib import ExitStack
import numpy as np
import concourse.bass as bass
import concourse.mybir as mybir
from concourse.bass_utils import run_bass_kernel_spmd

F32 = mybir.dt.float32
BF16 = mybir.dt.bfloat16
AF = mybir.ActivationFunctionType
ALU = mybir.AluOpType
AX = mybir.AxisListType

D = 1024
S = 2048
DEPTH = 4
NE = 64
ALPHA = (2 * DEPTH) ** 0.25
DIN = 5152
NPP = 144
TB = 512
NTB = S // TB


class Prog:
    def __init__(self, nc, stack):
        self.nc = nc
        self.stack = stack
        self.ops = []
        self.last_write = {}
        self.readers = {}
        self.epoch = 0
        self.op_epoch = []
        self.group_open = {}
        self.group_of = {}
        self.nsem = 0
        self.bar = None
        self.xeng = []

    def barrier(self, fn):
        allk = list(set(self.last_write.keys()) | set(k for k, v in self.readers.items() if v))
        oid = self._add("dve", fn, allk, allk)
        self.bar = oid
        self.last_write = {}
        self.readers = {}

    def new_sem(self, name):
        self.nsem += 1
        return self.stack.enter_context(self.nc.semaphore(f"{name}_{self.nsem}"))

    def new_epoch(self):
        self.epoch += 1

    def _add(self, eng, fn, reads, writes, chan=None):
        oid = len(self.ops)
        raw = set()
        oth = set()
        xe = set()
        for k in reads:
            w = self.last_write.get(k)
            if w is not None:
                raw.add(w)
            if isinstance(k, tuple) and k[0] in ("ps", "ps2o"):
                for r in self.readers.get(k, ()):
                    xe.add(r)
        for k in writes:
            w = self.last_write.get(k)
            if w is not None:
                oth.add(w)
            for r in self.readers.get(k, ()):
                oth.add(r)
        if self.bar is not None:
            oth.add(self.bar)
        oth -= raw
        raw.discard(oid)
        oth.discard(oid)
        xe -= raw
        xe -= oth
        xe.discard(oid)
        self.xeng.append(sorted(xe))
        self.ops.append([eng, fn, sorted(raw), sorted(oth), chan])
        self.op_epoch.append(self.epoch)
        for k in reads:
            self.readers.setdefault(k, []).append(oid)
        for k in writes:
            self.last_write[k] = oid
            self.readers[k] = []
        return oid

    def op(self, eng, fn, reads=(), writes=()):
        return self._add(eng, fn, reads, writes)

    def dma(self, chan, out, in_, reads=(), writes=(), eng="sp", more=False, **kw):
        def fn(e, out=out, in_=in_, kw=kw):
            return e.dma_start(out=out, in_=in_, **kw)
        oid = self._add(eng, fn, reads, writes, chan=chan)
        g = self.group_open.get(chan)
        if g is None:
            g = []
            self.group_open[chan] = g
        g.append(oid)
        self.group_of[oid] = g
        if not more:
            self.group_open[chan] = None
        return oid

    def emit(self):
        nc = self.nc
        n = len(self.ops)
        is_dma = [o[4] is not None for o in self.ops]
        need_sig = [False] * n
        deps_of = []
        for i, (eng, fn, raw, oth, chan) in enumerate(self.ops):
            deps = []
            for d in raw:
                if is_dma[d] or is_dma[i] or self.ops[d][0] != eng or eng != "pe":
                    deps.append(d)
            for d in oth:
                if is_dma[d] or is_dma[i] or self.ops[d][0] != eng or eng != "pe":
                    deps.append(d)
            for d in self.xeng[i]:
                if self.ops[d][0] != eng:
                    deps.append(d)
            deps_of.append(deps)
            for d in deps:
                if not is_dma[d]:
                    need_sig[d] = True
        sig = [None] * n
        cur = {}
        chan_sem = {}
        chan_cnt = {}
        for i, (eng, fn, raw, oth, chan) in enumerate(self.ops):
            if is_dma[i]:
                if chan not in chan_sem:
                    chan_sem[chan] = self.new_sem("d")
                    chan_cnt[chan] = 0
                chan_cnt[chan] += 16
                sig[i] = (chan_sem[chan], chan_cnt[chan])
            elif need_sig[i]:
                key = (eng, self.op_epoch[i])
                if key not in cur:
                    cur[key] = [self.new_sem(eng), 0]
                cur[key][1] += 1
                sig[i] = (cur[key][0], cur[key][1])
        for i in range(n):
            if is_dma[i]:
                last = self.group_of[i][-1]
                if last != i:
                    sig[i] = (sig[i][0], sig[last][1])
        streams = {}
        for i, (eng, fn, raw, oth, chan) in enumerate(self.ops):
            streams.setdefault(eng, []).append((i, fn, [sig[d] for d in deps_of[i]]))
        final_dma = [(chan_sem[c], chan_cnt[c]) for c in chan_sem]
        self.n_waits = 0

        def run_stream(e, items, tail):
            waited = {}
            for (i, fn, waits) in items:
                best = {}
                for (s, v) in waits:
                    k = s.num
                    if waited.get(k, 0) >= v:
                        continue
                    if k not in best or best[k][1] < v:
                        best[k] = (s, v)
                for k, (s, v) in best.items():
                    e.wait_ge(s, v)
                    waited[k] = v
                    self.n_waits += 1
                ins = fn(e)
                if is_dma[i]:
                    ins.then_inc(chan_sem[self.ops[i][4]], 16)
                elif sig[i] is not None:
                    ins.then_inc(sig[i][0], 1)
            for (s, v) in tail:
                e.wait_ge(s, v)

        with nc.Block() as block:
            names = {"pe": "tensor", "act": "scalar", "dve": "vector", "pool": "gpsimd", "sp": "sync"}
            for en, attr in names.items():
                items = streams.get(en, [])
                tail = final_dma if en == "sp" else []
                if not items and not tail:
                    continue

                def body(e, items=items, tail=tail):
                    run_stream(e, items, tail)
                getattr(block, attr)(body)


C_ID = 0
C_MASK = 128
C_SUF = 256
C_CH0 = 384
C_CH1 = 512
C_M64 = 640
C_ONESD = 704
C_ONE = 832
C_SEL = 960
C_MASKG = 1984
NCONST = 2240


def build_consts():
    c = np.zeros((128, NCONST), np.float32)
    idx = np.arange(128)
    same = (idx[:, None] // 64) == (idx[None, :] // 64)
    c[:, C_ID:C_ID + 128] = np.eye(128)
    c[:, C_MASK:C_MASK + 128] = same & (idx[:, None] <= idx[None, :])
    c[:, C_SUF:C_SUF + 128] = same & (idx[:, None] > idx[None, :])
    c[:64, C_CH0:C_CH0 + 128] = 1.0
    c[64:, C_CH1:C_CH1 + 128] = 1.0
    j = idx % 64
    c[:, C_M64:C_M64 + 64] = j[:, None] <= np.arange(64)[None, :]
    c[:, C_ONESD:C_ONESD + 128] = 1.0 / 1024.0
    c[:, C_ONE:C_ONE + 128] = 1.0
    for h in range(8):
        c[h, C_SEL + h * 128:C_SEL + (h + 1) * 128] = 1.0
    c[:, C_MASKG:C_MASKG + 128] = c[:, C_MASK:C_MASK + 128] * (-1.0 / 16.0)
    c[:, C_MASKG + 128:C_MASKG + 256] = c[:, C_SUF:C_SUF + 128] * (-1.0 / 16.0)
    return c


PP_LN1G, PP_LN1B, PP_LN2G, PP_LN2B = 0, 8, 16, 24
PP_LCW, PP_LCB, PP_LBA, PP_LBX, PP_LLAM = 32, 48, 52, 56, 60
PP_SCW, PP_SCB, PP_SNORM, PP_GNORM, PP_SD = 64, 112, 124, 132, 133
PR_DTB, PR_ALOG, PR_RB = 0, 16, 32
NPR = 96


def build_program(depth=DEPTH, do_lru=True, do_gla=True, do_ssd=True, do_moe=True, n_exp=NE + 1, debug=False):
    nc = bass.Bass("TRN2", target_bir_lowering=False)
    dr = {}

    def din(name, shape):
        dr[name] = nc.dram_tensor(name, list(shape), F32, kind="ExternalInput").ap()
        return dr[name]

    xT_d = din("xT", [D, S])
    cpc_d = din("cpc", [128, 8])
    consts_d = din("consts", [128, NCONST])
    pp_d = din("pp", [DEPTH, 128, NPP])
    prow_d = din("prow", [DEPTH, NPR])
    wada_d = din("w_ada", [DEPTH, D, 6 * D])
    bada_d = din("b_ada", [DEPTH, 6 * D])
    win_d = din("w_in", [DEPTH, D, DIN])
    wout_d = din("w_out", [DEPTH, 2 * D, D])
    wabd_d = din("lru_wa_bd", [DEPTH, 4, 128, 128])
    wxbd_d = din("lru_wx_bd", [DEPTH, 4, 128, 128])
    walpha_d = din("walpha_ext", [DEPTH, 32, 256])
    rw_d = din("router_w", [DEPTH, D, NE])
    ew1_d = din("exp_w1", [DEPTH, NE, D, 256])
    ew3_d = din("exp_w3", [DEPTH, NE, D, 256])
    ew2_d = din("exp_w2", [DEPTH, NE, 256, D])
    sw1_d = din("shared_w1", [DEPTH, D, 256])
    sw3_d = din("shared_w3", [DEPTH, D, 256])
    sw2_d = din("shared_w2", [DEPTH, 256, D])
    yT_d = nc.dram_tensor("yT", [D, S], F32, kind="ExternalOutput").ap()
    gscr_d = nc.dram_tensor("gscr", [2, NE, S], F32, kind="Internal").ap()
    dbg = {}

    with ExitStack() as st:
        p = Prog(nc, st)

        def sb(name, shape, dt=F32):
            return st.enter_context(nc.sbuf_tensor(name, list(shape), dt))

        xT = sb("xT_sb", [128, 8, S])
        hT = sb("hT_sb", [128, 8, S], BF16)
        consts = sb("consts_sb", [128, NCONST])
        pp = sb("pp_sb", [128, DEPTH, NPP])
        mod = sb("mod_sb", [128, DEPTH, 64])
        cond = sb("cond_sb", [128, 8])
        SCRW = 25300
        scr = sb("scr_sb", [128, SCRW])
        banks = [st.enter_context(nc.psum_tensor(f"bank{i}", [128, 512], F32)) for i in range(8)]

        def PS(i):
            return ("ps", i)

        class Carve:
            def __init__(self):
                self.off = 0

            def get(self, shape, dt=F32):
                n = int(np.prod(shape[1:]))
                words = n if dt == F32 else (n + 1) // 2
                a = scr[:, self.off:self.off + words]
                self.off += words
                assert self.off <= SCRW - 1, self.off
                if dt != F32:
                    a = a.bitcast(dt)
                    if n % 2:
                        a = a[:, 0:n]
                if len(shape) == 3:
                    a = a.rearrange("p (a b) -> p a b", a=shape[1])
                elif len(shape) == 4:
                    a = a.rearrange("p (a b c) -> p a b c", a=shape[1], b=shape[2])
                if shape[0] != 128:
                    a = a[0:shape[0]]
                return a

        ident = consts[:, C_ID:C_ID + 128]
        onesD = consts[:, C_ONESD:C_ONESD + 128]

        def barrier():
            tok = scr[:, SCRW - 1:SCRW]
            p.barrier(lambda e: e.memset(tok, 0.0))

        p.dma("ld0", consts[:], consts_d[:, :], writes=["consts"])
        p.dma("ld1", pp[:], pp_d.rearrange("l p n -> p l n"), writes=["pp"])
        p.dma("ld2", cond[:], cpc_d[:, :], writes=["cond"])
        p.dma("ldx", xT[:], xT_d.rearrange("(c p) t -> p c t", p=128), writes=[("x", c, tb) for c in range(8) for tb in range(NTB)])
        p.op("act", lambda e: e.activation(out=cond[:], in_=cond[:], func=AF.Silu), reads=["cond"], writes=["cond"])

        ADA_BLK = 256
        N_ADA = 6 * D // ADA_BLK
        ada_stg = [None, None]

        def adaln_block(l, blk, cv_bufs, bank):
            stg, brow, mrow = cv_bufs[blk % 2]
            key = ("adastg", blk % 2)
            c0 = blk * ADA_BLK
            nj = ADA_BLK // 128
            p.dma(("adab", blk % 2), brow, bada_d[l:l + 1, c0:c0 + ADA_BLK], writes=[("adabrow", blk % 2)])
            p.dma(("ada", blk % 2), stg, wada_d[l].rearrange("(kc p) f -> p kc f", p=128)[:, :, c0:c0 + ADA_BLK], writes=[key])

            def mm(e, stg=stg):
                ins = None
                for kc in range(8):
                    ins = e.matmul(banks[bank][0:1, 0:ADA_BLK], lhsT=cond[:, kc:kc + 1], rhs=stg[:, kc, :], start=(kc == 0), stop=(kc == 7))
                return ins
            p.op("pe", mm, reads=[key, "cond"], writes=[PS(bank)])
            p.op("dve", lambda e: e.tensor_tensor(out=mrow, in0=banks[bank][0:1, 0:ADA_BLK], in1=brow, op=ALU.add),
                 reads=[PS(bank), ("adabrow", blk % 2)], writes=[("adamrow", blk % 2)])

            def mm2(e):
                ins = None
                for j in range(nj):
                    ins = e.matmul(banks[bank][:, 256 + j:257 + j], lhsT=mrow[0:1, j * 128:(j + 1) * 128], rhs=consts[0:1, C_ONE:C_ONE + 1], start=True, stop=True)
                return ins
            p.op("pe", mm2, reads=[("adamrow", blk % 2), "consts"], writes=[PS(bank)])
            p.op("act", lambda e: e.copy(out=mod[:, l, blk * nj:(blk + 1) * nj], in_=banks[bank][:, 256:256 + nj]), reads=[PS(bank)], writes=[("mod", l)])

        def adaln_finish(l, bank):
            p.op("dve", lambda e: e.tensor_scalar(out=mod[:, l, 48:56], in0=mod[:, l, 8:16], scalar1=1.0, scalar2=1.0 / float(ALPHA), op0=ALU.add, op1=ALU.mult), reads=[("mod", l)], writes=[("modd", l)])
            p.op("dve", lambda e: e.tensor_scalar(out=mod[:, l, 56:64], in0=mod[:, l, 32:40], scalar1=1.0, scalar2=1.0 / float(ALPHA), op0=ALU.add, op1=ALU.mult), reads=[("mod", l)], writes=[("modd2", l)])

        def ada_bufs_alloc(cv):
            return [(cv.get([128, 8, ADA_BLK]), cv.get([1, ADA_BLK]), cv.get([1, ADA_BLK])) for _ in range(2)]

        def MOD(l, j, c):
            if j < 6:
                return mod[:, l, j * 8 + c:j * 8 + c + 1]
            return mod[:, l, 48 + (j - 6) * 8 + c:48 + (j - 6) * 8 + c + 1]

        def PPc(l, col):
            return pp[:, l, col:col + 1]

        modkeys = lambda l: [("mod", l), ("modd", l), ("modd2", l)]

        def modulate(l, which, tb, engs=("dve", "pool")):
            jsc, jsh = (6, 0) if which == 1 else (7, 3)
            for c in range(8):
                eng = engs[c % len(engs)]
                p.op(eng, lambda e, c=c: e.tensor_scalar(out=hT[:, c, tb * TB:(tb + 1) * TB], in0=xT[:, c, tb * TB:(tb + 1) * TB],
                                                         scalar1=MOD(l, jsc, c), scalar2=MOD(l, jsh, c), op0=ALU.mult, op1=ALU.add),
                     reads=[("x", c, tb)] + modkeys(l), writes=[("h", c, tb)])

        def scale_alpha(tb, engs=("pool",)):
            for c in range(8):
                eng = engs[c % len(engs)]
                if eng == "act":
                    p.op(eng, lambda e, c=c: e.mul(out=xT[:, c, tb * TB:(tb + 1) * TB], in_=xT[:, c, tb * TB:(tb + 1) * TB], mul=float(ALPHA)),
                         reads=[("x", c, tb)], writes=[("x", c, tb)])
                else:
                    p.op(eng, lambda e, c=c: e.tensor_scalar_mul(out=xT[:, c, tb * TB:(tb + 1) * TB], in0=xT[:, c, tb * TB:(tb + 1) * TB], scalar1=float(ALPHA)),
                         reads=[("x", c, tb)], writes=[("x", c, tb)])

        def layernorm(l, gcol, bcol, cv, bank_m, bank_q, final=False):
            def GB(col):
                return pp[:, l, col:col + 1] if final else ppA[:, l, col:col + 1]
            sq = [cv.get([128, TB]) for _ in range(2)]
            mean_sbs = [cv.get([128, TB]) for _ in range(2)]
            rstds = [cv.get([128, TB]) for _ in range(2)]
            tmp = [cv.get([128, TB]) for _ in range(2)]
            tmp2 = [cv.get([128, TB]) for _ in range(2)]
            bank_m0, bank_q0 = bank_m, bank_q
            for tb in range(NTB):
                sl = slice(tb * TB, (tb + 1) * TB)
                mean_sb = mean_sbs[tb % 2]
                rstd = rstds[tb % 2]
                bank_m = bank_m0 + 2 * (tb % 2)
                bank_q = bank_q0 + 2 * (tb % 2)
                KM = ("lnmean", tb % 2)
                KR = ("lnrstd", tb % 2)

                def mm_mean(e, sl=sl, bank_m=bank_m):
                    ins = None
                    for c in range(8):
                        ins = e.matmul(banks[bank_m][:, :], lhsT=onesD, rhs=xT[:, c, sl], start=(c == 0), stop=(c == 7))
                    return ins
                p.op("pe", mm_mean, reads=[("x", c, tb) for c in range(8)] + ["consts"], writes=[PS(bank_m)])
                for c in range(8):
                    p.op("act", lambda e, c=c, sl=sl: e.activation(out=sq[c % 2], in_=xT[:, c, sl], func=AF.Square), reads=[("x", c, tb)], writes=[("lnsq", c % 2)])
                    p.op("pe", lambda e, c=c, bank_q=bank_q: e.matmul(banks[bank_q][:, :], lhsT=onesD, rhs=sq[c % 2], start=(c == 0), stop=(c == 7)),
                         reads=[("lnsq", c % 2), "consts"], writes=[PS(bank_q)])
                p.op("act", lambda e, mean_sb=mean_sb, bank_m=bank_m: e.copy(out=mean_sb, in_=banks[bank_m][:, :]), reads=[PS(bank_m)], writes=[KM])
                p.op("dve", lambda e, rstd=rstd, mean_sb=mean_sb: e.tensor_tensor(out=rstd, in0=mean_sb, in1=mean_sb, op=ALU.mult), reads=[KM], writes=[KR])
                p.op("dve", lambda e, rstd=rstd, bank_q=bank_q: e.tensor_tensor(out=rstd, in0=banks[bank_q][:, :], in1=rstd, op=ALU.subtract), reads=[PS(bank_q), KR], writes=[KR])
                p.op("act", lambda e, rstd=rstd: e.activation(out=rstd, in_=rstd, func=AF.Ln, bias=LNEPS[:, 0:1]), reads=[KR, "lneps"], writes=[KR])
                p.op("act", lambda e, rstd=rstd: e.activation(out=rstd, in_=rstd, func=AF.Exp, scale=-0.5), reads=[KR], writes=[KR])
                for c in range(8):
                    k = c % 2
                    p.op("dve", lambda e, c=c, k=k, sl=sl, mean_sb=mean_sb: e.tensor_tensor(out=tmp[k], in0=xT[:, c, sl], in1=mean_sb, op=ALU.subtract),
                         reads=[("x", c, tb), KM], writes=[("lnt", k)])
                    p.op("pool", lambda e, k=k, rstd=rstd: e.tensor_tensor(out=tmp2[k], in0=tmp[k], in1=rstd, op=ALU.mult), reads=[("lnt", k), KR], writes=[("lnt2", k)])
                    p.op("act", lambda e, c=c, k=k, sl=sl: e.activation(out=xT[:, c, sl], in_=tmp2[k], func=AF.Identity, scale=GB(gcol + c), bias=GB(bcol + c)),
                         reads=[("lnt2", k), "pp", "ppA"], writes=[("x", c, tb)])

        ppA = sb("ppA_sb", [128, DEPTH, 32])
        p.op("dve", lambda e: e.tensor_scalar_mul(out=ppA[:], in0=pp[:, :, 0:32], scalar1=float(ALPHA)), reads=["pp"], writes=["ppA"])
        LNEPS = sb("lneps_sb", [128, 4])
        p.op("pool", lambda e: e.memset(LNEPS[:, 0:1], 1e-5), writes=["lneps"])
        p.op("pool", lambda e: e.memset(LNEPS[:, 1:2], 1e-6), reads=[], writes=["lneps"])
        p.op("pool", lambda e: e.memset(LNEPS[:, 2:3], 1.0), reads=[], writes=["lneps"])

        def router(l, cv, bank_l, bank_t):
            NI = 4
            rw = cv.get([128, 8, NE])
            rb = cv.get([128, NE])
            gT = cv.get([64, S])
            h32s = [cv.get([128, 8, 128]) for _ in range(NI)]
            Ws = [{n: cv.get([128, 64]) for n in ("sc", "bi", "eq", "b2", "mk", "sel", "gw", "gates")} for _ in range(NI)]
            Sms = [{n: cv.get([128, 8]) for n in ("m1", "m2", "gs", "t8", "gsel", "goff", "t8e")} for _ in range(NI)]
            s1s = [cv.get([128, 2]) for _ in range(NI)]
            p.dma("rw", rw, rw_d[l].rearrange("(kc p) e -> p kc e", p=128), writes=["rw"])
            p.dma("rb", rb, prow_d[l:l + 1, PR_RB:PR_RB + NE].partition_broadcast(128), writes=["rb"])
            g3 = lambda a: a.rearrange("p (g k) -> p g k", k=8)
            b3 = lambda a: a.unsqueeze(2).to_broadcast([128, 8, 8])

            def tile_ops(tt):
                j = tt % NI
                tb = tt // 4
                tsl = slice(tt * 128, (tt + 1) * 128)
                h32, W, Sm, s1 = h32s[j], Ws[j], Sms[j], s1s[j]
                bl, bt = j, 4 + j
                K = lambda n: (n, j)
                ops = []
                A = lambda eng, fn, r, w: ops.append((eng, fn, r, w))
                for c in range(8):
                    eng = ("dve", "pool")[c % 2]
                    A(eng, lambda e, c=c: e.tensor_scalar(out=h32[:, c, :], in0=xT[:, c, tsl], scalar1=MOD(l, 7, c), scalar2=MOD(l, 3, c), op0=ALU.mult, op1=ALU.add),
                      [("x", c, tb)] + modkeys(l), [("h32", j, c)])

                def mm(e):
                    ins = None
                    for c in range(8):
                        ins = e.matmul(banks[bl][:, 0:NE], lhsT=h32[:, c, :], rhs=rw[:, c, :], start=(c == 0), stop=(c == 7))
                    return ins
                A("pe", mm, [("h32", j, c) for c in range(8)] + ["rw"], [PS(bl)])
                A("act", lambda e: e.activation(out=W["sc"], in_=banks[bl][:, 0:NE], func=AF.Sigmoid), [PS(bl)], [K("r_sc")])
                V = lambda fn, r, w: A("dve", fn, r, w)
                V(lambda e: e.tensor_tensor(out=W["bi"], in0=W["sc"], in1=rb, op=ALU.add), [K("r_sc"), "rb"], [K("r_bi")])
                V(lambda e: e.tensor_reduce(out=Sm["m1"], in_=g3(W["bi"]), axis=AX.X, op=ALU.max), [K("r_bi")], [K("r_m1")])
                V(lambda e: e.tensor_tensor(out=g3(W["eq"]), in0=g3(W["bi"]), in1=b3(Sm["m1"]), op=ALU.is_equal), [K("r_bi"), K("r_m1")], [K("r_eq")])
                V(lambda e: e.scalar_tensor_tensor(out=W["b2"], in0=W["eq"], scalar=-10.0, in1=W["bi"], op0=ALU.mult, op1=ALU.add), [K("r_eq"), K("r_bi")], [K("r_b2")])
                V(lambda e: e.tensor_reduce(out=Sm["m2"], in_=g3(W["b2"]), axis=AX.X, op=ALU.max), [K("r_b2")], [K("r_m2")])
                V(lambda e: e.tensor_tensor(out=Sm["gs"], in0=Sm["m1"], in1=Sm["m2"], op=ALU.add), [K("r_m1"), K("r_m2")], [K("r_gs")])
                V(lambda e: e.max(out=Sm["t8"], in_=Sm["gs"]), [K("r_gs")], [K("r_t8")])
                V(lambda e: e.tensor_scalar(out=Sm["gsel"], in0=Sm["gs"], scalar1=Sm["t8"][:, 3:4], scalar2=None, op0=ALU.is_ge), [K("r_gs"), K("r_t8")], [K("r_gsel")])
                V(lambda e: e.tensor_scalar(out=Sm["goff"], in0=Sm["gsel"], scalar1=10.0, scalar2=-10.0, op0=ALU.mult, op1=ALU.add), [K("r_gsel")], [K("r_goff")])
                V(lambda e: e.tensor_tensor(out=g3(W["mk"]), in0=g3(W["bi"]), in1=b3(Sm["gsel"]), op=ALU.mult), [K("r_bi"), K("r_gsel")], [K("r_mk")])
                V(lambda e: e.tensor_tensor(out=g3(W["mk"]), in0=g3(W["mk"]), in1=b3(Sm["goff"]), op=ALU.add), [K("r_mk"), K("r_goff")], [K("r_mk")])
                V(lambda e: e.max(out=Sm["t8e"], in_=W["mk"]), [K("r_mk")], [K("r_t8e")])
                V(lambda e: e.tensor_scalar(out=W["sel"], in0=W["mk"], scalar1=Sm["t8e"][:, 7:8], scalar2=None, op0=ALU.is_ge), [K("r_mk"), K("r_t8e")], [K("r_sel")])
                V(lambda e: e.tensor_tensor(out=W["gw"], in0=W["sel"], in1=W["sc"], op=ALU.mult), [K("r_sel"), K("r_sc")], [K("r_gw")])
                V(lambda e: e.tensor_reduce(out=s1[:, 0:1], in_=W["gw"], axis=AX.X, op=ALU.add), [K("r_gw")], [K("r_s1")])
                V(lambda e: e.reciprocal(out=s1[:, 1:2], in_=s1[:, 0:1]), [K("r_s1")], [K("r_s2")])
                V(lambda e: e.tensor_scalar(out=W["gates"], in0=W["gw"], scalar1=s1[:, 1:2], scalar2=2.5, op0=ALU.mult, op1=ALU.mult), [K("r_gw"), K("r_s2")], [K("r_gates")])
                A("pe", lambda e: e.transpose(banks[bt][0:64, 0:128], W["gates"], ident), [K("r_gates"), "consts"], [PS(bt)])
                A("act", lambda e: e.copy(out=gT[:, tsl], in_=banks[bt][0:64, 0:128]), [PS(bt)], [("gT", tt)])
                return ops

            for g0 in range(0, S // 128, NI):
                lists = [tile_ops(tt) for tt in range(g0, g0 + NI)]
                for k in range(len(lists[0])):
                    for ol in lists:
                        eng, fn, r, w = ol[k]
                        p.op(eng, fn, reads=r, writes=w)
            p.dma("gst", gscr_d[l % 2], gT, reads=[("gT", tt) for tt in range(S // 128)], writes=[("gscr", l % 2)])

        def moe(l, cv, hooks):
            stg = {n: cv.get([128, 8, 256]) for n in ("w1", "w3")}
            stg["w2"] = cv.get([128, 2, D])
            wbf = [{"w1": cv.get([128, 8, 256], BF16), "w3": cv.get([128, 8, 256], BF16), "w2": cv.get([128, 2, D], BF16)} for _ in range(2)]
            gbc = [cv.get([128, S]) for _ in range(2)]
            sS = [[cv.get([128, TB], BF16) for f in range(2)] for _ in range(2)]
            tS = [[cv.get([128, TB], BF16) for f in range(2)] for _ in range(2)]
            hid = [[cv.get([128, TB], BF16) for f in range(2)] for _ in range(2)]
            steps = [(e, tb) for e in range(n_exp) for tb in range(NTB)]

            def load(e):
                sl = e % 2
                if e < NE:
                    srcs = {"w1": ew1_d[l, e], "w3": ew3_d[l, e], "w2": ew2_d[l, e]}
                else:
                    srcs = {"w1": sw1_d[l], "w3": sw3_d[l], "w2": sw2_d[l]}
                for n in ("w1", "w3", "w2"):
                    pat = "(kc p) f -> p kc f"
                    p.dma(("wst", n), stg[n], srcs[n].rearrange(pat, p=128), writes=[("stg", n)])
                if e < NE:
                    p.dma(("gbc", sl), gbc[sl], gscr_d[l % 2, e:e + 1, :].partition_broadcast(128), reads=[("gscr", l % 2)], writes=[("gbc", sl)])

            def cast(e):
                sl = e % 2
                for n in ("w1", "w3", "w2"):
                    if n == "w2":
                        parts = [(slice(0, 1), "act"), (slice(1, 2), "pool")]
                    else:
                        parts = [(slice(0, 3), "act"), (slice(3, 8), "pool")]
                    for (ps_, ce) in parts:
                        if ce == "act":
                            p.op("act", lambda e_, n=n, sl=sl, ps_=ps_: e_.copy(out=wbf[sl][n][:, ps_, :], in_=stg[n][:, ps_, :]), reads=[("stg", n)], writes=[("wbf", sl, n, ce)])
                        else:
                            p.op("pool", lambda e_, n=n, sl=sl, ps_=ps_: e_.tensor_copy(out=wbf[sl][n][:, ps_, :], in_=stg[n][:, ps_, :]), reads=[("stg", n)], writes=[("wbf", sl, n, ce)])

            def up(i, f):
                e, tb = steps[i]
                sl = e % 2
                for wi, n in enumerate(("w1", "w3")):
                    bk = f * 2 + wi

                    def mm(e_, n=n, bk=bk, sl=sl, tb=tb, f=f):
                        ins = None
                        for kc in range(8):
                            ins = e_.matmul(banks[bk][:, :], lhsT=wbf[sl][n][:, kc, f * 128:(f + 1) * 128], rhs=hT[:, kc, tb * TB:(tb + 1) * TB], start=(kc == 0), stop=(kc == 7))
                        return ins
                    p.op("pe", mm, reads=[("wbf", sl, n, "act"), ("wbf", sl, n, "pool")] + [("h", kc, tb) for kc in range(8)], writes=[PS(bk)])

            def gating(i, f):
                e, tb = steps[i]
                sl = e % 2
                par = i % 2
                p.op("act", lambda e_: e_.activation(out=sS[par][f], in_=banks[f * 2][:, :], func=AF.Silu), reads=[PS(f * 2)], writes=[("sS", par, f)])
                if e < NE:
                    p.op("dve", lambda e_: e_.tensor_tensor(out=tS[par][f], in0=banks[f * 2 + 1][:, :], in1=gbc[sl][:, tb * TB:(tb + 1) * TB], op=ALU.mult),
                         reads=[PS(f * 2 + 1), ("gbc", sl)], writes=[("tS", par, f)])
                    p.op("dve", lambda e_: e_.tensor_tensor(out=hid[par][f], in0=sS[par][f], in1=tS[par][f], op=ALU.mult),
                         reads=[("sS", par, f), ("tS", par, f)], writes=[("hid", par, f)])
                else:
                    p.op("dve", lambda e_: e_.tensor_tensor(out=hid[par][f], in0=banks[f * 2 + 1][:, :], in1=sS[par][f], op=ALU.mult),
                         reads=[PS(f * 2 + 1), ("sS", par, f)], writes=[("hid", par, f)])

            def down(i, dh):
                e, tb = steps[i]
                sl = e % 2
                par = i % 2
                for dq in range(4):
                    d = dh * 4 + dq
                    bk = 4 + dq

                    def mm(e_, d=d, bk=bk):
                        ins = None
                        for f in range(2):
                            ins = e_.matmul(banks[bk][:, :], lhsT=wbf[sl]["w2"][:, f, d * 128:(d + 1) * 128], rhs=hid[par][f], start=(f == 0), stop=(f == 1))
                        return ins
                    p.op("pe", mm, reads=[("wbf", sl, "w2", "act"), ("wbf", sl, "w2", "pool"), ("hid", par, 0), ("hid", par, 1)], writes=[PS(bk)])
                    p.op("dve", lambda e_, d=d, bk=bk: e_.scalar_tensor_tensor(out=xT[:, d, tb * TB:(tb + 1) * TB], in0=banks[bk][:, :], scalar=MOD(l, 5, d), in1=xT[:, d, tb * TB:(tb + 1) * TB], op0=ALU.mult, op1=ALU.add),
                         reads=[PS(bk), ("x", d, tb)] + modkeys(l), writes=[("x", d, tb)])

            load(0)
            cast(0)
            if n_exp > 1:
                load(1)
                cast(1)
            up(0, 0)
            up(0, 1)
            gating(0, 0)
            gating(0, 1)
            for i in range(len(steps)):
                e, tb = steps[i]
                if tb == 0 and i > 0 and e + 1 < n_exp:
                    load(e + 1)
                if tb == 2 and e > 0 and e + 1 < n_exp:
                    cast(e + 1)
                if tb == 1 and e in hooks:
                    hooks[e]()
                nxt = i + 1 < len(steps)
                if nxt:
                    up(i + 1, 0)
                down(i, 0)
                if nxt:
                    gating(i + 1, 0)
                    up(i + 1, 1)
                down(i, 1)
                if nxt:
                    gating(i + 1, 1)


        def ssd_units(l, cv, mark, load_win, load_wout, proj_fm, proj_tm, out_proj, conv_silu, yblk, identb, ones512b):
            cv.off = mark
            dtb = cv.get([128, 16]); alog = cv.get([128, 16]); aneg = cv.get([128, 16])
            cbuf = [cv.get([128, 3 + TB]) for _ in range(6)]
            ctmp = cv.get([128, TB])
            xs = [cv.get([128, TB], BF16) for _ in range(4)]
            BT = cv.get([128, TB], BF16); CT = cv.get([128, TB], BF16)
            sz = [cv.get([128, TB], BF16) for _ in range(4)]
            yg = cv.get([128, 4, TB])
            dt_tm = cv.get([128, 8]); dA = cv.get([128, 8]); acs = cv.get([128, 8]); dte = cv.get([128, 8])
            w2 = cv.get([128, 8]); draw = cv.get([128, 8]); ex = cv.get([128, 8])
            dAb = cv.get([128, 8, 128])
            L = dAb
            Btmz = [cv.get([128, 128], BF16) for _ in range(2)]
            decbc = cv.get([128, 2, 8])
            eD = cv.get([128, 8, 128], BF16)
            cbm = cv.get([128, 128])
            MT = cv.get([128, 8, 128], BF16)
            CTs = cv.get([128, 8, 128], BF16)
            xdt = cv.get([128, 8, 64], BF16); xw = cv.get([128, 8, 64], BF16)
            Btm = cv.get([128, 128], BF16)
            S32 = cv.get([128, 8, 64])
            Sbf = [cv.get([128, 8, 64], BF16) for _ in range(2)]
            sqb = cv.get([128, TB], BF16); rs = ctmp
            b7 = banks[7][:, :].bitcast(BF16)
            MASK = consts[:, C_MASK:C_MASK + 128]
            SUF = consts[:, C_SUF:C_SUF + 128]
            p.dma("dtb", dtb, prow_d[l:l + 1, PR_DTB:PR_DTB + 16].partition_broadcast(128), writes=["dtb"])
            p.dma("alog", alog, prow_d[l:l + 1, PR_ALOG:PR_ALOG + 16].partition_broadcast(128), writes=["alog"])
            p.op("act", lambda e: e.activation(out=aneg, in_=alog, func=AF.Exp), reads=["alog"], writes=["aneg"])
            p.op("dve", lambda e: e.tensor_scalar_mul(out=aneg, in0=aneg, scalar1=-1.0), reads=["aneg"], writes=["aneg"])
            for g in range(2):
                load_win(3600 + g * 512, 512, 0)
                load_win(2576 + g * 512, 512, 512)
                load_win(4624 + g * 128, 128, 1024)
                load_win(4880 + g * 128, 128, 1152)
                load_win(5136 + g * 8, 8, 1280)
                load_wout(8 + g * 4, 4)
                for j6 in range(6):
                    p.op("pool", lambda e, j6=j6: e.memset(cbuf[j6][:, 0:3], 0.0), writes=[("cbuf", j6)])
                p.op("pool", lambda e: e.memset(S32, 0.0), writes=["s_S32"])
                p.op("pool", lambda e: e.memset(Sbf[0], 0.0), writes=[("s_Sbf", 0)])
                for half in range(2):
                    p.op("pool", lambda e, half=half: e.memset(Btmz[half], 0.0), writes=[("s_Btmz", half)])
                par = 0
                gs = slice(g * 8, g * 8 + 8)
                for tb in range(NTB):
                    for j6 in range(6):
                        off = j6 * 128 if j6 < 4 else (1024 if j6 == 4 else 1152)
                        jc = g * 4 + j6 if j6 < 4 else (8 + g if j6 == 4 else 10 + g)
                        bank = j6 % 2
                        proj_fm(off, 128, tb, bank)
                        dst = xs[j6] if j6 < 4 else (BT if j6 == 4 else CT)
                        conv_silu(cbuf[j6], ("cbuf", j6), tb, bank, PP_SCW + jc * 4, PP_SCB + jc, ctmp, "s_ctmp", dst, ("s_fm", j6), AF.Silu, eng="dve")
                    for q in range(4):
                        proj_fm(512 + q * 128, 128, tb, q % 2)
                        p.op("act", lambda e, q=q: e.activation(out=sz[q], in_=banks[q % 2][:, :], func=AF.Silu), reads=[PS(q % 2)], writes=[("s_sz", q)])
                    for tt in range(4):
                        tsl = slice(tt * 128, (tt + 1) * 128)
                        proj_tm(1280, 8, tb, tt, 5)
                        p.op("dve", lambda e, gs=gs: e.tensor_tensor(out=draw, in0=banks[5][:, 0:8], in1=dtb[:, gs], op=ALU.add), reads=[PS(5), "dtb"], writes=["s_draw"])
                        p.op("act", lambda e: e.activation(out=ex, in_=draw, func=AF.Exp), reads=["s_draw"], writes=["s_ex"])
                        p.op("act", lambda e: e.activation(out=dt_tm, in_=ex, func=AF.Ln, bias=LNEPS[:, 2:3]), reads=["s_ex", "lneps"], writes=["s_dt"])
                        p.op("dve", lambda e, gs=gs: e.tensor_tensor(out=dA, in0=dt_tm, in1=aneg[:, gs], op=ALU.mult), reads=["s_dt", "aneg"], writes=["s_dA"])

                        def mm5(e):
                            e.matmul(banks[5][:, 8:16], lhsT=MASK, rhs=dA, start=True, stop=True)
                            e.matmul(banks[5][:, 16:24], lhsT=SUF, rhs=dA, start=True, stop=True)
                            e.matmul(banks[5][:, 160:168], lhsT=consts[:, C_CH0:C_CH0 + 128], rhs=dA, start=True, stop=True)
                            return e.matmul(banks[5][:, 168:176], lhsT=consts[:, C_CH1:C_CH1 + 128], rhs=dA, start=True, stop=True)
                        p.op("pe", mm5, reads=["s_dA", "consts"], writes=[PS(5)])
                        p.op("act", lambda e: e.copy(out=acs, in_=banks[5][:, 8:16]), reads=[PS(5)], writes=["s_acs"])
                        p.op("act", lambda e: e.activation(out=dte, in_=banks[5][:, 16:24], func=AF.Exp), reads=[PS(5)], writes=["s_dte"])
                        p.op("act", lambda e: e.copy(out=dAb, in_=dA.unsqueeze(2).to_broadcast([128, 8, 128])), reads=["s_dA"], writes=["s_dAb", ("s_L", 0), ("s_L", 1), "s_Lm", "s_Le"])
                        p.op("act", lambda e: e.activation(out=decbc, in_=banks[5][:, 160:176].rearrange("p (a b) -> p a b", a=2), func=AF.Exp), reads=[PS(5)], writes=["s_dec"])
                        p.op("dve", lambda e: e.tensor_tensor(out=w2, in0=dt_tm, in1=dte, op=ALU.mult), reads=["s_dt", "s_dte"], writes=["s_w2"])

                        def mmD(e):
                            ins = None
                            for h in range(8):
                                ins = e.matmul(banks[2 + h // 4][:, (h % 4) * 128:(h % 4 + 1) * 128], lhsT=dAb[:, h, :], rhs=MASK, start=True, stop=True)
                            return ins
                        p.op("pe", mmD, reads=["s_dAb", "consts"], writes=[PS(2), PS(3)])
                        for k in range(2):
                            p.op("dve", lambda e, k=k: e.tensor_tensor(out=L[:, 4 * k:4 * k + 4, :], in0=banks[2 + k][:, :].rearrange("p (a b) -> p a b", a=4),
                                                                       in1=acs[:, 4 * k:4 * k + 4].unsqueeze(2).to_broadcast([128, 4, 128]), op=ALU.subtract),
                                 reads=[PS(2 + k), "s_acs"], writes=[("s_L", k)])
                            p.op("act", lambda e, k=k: e.activation(out=eD[:, 4 * k:4 * k + 4, :], in_=banks[2 + k][:, :].rearrange("p (a b) -> p a b", a=4), func=AF.Exp), reads=[PS(2 + k)], writes=[("s_eD", k)])
                        p.op("dve", lambda e: e.tensor_scalar_min(out=L, in0=L, scalar1=0.0), reads=[("s_L", 0), ("s_L", 1)], writes=["s_Lm"])
                        p.op("act", lambda e: e.activation(out=L, in_=L, func=AF.Exp), reads=["s_Lm"], writes=["s_Le"])
                        p.op("pe", lambda e, tsl=tsl: e.matmul(banks[5][:, 256:384], lhsT=BT[:, tsl], rhs=CT[:, tsl], start=True, stop=True), reads=[("s_fm", 4), ("s_fm", 5)], writes=[PS(5)])
                        p.op("dve", lambda e: e.tensor_tensor(out=cbm, in0=banks[5][:, 256:384], in1=MASK, op=ALU.mult), reads=[PS(5), "consts"], writes=["s_cbm"])
                        p.op("dve", lambda e: e.tensor_tensor(out=MT, in0=L, in1=cbm.unsqueeze(1).to_broadcast([128, 8, 128]), op=ALU.mult), reads=["s_Le", "s_cbm"], writes=["s_MT"])
                        p.op("dve", lambda e, tsl=tsl: e.tensor_tensor(out=CTs, in0=eD, in1=CT[:, tsl].unsqueeze(1).to_broadcast([128, 8, 128]), op=ALU.mult), reads=[("s_eD", 0), ("s_eD", 1), ("s_fm", 5)], writes=["s_CTs"])

                        def mmT(e, tsl=tsl):
                            for q in range(4):
                                e.transpose(b7[:, q * 128:(q + 1) * 128], xs[q][:, tsl], identb)
                            return e.transpose(b7[:, 512:640], BT[:, tsl], identb)
                        p.op("pe", mmT, reads=[("s_fm", j) for j in range(5)] + ["identb"], writes=[PS(7)])
                        xtm = b7[:, 0:512].rearrange("p (h k) -> p h k", h=8)
                        p.op("dve", lambda e: e.tensor_tensor(out=xdt, in0=xtm, in1=dt_tm.unsqueeze(2).to_broadcast([128, 8, 64]), op=ALU.mult), reads=[PS(7), "s_dt"], writes=["s_xdt"])
                        p.op("dve", lambda e: e.tensor_tensor(out=xw, in0=xtm, in1=w2.unsqueeze(2).to_broadcast([128, 8, 64]), op=ALU.mult), reads=[PS(7), "s_w2"], writes=["s_xw"])
                        for half in range(2):
                            p.op("act", lambda e, half=half: e.copy(out=Btmz[half][half * 64:(half + 1) * 64, :], in_=b7[half * 64:(half + 1) * 64, 512:640]), reads=[PS(7)], writes=[("s_Btmz", half)])

                        def mmY(e):
                            ins = None
                            for h in range(8):
                                q, hq = h // 2, h % 2
                                ins = e.matmul(banks[4][hq * 64:(hq + 1) * 64, q * 128:(q + 1) * 128], lhsT=xdt[:, h, :], rhs=MT[:, h, :], start=True, stop=True)
                            return ins
                        p.op("pe", mmY, reads=["s_xdt", "s_MT"], writes=[PS(4)])
                        for half in range(2):
                            hs = slice(half * 64, (half + 1) * 64)

                            def mmO(e, half=half, par=par):
                                ins = None
                                for h in range(8):
                                    q, hq = h // 2, h % 2
                                    ins = e.matmul(banks[2][hq * 64:(hq + 1) * 64, q * 128 + half * 64:q * 128 + half * 64 + 64], lhsT=Sbf[par][:, h, :], rhs=CTs[:, h, half * 64:(half + 1) * 64], start=True, stop=True)
                                return ins
                            p.op("pe", mmO, reads=[("s_Sbf", par), "s_CTs"], writes=[("ps2o", half)] + ([PS(2)] if half == 0 else []))
                            p.op("pe", lambda e, half=half: e.matmul(banks[6][:, :], lhsT=Btmz[half], rhs=xw.rearrange("p h k -> p (h k)"), start=True, stop=True), reads=[("s_Btmz", half), "s_xw"], writes=[PS(6)])
                            p.op("dve", lambda e, half=half: e.tensor_tensor(out=S32, in0=S32, in1=decbc[:, half, :].unsqueeze(2).to_broadcast([128, 8, 64]), op=ALU.mult), reads=["s_S32", "s_dec"], writes=["s_S32"])
                            p.op("dve", lambda e: e.tensor_tensor(out=S32, in0=S32, in1=banks[6][:, :].rearrange("p (h k) -> p h k", h=8), op=ALU.add), reads=["s_S32", PS(6)], writes=["s_S32"])
                            p.op("act", lambda e, par=par: e.copy(out=Sbf[1 - par], in_=S32), reads=["s_S32"], writes=[("s_Sbf", 1 - par)])
                            par = 1 - par
                        for q in range(4):
                            p.op("dve", lambda e, q=q, tsl=tsl, g=g: e.scalar_tensor_tensor(out=yg[:, q, tsl], in0=xs[q][:, tsl], scalar=PPc(l, PP_SD + g * 4 + q), in1=banks[4][:, q * 128:(q + 1) * 128], op0=ALU.mult, op1=ALU.add),
                                 reads=[("s_fm", q), PS(4), "pp"], writes=[("s_yg", q)])
                            p.op("dve", lambda e, q=q, tsl=tsl: e.tensor_tensor(out=yg[:, q, tsl], in0=yg[:, q, tsl], in1=banks[2][:, q * 128:(q + 1) * 128], op=ALU.add),
                                 reads=[("s_yg", q), PS(2), ("ps2o", 0), ("ps2o", 1)], writes=[("s_yg", q)])
                    for q in range(4):
                        p.op("dve", lambda e, q=q: e.tensor_tensor(out=yg[:, q, :], in0=yg[:, q, :], in1=sz[q], op=ALU.mult), reads=[("s_yg", q), ("s_sz", q)], writes=[("s_yg", q)])
                    for q in range(4):
                        p.op("act", lambda e, q=q: e.activation(out=sqb, in_=yg[:, q, :], func=AF.Square), reads=[("s_yg", q)], writes=["s_sqb"])
                        p.op("pe", lambda e, q=q: e.matmul(banks[5][:, :], lhsT=ones512b, rhs=sqb, start=(q == 0), stop=(q == 3)), reads=["s_sqb", "ones512b"], writes=[PS(5)])
                    p.op("act", lambda e: e.activation(out=rs, in_=banks[5][:, :], func=AF.Ln, bias=LNEPS[:, 1:2]), reads=[PS(5), "lneps"], writes=["s_ctmp"])
                    p.op("act", lambda e: e.activation(out=rs, in_=rs, func=AF.Exp, scale=-0.5), reads=["s_ctmp"], writes=["s_ctmp"])
                    for q in range(4):
                        p.op("dve", lambda e, q=q, g=g: e.scalar_tensor_tensor(out=yblk[:, q, :], in0=yg[:, q, :], scalar=PPc(l, PP_SNORM + g * 4 + q), in1=rs, op0=ALU.mult, op1=ALU.mult),
                             reads=[("s_yg", q), "s_ctmp", "pp"], writes=[("yblk", q)])
                    out_proj(tb, 4, [0, 1])

        def mixer(l):
            cv = Carve()
            wst = [cv.get([128, 8, 128]) for _ in range(2)]
            wunit = cv.get([128, 8, 1408], BF16)
            woutst = [cv.get([128, D])] * 2
            wout = cv.get([128, 4, D], BF16)
            yblk = cv.get([128, 4, TB], BF16)
            identb = cv.get([128, 128], BF16)
            ones128b = cv.get([128, 128], BF16)
            ones512b = cv.get([128, 128], BF16)
            p.op("act", lambda e: e.copy(out=identb, in_=ident), reads=["consts"], writes=["identb"])
            p.op("pool", lambda e: e.memset(ones128b, 1.0 / 128.0), writes=["ones128b"])
            p.op("pool", lambda e: e.memset(ones512b, 1.0 / 512.0), writes=["ones512b"])
            wcnt = [0]

            def load_win(col0, ncols, dst):
                c = 0
                while c < ncols:
                    n = min(128, ncols - c)
                    k = wcnt[0] % 2
                    wcnt[0] += 1
                    p.dma(("wst", k), wst[k][:, :, 0:n], win_d[l].rearrange("(kc p) f -> p kc f", p=128)[:, :, col0 + c:col0 + c + n], writes=[("wst", k)])
                    eng = ("act", "pool")[k]
                    if eng == "act":
                        p.op("act", lambda e, k=k, n=n, c=c: e.copy(out=wunit[:, :, dst + c:dst + c + n], in_=wst[k][:, :, 0:n]), reads=[("wst", k)], writes=["wunit"])
                    else:
                        p.op("pool", lambda e, k=k, n=n, c=c: e.tensor_copy(out=wunit[:, :, dst + c:dst + c + n], in_=wst[k][:, :, 0:n]), reads=[("wst", k)], writes=["wunit"])
                    c += n

            def load_wout(ych0, n):
                for j in range(n):
                    k = 0
                    p.dma(("wost", k), woutst[k], wout_d[l, (ych0 + j) * 128:(ych0 + j + 1) * 128, :], writes=[("wost", k)])
                    p.op("pool", lambda e, k=k, j=j: e.tensor_copy(out=wout[:, j, :], in_=woutst[k]), reads=[("wost", k)], writes=["wout"])

            def proj_fm(off, ncols, tb, bank):
                def mm(e):
                    ins = None
                    for kc in range(8):
                        ins = e.matmul(banks[bank][0:ncols, :], lhsT=wunit[:, kc, off:off + ncols], rhs=hT[:, kc, tb * TB:(tb + 1) * TB], start=(kc == 0), stop=(kc == 7))
                    return ins
                p.op("pe", mm, reads=["wunit"] + [("h", kc, tb) for kc in range(8)], writes=[PS(bank)])

            def proj_tm(off, ncols, tb, tt, bank, col0=0):
                t0 = tb * TB + tt * 128

                def mm(e):
                    ins = None
                    for kc in range(8):
                        ins = e.matmul(banks[bank][:, col0:col0 + ncols], lhsT=hT[:, kc, t0:t0 + 128], rhs=wunit[:, kc, off:off + ncols], start=(kc == 0), stop=(kc == 7))
                    return ins
                p.op("pe", mm, reads=["wunit"] + [("h", kc, tb) for kc in range(8)], writes=[PS(bank)])

            def out_proj(tb, nych, bks):
                for d in range(8):
                    bk = bks[d % len(bks)]

                    def mm(e, d=d, bk=bk):
                        ins = None
                        for j in range(nych):
                            ins = e.matmul(banks[bk][:, :], lhsT=wout[:, j, d * 128:(d + 1) * 128], rhs=yblk[:, j, :], start=(j == 0), stop=(j == nych - 1))
                        return ins
                    p.op("pe", mm, reads=["wout"] + [("yblk", j) for j in range(nych)], writes=[PS(bk)])
                    p.op("dve", lambda e, d=d, bk=bk: e.scalar_tensor_tensor(out=xT[:, d, tb * TB:(tb + 1) * TB], in0=banks[bk][:, :], scalar=MOD(l, 2, d), in1=xT[:, d, tb * TB:(tb + 1) * TB], op0=ALU.mult, op1=ALU.add),
                         reads=[PS(bk), ("x", d, tb)] + modkeys(l), writes=[("x", d, tb)])

            def conv_silu(buf, key, tb, bank, wcol, bcol, tmp, tmpkey, dst, dstkey, act_func, eng="dve"):
                p.op("act", lambda e: e.copy(out=buf[:, 3:3 + TB], in_=banks[bank][:, :]), reads=[PS(bank)], writes=[key])
                p.op(eng, lambda e: e.tensor_scalar(out=tmp, in0=buf[:, 0:TB], scalar1=PPc(l, wcol), scalar2=PPc(l, bcol), op0=ALU.mult, op1=ALU.add), reads=[key, "pp"], writes=[tmpkey])
                for k in range(1, 4):
                    p.op(eng, lambda e, k=k: e.scalar_tensor_tensor(out=tmp, in0=buf[:, k:k + TB], scalar=PPc(l, wcol + k), in1=tmp, op0=ALU.mult, op1=ALU.add), reads=[key, tmpkey, "pp"], writes=[tmpkey])
                p.op("pool", lambda e: e.tensor_copy(out=buf[:, 0:3], in_=buf[:, TB:TB + 3]), reads=[key], writes=[key])
                if dst is not None:
                    p.op("act", lambda e: e.activation(out=dst, in_=tmp, func=act_func), reads=[tmpkey], writes=[dstkey])

            mark = cv.off

            if do_lru:
                cv.off = mark
                wabd = cv.get([128, 4, 128], BF16)
                wxbd = cv.get([128, 4, 128], BF16)
                nsp8 = cv.get([128, 4])
                hcar = cv.get([128, 4])
                xbuf = [cv.get([128, 3 + TB]) for _ in range(4)]
                Ts = [{n: cv.get([128, TB]) for n in ("xc", "r", "i", "a", "om", "ix", "u", "h", "gg")} for _ in range(2)]
                xcbs = [cv.get([128, TB], BF16) for _ in range(2)]
                for nm, src, dst in (("wa", wabd_d, wabd), ("wx", wxbd_d, wxbd)):
                    p.dma(("wost", 0), woutst[0][:, 0:512].rearrange("p (m j) -> p m j", m=4), src[l].rearrange("m i j -> i m j"), writes=[("wost", 0)])
                    p.op("pool", lambda e, dst=dst: e.tensor_copy(out=dst, in_=woutst[0][:, 0:512].rearrange("p (m j) -> p m j", m=4)), reads=[("wost", 0)], writes=[nm])
                p.op("act", lambda e: e.activation(out=nsp8, in_=pp[:, l, PP_LLAM:PP_LLAM + 4], func=AF.Exp, scale=-1.0), reads=["pp"], writes=["nsp8"])
                p.op("act", lambda e: e.activation(out=nsp8, in_=nsp8, func=AF.Ln, bias=LNEPS[:, 2:3]), reads=["nsp8", "lneps"], writes=["nsp8"])
                p.op("dve", lambda e: e.tensor_scalar_mul(out=nsp8, in0=nsp8, scalar1=-8.0), reads=["nsp8"], writes=["nsp8"])
                for m in range(4):
                    p.op("pool", lambda e, m=m: e.memset(xbuf[m][:, 0:3], 0.0), writes=[("xbuf", m)])
                load_win(0, 1024, 0)
                load_wout(0, 4)
                for tb in range(NTB):
                    for m in range(4):
                        par = m % 2
                        T = Ts[par]
                        xcb = xcbs[par]
                        B0, B1, B2, B3 = (0, 1, 2, 3) if par == 0 else (4, 5, 6, 7)
                        KK = lambda n, par=par: (n, par)
                        proj_fm(m * 128, 128, tb, B0)
                        proj_fm(512 + m * 128, 128, tb, B1)
                        conv_silu(xbuf[m], ("xbuf", m), tb, B0, PP_LCW + m * 4, PP_LCB + m, T["xc"], KK("l_xc"), None, None, None)
                        p.op("act", lambda e, T=T, xcb=xcb: e.copy(out=xcb, in_=T["xc"]), reads=[KK("l_xc")], writes=[KK("l_xcb")])
                        p.op("pe", lambda e, m=m, xcb=xcb, B2=B2: e.matmul(banks[B2][:, :], lhsT=wabd[:, m, :], rhs=xcb, start=True, stop=True), reads=["wa", KK("l_xcb")], writes=[PS(B2)])
                        p.op("pe", lambda e, m=m, xcb=xcb, B3=B3: e.matmul(banks[B3][:, :], lhsT=wxbd[:, m, :], rhs=xcb, start=True, stop=True), reads=["wx", KK("l_xcb")], writes=[PS(B3)])
                        p.op("act", lambda e, m=m, T=T, B2=B2: e.activation(out=T["r"], in_=banks[B2][:, :], func=AF.Sigmoid, bias=PPc(l, PP_LBA + m)), reads=[PS(B2), "pp"], writes=[KK("l_r")])
                        p.op("act", lambda e, m=m, T=T, B3=B3: e.activation(out=T["i"], in_=banks[B3][:, :], func=AF.Sigmoid, bias=PPc(l, PP_LBX + m)), reads=[PS(B3), "pp"], writes=[KK("l_i")])
                        p.op("act", lambda e, m=m, T=T: e.activation(out=T["a"], in_=T["r"], func=AF.Exp, scale=nsp8[:, m:m + 1]), reads=[KK("l_r"), "nsp8"], writes=[KK("l_a")])
                        p.op("pool", lambda e, T=T: e.tensor_tensor(out=T["om"], in0=T["a"], in1=T["a"], op=ALU.mult), reads=[KK("l_a")], writes=[KK("l_om")])
                        p.op("pool", lambda e, T=T: e.tensor_scalar(out=T["om"], in0=T["om"], scalar1=-1.0, scalar2=1.0, op0=ALU.mult, op1=ALU.add), reads=[KK("l_om")], writes=[KK("l_om")])
                        p.op("act", lambda e, T=T: e.activation(out=T["om"], in_=T["om"], func=AF.Sqrt), reads=[KK("l_om")], writes=[KK("l_om")])
                        p.op("pool", lambda e, T=T: e.tensor_tensor(out=T["ix"], in0=T["i"], in1=T["xc"], op=ALU.mult), reads=[KK("l_i"), KK("l_xc")], writes=[KK("l_ix")])
                        p.op("dve", lambda e, T=T: e.tensor_tensor(out=T["u"], in0=T["om"], in1=T["ix"], op=ALU.mult), reads=[KK("l_om"), KK("l_ix")], writes=[KK("l_u")])
                        if tb == 0:
                            p.op("dve", lambda e, T=T: e.tensor_tensor_scan(out=T["h"], data0=T["a"], data1=T["u"], initial=0.0, op0=ALU.mult, op1=ALU.add), reads=[KK("l_a"), KK("l_u")], writes=[KK("l_h")])
                        else:
                            p.op("dve", lambda e, m=m, T=T: e.tensor_tensor_scan(out=T["h"], data0=T["a"], data1=T["u"], initial=hcar[:, m:m + 1], op0=ALU.mult, op1=ALU.add), reads=[KK("l_a"), KK("l_u"), ("hcar", m)], writes=[KK("l_h")])
                        p.op("pool", lambda e, m=m, T=T: e.tensor_copy(out=hcar[:, m:m + 1], in_=T["h"][:, TB - 1:TB]), reads=[KK("l_h")], writes=[("hcar", m)])
                        p.op("act", lambda e, T=T, B1=B1: e.activation(out=T["gg"], in_=banks[B1][:, :], func=AF.Gelu_apprx_tanh), reads=[PS(B1)], writes=[KK("l_gg")])
                        p.op("dve", lambda e, m=m, T=T: e.tensor_tensor(out=yblk[:, m, :], in0=T["h"], in1=T["gg"], op=ALU.mult), reads=[KK("l_h"), KK("l_gg")], writes=[("yblk", m)])
                    out_proj(tb, 4, [0, 1, 2, 3, 4, 5, 6, 7])

            if do_gla:
                cv.off = mark
                walb = cv.get([32, 256], BF16)
                rT = cv.get([32, TB], BF16)
                e1 = cv.get([128, 128])
                l1 = cv.get([128, 128])
                ecp = cv.get([128, TB])
                ecn = cv.get([128, TB])
                esuf = [cv.get([128, 128]) for _ in range(4)]
                qd = cv.get([128, TB], BF16)
                kd = cv.get([128, TB], BF16)
                kend = [cv.get([128, 128], BF16) for _ in range(4)]
                kendz = [[cv.get([128, 128], BF16) for _ in range(2)] for _ in range(4)]
                qdz = [cv.get([128, TB], BF16) for _ in range(2)]
                CHM = [consts[:, C_CH0:C_CH0 + 128], consts[:, C_CH1:C_CH1 + 128]]
                vtm = [cv.get([128, 256], BF16) for _ in range(4)]
                sg = [cv.get([128, TB]) for _ in range(2)]
                attm = cv.get([128, 4, 128], BF16)
                S32 = cv.get([128, 128])
                Sbf = [cv.get([128, 128], BF16) for _ in range(2)]
                sqb = cv.get([128, TB], BF16)
                rs = cv.get([128, TB])
                t1 = cv.get([128, TB])
                p.dma(("wost", 0), woutst[0][0:32, 0:256], walpha_d[l], writes=[("wost", 0)])
                p.op("pool", lambda e: e.tensor_copy(out=walb, in_=woutst[0][0:32, 0:256]), reads=[("wost", 0)], writes=["walb"])
                p.op("pool", lambda e: e.memset(rT, 1.0), writes=["rT"])
                m64b = consts[:, C_MASK:C_MASK + 128].unsqueeze(1).to_broadcast([128, 4, 128])
                for hp in range(2):
                    load_win(1024 + hp * 128, 128, 0)
                    load_win(1280 + hp * 128, 128, 128)
                    load_win(1536 + hp * 256, 256, 256)
                    load_win(2048 + hp * 256, 256, 512)
                    load_win(2560, 16, 768)
                    load_wout(4 + hp * 2, 2)
                    p.op("pool", lambda e: e.memset(S32, 0.0), writes=["S32"])
                    p.op("pool", lambda e: e.memset(Sbf[0], 0.0), writes=[("Sbf", 0)])
                    if hp == 0:
                        for hh in range(2):
                            p.op("pool", lambda e, hh=hh: e.memset(qdz[hh], 0.0), writes=[("g_qdz", hh)])
                        for tt in range(4):
                            for half in range(2):
                                p.op("pool", lambda e, tt=tt, half=half: e.memset(kendz[tt][half], 0.0), writes=[("g_kendz", tt, half)])
                    par_state = {0: 0, 1: 0}
                    for tb in range(NTB):
                        proj_fm(768, 16, tb, 0)
                        p.op("act", lambda e: e.copy(out=rT[0:16, :], in_=banks[0][0:16, :]), reads=[PS(0)], writes=["rT"])
                        for tt in range(4):
                            tsl = slice(tt * 128, (tt + 1) * 128)
                            p.op("pe", lambda e, tsl=tsl, hp=hp: e.matmul(banks[5][:, 0:128], lhsT=rT[:, tsl], rhs=walb[:, hp * 128:(hp + 1) * 128], start=True, stop=True), reads=["rT", "walb"], writes=[PS(5)])
                            p.op("act", lambda e: e.activation(out=e1, in_=banks[5][:, 0:128], func=AF.Exp, scale=-1.0), reads=[PS(5)], writes=["g_e1"])
                            p.op("act", lambda e: e.activation(out=l1, in_=e1, func=AF.Ln, bias=LNEPS[:, 2:3]), reads=["g_e1", "lneps"], writes=["g_l1"])
                            p.op("pe", lambda e: e.matmul(banks[5][:, 128:256], lhsT=l1, rhs=consts[:, C_MASKG:C_MASKG + 128], start=True, stop=True), reads=["g_l1", "consts"], writes=[PS(5)])
                            p.op("pe", lambda e: e.matmul(banks[5][:, 256:384], lhsT=consts[:, C_MASKG + 128:C_MASKG + 256], rhs=l1, start=True, stop=True), reads=["g_l1", "consts"], writes=[PS(5)])
                            p.op("act", lambda e, tsl=tsl: e.activation(out=ecp[:, tsl], in_=banks[5][:, 128:256], func=AF.Exp), reads=[PS(5)], writes=["g_ecp"])
                            p.op("act", lambda e, tsl=tsl: e.activation(out=ecn[:, tsl], in_=banks[5][:, 128:256], func=AF.Exp, scale=-1.0), reads=[PS(5)], writes=["g_ecn"])
                            p.op("act", lambda e, tt=tt: e.activation(out=esuf[tt], in_=banks[5][:, 256:384], func=AF.Exp), reads=[PS(5)], writes=[("g_esuf", tt)])
                        proj_fm(0, 128, tb, 0)
                        for hh in range(2):
                            p.op("dve", lambda e, hh=hh: e.scalar_tensor_tensor(out=qdz[hh][hh * 64:(hh + 1) * 64, :], in0=banks[0][hh * 64:(hh + 1) * 64, :], scalar=0.125, in1=ecp[hh * 64:(hh + 1) * 64, :], op0=ALU.mult, op1=ALU.mult),
                                 reads=[PS(0), "g_ecp"], writes=[("g_qdz", hh)])
                        proj_fm(128, 128, tb, 1)
                        p.op("dve", lambda e: e.tensor_tensor(out=kd, in0=banks[1][:, :], in1=ecn, op=ALU.mult), reads=[PS(1), "g_ecn"], writes=["g_kd"])
                        for tt in range(4):
                            proj_tm(128, 128, tb, tt, 0)
                            for half in range(2):
                                p.op("dve", lambda e, tt=tt, half=half: e.tensor_tensor(out=kendz[tt][half][half * 64:(half + 1) * 64, :], in0=banks[0][half * 64:(half + 1) * 64, 0:128], in1=esuf[tt][half * 64:(half + 1) * 64, :], op=ALU.mult),
                                     reads=[PS(0), ("g_esuf", tt)], writes=[("g_kendz", tt, half)])
                            proj_tm(256, 256, tb, tt, 1)
                            p.op("act", lambda e, tt=tt: e.copy(out=vtm[tt], in_=banks[1][:, 0:256]), reads=[PS(1)], writes=[("g_vtm", tt)])
                        for hh in range(2):
                            proj_fm(512 + hh * 128, 128, tb, hh)
                            p.op("act", lambda e, hh=hh: e.activation(out=sg[hh], in_=banks[hh][:, :], func=AF.Silu), reads=[PS(hh)], writes=[("g_sg", hh)])
                        for hh in range(2):
                            b0 = hh * 64

                            def att(e, hh=hh):
                                ins = None
                                for tt in range(4):
                                    tsl = slice(tt * 128, (tt + 1) * 128)
                                    ins = e.matmul(banks[2][:, tsl], lhsT=kd[:, tsl], rhs=qdz[hh][:, tsl], start=True, stop=True)
                                return ins
                            p.op("pe", att, reads=["g_kd", ("g_qdz", hh)], writes=[PS(2)])
                            p.op("dve", lambda e: e.tensor_tensor(out=attm, in0=banks[2][:, :].rearrange("p (a b) -> p a b", a=4), in1=m64b, op=ALU.mult), reads=[PS(2), "consts"], writes=["g_attm"])
                            for c in range(8):
                                tt, half = c // 2, c % 2
                                csl = slice(c * 64, (c + 1) * 64)
                                tsl = slice(tt * 128, (tt + 1) * 128)
                                cur_par = par_state[hh]
                                if half == 0:
                                    p.op("pe", lambda e, tt=tt, tsl=tsl, hh=hh: e.matmul(banks[3][:, tsl], lhsT=vtm[tt][:, hh * 128:(hh + 1) * 128], rhs=attm[:, tt, :], start=True, stop=False),
                                         reads=[("g_vtm", tt), "g_attm"], writes=[PS(3)])
                                p.op("pe", lambda e, csl=csl, hh=hh, cur_par=cur_par, half=half: e.matmul(banks[3][:, csl], lhsT=Sbf[cur_par], rhs=qdz[hh][:, csl], start=False, stop=(half == 1)),
                                     reads=[("Sbf", cur_par), ("g_qdz", hh)], writes=[PS(3)])
                                p.op("pe", lambda e, tt=tt, half=half, hh=hh: e.matmul(banks[4][:, 0:128], lhsT=kendz[tt][half], rhs=vtm[tt][:, hh * 128:(hh + 1) * 128], start=True, stop=True),
                                     reads=[("g_kendz", tt, half), ("g_vtm", tt)], writes=[PS(4)])
                                col = c * 64 + 63
                                p.op("dve", lambda e, b0=b0, col=col: e.scalar_tensor_tensor(out=S32[b0:b0 + 64, :], in0=S32[b0:b0 + 64, :], scalar=ecp[b0:b0 + 64, col:col + 1], in1=banks[4][b0:b0 + 64, 0:128], op0=ALU.mult, op1=ALU.add),
                                     reads=["S32", "g_ecp", PS(4)], writes=["S32"])
                                nxt = 1 - cur_par
                                p.op("act", lambda e, nxt=nxt: e.copy(out=Sbf[nxt], in_=S32), reads=["S32"], writes=[("Sbf", nxt)])
                                par_state[hh] = nxt
                            p.op("act", lambda e: e.activation(out=sqb, in_=banks[3][:, :], func=AF.Square), reads=[PS(3)], writes=["g_sqb"])
                            p.op("pe", lambda e: e.matmul(banks[5][:, :], lhsT=ones128b, rhs=sqb, start=True, stop=True), reads=["ones128b", "g_sqb"], writes=[PS(5)])
                            p.op("act", lambda e: e.activation(out=rs, in_=banks[5][:, :], func=AF.Ln, bias=LNEPS[:, 1:2]), reads=[PS(5), "lneps"], writes=["g_rs"])
                            p.op("act", lambda e: e.activation(out=rs, in_=rs, func=AF.Exp, scale=-0.5), reads=["g_rs"], writes=["g_rs"])
                            p.op("dve", lambda e: e.tensor_tensor(out=t1, in0=banks[3][:, :], in1=rs, op=ALU.mult), reads=[PS(3), "g_rs"], writes=["g_t1"])
                            p.op("dve", lambda e, hh=hh: e.scalar_tensor_tensor(out=yblk[:, hh, :], in0=t1, scalar=PPc(l, PP_GNORM), in1=sg[hh], op0=ALU.mult, op1=ALU.mult), reads=["g_t1", ("g_sg", hh), "pp"], writes=[("yblk", hh)])
                        out_proj(tb, 2, [6, 7])

            if do_ssd:
                ssd_units(l, cv, mark, load_win, load_wout, proj_fm, proj_tm, out_proj, conv_silu, yblk, identb, ones512b)

        cv = Carve()
        ada_bufs = ada_bufs_alloc(cv)
        for blk in range(N_ADA):
            adaln_block(0, blk, ada_bufs, blk % 4)
        adaln_finish(0, 1)

        for tb in range(NTB):
            scale_alpha(tb, engs=("pool", "dve", "act"))
        for l in range(depth):
            p.new_epoch()
            for tb in range(NTB):
                modulate(l, 1, tb)
            barrier()
            if do_lru or do_gla or do_ssd:
                mixer(l)
            barrier()
            cv = Carve()
            layernorm(l, PP_LN1G, PP_LN1B, cv, 0, 1)
            for tb in range(NTB):
                modulate(l, 2, tb)
            barrier()
            cv = Carve()
            if do_moe:
                router(l, cv, 2, 3)
            barrier()
            cv = Carve()
            hooks = {}
            if l + 1 < depth:
                ada_bufs2 = ada_bufs_alloc(cv)
                for blk in range(N_ADA):
                    hooks[2 + blk] = (lambda blk=blk: adaln_block(l + 1, blk, ada_bufs2, 4 + blk % 4))
                hooks[2 + N_ADA] = (lambda: adaln_finish(l + 1, 4))
            if do_moe:
                moe(l, cv, hooks)
            else:
                for k in sorted(hooks):
                    hooks[k]()
            barrier()
            cv = Carve()
            layernorm(l, PP_LN2G, PP_LN2B, cv, 0, 1, final=(l == depth - 1))
            barrier()

        p.dma("out", yT_d.rearrange("(c p) t -> p c t", p=128), xT[:], reads=[("x", c, tb) for c in range(8) for tb in range(NTB)])
        p.emit()
    return nc


def prep_inputs(inputs):
    f = lambda a: np.ascontiguousarray(np.asarray(a, dtype=np.float32))
    L = DEPTH
    shared = {}
    shared["consts"] = build_consts()
    pc = lambda v, n: np.asarray(v, np.float32).reshape(n, 128).T
    pp = np.zeros((L, 128, NPP), np.float32)
    prow = np.zeros((L, NPR), np.float32)
    wabd = np.zeros((L, 4, 128, 128), np.float32)
    wxbd = np.zeros((L, 4, 128, 128), np.float32)
    wal = np.zeros((L, 32, 256), np.float32)
    for l in range(L):
        pp[l, :, PP_LN1G:PP_LN1G + 8] = pc(inputs["ln1_g"][l], 8)
        pp[l, :, PP_LN1B:PP_LN1B + 8] = pc(inputs["ln1_b"][l], 8)
        pp[l, :, PP_LN2G:PP_LN2G + 8] = pc(inputs["ln2_g"][l], 8)
        pp[l, :, PP_LN2B:PP_LN2B + 8] = pc(inputs["ln2_b"][l], 8)
        for m in range(4):
            for k in range(4):
                pp[l, :, PP_LCW + m * 4 + k] = inputs["lru_conv_w"][l, k, m * 128:(m + 1) * 128]
        pp[l, :, PP_LCB:PP_LCB + 4] = pc(inputs["lru_conv_b"][l], 4)
        pp[l, :, PP_LBA:PP_LBA + 4] = pc(inputs["lru_b_a"][l], 4)
        pp[l, :, PP_LBX:PP_LBX + 4] = pc(inputs["lru_b_x"][l], 4)
        pp[l, :, PP_LLAM:PP_LLAM + 4] = pc(inputs["lru_lambda"][l], 4)
        for j in range(12):
            for k in range(4):
                pp[l, :, PP_SCW + j * 4 + k] = inputs["ssd_conv_w"][l, k, j * 128:(j + 1) * 128]
        pp[l, :, PP_SCB:PP_SCB + 12] = pc(inputs["ssd_conv_b"][l], 12)
        pp[l, :, PP_SNORM:PP_SNORM + 8] = pc(inputs["ssd_norm"][l], 8)
        pp[l, :, PP_GNORM] = inputs["gla_norm"][l]
        pp[l, :, PP_SD:PP_SD + 8] = pc(np.repeat(np.asarray(inputs["ssd_d"][l]), 64), 8)
        prow[l, PR_DTB:PR_DTB + 16] = inputs["ssd_dt_bias"][l]
        prow[l, PR_ALOG:PR_ALOG + 16] = inputs["ssd_a_log"][l]
        prow[l, PR_RB:PR_RB + NE] = inputs["router_bias"][l]
        for m in range(4):
            for q in range(2):
                wabd[l, m, q * 64:(q + 1) * 64, q * 64:(q + 1) * 64] = inputs["lru_w_a"][l, 2 * m + q]
                wxbd[l, m, q * 64:(q + 1) * 64, q * 64:(q + 1) * 64] = inputs["lru_w_x"][l, 2 * m + q]
        wal[l, 0:16] = inputs["gla_w_alpha"][l]
        wal[l, 16] = inputs["gla_b_alpha"][l]
    shared.update(pp=pp, prow=prow, lru_wa_bd=wabd, lru_wx_bd=wxbd, walpha_ext=wal)
    for k in ("w_ada", "b_ada", "w_in", "w_out", "router_w", "exp_w1", "exp_w3", "exp_w2", "shared_w1", "shared_w3", "shared_w2"):
        shared[k] = f(inputs[k])
    x = np.asarray(inputs["x"], np.float32)
    c = np.asarray(inputs["c"], np.float32)
    maps = []
    for b in range(x.shape[0]):
        m = dict(shared)
        m["xT"] = np.ascontiguousarray(x[b].T)
        m["cpc"] = np.ascontiguousarray(c[b].reshape(8, 128).T)
        maps.append(m)
    return maps


_NC_CACHE = {}


def kernel(**inputs):
    maps = prep_inputs(inputs)
    if "nc" not in _NC_CACHE:
        _NC_CACHE["nc"] = build_program()
    nc = _NC_CACHE["nc"]
    res = run_bass_kernel_spmd(nc, maps, core_ids=list(range(len(maps))))
    out = np.stack([np.ascontiguousarray(r["yT"].T) for r in res.results], axis=0)
    return out.astype(np.float32)
```

```python
from contextlib import ExitStack
import numpy as np
import concourse.bass as bass
import concourse.mybir as mybir
from concourse.bass_utils import run_bass_kernel_spmd

F32 = mybir.dt.float32
BF16 = mybir.dt.bfloat16
AF = mybir.ActivationFunctionType
ALU = mybir.AluOpType
AX = mybir.AxisListType

D = 1024
S = 2048
DEPTH = 4
NE = 64
ALPHA = (2 * DEPTH) ** 0.25
DIN = 5152
NPP = 144
TB = 512
NTB = S // TB


class Prog:
    def __init__(self, nc, stack):
        self.nc = nc
        self.stack = stack
        self.ops = []
        self.last_write = {}
        self.readers = {}
        self.epoch = 0
        self.op_epoch = []
        self.group_open = {}
        self.group_of = {}
        self.nsem = 0
        self.bar = None
        self.xeng = []
        self.cap = None

    def barrier(self, fn):
        allk = list(set(self.last_write.keys()) | set(k for k, v in self.readers.items() if v))
        oid = self._add("dve", fn, allk, allk)
        self.bar = oid
        self.last_write = {}
        self.readers = {}

    def new_sem(self, name):
        self.nsem += 1
        return self.stack.enter_context(self.nc.semaphore(f"{name}_{self.nsem}"))

    def new_epoch(self):
        self.epoch += 1

    def _add(self, eng, fn, reads, writes, chan=None):
        oid = len(self.ops)
        raw = set()
        oth = set()
        xe = set()
        for k in reads:
            w = self.last_write.get(k)
            if w is not None:
                raw.add(w)
            if isinstance(k, tuple) and k[0] in ("ps", "ps2o"):
                for r in self.readers.get(k, ()):
                    xe.add(r)
        for k in writes:
            w = self.last_write.get(k)
            if w is not None:
                oth.add(w)
            for r in self.readers.get(k, ()):
                oth.add(r)
        if self.bar is not None:
            oth.add(self.bar)
        oth -= raw
        raw.discard(oid)
        oth.discard(oid)
        xe -= raw
        xe -= oth
        xe.discard(oid)
        self.xeng.append(sorted(xe))
        self.ops.append([eng, fn, sorted(raw), sorted(oth), chan])
        self.op_epoch.append(self.epoch)
        for k in reads:
            self.readers.setdefault(k, []).append(oid)
        for k in writes:
            self.last_write[k] = oid
            self.readers[k] = []
        return oid

    def op(self, eng, fn, reads=(), writes=()):
        if self.cap is not None:
            self.cap.append((eng, fn, list(reads), list(writes)))
            return None
        return self._add(eng, fn, reads, writes)

    def dma(self, chan, out, in_, reads=(), writes=(), eng="sp", more=False, **kw):
        def fn(e, out=out, in_=in_, kw=kw):
            return e.dma_start(out=out, in_=in_, **kw)
        oid = self._add(eng, fn, reads, writes, chan=chan)
        g = self.group_open.get(chan)
        if g is None:
            g = []
            self.group_open[chan] = g
        g.append(oid)
        self.group_of[oid] = g
        if not more:
            self.group_open[chan] = None
        return oid

    def emit(self):
        nc = self.nc
        n = len(self.ops)
        is_dma = [o[4] is not None for o in self.ops]
        need_sig = [False] * n
        deps_of = []
        for i, (eng, fn, raw, oth, chan) in enumerate(self.ops):
            deps = []
            for d in raw:
                if is_dma[d] or is_dma[i] or self.ops[d][0] != eng or eng != "pe":
                    deps.append(d)
            for d in oth:
                if is_dma[d] or is_dma[i] or self.ops[d][0] != eng or eng != "pe":
                    deps.append(d)
            for d in self.xeng[i]:
                if self.ops[d][0] != eng:
                    deps.append(d)
            deps_of.append(deps)
            for d in deps:
                if not is_dma[d]:
                    need_sig[d] = True
        sig = [None] * n
        cur = {}
        chan_sem = {}
        chan_cnt = {}
        for i, (eng, fn, raw, oth, chan) in enumerate(self.ops):
            if is_dma[i]:
                if chan not in chan_sem:
                    chan_sem[chan] = self.new_sem("d")
                    chan_cnt[chan] = 0
                chan_cnt[chan] += 16
                sig[i] = (chan_sem[chan], chan_cnt[chan])
            elif need_sig[i]:
                key = (eng, self.op_epoch[i])
                if key not in cur:
                    cur[key] = [self.new_sem(eng), 0]
                cur[key][1] += 1
                sig[i] = (cur[key][0], cur[key][1])
        for i in range(n):
            if is_dma[i]:
                last = self.group_of[i][-1]
                if last != i:
                    sig[i] = (sig[i][0], sig[last][1])
        streams = {}
        for i, (eng, fn, raw, oth, chan) in enumerate(self.ops):
            streams.setdefault(eng, []).append((i, fn, [sig[d] for d in deps_of[i]]))
        final_dma = [(chan_sem[c], chan_cnt[c]) for c in chan_sem]
        self.n_waits = 0

        def run_stream(e, items, tail):
            waited = {}
            for (i, fn, waits) in items:
                best = {}
                for (s, v) in waits:
                    k = s.num
                    if waited.get(k, 0) >= v:
                        continue
                    if k not in best or best[k][1] < v:
                        best[k] = (s, v)
                for k, (s, v) in best.items():
                    e.wait_ge(s, v)
                    waited[k] = v
                    self.n_waits += 1
                ins = fn(e)
                if is_dma[i]:
                    ins.then_inc(chan_sem[self.ops[i][4]], 16)
                elif sig[i] is not None:
                    ins.then_inc(sig[i][0], 1)
            for (s, v) in tail:
                e.wait_ge(s, v)

        with nc.Block() as block:
            names = {"pe": "tensor", "act": "scalar", "dve": "vector", "pool": "gpsimd", "sp": "sync"}
            for en, attr in names.items():
                items = streams.get(en, [])
                tail = final_dma if en == "sp" else []
                if not items and not tail:
                    continue

                def body(e, items=items, tail=tail):
                    run_stream(e, items, tail)
                getattr(block, attr)(body)


C_ID = 0
C_MASK = 128
C_SUF = 256
C_CH0 = 384
C_CH1 = 512
C_M64 = 640
C_ONESD = 704
C_ONE = 832
C_SEL = 960
C_MASKG = 1984
NCONST = 2240


def build_consts():
    c = np.zeros((128, NCONST), np.float32)
    idx = np.arange(128)
    same = (idx[:, None] // 64) == (idx[None, :] // 64)
    c[:, C_ID:C_ID + 128] = np.eye(128)
    c[:, C_MASK:C_MASK + 128] = same & (idx[:, None] <= idx[None, :])
    c[:, C_SUF:C_SUF + 128] = same & (idx[:, None] > idx[None, :])
    c[:64, C_CH0:C_CH0 + 128] = 1.0
    c[64:, C_CH1:C_CH1 + 128] = 1.0
    j = idx % 64
    c[:, C_M64:C_M64 + 64] = j[:, None] <= np.arange(64)[None, :]
    c[:, C_ONESD:C_ONESD + 128] = 1.0 / 1024.0
    c[:, C_ONE:C_ONE + 128] = 1.0
    for h in range(8):
        c[h, C_SEL + h * 128:C_SEL + (h + 1) * 128] = 1.0
    c[:, C_MASKG:C_MASKG + 128] = c[:, C_MASK:C_MASK + 128] * (-1.0 / 16.0)
    c[:, C_MASKG + 128:C_MASKG + 256] = c[:, C_SUF:C_SUF + 128] * (-1.0 / 16.0)
    return c


PP_LN1G, PP_LN1B, PP_LN2G, PP_LN2B = 0, 8, 16, 24
PP_LCW, PP_LCB, PP_LBA, PP_LBX, PP_LLAM = 32, 48, 52, 56, 60
PP_SCW, PP_SCB, PP_SNORM, PP_GNORM, PP_SD = 64, 112, 124, 132, 133
PR_DTB, PR_ALOG, PR_RB = 0, 16, 32
NPR = 96


def build_program(depth=DEPTH, do_lru=True, do_gla=True, do_ssd=True, do_moe=True, n_exp=NE + 1, debug=False):
    nc = bass.Bass("TRN2", target_bir_lowering=False, dynamic_dma_scratch_size=512)
    dr = {}

    def din(name, shape):
        dr[name] = nc.dram_tensor(name, list(shape), F32, kind="ExternalInput").ap()
        return dr[name]

    xT_d = din("xT", [D, S])
    cpc_d = din("cpc", [128, 8])
    consts_d = din("consts", [128, NCONST])
    pp_d = din("pp", [DEPTH, 128, NPP])
    prow_d = din("prow", [DEPTH, NPR])
    wada_d = din("w_ada", [DEPTH, D, 6 * D])
    bada_d = din("b_ada", [DEPTH, 6 * D])
    win_d = din("w_in", [DEPTH, D, DIN])
    wout_d = din("w_out", [DEPTH, 2 * D, D])
    wabd_d = din("lru_wa_bd", [DEPTH, 4, 128, 128])
    wxbd_d = din("lru_wx_bd", [DEPTH, 4, 128, 128])
    walpha_d = din("walpha_ext", [DEPTH, 32, 256])
    rw_d = din("router_w", [DEPTH, D, NE])
    ew1_d = din("exp_w1", [DEPTH, NE, D, 256])
    ew3_d = din("exp_w3", [DEPTH, NE, D, 256])
    ew2_d = din("exp_w2", [DEPTH, NE, 256, D])
    sw1_d = din("shared_w1", [DEPTH, D, 256])
    sw3_d = din("shared_w3", [DEPTH, D, 256])
    sw2_d = din("shared_w2", [DEPTH, 256, D])
    yT_d = nc.dram_tensor("yT", [D, S], F32, kind="ExternalOutput").ap()
    gscr_d = nc.dram_tensor("gscr", [2, NE, S], F32, kind="Internal").ap()
    dbg = {}

    with ExitStack() as st:
        p = Prog(nc, st)

        def sb(name, shape, dt=F32):
            return st.enter_context(nc.sbuf_tensor(name, list(shape), dt))

        xT = sb("xT_sb", [128, 8, S])
        hT = sb("hT_sb", [128, 8, S], BF16)
        consts = sb("consts_sb", [128, NCONST])
        pp = sb("pp_sb", [128, DEPTH, NPP])
        mod = sb("mod_sb", [128, DEPTH, 64])
        cond = sb("cond_sb", [128, 8])
        SCRW = 29150
        scr = sb("scr_sb", [128, SCRW])
        banks = [st.enter_context(nc.psum_tensor(f"bank{i}", [128, 512], F32)) for i in range(8)]

        def PS(i):
            return ("ps", i)

        class Carve:
            def __init__(self):
                self.off = 0

            def get(self, shape, dt=F32):
                n = int(np.prod(shape[1:]))
                words = n if dt == F32 else (n + 1) // 2
                a = scr[:, self.off:self.off + words]
                self.off += words
                assert self.off <= SCRW - 1, self.off
                if dt != F32:
                    a = a.bitcast(dt)
                    if n % 2:
                        a = a[:, 0:n]
                if len(shape) == 3:
                    a = a.rearrange("p (a b) -> p a b", a=shape[1])
                elif len(shape) == 4:
                    a = a.rearrange("p (a b c) -> p a b c", a=shape[1], b=shape[2])
                if shape[0] != 128:
                    a = a[0:shape[0]]
                return a

        ident = consts[:, C_ID:C_ID + 128]
        onesD = consts[:, C_ONESD:C_ONESD + 128]

        def barrier():
            tok = scr[:, SCRW - 1:SCRW]
            p.barrier(lambda e: e.memset(tok, 0.0))

        p.dma("ld0", consts[:], consts_d[:, :], writes=["consts"])
        p.dma("ld1", pp[:], pp_d.rearrange("l p n -> p l n"), writes=["pp"])
        p.dma("ld2", cond[:], cpc_d[:, :], writes=["cond"])
        p.dma("ldx", xT[:], xT_d.rearrange("(c p) t -> p c t", p=128), writes=[("x", c, tb) for c in range(8) for tb in range(NTB)])
        p.op("act", lambda e: e.activation(out=cond[:], in_=cond[:], func=AF.Silu), reads=["cond"], writes=["cond"])

        ADA_BLK = 256
        N_ADA = 6 * D // ADA_BLK
        ada_stg = [None, None]

        def adaln_block(l, blk, cv_bufs, bank):
            stg, brow, mrow = cv_bufs[blk % 2]
            key = ("adastg", blk % 2)
            c0 = blk * ADA_BLK
            nj = ADA_BLK // 128
            p.dma(("adab", blk % 2), brow, bada_d[l:l + 1, c0:c0 + ADA_BLK], writes=[("adabrow", blk % 2)])
            p.dma(("ada", blk % 2), stg, wada_d[l].rearrange("(kc p) f -> p kc f", p=128)[:, :, c0:c0 + ADA_BLK], writes=[key])

            def mm(e, stg=stg):
                ins = None
                for kc in range(8):
                    ins = e.matmul(banks[bank][0:1, 0:ADA_BLK], lhsT=cond[:, kc:kc + 1], rhs=stg[:, kc, :], start=(kc == 0), stop=(kc == 7))
                return ins
            p.op("pe", mm, reads=[key, "cond"], writes=[PS(bank)])
            p.op("dve", lambda e: e.tensor_tensor(out=mrow, in0=banks[bank][0:1, 0:ADA_BLK], in1=brow, op=ALU.add),
                 reads=[PS(bank), ("adabrow", blk % 2)], writes=[("adamrow", blk % 2)])

            def mm2(e):
                ins = None
                for j in range(nj):
                    ins = e.matmul(banks[bank][:, 256 + j:257 + j], lhsT=mrow[0:1, j * 128:(j + 1) * 128], rhs=consts[0:1, C_ONE:C_ONE + 1], start=True, stop=True)
                return ins
            p.op("pe", mm2, reads=[("adamrow", blk % 2), "consts"], writes=[PS(bank)])
            p.op("act", lambda e: e.copy(out=mod[:, l, blk * nj:(blk + 1) * nj], in_=banks[bank][:, 256:256 + nj]), reads=[PS(bank)], writes=[("mod", l)])

        def adaln_finish(l, bank):
            p.op("dve", lambda e: e.tensor_scalar(out=mod[:, l, 48:56], in0=mod[:, l, 8:16], scalar1=1.0, scalar2=1.0 / float(ALPHA), op0=ALU.add, op1=ALU.mult), reads=[("mod", l)], writes=[("modd", l)])
            p.op("dve", lambda e: e.tensor_scalar(out=mod[:, l, 56:64], in0=mod[:, l, 32:40], scalar1=1.0, scalar2=1.0 / float(ALPHA), op0=ALU.add, op1=ALU.mult), reads=[("mod", l)], writes=[("modd2", l)])

        def ada_bufs_alloc(cv):
            return [(cv.get([128, 8, ADA_BLK]), cv.get([1, ADA_BLK]), cv.get([1, ADA_BLK])) for _ in range(2)]

        def MOD(l, j, c):
            if j < 6:
                return mod[:, l, j * 8 + c:j * 8 + c + 1]
            return mod[:, l, 48 + (j - 6) * 8 + c:48 + (j - 6) * 8 + c + 1]

        def PPc(l, col):
            return pp[:, l, col:col + 1]

        modkeys = lambda l: [("mod", l), ("modd", l), ("modd2", l)]

        def modulate(l, which, tb, engs=("dve", "pool")):
            jsc, jsh = (6, 0) if which == 1 else (7, 3)
            for c in range(8):
                eng = engs[c % len(engs)]
                p.op(eng, lambda e, c=c: e.tensor_scalar(out=hT[:, c, tb * TB:(tb + 1) * TB], in0=xT[:, c, tb * TB:(tb + 1) * TB],
                                                         scalar1=MOD(l, jsc, c), scalar2=MOD(l, jsh, c), op0=ALU.mult, op1=ALU.add),
                     reads=[("x", c, tb)] + modkeys(l), writes=[("h", c, tb)])

        def scale_alpha(tb, engs=("pool",)):
            for c in range(8):
                eng = engs[c % len(engs)]
                if eng == "act":
                    p.op(eng, lambda e, c=c: e.mul(out=xT[:, c, tb * TB:(tb + 1) * TB], in_=xT[:, c, tb * TB:(tb + 1) * TB], mul=float(ALPHA)),
                         reads=[("x", c, tb)], writes=[("x", c, tb)])
                else:
                    p.op(eng, lambda e, c=c: e.tensor_scalar_mul(out=xT[:, c, tb * TB:(tb + 1) * TB], in0=xT[:, c, tb * TB:(tb + 1) * TB], scalar1=float(ALPHA)),
                         reads=[("x", c, tb)], writes=[("x", c, tb)])

        def layernorm(l, gcol, bcol, cv, bank_m, bank_q, final=False):
            def GB(col):
                return pp[:, l, col:col + 1] if final else ppA[:, l, col:col + 1]
            sq = [cv.get([128, TB]) for _ in range(2)]
            mean_sbs = [cv.get([128, TB]) for _ in range(2)]
            rstds = [cv.get([128, TB]) for _ in range(2)]
            tmp = [cv.get([128, TB]) for _ in range(2)]
            tmp2 = [cv.get([128, TB]) for _ in range(2)]
            bank_m0, bank_q0 = bank_m, bank_q
            for tb in range(NTB):
                sl = slice(tb * TB, (tb + 1) * TB)
                mean_sb = mean_sbs[tb % 2]
                rstd = rstds[tb % 2]
                bank_m = bank_m0 + 2 * (tb % 2)
                bank_q = bank_q0 + 2 * (tb % 2)
                KM = ("lnmean", tb % 2)
                KR = ("lnrstd", tb % 2)

                def mm_mean(e, sl=sl, bank_m=bank_m):
                    ins = None
                    for c in range(8):
                        ins = e.matmul(banks[bank_m][:, :], lhsT=onesD, rhs=xT[:, c, sl], start=(c == 0), stop=(c == 7))
                    return ins
                p.op("pe", mm_mean, reads=[("x", c, tb) for c in range(8)] + ["consts"], writes=[PS(bank_m)])
                for c in range(8):
                    p.op("act", lambda e, c=c, sl=sl: e.activation(out=sq[c % 2], in_=xT[:, c, sl], func=AF.Square), reads=[("x", c, tb)], writes=[("lnsq", c % 2)])
                    p.op("pe", lambda e, c=c, bank_q=bank_q: e.matmul(banks[bank_q][:, :], lhsT=onesD, rhs=sq[c % 2], start=(c == 0), stop=(c == 7)),
                         reads=[("lnsq", c % 2), "consts"], writes=[PS(bank_q)])
                p.op("act", lambda e, mean_sb=mean_sb, bank_m=bank_m: e.copy(out=mean_sb, in_=banks[bank_m][:, :]), reads=[PS(bank_m)], writes=[KM])
                p.op("dve", lambda e, rstd=rstd, mean_sb=mean_sb: e.tensor_tensor(out=rstd, in0=mean_sb, in1=mean_sb, op=ALU.mult), reads=[KM], writes=[KR])
                p.op("dve", lambda e, rstd=rstd, bank_q=bank_q: e.tensor_tensor(out=rstd, in0=banks[bank_q][:, :], in1=rstd, op=ALU.subtract), reads=[PS(bank_q), KR], writes=[KR])
                p.op("act", lambda e, rstd=rstd: e.activation(out=rstd, in_=rstd, func=AF.Ln, bias=LNEPS[:, 0:1]), reads=[KR, "lneps"], writes=[KR])
                p.op("act", lambda e, rstd=rstd: e.activation(out=rstd, in_=rstd, func=AF.Exp, scale=-0.5), reads=[KR], writes=[KR])
                for c in range(8):
                    k = c % 2
                    p.op("dve", lambda e, c=c, k=k, sl=sl, mean_sb=mean_sb: e.tensor_tensor(out=tmp[k], in0=xT[:, c, sl], in1=mean_sb, op=ALU.subtract),
                         reads=[("x", c, tb), KM], writes=[("lnt", k)])
                    p.op("pool", lambda e, k=k, rstd=rstd: e.tensor_tensor(out=tmp2[k], in0=tmp[k], in1=rstd, op=ALU.mult), reads=[("lnt", k), KR], writes=[("lnt2", k)])
                    p.op("act", lambda e, c=c, k=k, sl=sl: e.activation(out=xT[:, c, sl], in_=tmp2[k], func=AF.Identity, scale=GB(gcol + c), bias=GB(bcol + c)),
                         reads=[("lnt2", k), "pp", "ppA"], writes=[("x", c, tb)])

        ppA = sb("ppA_sb", [128, DEPTH, 32])
        p.op("dve", lambda e: e.tensor_scalar_mul(out=ppA[:], in0=pp[:, :, 0:32], scalar1=float(ALPHA)), reads=["pp"], writes=["ppA"])
        LNEPS = sb("lneps_sb", [128, 4])
        p.op("pool", lambda e: e.memset(LNEPS[:, 0:1], 1e-5), writes=["lneps"])
        p.op("pool", lambda e: e.memset(LNEPS[:, 1:2], 1e-6), reads=[], writes=["lneps"])
        p.op("pool", lambda e: e.memset(LNEPS[:, 2:3], 1.0), reads=[], writes=["lneps"])

        def router(l, cv, bank_l, bank_t):
            NI = 4
            rw = cv.get([128, 8, NE])
            rb = cv.get([128, NE])
            gT = cv.get([64, S])
            h32s = [cv.get([128, 8, 128]) for _ in range(NI)]
            Ws = [{n: cv.get([128, 64]) for n in ("sc", "bi", "eq", "b2", "mk", "sel", "gw", "gates")} for _ in range(NI)]
            Sms = [{n: cv.get([128, 8]) for n in ("m1", "m2", "gs", "t8", "gsel", "goff", "t8e")} for _ in range(NI)]
            s1s = [cv.get([128, 2]) for _ in range(NI)]
            p.dma("rw", rw, rw_d[l].rearrange("(kc p) e -> p kc e", p=128), writes=["rw"])
            p.dma("rb", rb, prow_d[l:l + 1, PR_RB:PR_RB + NE].partition_broadcast(128), writes=["rb"])
            g3 = lambda a: a.rearrange("p (g k) -> p g k", k=8)
            b3 = lambda a: a.unsqueeze(2).to_broadcast([128, 8, 8])

            def tile_ops(tt):
                j = tt % NI
                tb = tt // 4
                tsl = slice(tt * 128, (tt + 1) * 128)
                h32, W, Sm, s1 = h32s[j], Ws[j], Sms[j], s1s[j]
                bl, bt = j, 4 + j
                K = lambda n: (n, j)
                ops = []
                A = lambda eng, fn, r, w: ops.append((eng, fn, r, w))
                for c in range(8):
                    eng = ("dve", "pool")[c % 2]
                    A(eng, lambda e, c=c: e.tensor_scalar(out=h32[:, c, :], in0=xT[:, c, tsl], scalar1=MOD(l, 7, c), scalar2=MOD(l, 3, c), op0=ALU.mult, op1=ALU.add),
                      [("x", c, tb)] + modkeys(l), [("h32", j, c)])

                def mm(e):
                    ins = None
                    for c in range(8):
                        ins = e.matmul(banks[bl][:, 0:NE], lhsT=h32[:, c, :], rhs=rw[:, c, :], start=(c == 0), stop=(c == 7))
                    return ins
                A("pe", mm, [("h32", j, c) for c in range(8)] + ["rw"], [PS(bl)])
                A("act", lambda e: e.activation(out=W["sc"], in_=banks[bl][:, 0:NE], func=AF.Sigmoid), [PS(bl)], [K("r_sc")])
                V = lambda fn, r, w: A("dve", fn, r, w)
                V(lambda e: e.tensor_tensor(out=W["bi"], in0=W["sc"], in1=rb, op=ALU.add), [K("r_sc"), "rb"], [K("r_bi")])
                V(lambda e: e.tensor_reduce(out=Sm["m1"], in_=g3(W["bi"]), axis=AX.X, op=ALU.max), [K("r_bi")], [K("r_m1")])
                V(lambda e: e.tensor_tensor(out=g3(W["eq"]), in0=g3(W["bi"]), in1=b3(Sm["m1"]), op=ALU.is_equal), [K("r_bi"), K("r_m1")], [K("r_eq")])
                V(lambda e: e.scalar_tensor_tensor(out=W["b2"], in0=W["eq"], scalar=-10.0, in1=W["bi"], op0=ALU.mult, op1=ALU.add), [K("r_eq"), K("r_bi")], [K("r_b2")])
                V(lambda e: e.tensor_reduce(out=Sm["m2"], in_=g3(W["b2"]), axis=AX.X, op=ALU.max), [K("r_b2")], [K("r_m2")])
                V(lambda e: e.tensor_tensor(out=Sm["gs"], in0=Sm["m1"], in1=Sm["m2"], op=ALU.add), [K("r_m1"), K("r_m2")], [K("r_gs")])
                V(lambda e: e.max(out=Sm["t8"], in_=Sm["gs"]), [K("r_gs")], [K("r_t8")])
                V(lambda e: e.tensor_scalar(out=Sm["gsel"], in0=Sm["gs"], scalar1=Sm["t8"][:, 3:4], scalar2=None, op0=ALU.is_ge), [K("r_gs"), K("r_t8")], [K("r_gsel")])
                V(lambda e: e.tensor_scalar(out=Sm["goff"], in0=Sm["gsel"], scalar1=10.0, scalar2=-10.0, op0=ALU.mult, op1=ALU.add), [K("r_gsel")], [K("r_goff")])
                V(lambda e: e.tensor_tensor(out=g3(W["mk"]), in0=g3(W["bi"]), in1=b3(Sm["gsel"]), op=ALU.mult), [K("r_bi"), K("r_gsel")], [K("r_mk")])
                V(lambda e: e.tensor_tensor(out=g3(W["mk"]), in0=g3(W["mk"]), in1=b3(Sm["goff"]), op=ALU.add), [K("r_mk"), K("r_goff")], [K("r_mk")])
                V(lambda e: e.max(out=Sm["t8e"], in_=W["mk"]), [K("r_mk")], [K("r_t8e")])
                V(lambda e: e.tensor_scalar(out=W["sel"], in0=W["mk"], scalar1=Sm["t8e"][:, 7:8], scalar2=None, op0=ALU.is_ge), [K("r_mk"), K("r_t8e")], [K("r_sel")])
                V(lambda e: e.tensor_tensor(out=W["gw"], in0=W["sel"], in1=W["sc"], op=ALU.mult), [K("r_sel"), K("r_sc")], [K("r_gw")])
                V(lambda e: e.tensor_reduce(out=s1[:, 0:1], in_=W["gw"], axis=AX.X, op=ALU.add), [K("r_gw")], [K("r_s1")])
                V(lambda e: e.reciprocal(out=s1[:, 1:2], in_=s1[:, 0:1]), [K("r_s1")], [K("r_s2")])
                V(lambda e: e.tensor_scalar(out=W["gates"], in0=W["gw"], scalar1=s1[:, 1:2], scalar2=2.5, op0=ALU.mult, op1=ALU.mult), [K("r_gw"), K("r_s2")], [K("r_gates")])
                A("pe", lambda e: e.transpose(banks[bt][0:64, 0:128], W["gates"], ident), [K("r_gates"), "consts"], [PS(bt)])
                A("act", lambda e: e.copy(out=gT[:, tsl], in_=banks[bt][0:64, 0:128]), [PS(bt)], [("gT", tt)])
                return ops

            for g0 in range(0, S // 128, NI):
                lists = [tile_ops(tt) for tt in range(g0, g0 + NI)]
                for k in range(len(lists[0])):
                    for ol in lists:
                        eng, fn, r, w = ol[k]
                        p.op(eng, fn, reads=r, writes=w)
            p.dma("gst", gscr_d[l % 2], gT, reads=[("gT", tt) for tt in range(S // 128)], writes=[("gscr", l % 2)])

        def moe(l, cv, hooks):
            stg = {n: cv.get([128, 8, 256]) for n in ("w1", "w3")}
            stg["w2"] = cv.get([128, 2, D])
            wbf = [{"w1": cv.get([128, 8, 256], BF16), "w3": cv.get([128, 8, 256], BF16), "w2": cv.get([128, 2, D], BF16)} for _ in range(2)]
            gbc = [cv.get([128, S]) for _ in range(2)]
            sS = [[cv.get([128, TB], BF16) for f in range(2)] for _ in range(2)]
            tS = [[cv.get([128, TB], BF16) for f in range(2)] for _ in range(2)]
            hid = [[cv.get([128, TB], BF16) for f in range(2)] for _ in range(2)]
            steps = [(e, tb) for e in range(n_exp) for tb in range(NTB)]

            def load(e):
                sl = e % 2
                if e < NE:
                    srcs = {"w1": ew1_d[l, e], "w3": ew3_d[l, e], "w2": ew2_d[l, e]}
                else:
                    srcs = {"w1": sw1_d[l], "w3": sw3_d[l], "w2": sw2_d[l]}
                for n in ("w1", "w3", "w2"):
                    pat = "(kc p) f -> p kc f"
                    p.dma(("wst", n), stg[n], srcs[n].rearrange(pat, p=128), writes=[("stg", n)])
                if e < NE:
                    p.dma(("gbc", sl), gbc[sl], gscr_d[l % 2, e:e + 1, :].partition_broadcast(128), reads=[("gscr", l % 2)], writes=[("gbc", sl)])

            def cast(e):
                sl = e % 2
                for n in ("w1", "w3", "w2"):
                    if n == "w2":
                        parts = [(slice(0, 1), "act"), (slice(1, 2), "pool")]
                    else:
                        parts = [(slice(0, 3), "act"), (slice(3, 8), "pool")]
                    for (ps_, ce) in parts:
                        if ce == "act":
                            p.op("act", lambda e_, n=n, sl=sl, ps_=ps_: e_.copy(out=wbf[sl][n][:, ps_, :], in_=stg[n][:, ps_, :]), reads=[("stg", n)], writes=[("wbf", sl, n, ce)])
                        else:
                            p.op("pool", lambda e_, n=n, sl=sl, ps_=ps_: e_.tensor_copy(out=wbf[sl][n][:, ps_, :], in_=stg[n][:, ps_, :]), reads=[("stg", n)], writes=[("wbf", sl, n, ce)])

            def up(i, f):
                e, tb = steps[i]
                sl = e % 2
                for wi, n in enumerate(("w1", "w3")):
                    bk = f * 2 + wi

                    def mm(e_, n=n, bk=bk, sl=sl, tb=tb, f=f):
                        ins = None
                        for kc in range(8):
                            ins = e_.matmul(banks[bk][:, :], lhsT=wbf[sl][n][:, kc, f * 128:(f + 1) * 128], rhs=hT[:, kc, tb * TB:(tb + 1) * TB], start=(kc == 0), stop=(kc == 7))
                        return ins
                    p.op("pe", mm, reads=[("wbf", sl, n, "act"), ("wbf", sl, n, "pool")] + [("h", kc, tb) for kc in range(8)], writes=[PS(bk)])

            def gating(i, f):
                e, tb = steps[i]
                sl = e % 2
                par = i % 2
                p.op("act", lambda e_: e_.activation(out=sS[par][f], in_=banks[f * 2][:, :], func=AF.Silu), reads=[PS(f * 2)], writes=[("sS", par, f)])
                if e < NE:
                    p.op("dve", lambda e_: e_.tensor_tensor(out=tS[par][f], in0=banks[f * 2 + 1][:, :], in1=gbc[sl][:, tb * TB:(tb + 1) * TB], op=ALU.mult),
                         reads=[PS(f * 2 + 1), ("gbc", sl)], writes=[("tS", par, f)])
                    p.op("dve", lambda e_: e_.tensor_tensor(out=hid[par][f], in0=sS[par][f], in1=tS[par][f], op=ALU.mult),
                         reads=[("sS", par, f), ("tS", par, f)], writes=[("hid", par, f)])
                else:
                    p.op("dve", lambda e_: e_.tensor_tensor(out=hid[par][f], in0=banks[f * 2 + 1][:, :], in1=sS[par][f], op=ALU.mult),
                         reads=[PS(f * 2 + 1), ("sS", par, f)], writes=[("hid", par, f)])

            def down(i, dh):
                e, tb = steps[i]
                sl = e % 2
                par = i % 2
                for dq in range(4):
                    d = dh * 4 + dq
                    bk = 4 + dq

                    def mm(e_, d=d, bk=bk):
                        ins = None
                        for f in range(2):
                            ins = e_.matmul(banks[bk][:, :], lhsT=wbf[sl]["w2"][:, f, d * 128:(d + 1) * 128], rhs=hid[par][f], start=(f == 0), stop=(f == 1))
                        return ins
                    p.op("pe", mm, reads=[("wbf", sl, "w2", "act"), ("wbf", sl, "w2", "pool"), ("hid", par, 0), ("hid", par, 1)], writes=[PS(bk)])
                    p.op("dve", lambda e_, d=d, bk=bk: e_.scalar_tensor_tensor(out=xT[:, d, tb * TB:(tb + 1) * TB], in0=banks[bk][:, :], scalar=MOD(l, 5, d), in1=xT[:, d, tb * TB:(tb + 1) * TB], op0=ALU.mult, op1=ALU.add),
                         reads=[PS(bk), ("x", d, tb)] + modkeys(l), writes=[("x", d, tb)])

            load(0)
            cast(0)
            if n_exp > 1:
                load(1)
                cast(1)
            up(0, 0)
            up(0, 1)
            gating(0, 0)
            gating(0, 1)
            for i in range(len(steps)):
                e, tb = steps[i]
                if tb == 0 and i > 0 and e + 1 < n_exp:
                    load(e + 1)
                if tb == 2 and e > 0 and e + 1 < n_exp:
                    cast(e + 1)
                if tb == 1 and e in hooks:
                    hooks[e]()
                nxt = i + 1 < len(steps)
                if nxt:
                    up(i + 1, 0)
                down(i, 0)
                if nxt:
                    gating(i + 1, 0)
                    up(i + 1, 1)
                down(i, 1)
                if nxt:
                    gating(i + 1, 1)


        def ssd_units(l, cv, mark, load_win, load_wout, proj_fm, proj_tm, out_proj, conv_silu, yblk, identb, ones512b):
            cv.off = mark
            dtb = cv.get([128, 16]); alog = cv.get([128, 16]); aneg = cv.get([128, 16])
            cbuf = [cv.get([128, 3 + TB]) for _ in range(6)]
            ctmp = cv.get([128, TB])
            xs = [cv.get([128, TB], BF16) for _ in range(4)]
            BT = cv.get([128, TB], BF16); CT = cv.get([128, TB], BF16)
            sz = [cv.get([128, TB], BF16) for _ in range(4)]
            yg = cv.get([128, 4, TB])
            dt_tm = cv.get([128, 8]); dA = cv.get([128, 8]); acs = cv.get([128, 8]); dte = cv.get([128, 8])
            w2 = cv.get([128, 8]); draw = cv.get([128, 8]); ex = cv.get([128, 8])
            dAb = cv.get([128, 8, 128])
            L = dAb
            Btmzs = [[cv.get([128, 128], BF16) for _ in range(2)] for _ in range(2)]
            decbcs = [cv.get([128, 2, 8]) for _ in range(2)]
            eD = cv.get([128, 8, 128], BF16)
            cbm = cv.get([128, 128])
            MTs = [cv.get([128, 8, 128], BF16) for _ in range(2)]
            CTss = [cv.get([128, 8, 128], BF16) for _ in range(2)]
            xdts = [cv.get([128, 8, 64], BF16) for _ in range(2)]
            xws = [cv.get([128, 8, 64], BF16) for _ in range(2)]
            pa_ctr = [0]
            Btm = cv.get([128, 128], BF16)
            S32 = cv.get([128, 8, 64])
            Sbf = [cv.get([128, 8, 64], BF16) for _ in range(2)]
            sqb = cv.get([128, TB], BF16); rs = ctmp
            b7 = banks[7][:, :].bitcast(BF16)
            MASK = consts[:, C_MASK:C_MASK + 128]
            SUF = consts[:, C_SUF:C_SUF + 128]
            p.dma("dtb", dtb, prow_d[l:l + 1, PR_DTB:PR_DTB + 16].partition_broadcast(128), writes=["dtb"])
            p.dma("alog", alog, prow_d[l:l + 1, PR_ALOG:PR_ALOG + 16].partition_broadcast(128), writes=["alog"])
            p.op("act", lambda e: e.activation(out=aneg, in_=alog, func=AF.Exp), reads=["alog"], writes=["aneg"])
            p.op("dve", lambda e: e.tensor_scalar_mul(out=aneg, in0=aneg, scalar1=-1.0), reads=["aneg"], writes=["aneg"])
            for g in range(2):
                load_win(3600 + g * 512, 512, 0)
                load_win(2576 + g * 512, 512, 512)
                load_win(4624 + g * 128, 128, 1024)
                load_win(4880 + g * 128, 128, 1152)
                load_win(5136 + g * 8, 8, 1280)
                load_wout(8 + g * 4, 4)
                for j6 in range(6):
                    p.op("pool", lambda e, j6=j6: e.memset(cbuf[j6][:, 0:3], 0.0), writes=[("cbuf", j6)])
                p.op("pool", lambda e: e.memset(S32, 0.0), writes=["s_S32"])
                p.op("pool", lambda e: e.memset(Sbf[0], 0.0), writes=[("s_Sbf", 0)])
                for pa in range(2):
                    for half in range(2):
                        p.op("pool", lambda e, half=half, pa=pa: e.memset(Btmzs[pa][half], 0.0), writes=[("s_Btmz", pa, half)])
                par = 0
                gs = slice(g * 8, g * 8 + 8)
                for tb in range(NTB):
                    for j6 in range(6):
                        off = j6 * 128 if j6 < 4 else (1024 if j6 == 4 else 1152)
                        jc = g * 4 + j6 if j6 < 4 else (8 + g if j6 == 4 else 10 + g)
                        bank = j6 % 2
                        proj_fm(off, 128, tb, bank)
                        dst = xs[j6] if j6 < 4 else (BT if j6 == 4 else CT)
                        conv_silu(cbuf[j6], ("cbuf", j6), tb, bank, PP_SCW + jc * 4, PP_SCB + jc, ctmp, "s_ctmp", dst, ("s_fm", j6), AF.Silu, eng="dve")
                    for q in range(4):
                        proj_fm(512 + q * 128, 128, tb, q % 2)
                        p.op("act", lambda e, q=q: e.activation(out=sz[q], in_=banks[q % 2][:, :], func=AF.Silu), reads=[PS(q % 2)], writes=[("s_sz", q)])
                    def _aliases(pa):
                        return MTs[pa], CTss[pa], xdts[pa], xws[pa], Btmzs[pa], decbcs[pa]

                    def stageA(tt, pa):
                        MT, CTs, xdt, xw, Btmz, decbc = _aliases(pa)
                        KP = lambda n: (n, pa)
                        tsl = slice(tt * 128, (tt + 1) * 128)
                        proj_tm(1280, 8, tb, tt, 5)
                        p.op("dve", lambda e, gs=gs: e.tensor_tensor(out=draw, in0=banks[5][:, 0:8], in1=dtb[:, gs], op=ALU.add), reads=[PS(5), "dtb"], writes=["s_draw"])
                        p.op("act", lambda e: e.activation(out=ex, in_=draw, func=AF.Exp), reads=["s_draw"], writes=["s_ex"])
                        p.op("act", lambda e: e.activation(out=dt_tm, in_=ex, func=AF.Ln, bias=LNEPS[:, 2:3]), reads=["s_ex", "lneps"], writes=["s_dt"])
                        p.op("dve", lambda e, gs=gs: e.tensor_tensor(out=dA, in0=dt_tm, in1=aneg[:, gs], op=ALU.mult), reads=["s_dt", "aneg"], writes=["s_dA"])

                        def mm5(e):
                            e.matmul(banks[5][:, 8:16], lhsT=MASK, rhs=dA, start=True, stop=True)
                            e.matmul(banks[5][:, 16:24], lhsT=SUF, rhs=dA, start=True, stop=True)
                            e.matmul(banks[5][:, 160:168], lhsT=consts[:, C_CH0:C_CH0 + 128], rhs=dA, start=True, stop=True)
                            return e.matmul(banks[5][:, 168:176], lhsT=consts[:, C_CH1:C_CH1 + 128], rhs=dA, start=True, stop=True)
                        p.op("pe", mm5, reads=["s_dA", "consts"], writes=[PS(5)])
                        p.op("act", lambda e: e.copy(out=acs, in_=banks[5][:, 8:16]), reads=[PS(5)], writes=["s_acs"])
                        p.op("act", lambda e: e.activation(out=dte, in_=banks[5][:, 16:24], func=AF.Exp), reads=[PS(5)], writes=["s_dte"])
                        p.op("act", lambda e: e.copy(out=dAb, in_=dA.unsqueeze(2).to_broadcast([128, 8, 128])), reads=["s_dA"], writes=["s_dAb", ("s_L", 0), ("s_L", 1), "s_Lm", "s_Le"])
                        p.op("act", lambda e: e.activation(out=decbc, in_=banks[5][:, 160:176].rearrange("p (a b) -> p a b", a=2), func=AF.Exp), reads=[PS(5)], writes=[KP("s_dec")])
                        p.op("dve", lambda e: e.tensor_tensor(out=w2, in0=dt_tm, in1=dte, op=ALU.mult), reads=["s_dt", "s_dte"], writes=["s_w2"])

                        def mmD(e):
                            ins = None
                            for h in range(8):
                                ins = e.matmul(banks[2 + h // 4][:, (h % 4) * 128:(h % 4 + 1) * 128], lhsT=dAb[:, h, :], rhs=MASK, start=True, stop=True)
                            return ins
                        p.op("pe", mmD, reads=["s_dAb", "consts"], writes=[PS(2), PS(3)])
                        for k in range(2):
                            p.op("dve", lambda e, k=k: e.tensor_tensor(out=L[:, 4 * k:4 * k + 4, :], in0=banks[2 + k][:, :].rearrange("p (a b) -> p a b", a=4),
                                                                       in1=acs[:, 4 * k:4 * k + 4].unsqueeze(2).to_broadcast([128, 4, 128]), op=ALU.subtract),
                                 reads=[PS(2 + k), "s_acs"], writes=[("s_L", k)])
                            p.op("act", lambda e, k=k: e.activation(out=eD[:, 4 * k:4 * k + 4, :], in_=banks[2 + k][:, :].rearrange("p (a b) -> p a b", a=4), func=AF.Exp), reads=[PS(2 + k)], writes=[("s_eD", k)])
                        p.op("dve", lambda e: e.tensor_scalar_min(out=L, in0=L, scalar1=0.0), reads=[("s_L", 0), ("s_L", 1)], writes=["s_Lm"])
                        p.op("act", lambda e: e.activation(out=L, in_=L, func=AF.Exp), reads=["s_Lm"], writes=["s_Le"])
                        p.op("pe", lambda e, tsl=tsl: e.matmul(banks[5][:, 256:384], lhsT=BT[:, tsl], rhs=CT[:, tsl], start=True, stop=True), reads=[("s_fm", 4), ("s_fm", 5)], writes=[PS(5)])
                        p.op("dve", lambda e: e.tensor_tensor(out=cbm, in0=banks[5][:, 256:384], in1=MASK, op=ALU.mult), reads=[PS(5), "consts"], writes=["s_cbm"])
                        p.op("dve", lambda e: e.tensor_tensor(out=MT, in0=L, in1=cbm.unsqueeze(1).to_broadcast([128, 8, 128]), op=ALU.mult), reads=["s_Le", "s_cbm"], writes=[KP("s_MT")])
                        p.op("dve", lambda e, tsl=tsl: e.tensor_tensor(out=CTs, in0=eD, in1=CT[:, tsl].unsqueeze(1).to_broadcast([128, 8, 128]), op=ALU.mult), reads=[("s_eD", 0), ("s_eD", 1), ("s_fm", 5)], writes=[KP("s_CTs")])

                        def mmT(e, tsl=tsl):
                            for q in range(4):
                                e.transpose(b7[:, q * 128:(q + 1) * 128], xs[q][:, tsl], identb)
                            return e.transpose(b7[:, 512:640], BT[:, tsl], identb)
                        p.op("pe", mmT, reads=[("s_fm", j) for j in range(5)] + ["identb"], writes=[PS(7)])
                        xtm = b7[:, 0:512].rearrange("p (h k) -> p h k", h=8)
                        p.op("dve", lambda e: e.tensor_tensor(out=xdt, in0=xtm, in1=dt_tm.unsqueeze(2).to_broadcast([128, 8, 64]), op=ALU.mult), reads=[PS(7), "s_dt"], writes=[KP("s_xdt")])
                        p.op("dve", lambda e: e.tensor_tensor(out=xw, in0=xtm, in1=w2.unsqueeze(2).to_broadcast([128, 8, 64]), op=ALU.mult), reads=[PS(7), "s_w2"], writes=[KP("s_xw")])
                        for half in range(2):
                            p.op("act", lambda e, half=half: e.copy(out=Btmz[half][half * 64:(half + 1) * 64, :], in_=b7[half * 64:(half + 1) * 64, 512:640]), reads=[PS(7)], writes=[("s_Btmz", pa, half)])


                    def stageB(tt, pa, par):
                        MT, CTs, xdt, xw, Btmz, decbc = _aliases(pa)
                        KP = lambda n: (n, pa)
                        tsl = slice(tt * 128, (tt + 1) * 128)
                        def mmY(e):
                            ins = None
                            for h in range(8):
                                q, hq = h // 2, h % 2
                                ins = e.matmul(banks[4][hq * 64:(hq + 1) * 64, q * 128:(q + 1) * 128], lhsT=xdt[:, h, :], rhs=MT[:, h, :], start=True, stop=True)
                            return ins
                        p.op("pe", mmY, reads=[KP("s_xdt"), KP("s_MT")], writes=[PS(4)])
                        for half in range(2):
                            hs = slice(half * 64, (half + 1) * 64)

                            def mmO(e, half=half, par=par):
                                ins = None
                                for h in range(8):
                                    q, hq = h // 2, h % 2
                                    ins = e.matmul(banks[0][hq * 64:(hq + 1) * 64, q * 128 + half * 64:q * 128 + half * 64 + 64], lhsT=Sbf[par][:, h, :], rhs=CTs[:, h, half * 64:(half + 1) * 64], start=True, stop=True)
                                return ins
                            p.op("pe", mmO, reads=[("s_Sbf", par), KP("s_CTs")], writes=[("ps0o", half)] + ([PS(0)] if half == 0 else []))
                            p.op("pe", lambda e, half=half: e.matmul(banks[6][:, :], lhsT=Btmz[half], rhs=xw.rearrange("p h k -> p (h k)"), start=True, stop=True), reads=[("s_Btmz", pa, half), KP("s_xw")], writes=[PS(6)])
                            p.op("dve", lambda e, half=half: e.tensor_tensor(out=S32, in0=S32, in1=decbc[:, half, :].unsqueeze(2).to_broadcast([128, 8, 64]), op=ALU.mult), reads=["s_S32", KP("s_dec")], writes=["s_S32"])
                            p.op("dve", lambda e: e.tensor_tensor(out=S32, in0=S32, in1=banks[6][:, :].rearrange("p (h k) -> p h k", h=8), op=ALU.add), reads=["s_S32", PS(6)], writes=["s_S32"])
                            p.op("act", lambda e, par=par: e.copy(out=Sbf[1 - par], in_=S32), reads=["s_S32"], writes=[("s_Sbf", 1 - par)])
                            par = 1 - par
                        for q in range(4):
                            p.op("dve", lambda e, q=q, tsl=tsl, g=g: e.scalar_tensor_tensor(out=yg[:, q, tsl], in0=xs[q][:, tsl], scalar=PPc(l, PP_SD + g * 4 + q), in1=banks[4][:, q * 128:(q + 1) * 128], op0=ALU.mult, op1=ALU.add),
                                 reads=[("s_fm", q), PS(4), "pp"], writes=[("s_yg", q)])
                            p.op("dve", lambda e, q=q, tsl=tsl: e.tensor_tensor(out=yg[:, q, tsl], in0=yg[:, q, tsl], in1=banks[0][:, q * 128:(q + 1) * 128], op=ALU.add),
                                 reads=[("s_yg", q), PS(0), ("ps0o", 0), ("ps0o", 1)], writes=[("s_yg", q)])
                        return par

                    def capture(fn, *a):
                        lst = []
                        p.cap = lst
                        r = fn(*a)
                        p.cap = None
                        return lst, r

                    def replay(lst):
                        for (eng, fn, r, w) in lst:
                            p.op(eng, fn, reads=r, writes=w)

                    def merge(la, lb):
                        out = []
                        ia = ib = 0
                        na, nb = len(la), len(lb)
                        while ia < na or ib < nb:
                            if ib >= nb or (ia < na and ia * nb <= ib * na):
                                out.append(la[ia]); ia += 1
                            else:
                                out.append(lb[ib]); ib += 1
                        return out

                    lA, _ = capture(stageA, 0, pa_ctr[0] % 2)
                    replay(lA)
                    for tt in range(4):
                        pa = pa_ctr[0] % 2
                        lB, par = capture(stageB, tt, pa, par)
                        if tt + 1 < 4:
                            lA, _ = capture(stageA, tt + 1, (pa_ctr[0] + 1) % 2)
                            replay(merge(lA, lB))
                        else:
                            replay(lB)
                        pa_ctr[0] += 1
                    for q in range(4):
                        p.op("dve", lambda e, q=q: e.tensor_tensor(out=yg[:, q, :], in0=yg[:, q, :], in1=sz[q], op=ALU.mult), reads=[("s_yg", q), ("s_sz", q)], writes=[("s_yg", q)])
                    for q in range(4):
                        p.op("act", lambda e, q=q: e.activation(out=sqb, in_=yg[:, q, :], func=AF.Square), reads=[("s_yg", q)], writes=["s_sqb"])
                        p.op("pe", lambda e, q=q: e.matmul(banks[5][:, :], lhsT=ones512b, rhs=sqb, start=(q == 0), stop=(q == 3)), reads=["s_sqb", "ones512b"], writes=[PS(5)])
                    p.op("act", lambda e: e.activation(out=rs, in_=banks[5][:, :], func=AF.Ln, bias=LNEPS[:, 1:2]), reads=[PS(5), "lneps"], writes=["s_ctmp"])
                    p.op("act", lambda e: e.activation(out=rs, in_=rs, func=AF.Exp, scale=-0.5), reads=["s_ctmp"], writes=["s_ctmp"])
                    for q in range(4):
                        p.op("dve", lambda e, q=q, g=g: e.scalar_tensor_tensor(out=yblk[:, q, :], in0=yg[:, q, :], scalar=PPc(l, PP_SNORM + g * 4 + q), in1=rs, op0=ALU.mult, op1=ALU.mult),
                             reads=[("s_yg", q), "s_ctmp", "pp"], writes=[("yblk", q)])
                    out_proj(tb, 4, [0, 1])

        def mixer(l):
            cv = Carve()
            wst = [cv.get([128, 8, 128]) for _ in range(2)]
            wunit = cv.get([128, 8, 1408], BF16)
            woutst = [cv.get([128, D])] * 2
            wout = cv.get([128, 4, D], BF16)
            yblk = cv.get([128, 4, TB], BF16)
            identb = cv.get([128, 128], BF16)
            ones128b = cv.get([128, 128], BF16)
            ones512b = cv.get([128, 128], BF16)
            p.op("act", lambda e: e.copy(out=identb, in_=ident), reads=["consts"], writes=["identb"])
            p.op("pool", lambda e: e.memset(ones128b, 1.0 / 128.0), writes=["ones128b"])
            p.op("pool", lambda e: e.memset(ones512b, 1.0 / 512.0), writes=["ones512b"])
            wcnt = [0]

            def load_win(col0, ncols, dst):
                c = 0
                while c < ncols:
                    n = min(128, ncols - c)
                    k = wcnt[0] % 2
                    wcnt[0] += 1
                    p.dma(("wst", k), wst[k][:, :, 0:n], win_d[l].rearrange("(kc p) f -> p kc f", p=128)[:, :, col0 + c:col0 + c + n], writes=[("wst", k)])
                    eng = ("act", "pool")[k]
                    if eng == "act":
                        p.op("act", lambda e, k=k, n=n, c=c: e.copy(out=wunit[:, :, dst + c:dst + c + n], in_=wst[k][:, :, 0:n]), reads=[("wst", k)], writes=["wunit"])
                    else:
                        p.op("pool", lambda e, k=k, n=n, c=c: e.tensor_copy(out=wunit[:, :, dst + c:dst + c + n], in_=wst[k][:, :, 0:n]), reads=[("wst", k)], writes=["wunit"])
                    c += n

            def load_wout(ych0, n):
                for j in range(n):
                    k = 0
                    p.dma(("wost", k), woutst[k], wout_d[l, (ych0 + j) * 128:(ych0 + j + 1) * 128, :], writes=[("wost", k)])
                    p.op("pool", lambda e, k=k, j=j: e.tensor_copy(out=wout[:, j, :], in_=woutst[k]), reads=[("wost", k)], writes=["wout"])

            def proj_fm(off, ncols, tb, bank):
                def mm(e):
                    ins = None
                    for kc in range(8):
                        ins = e.matmul(banks[bank][0:ncols, :], lhsT=wunit[:, kc, off:off + ncols], rhs=hT[:, kc, tb * TB:(tb + 1) * TB], start=(kc == 0), stop=(kc == 7))
                    return ins
                p.op("pe", mm, reads=["wunit"] + [("h", kc, tb) for kc in range(8)], writes=[PS(bank)])

            def proj_tm(off, ncols, tb, tt, bank, col0=0):
                t0 = tb * TB + tt * 128

                def mm(e):
                    ins = None
                    for kc in range(8):
                        ins = e.matmul(banks[bank][:, col0:col0 + ncols], lhsT=hT[:, kc, t0:t0 + 128], rhs=wunit[:, kc, off:off + ncols], start=(kc == 0), stop=(kc == 7))
                    return ins
                p.op("pe", mm, reads=["wunit"] + [("h", kc, tb) for kc in range(8)], writes=[PS(bank)])

            def out_proj(tb, nych, bks):
                for d in range(8):
                    bk = bks[d % len(bks)]

                    def mm(e, d=d, bk=bk):
                        ins = None
                        for j in range(nych):
                            ins = e.matmul(banks[bk][:, :], lhsT=wout[:, j, d * 128:(d + 1) * 128], rhs=yblk[:, j, :], start=(j == 0), stop=(j == nych - 1))
                        return ins
                    p.op("pe", mm, reads=["wout"] + [("yblk", j) for j in range(nych)], writes=[PS(bk)])
                    p.op("dve", lambda e, d=d, bk=bk: e.scalar_tensor_tensor(out=xT[:, d, tb * TB:(tb + 1) * TB], in0=banks[bk][:, :], scalar=MOD(l, 2, d), in1=xT[:, d, tb * TB:(tb + 1) * TB], op0=ALU.mult, op1=ALU.add),
                         reads=[PS(bk), ("x", d, tb)] + modkeys(l), writes=[("x", d, tb)])

            def conv_silu(buf, key, tb, bank, wcol, bcol, tmp, tmpkey, dst, dstkey, act_func, eng="dve"):
                p.op("act", lambda e: e.copy(out=buf[:, 3:3 + TB], in_=banks[bank][:, :]), reads=[PS(bank)], writes=[key])
                p.op(eng, lambda e: e.tensor_scalar(out=tmp, in0=buf[:, 0:TB], scalar1=PPc(l, wcol), scalar2=PPc(l, bcol), op0=ALU.mult, op1=ALU.add), reads=[key, "pp"], writes=[tmpkey])
                for k in range(1, 4):
                    p.op(eng, lambda e, k=k: e.scalar_tensor_tensor(out=tmp, in0=buf[:, k:k + TB], scalar=PPc(l, wcol + k), in1=tmp, op0=ALU.mult, op1=ALU.add), reads=[key, tmpkey, "pp"], writes=[tmpkey])
                p.op("pool", lambda e: e.tensor_copy(out=buf[:, 0:3], in_=buf[:, TB:TB + 3]), reads=[key], writes=[key])
                if dst is not None:
                    p.op("act", lambda e: e.activation(out=dst, in_=tmp, func=act_func), reads=[tmpkey], writes=[dstkey])

            mark = cv.off

            if do_lru:
                cv.off = mark
                wabd = cv.get([128, 4, 128], BF16)
                wxbd = cv.get([128, 4, 128], BF16)
                nsp8 = cv.get([128, 4])
                hcar = cv.get([128, 4])
                xbuf = [cv.get([128, 3 + TB]) for _ in range(4)]
                Ts = [{n: cv.get([128, TB]) for n in ("xc", "r", "i", "om", "h", "gg")} for _ in range(4)]
                xcbs = [cv.get([128, TB], BF16) for _ in range(4)]
                for nm, src, dst in (("wa", wabd_d, wabd), ("wx", wxbd_d, wxbd)):
                    p.dma(("wost", 0), woutst[0][:, 0:512].rearrange("p (m j) -> p m j", m=4), src[l].rearrange("m i j -> i m j"), writes=[("wost", 0)])
                    p.op("pool", lambda e, dst=dst: e.tensor_copy(out=dst, in_=woutst[0][:, 0:512].rearrange("p (m j) -> p m j", m=4)), reads=[("wost", 0)], writes=[nm])
                p.op("act", lambda e: e.activation(out=nsp8, in_=pp[:, l, PP_LLAM:PP_LLAM + 4], func=AF.Exp, scale=-1.0), reads=["pp"], writes=["nsp8"])
                p.op("act", lambda e: e.activation(out=nsp8, in_=nsp8, func=AF.Ln, bias=LNEPS[:, 2:3]), reads=["nsp8", "lneps"], writes=["nsp8"])
                p.op("dve", lambda e: e.tensor_scalar_mul(out=nsp8, in0=nsp8, scalar1=-8.0), reads=["nsp8"], writes=["nsp8"])
                for m in range(4):
                    p.op("pool", lambda e, m=m: e.memset(xbuf[m][:, 0:3], 0.0), writes=[("xbuf", m)])
                load_win(0, 1024, 0)
                load_wout(0, 4)

                def lru_chain(tb, m):
                    T = Ts[m]
                    xcb = xcbs[m]
                    BA, BB = 2 * m, 2 * m + 1
                    KK = lambda n: (n, m)
                    ops = []
                    A = lambda eng, fn, r, w: ops.append((eng, fn, r, w))
                    buf = xbuf[m]
                    key = ("xbuf", m)
                    wcol, bcol = PP_LCW + m * 4, PP_LCB + m
                    sl_h = [("h", kc, tb) for kc in range(8)]

                    def mmx(e):
                        ins = None
                        for kc in range(8):
                            ins = e.matmul(banks[BA][:, :], lhsT=wunit[:, kc, m * 128:(m + 1) * 128], rhs=hT[:, kc, tb * TB:(tb + 1) * TB], start=(kc == 0), stop=(kc == 7))
                        return ins

                    def mmg(e):
                        ins = None
                        for kc in range(8):
                            ins = e.matmul(banks[BB][:, :], lhsT=wunit[:, kc, 512 + m * 128:512 + (m + 1) * 128], rhs=hT[:, kc, tb * TB:(tb + 1) * TB], start=(kc == 0), stop=(kc == 7))
                        return ins
                    A("pe", mmx, ["wunit"] + sl_h, [PS(BA)])
                    A("pe", mmg, ["wunit"] + sl_h, [PS(BB)])
                    A("act", lambda e: e.copy(out=buf[:, 3:3 + TB], in_=banks[BA][:, :]), [PS(BA)], [key])
                    A("act", lambda e: e.activation(out=T["gg"], in_=banks[BB][:, :], func=AF.Gelu_apprx_tanh), [PS(BB)], [KK("l_gg")])
                    A("dve", lambda e: e.tensor_scalar(out=T["xc"], in0=buf[:, 0:TB], scalar1=PPc(l, wcol), scalar2=PPc(l, bcol), op0=ALU.mult, op1=ALU.add), [key, "pp"], [KK("l_xc")])
                    for k in range(1, 4):
                        A("dve", lambda e, k=k: e.scalar_tensor_tensor(out=T["xc"], in0=buf[:, k:k + TB], scalar=PPc(l, wcol + k), in1=T["xc"], op0=ALU.mult, op1=ALU.add), [key, KK("l_xc"), "pp"], [KK("l_xc")])
                    A("pool", lambda e: e.tensor_copy(out=buf[:, 0:3], in_=buf[:, TB:TB + 3]), [key], [key])
                    A("act", lambda e: e.copy(out=xcb, in_=T["xc"]), [KK("l_xc")], [KK("l_xcb")])
                    A("pe", lambda e: e.matmul(banks[BA][:, :], lhsT=wabd[:, m, :], rhs=xcb, start=True, stop=True), ["wa", KK("l_xcb")], [PS(BA)])
                    A("pe", lambda e: e.matmul(banks[BB][:, :], lhsT=wxbd[:, m, :], rhs=xcb, start=True, stop=True), ["wx", KK("l_xcb")], [PS(BB)])
                    A("act", lambda e: e.activation(out=T["r"], in_=banks[BA][:, :], func=AF.Sigmoid, bias=PPc(l, PP_LBA + m)), [PS(BA), "pp"], [KK("l_r")])
                    A("act", lambda e: e.activation(out=T["i"], in_=banks[BB][:, :], func=AF.Sigmoid, bias=PPc(l, PP_LBX + m)), [PS(BB), "pp"], [KK("l_i")])
                    A("act", lambda e: e.activation(out=T["r"], in_=T["r"], func=AF.Exp, scale=nsp8[:, m:m + 1]), [KK("l_r"), "nsp8"], [KK("l_r")])
                    A("pool", lambda e: e.tensor_tensor(out=T["om"], in0=T["r"], in1=T["r"], op=ALU.mult), [KK("l_r")], [KK("l_om")])
                    A("pool", lambda e: e.tensor_scalar(out=T["om"], in0=T["om"], scalar1=-1.0, scalar2=1.0, op0=ALU.mult, op1=ALU.add), [KK("l_om")], [KK("l_om")])
                    A("act", lambda e: e.activation(out=T["om"], in_=T["om"], func=AF.Sqrt), [KK("l_om")], [KK("l_om")])
                    A("pool", lambda e: e.tensor_tensor(out=T["i"], in0=T["i"], in1=T["xc"], op=ALU.mult), [KK("l_i"), KK("l_xc")], [KK("l_i")])
                    A("dve", lambda e: e.tensor_tensor(out=T["om"], in0=T["om"], in1=T["i"], op=ALU.mult), [KK("l_om"), KK("l_i")], [KK("l_om")])
                    if tb == 0:
                        A("dve", lambda e: e.tensor_tensor_scan(out=T["h"], data0=T["r"], data1=T["om"], initial=0.0, op0=ALU.mult, op1=ALU.add), [KK("l_r"), KK("l_om")], [KK("l_h")])
                    else:
                        A("dve", lambda e: e.tensor_tensor_scan(out=T["h"], data0=T["r"], data1=T["om"], initial=hcar[:, m:m + 1], op0=ALU.mult, op1=ALU.add), [KK("l_r"), KK("l_om"), ("hcar", m)], [KK("l_h")])
                    A("pool", lambda e: e.tensor_copy(out=hcar[:, m:m + 1], in_=T["h"][:, TB - 1:TB]), [KK("l_h")], [("hcar", m)])
                    A("dve", lambda e: e.tensor_tensor(out=yblk[:, m, :], in0=T["h"], in1=T["gg"], op=ALU.mult), [KK("l_h"), KK("l_gg")], [("yblk", m)])
                    return ops

                for tb in range(NTB):
                    lists = [lru_chain(tb, m) for m in range(4)]
                    for k in range(len(lists[0])):
                        for ol in lists:
                            eng, fn, r, w = ol[k]
                            p.op(eng, fn, reads=r, writes=w)
                    out_proj(tb, 4, [0, 1, 2, 3, 4, 5, 6, 7])

            if do_gla:
                cv.off = mark
                walb = cv.get([32, 256], BF16)
                rT = cv.get([32, TB], BF16)
                e1 = cv.get([128, 128])
                l1 = cv.get([128, 128])
                ecp = cv.get([128, TB])
                ecn = cv.get([128, TB])
                esuf = [cv.get([128, 128]) for _ in range(4)]
                qd = cv.get([128, TB], BF16)
                kd = cv.get([128, TB], BF16)
                kend = [cv.get([128, 128], BF16) for _ in range(4)]
                kendz = [[cv.get([128, 128], BF16) for _ in range(2)] for _ in range(4)]
                qdz = [cv.get([128, TB], BF16) for _ in range(2)]
                CHM = [consts[:, C_CH0:C_CH0 + 128], consts[:, C_CH1:C_CH1 + 128]]
                vtm = [cv.get([128, 256], BF16) for _ in range(4)]
                sg = [cv.get([128, TB]) for _ in range(2)]
                attm = cv.get([128, 4, 128], BF16)
                S32 = cv.get([128, 128])
                Sbf = [cv.get([128, 128], BF16) for _ in range(2)]
                sqb = cv.get([128, TB], BF16)
                rs = cv.get([128, TB])
                t1 = cv.get([128, TB])
                p.dma(("wost", 0), woutst[0][0:32, 0:256], walpha_d[l], writes=[("wost", 0)])
                p.op("pool", lambda e: e.tensor_copy(out=walb, in_=woutst[0][0:32, 0:256]), reads=[("wost", 0)], writes=["walb"])
                p.op("pool", lambda e: e.memset(rT, 1.0), writes=["rT"])
                m64b = consts[:, C_MASK:C_MASK + 128].unsqueeze(1).to_broadcast([128, 4, 128])
                for hp in range(2):
                    load_win(1024 + hp * 128, 128, 0)
                    load_win(1280 + hp * 128, 128, 128)
                    load_win(1536 + hp * 256, 256, 256)
                    load_win(2048 + hp * 256, 256, 512)
                    load_win(2560, 16, 768)
                    load_wout(4 + hp * 2, 2)
                    p.op("pool", lambda e: e.memset(S32, 0.0), writes=["S32"])
                    p.op("pool", lambda e: e.memset(Sbf[0], 0.0), writes=[("Sbf", 0)])
                    if hp == 0:
                        for hh in range(2):
                            p.op("pool", lambda e, hh=hh: e.memset(qdz[hh], 0.0), writes=[("g_qdz", hh)])
                        for tt in range(4):
                            for half in range(2):
                                p.op("pool", lambda e, tt=tt, half=half: e.memset(kendz[tt][half], 0.0), writes=[("g_kendz", tt, half)])
                    par_state = {0: 0, 1: 0}
                    for tb in range(NTB):
                        proj_fm(768, 16, tb, 0)
                        p.op("act", lambda e: e.copy(out=rT[0:16, :], in_=banks[0][0:16, :]), reads=[PS(0)], writes=["rT"])
                        for tt in range(4):
                            tsl = slice(tt * 128, (tt + 1) * 128)
                            p.op("pe", lambda e, tsl=tsl, hp=hp: e.matmul(banks[5][:, 0:128], lhsT=rT[:, tsl], rhs=walb[:, hp * 128:(hp + 1) * 128], start=True, stop=True), reads=["rT", "walb"], writes=[PS(5)])
                            p.op("act", lambda e: e.activation(out=e1, in_=banks[5][:, 0:128], func=AF.Exp, scale=-1.0), reads=[PS(5)], writes=["g_e1"])
                            p.op("act", lambda e: e.activation(out=l1, in_=e1, func=AF.Ln, bias=LNEPS[:, 2:3]), reads=["g_e1", "lneps"], writes=["g_l1"])
                            p.op("pe", lambda e: e.matmul(banks[5][:, 128:256], lhsT=l1, rhs=consts[:, C_MASKG:C_MASKG + 128], start=True, stop=True), reads=["g_l1", "consts"], writes=[PS(5)])
                            p.op("pe", lambda e: e.matmul(banks[5][:, 256:384], lhsT=consts[:, C_MASKG + 128:C_MASKG + 256], rhs=l1, start=True, stop=True), reads=["g_l1", "consts"], writes=[PS(5)])
                            p.op("act", lambda e, tsl=tsl: e.activation(out=ecp[:, tsl], in_=banks[5][:, 128:256], func=AF.Exp), reads=[PS(5)], writes=["g_ecp"])
                            p.op("act", lambda e, tsl=tsl: e.activation(out=ecn[:, tsl], in_=banks[5][:, 128:256], func=AF.Exp, scale=-1.0), reads=[PS(5)], writes=["g_ecn"])
                            p.op("act", lambda e, tt=tt: e.activation(out=esuf[tt], in_=banks[5][:, 256:384], func=AF.Exp), reads=[PS(5)], writes=[("g_esuf", tt)])
                        proj_fm(0, 128, tb, 0)
                        for hh in range(2):
                            p.op("dve", lambda e, hh=hh: e.scalar_tensor_tensor(out=qdz[hh][hh * 64:(hh + 1) * 64, :], in0=banks[0][hh * 64:(hh + 1) * 64, :], scalar=0.125, in1=ecp[hh * 64:(hh + 1) * 64, :], op0=ALU.mult, op1=ALU.mult),
                                 reads=[PS(0), "g_ecp"], writes=[("g_qdz", hh)])
                        proj_fm(128, 128, tb, 1)
                        p.op("dve", lambda e: e.tensor_tensor(out=kd, in0=banks[1][:, :], in1=ecn, op=ALU.mult), reads=[PS(1), "g_ecn"], writes=["g_kd"])
                        for tt in range(4):
                            proj_tm(128, 128, tb, tt, 0)
                            for half in range(2):
                                p.op("dve", lambda e, tt=tt, half=half: e.tensor_tensor(out=kendz[tt][half][half * 64:(half + 1) * 64, :], in0=banks[0][half * 64:(half + 1) * 64, 0:128], in1=esuf[tt][half * 64:(half + 1) * 64, :], op=ALU.mult),
                                     reads=[PS(0), ("g_esuf", tt)], writes=[("g_kendz", tt, half)])
                            proj_tm(256, 256, tb, tt, 1)
                            p.op("act", lambda e, tt=tt: e.copy(out=vtm[tt], in_=banks[1][:, 0:256]), reads=[PS(1)], writes=[("g_vtm", tt)])
                        for hh in range(2):
                            proj_fm(512 + hh * 128, 128, tb, hh)
                            p.op("act", lambda e, hh=hh: e.activation(out=sg[hh], in_=banks[hh][:, :], func=AF.Silu), reads=[PS(hh)], writes=[("g_sg", hh)])
                        for hh in range(2):
                            b0 = hh * 64

                            def att(e, hh=hh):
                                ins = None
                                for tt in range(4):
                                    tsl = slice(tt * 128, (tt + 1) * 128)
                                    ins = e.matmul(banks[2][:, tsl], lhsT=kd[:, tsl], rhs=qdz[hh][:, tsl], start=True, stop=True)
                                return ins
                            p.op("pe", att, reads=["g_kd", ("g_qdz", hh)], writes=[PS(2)])
                            p.op("dve", lambda e: e.tensor_tensor(out=attm, in0=banks[2][:, :].rearrange("p (a b) -> p a b", a=4), in1=m64b, op=ALU.mult), reads=[PS(2), "consts"], writes=["g_attm"])
                            for c in range(8):
                                tt, half = c // 2, c % 2
                                csl = slice(c * 64, (c + 1) * 64)
                                tsl = slice(tt * 128, (tt + 1) * 128)
                                cur_par = par_state[hh]
                                if half == 0:
                                    p.op("pe", lambda e, tt=tt, tsl=tsl, hh=hh: e.matmul(banks[3][:, tsl], lhsT=vtm[tt][:, hh * 128:(hh + 1) * 128], rhs=attm[:, tt, :], start=True, stop=False),
                                         reads=[("g_vtm", tt), "g_attm"], writes=[PS(3)])
                                p.op("pe", lambda e, csl=csl, hh=hh, cur_par=cur_par, half=half: e.matmul(banks[3][:, csl], lhsT=Sbf[cur_par], rhs=qdz[hh][:, csl], start=False, stop=(half == 1)),
                                     reads=[("Sbf", cur_par), ("g_qdz", hh)], writes=[PS(3)])
                                p.op("pe", lambda e, tt=tt, half=half, hh=hh: e.matmul(banks[4][:, 0:128], lhsT=kendz[tt][half], rhs=vtm[tt][:, hh * 128:(hh + 1) * 128], start=True, stop=True),
                                     reads=[("g_kendz", tt, half), ("g_vtm", tt)], writes=[PS(4)])
                                col = c * 64 + 63
                                p.op("dve", lambda e, b0=b0, col=col: e.scalar_tensor_tensor(out=S32[b0:b0 + 64, :], in0=S32[b0:b0 + 64, :], scalar=ecp[b0:b0 + 64, col:col + 1], in1=banks[4][b0:b0 + 64, 0:128], op0=ALU.mult, op1=ALU.add),
                                     reads=["S32", "g_ecp", PS(4)], writes=["S32"])
                                nxt = 1 - cur_par
                                p.op("act", lambda e, nxt=nxt: e.copy(out=Sbf[nxt], in_=S32), reads=["S32"], writes=[("Sbf", nxt)])
                                par_state[hh] = nxt
                            p.op("act", lambda e: e.activation(out=sqb, in_=banks[3][:, :], func=AF.Square), reads=[PS(3)], writes=["g_sqb"])
                            p.op("pe", lambda e: e.matmul(banks[5][:, :], lhsT=ones128b, rhs=sqb, start=True, stop=True), reads=["ones128b", "g_sqb"], writes=[PS(5)])
                            p.op("act", lambda e: e.activation(out=rs, in_=banks[5][:, :], func=AF.Ln, bias=LNEPS[:, 1:2]), reads=[PS(5), "lneps"], writes=["g_rs"])
                            p.op("act", lambda e: e.activation(out=rs, in_=rs, func=AF.Exp, scale=-0.5), reads=["g_rs"], writes=["g_rs"])
                            p.op("dve", lambda e: e.tensor_tensor(out=t1, in0=banks[3][:, :], in1=rs, op=ALU.mult), reads=[PS(3), "g_rs"], writes=["g_t1"])
                            p.op("dve", lambda e, hh=hh: e.scalar_tensor_tensor(out=yblk[:, hh, :], in0=t1, scalar=PPc(l, PP_GNORM), in1=sg[hh], op0=ALU.mult, op1=ALU.mult), reads=["g_t1", ("g_sg", hh), "pp"], writes=[("yblk", hh)])
                        out_proj(tb, 2, [6, 7])

            if do_ssd:
                ssd_units(l, cv, mark, load_win, load_wout, proj_fm, proj_tm, out_proj, conv_silu, yblk, identb, ones512b)

        cv = Carve()
        ada_bufs = ada_bufs_alloc(cv)
        for blk in range(N_ADA):
            adaln_block(0, blk, ada_bufs, blk % 4)
        adaln_finish(0, 1)

        for tb in range(NTB):
            scale_alpha(tb, engs=("pool", "dve", "act"))
        for l in range(depth):
            p.new_epoch()
            for tb in range(NTB):
                modulate(l, 1, tb)
            barrier()
            if do_lru or do_gla or do_ssd:
                mixer(l)
            barrier()
            cv = Carve()
            layernorm(l, PP_LN1G, PP_LN1B, cv, 0, 1)
            for tb in range(NTB):
                modulate(l, 2, tb)
            barrier()
            cv = Carve()
            if do_moe:
                router(l, cv, 2, 3)
            barrier()
            cv = Carve()
            hooks = {}
            if l + 1 < depth:
                ada_bufs2 = ada_bufs_alloc(cv)
                for blk in range(N_ADA):
                    hooks[2 + blk] = (lambda blk=blk: adaln_block(l + 1, blk, ada_bufs2, 4 + blk % 4))
                hooks[2 + N_ADA] = (lambda: adaln_finish(l + 1, 4))
            if do_moe:
                moe(l, cv, hooks)
            else:
                for k in sorted(hooks):
                    hooks[k]()
            barrier()
            cv = Carve()
            layernorm(l, PP_LN2G, PP_LN2B, cv, 0, 1, final=(l == depth - 1))
            barrier()

        p.dma("out", yT_d.rearrange("(c p) t -> p c t", p=128), xT[:], reads=[("x", c, tb) for c in range(8) for tb in range(NTB)])
        p.emit()
    return nc


def prep_inputs(inputs):
    f = lambda a: np.ascontiguousarray(np.asarray(a, dtype=np.float32))
    L = DEPTH
    shared = {}
    shared["consts"] = build_consts()
    pc = lambda v, n: np.asarray(v, np.float32).reshape(n, 128).T
    pp = np.zeros((L, 128, NPP), np.float32)
    prow = np.zeros((L, NPR), np.float32)
    wabd = np.zeros((L, 4, 128, 128), np.float32)
    wxbd = np.zeros((L, 4, 128, 128), np.float32)
    wal = np.zeros((L, 32, 256), np.float32)
    for l in range(L):
        pp[l, :, PP_LN1G:PP_LN1G + 8] = pc(inputs["ln1_g"][l], 8)
        pp[l, :, PP_LN1B:PP_LN1B + 8] = pc(inputs["ln1_b"][l], 8)
        pp[l, :, PP_LN2G:PP_LN2G + 8] = pc(inputs["ln2_g"][l], 8)
        pp[l, :, PP_LN2B:PP_LN2B + 8] = pc(inputs["ln2_b"][l], 8)
        for m in range(4):
            for k in range(4):
                pp[l, :, PP_LCW + m * 4 + k] = inputs["lru_conv_w"][l, k, m * 128:(m + 1) * 128]
        pp[l, :, PP_LCB:PP_LCB + 4] = pc(inputs["lru_conv_b"][l], 4)
        pp[l, :, PP_LBA:PP_LBA + 4] = pc(inputs["lru_b_a"][l], 4)
        pp[l, :, PP_LBX:PP_LBX + 4] = pc(inputs["lru_b_x"][l], 4)
        pp[l, :, PP_LLAM:PP_LLAM + 4] = pc(inputs["lru_lambda"][l], 4)
        for j in range(12):
            for k in range(4):
                pp[l, :, PP_SCW + j * 4 + k] = inputs["ssd_conv_w"][l, k, j * 128:(j + 1) * 128]
        pp[l, :, PP_SCB:PP_SCB + 12] = pc(inputs["ssd_conv_b"][l], 12)
        pp[l, :, PP_SNORM:PP_SNORM + 8] = pc(inputs["ssd_norm"][l], 8)
        pp[l, :, PP_GNORM] = inputs["gla_norm"][l]
        pp[l, :, PP_SD:PP_SD + 8] = pc(np.repeat(np.asarray(inputs["ssd_d"][l]), 64), 8)
        prow[l, PR_DTB:PR_DTB + 16] = inputs["ssd_dt_bias"][l]
        prow[l, PR_ALOG:PR_ALOG + 16] = inputs["ssd_a_log"][l]
        prow[l, PR_RB:PR_RB + NE] = inputs["router_bias"][l]
        for m in range(4):
            for q in range(2):
                wabd[l, m, q * 64:(q + 1) * 64, q * 64:(q + 1) * 64] = inputs["lru_w_a"][l, 2 * m + q]
                wxbd[l, m, q * 64:(q + 1) * 64, q * 64:(q + 1) * 64] = inputs["lru_w_x"][l, 2 * m + q]
        wal[l, 0:16] = inputs["gla_w_alpha"][l]
        wal[l, 16] = inputs["gla_b_alpha"][l]
    shared.update(pp=pp, prow=prow, lru_wa_bd=wabd, lru_wx_bd=wxbd, walpha_ext=wal)
    for k in ("w_ada", "b_ada", "w_in", "w_out", "router_w", "exp_w1", "exp_w3", "exp_w2", "shared_w1", "shared_w3", "shared_w2"):
        shared[k] = f(inputs[k])
    x = np.asarray(inputs["x"], np.float32)
    c = np.asarray(inputs["c"], np.float32)
    maps = []
    for b in range(x.shape[0]):
        m = dict(shared)
        m["xT"] = np.ascontiguousarray(x[b].T)
        m["cpc"] = np.ascontiguousarray(c[b].reshape(8, 128).T)
        maps.append(m)
    return maps


_NC_CACHE = {}


def kernel(**inputs):
    maps = prep_inputs(inputs)
    if "nc" not in _NC_CACHE:
        _NC_CACHE["nc"] = build_program()
    nc = _NC_CACHE["nc"]
    res = run_bass_kernel_spmd(nc, maps, core_ids=list(range(len(maps))))
    out = np.stack([np.ascontiguousarray(r["yT"].T) for r in res.results], axis=0)
    return out.astype(np.float32)
```

```python
from contextlib import ExitStack
import numpy as np
import concourse.bass as bass
import concourse.mybir as mybir
from concourse.bass_utils import run_bass_kernel_spmd

F32 = mybir.dt.float32
BF16 = mybir.dt.bfloat16
AF = mybir.ActivationFunctionType
ALU = mybir.AluOpType
AX = mybir.AxisListType

D = 1024
S = 2048
DEPTH = 4
NE = 64
ALPHA = (2 * DEPTH) ** 0.25
DIN = 5152
NPP = 144
TB = 512
NTB = S // TB


class Prog:
    def __init__(self, nc, stack):
        self.nc = nc
        self.stack = stack
        self.ops = []
        self.last_write = {}
        self.readers = {}
        self.epoch = 0
        self.op_epoch = []
        self.group_open = {}
        self.group_of = {}
        self.nsem = 0
        self.bar = None
        self.xeng = []
        self.cap = None

    def barrier(self, fn):
        allk = list(set(self.last_write.keys()) | set(k for k, v in self.readers.items() if v))
        oid = self._add("dve", fn, allk, allk)
        self.bar = oid
        self.last_write = {}
        self.readers = {}

    def new_sem(self, name):
        self.nsem += 1
        return self.stack.enter_context(self.nc.semaphore(f"{name}_{self.nsem}"))

    def new_epoch(self):
        self.epoch += 1

    def _add(self, eng, fn, reads, writes, chan=None):
        oid = len(self.ops)
        raw = set()
        oth = set()
        xe = set()
        for k in reads:
            w = self.last_write.get(k)
            if w is not None:
                raw.add(w)
            if isinstance(k, tuple) and k[0] in ("ps", "ps2o"):
                for r in self.readers.get(k, ()):
                    xe.add(r)
        for k in writes:
            w = self.last_write.get(k)
            if w is not None:
                oth.add(w)
            for r in self.readers.get(k, ()):
                oth.add(r)
        if self.bar is not None:
            oth.add(self.bar)
        oth -= raw
        raw.discard(oid)
        oth.discard(oid)
        xe -= raw
        xe -= oth
        xe.discard(oid)
        self.xeng.append(sorted(xe))
        self.ops.append([eng, fn, sorted(raw), sorted(oth), chan])
        self.op_epoch.append(self.epoch)
        for k in reads:
            self.readers.setdefault(k, []).append(oid)
        for k in writes:
            self.last_write[k] = oid
            self.readers[k] = []
        return oid

    def op(self, eng, fn, reads=(), writes=()):
        if self.cap is not None:
            self.cap.append((eng, fn, list(reads), list(writes)))
            return None
        return self._add(eng, fn, reads, writes)

    def dma(self, chan, out, in_, reads=(), writes=(), eng="sp", more=False, **kw):
        def fn(e, out=out, in_=in_, kw=kw):
            return e.dma_start(out=out, in_=in_, **kw)
        oid = self._add(eng, fn, reads, writes, chan=chan)
        g = self.group_open.get(chan)
        if g is None:
            g = []
            self.group_open[chan] = g
        g.append(oid)
        self.group_of[oid] = g
        if not more:
            self.group_open[chan] = None
        return oid

    def emit(self):
        nc = self.nc
        n = len(self.ops)
        is_dma = [o[4] is not None for o in self.ops]
        need_sig = [False] * n
        deps_of = []
        for i, (eng, fn, raw, oth, chan) in enumerate(self.ops):
            deps = []
            for d in raw:
                if is_dma[d] or is_dma[i] or self.ops[d][0] != eng or eng != "pe":
                    deps.append(d)
            for d in oth:
                if is_dma[d] or is_dma[i] or self.ops[d][0] != eng or eng != "pe":
                    deps.append(d)
            for d in self.xeng[i]:
                if self.ops[d][0] != eng:
                    deps.append(d)
            deps_of.append(deps)
            for d in deps:
                if not is_dma[d]:
                    need_sig[d] = True
        sig = [None] * n
        cur = {}
        chan_sem = {}
        chan_cnt = {}
        for i, (eng, fn, raw, oth, chan) in enumerate(self.ops):
            if is_dma[i]:
                if chan not in chan_sem:
                    chan_sem[chan] = self.new_sem("d")
                    chan_cnt[chan] = 0
                chan_cnt[chan] += 16
                sig[i] = (chan_sem[chan], chan_cnt[chan])
            elif need_sig[i]:
                key = (eng, self.op_epoch[i])
                if key not in cur:
                    cur[key] = [self.new_sem(eng), 0]
                cur[key][1] += 1
                sig[i] = (cur[key][0], cur[key][1])
        for i in range(n):
            if is_dma[i]:
                last = self.group_of[i][-1]
                if last != i:
                    sig[i] = (sig[i][0], sig[last][1])
        streams = {}
        for i, (eng, fn, raw, oth, chan) in enumerate(self.ops):
            streams.setdefault(eng, []).append((i, fn, [sig[d] for d in deps_of[i]]))
        final_dma = [(chan_sem[c], chan_cnt[c]) for c in chan_sem]
        self.n_waits = 0

        def run_stream(e, items, tail):
            waited = {}
            for (i, fn, waits) in items:
                best = {}
                for (s, v) in waits:
                    k = s.num
                    if waited.get(k, 0) >= v:
                        continue
                    if k not in best or best[k][1] < v:
                        best[k] = (s, v)
                for k, (s, v) in best.items():
                    e.wait_ge(s, v)
                    waited[k] = v
                    self.n_waits += 1
                ins = fn(e)
                if is_dma[i]:
                    ins.then_inc(chan_sem[self.ops[i][4]], 16)
                elif sig[i] is not None:
                    ins.then_inc(sig[i][0], 1)
            for (s, v) in tail:
                e.wait_ge(s, v)

        with nc.Block() as block:
            names = {"pe": "tensor", "act": "scalar", "dve": "vector", "pool": "gpsimd", "sp": "sync"}
            for en, attr in names.items():
                items = streams.get(en, [])
                tail = final_dma if en == "sp" else []
                if not items and not tail:
                    continue

                def body(e, items=items, tail=tail):
                    run_stream(e, items, tail)
                getattr(block, attr)(body)


C_ID = 0
C_MASK = 128
C_SUF = 256
C_CH0 = 384
C_CH1 = 512
C_M64 = 640
C_ONESD = 704
C_ONE = 832
C_SEL = 960
C_MASKG = 1984
NCONST = 2240


def build_consts():
    c = np.zeros((128, NCONST), np.float32)
    idx = np.arange(128)
    same = (idx[:, None] // 64) == (idx[None, :] // 64)
    c[:, C_ID:C_ID + 128] = np.eye(128)
    c[:, C_MASK:C_MASK + 128] = same & (idx[:, None] <= idx[None, :])
    c[:, C_SUF:C_SUF + 128] = same & (idx[:, None] > idx[None, :])
    c[:64, C_CH0:C_CH0 + 128] = 1.0
    c[64:, C_CH1:C_CH1 + 128] = 1.0
    j = idx % 64
    c[:, C_M64:C_M64 + 64] = j[:, None] <= np.arange(64)[None, :]
    c[:, C_ONESD:C_ONESD + 128] = 1.0 / 1024.0
    c[:, C_ONE:C_ONE + 128] = 1.0
    for h in range(8):
        c[h, C_SEL + h * 128:C_SEL + (h + 1) * 128] = 1.0
    c[:, C_MASKG:C_MASKG + 128] = c[:, C_MASK:C_MASK + 128] * (-1.0 / 16.0)
    c[:, C_MASKG + 128:C_MASKG + 256] = c[:, C_SUF:C_SUF + 128] * (-1.0 / 16.0)
    return c


PP_LN1G, PP_LN1B, PP_LN2G, PP_LN2B = 0, 8, 16, 24
PP_LCW, PP_LCB, PP_LBA, PP_LBX, PP_LLAM = 32, 48, 52, 56, 60
PP_SCW, PP_SCB, PP_SNORM, PP_GNORM, PP_SD = 64, 112, 124, 132, 133
PR_DTB, PR_ALOG, PR_RB = 0, 16, 32
NPR = 96


def build_program(depth=DEPTH, do_lru=True, do_gla=True, do_ssd=True, do_moe=True, n_exp=NE + 1, debug=False):
    nc = bass.Bass("TRN2", target_bir_lowering=False, dynamic_dma_scratch_size=512)
    dr = {}

    def din(name, shape):
        dr[name] = nc.dram_tensor(name, list(shape), F32, kind="ExternalInput").ap()
        return dr[name]

    xT_d = din("xT", [D, S])
    cpc_d = din("cpc", [128, 8])
    consts_d = din("consts", [128, NCONST])
    pp_d = din("pp", [DEPTH, 128, NPP])
    prow_d = din("prow", [DEPTH, NPR])
    wada_d = din("w_ada", [DEPTH, D, 6 * D])
    bada_d = din("b_ada", [DEPTH, 6 * D])
    win_d = din("w_in", [DEPTH, D, DIN])
    wout_d = din("w_out", [DEPTH, 2 * D, D])
    wabd_d = din("lru_wa_bd", [DEPTH, 4, 128, 128])
    wxbd_d = din("lru_wx_bd", [DEPTH, 4, 128, 128])
    walpha_d = din("walpha_ext", [DEPTH, 32, 256])
    rw_d = din("router_w", [DEPTH, D, NE])
    ew1_d = din("exp_w1", [DEPTH, NE, D, 256])
    ew3_d = din("exp_w3", [DEPTH, NE, D, 256])
    ew2_d = din("exp_w2", [DEPTH, NE, 256, D])
    sw1_d = din("shared_w1", [DEPTH, D, 256])
    sw3_d = din("shared_w3", [DEPTH, D, 256])
    sw2_d = din("shared_w2", [DEPTH, 256, D])
    yT_d = nc.dram_tensor("yT", [D, S], F32, kind="ExternalOutput").ap()
    gscr_d = nc.dram_tensor("gscr", [2, NE, S], F32, kind="Internal").ap()
    dbg = {}

    with ExitStack() as st:
        p = Prog(nc, st)

        def sb(name, shape, dt=F32):
            return st.enter_context(nc.sbuf_tensor(name, list(shape), dt))

        def cap_ops(fn, *a):
            lst = []
            p.cap = lst
            r = fn(*a)
            p.cap = None
            return lst

        def replay_ops(lst):
            for (eng, fn, r, w) in lst:
                p.op(eng, fn, reads=r, writes=w)

        def interleave(lists):
            out = []
            n = max(len(x) for x in lists)
            for k in range(n):
                for x in lists:
                    if k < len(x):
                        out.append(x[k])
            return out

        xT = sb("xT_sb", [128, 8, S])
        hT = sb("hT_sb", [128, 8, S], BF16)
        consts = sb("consts_sb", [128, NCONST])
        pp = sb("pp_sb", [128, DEPTH, NPP])
        mod = sb("mod_sb", [128, DEPTH, 64])
        cond = sb("cond_sb", [128, 8])
        SCRW = 29150
        scr = sb("scr_sb", [128, SCRW])
        banks = [st.enter_context(nc.psum_tensor(f"bank{i}", [128, 512], F32)) for i in range(8)]

        def PS(i):
            return ("ps", i)

        class Carve:
            def __init__(self):
                self.off = 0

            def get(self, shape, dt=F32):
                n = int(np.prod(shape[1:]))
                words = n if dt == F32 else (n + 1) // 2
                a = scr[:, self.off:self.off + words]
                self.off += words
                assert self.off <= SCRW - 1, self.off
                if dt != F32:
                    a = a.bitcast(dt)
                    if n % 2:
                        a = a[:, 0:n]
                if len(shape) == 3:
                    a = a.rearrange("p (a b) -> p a b", a=shape[1])
                elif len(shape) == 4:
                    a = a.rearrange("p (a b c) -> p a b c", a=shape[1], b=shape[2])
                if shape[0] != 128:
                    a = a[0:shape[0]]
                return a

        ident = consts[:, C_ID:C_ID + 128]
        onesD = consts[:, C_ONESD:C_ONESD + 128]

        def barrier():
            tok = scr[:, SCRW - 1:SCRW]
            p.barrier(lambda e: e.memset(tok, 0.0))

        p.dma("ld0", consts[:], consts_d[:, :], writes=["consts"])
        p.dma("ld1", pp[:], pp_d.rearrange("l p n -> p l n"), writes=["pp"])
        p.dma("ld2", cond[:], cpc_d[:, :], writes=["cond"])
        p.dma("ldx", xT[:], xT_d.rearrange("(c p) t -> p c t", p=128), writes=[("x", c, tb) for c in range(8) for tb in range(NTB)])
        p.op("act", lambda e: e.activation(out=cond[:], in_=cond[:], func=AF.Silu), reads=["cond"], writes=["cond"])

        ADA_BLK = 256
        N_ADA = 6 * D // ADA_BLK
        ada_stg = [None, None]

        def adaln_block(l, blk, cv_bufs, bank):
            stg, brow, mrow = cv_bufs[blk % 2]
            key = ("adastg", blk % 2)
            c0 = blk * ADA_BLK
            nj = ADA_BLK // 128
            p.dma(("adab", blk % 2), brow, bada_d[l:l + 1, c0:c0 + ADA_BLK], writes=[("adabrow", blk % 2)])
            p.dma(("ada", blk % 2), stg, wada_d[l].rearrange("(kc p) f -> p kc f", p=128)[:, :, c0:c0 + ADA_BLK], writes=[key])

            def mm(e, stg=stg):
                ins = None
                for kc in range(8):
                    ins = e.matmul(banks[bank][0:1, 0:ADA_BLK], lhsT=cond[:, kc:kc + 1], rhs=stg[:, kc, :], start=(kc == 0), stop=(kc == 7))
                return ins
            p.op("pe", mm, reads=[key, "cond"], writes=[PS(bank)])
            p.op("dve", lambda e: e.tensor_tensor(out=mrow, in0=banks[bank][0:1, 0:ADA_BLK], in1=brow, op=ALU.add),
                 reads=[PS(bank), ("adabrow", blk % 2)], writes=[("adamrow", blk % 2)])

            def mm2(e):
                ins = None
                for j in range(nj):
                    ins = e.matmul(banks[bank][:, 256 + j:257 + j], lhsT=mrow[0:1, j * 128:(j + 1) * 128], rhs=consts[0:1, C_ONE:C_ONE + 1], start=True, stop=True)
                return ins
            p.op("pe", mm2, reads=[("adamrow", blk % 2), "consts"], writes=[PS(bank)])
            p.op("act", lambda e: e.copy(out=mod[:, l, blk * nj:(blk + 1) * nj], in_=banks[bank][:, 256:256 + nj]), reads=[PS(bank)], writes=[("mod", l)])

        def adaln_finish(l, bank):
            p.op("dve", lambda e: e.tensor_scalar(out=mod[:, l, 48:56], in0=mod[:, l, 8:16], scalar1=1.0, scalar2=1.0 / float(ALPHA), op0=ALU.add, op1=ALU.mult), reads=[("mod", l)], writes=[("modd", l)])
            p.op("dve", lambda e: e.tensor_scalar(out=mod[:, l, 56:64], in0=mod[:, l, 32:40], scalar1=1.0, scalar2=1.0 / float(ALPHA), op0=ALU.add, op1=ALU.mult), reads=[("mod", l)], writes=[("modd2", l)])

        def ada_bufs_alloc(cv):
            return [(cv.get([128, 8, ADA_BLK]), cv.get([1, ADA_BLK]), cv.get([1, ADA_BLK])) for _ in range(2)]

        def MOD(l, j, c):
            if j < 6:
                return mod[:, l, j * 8 + c:j * 8 + c + 1]
            return mod[:, l, 48 + (j - 6) * 8 + c:48 + (j - 6) * 8 + c + 1]

        def PPc(l, col):
            return pp[:, l, col:col + 1]

        modkeys = lambda l: [("mod", l), ("modd", l), ("modd2", l)]

        def modulate(l, which, tb, engs=("dve", "pool")):
            jsc, jsh = (6, 0) if which == 1 else (7, 3)
            for c in range(8):
                eng = engs[c % len(engs)]
                p.op(eng, lambda e, c=c: e.tensor_scalar(out=hT[:, c, tb * TB:(tb + 1) * TB], in0=xT[:, c, tb * TB:(tb + 1) * TB],
                                                         scalar1=MOD(l, jsc, c), scalar2=MOD(l, jsh, c), op0=ALU.mult, op1=ALU.add),
                     reads=[("x", c, tb)] + modkeys(l), writes=[("h", c, tb)])

        def scale_alpha(tb, engs=("pool",)):
            for c in range(8):
                eng = engs[c % len(engs)]
                if eng == "act":
                    p.op(eng, lambda e, c=c: e.mul(out=xT[:, c, tb * TB:(tb + 1) * TB], in_=xT[:, c, tb * TB:(tb + 1) * TB], mul=float(ALPHA)),
                         reads=[("x", c, tb)], writes=[("x", c, tb)])
                else:
                    p.op(eng, lambda e, c=c: e.tensor_scalar_mul(out=xT[:, c, tb * TB:(tb + 1) * TB], in0=xT[:, c, tb * TB:(tb + 1) * TB], scalar1=float(ALPHA)),
                         reads=[("x", c, tb)], writes=[("x", c, tb)])

        def layernorm(l, gcol, bcol, cv, bank_m, bank_q, final=False):
            def GB(col):
                return pp[:, l, col:col + 1] if final else ppA[:, l, col:col + 1]
            sq = [cv.get([128, TB]) for _ in range(2)]
            mean_sbs = [cv.get([128, TB]) for _ in range(2)]
            rstds = [cv.get([128, TB]) for _ in range(2)]
            tmp = [cv.get([128, TB]) for _ in range(2)]
            tmp2 = [cv.get([128, TB]) for _ in range(2)]
            bank_m0, bank_q0 = bank_m, bank_q
            for tb in range(NTB):
                sl = slice(tb * TB, (tb + 1) * TB)
                mean_sb = mean_sbs[tb % 2]
                rstd = rstds[tb % 2]
                bank_m = bank_m0 + 2 * (tb % 2)
                bank_q = bank_q0 + 2 * (tb % 2)
                KM = ("lnmean", tb % 2)
                KR = ("lnrstd", tb % 2)

                def mm_mean(e, sl=sl, bank_m=bank_m):
                    ins = None
                    for c in range(8):
                        ins = e.matmul(banks[bank_m][:, :], lhsT=onesD, rhs=xT[:, c, sl], start=(c == 0), stop=(c == 7))
                    return ins
                p.op("pe", mm_mean, reads=[("x", c, tb) for c in range(8)] + ["consts"], writes=[PS(bank_m)])
                for c in range(8):
                    p.op("act", lambda e, c=c, sl=sl: e.activation(out=sq[c % 2], in_=xT[:, c, sl], func=AF.Square), reads=[("x", c, tb)], writes=[("lnsq", c % 2)])
                    p.op("pe", lambda e, c=c, bank_q=bank_q: e.matmul(banks[bank_q][:, :], lhsT=onesD, rhs=sq[c % 2], start=(c == 0), stop=(c == 7)),
                         reads=[("lnsq", c % 2), "consts"], writes=[PS(bank_q)])
                p.op("act", lambda e, mean_sb=mean_sb, bank_m=bank_m: e.copy(out=mean_sb, in_=banks[bank_m][:, :]), reads=[PS(bank_m)], writes=[KM])
                p.op("dve", lambda e, rstd=rstd, mean_sb=mean_sb: e.tensor_tensor(out=rstd, in0=mean_sb, in1=mean_sb, op=ALU.mult), reads=[KM], writes=[KR])
                p.op("dve", lambda e, rstd=rstd, bank_q=bank_q: e.tensor_tensor(out=rstd, in0=banks[bank_q][:, :], in1=rstd, op=ALU.subtract), reads=[PS(bank_q), KR], writes=[KR])
                p.op("act", lambda e, rstd=rstd: e.activation(out=rstd, in_=rstd, func=AF.Ln, bias=LNEPS[:, 0:1]), reads=[KR, "lneps"], writes=[KR])
                p.op("act", lambda e, rstd=rstd: e.activation(out=rstd, in_=rstd, func=AF.Exp, scale=-0.5), reads=[KR], writes=[KR])
                for c in range(8):
                    k = c % 2
                    p.op("dve", lambda e, c=c, k=k, sl=sl, mean_sb=mean_sb: e.tensor_tensor(out=tmp[k], in0=xT[:, c, sl], in1=mean_sb, op=ALU.subtract),
                         reads=[("x", c, tb), KM], writes=[("lnt", k)])
                    p.op("pool", lambda e, k=k, rstd=rstd: e.tensor_tensor(out=tmp2[k], in0=tmp[k], in1=rstd, op=ALU.mult), reads=[("lnt", k), KR], writes=[("lnt2", k)])
                    p.op("act", lambda e, c=c, k=k, sl=sl: e.activation(out=xT[:, c, sl], in_=tmp2[k], func=AF.Identity, scale=GB(gcol + c), bias=GB(bcol + c)),
                         reads=[("lnt2", k), "pp", "ppA"], writes=[("x", c, tb)])

        ppA = sb("ppA_sb", [128, DEPTH, 32])
        p.op("dve", lambda e: e.tensor_scalar_mul(out=ppA[:], in0=pp[:, :, 0:32], scalar1=float(ALPHA)), reads=["pp"], writes=["ppA"])
        LNEPS = sb("lneps_sb", [128, 4])
        p.op("pool", lambda e: e.memset(LNEPS[:, 0:1], 1e-5), writes=["lneps"])
        p.op("pool", lambda e: e.memset(LNEPS[:, 1:2], 1e-6), reads=[], writes=["lneps"])
        p.op("pool", lambda e: e.memset(LNEPS[:, 2:3], 1.0), reads=[], writes=["lneps"])

        def router(l, cv, bank_l, bank_t):
            NI = 4
            rw = cv.get([128, 8, NE])
            rb = cv.get([128, NE])
            gT = cv.get([64, S])
            h32s = [cv.get([128, 8, 128]) for _ in range(NI)]
            Ws = [{n: cv.get([128, 64]) for n in ("sc", "bi", "eq", "b2", "mk", "sel", "gw", "gates")} for _ in range(NI)]
            Sms = [{n: cv.get([128, 8]) for n in ("m1", "m2", "gs", "t8", "gsel", "goff", "t8e")} for _ in range(NI)]
            s1s = [cv.get([128, 2]) for _ in range(NI)]
            p.dma("rw", rw, rw_d[l].rearrange("(kc p) e -> p kc e", p=128), writes=["rw"])
            p.dma("rb", rb, prow_d[l:l + 1, PR_RB:PR_RB + NE].partition_broadcast(128), writes=["rb"])
            g3 = lambda a: a.rearrange("p (g k) -> p g k", k=8)
            b3 = lambda a: a.unsqueeze(2).to_broadcast([128, 8, 8])

            def tile_ops(tt):
                j = tt % NI
                tb = tt // 4
                tsl = slice(tt * 128, (tt + 1) * 128)
                h32, W, Sm, s1 = h32s[j], Ws[j], Sms[j], s1s[j]
                bl, bt = j, 4 + j
                K = lambda n: (n, j)
                ops = []
                A = lambda eng, fn, r, w: ops.append((eng, fn, r, w))
                for c in range(8):
                    eng = ("dve", "pool")[c % 2]
                    A(eng, lambda e, c=c: e.tensor_scalar(out=h32[:, c, :], in0=xT[:, c, tsl], scalar1=MOD(l, 7, c), scalar2=MOD(l, 3, c), op0=ALU.mult, op1=ALU.add),
                      [("x", c, tb)] + modkeys(l), [("h32", j, c)])

                def mm(e):
                    ins = None
                    for c in range(8):
                        ins = e.matmul(banks[bl][:, 0:NE], lhsT=h32[:, c, :], rhs=rw[:, c, :], start=(c == 0), stop=(c == 7))
                    return ins
                A("pe", mm, [("h32", j, c) for c in range(8)] + ["rw"], [PS(bl)])
                A("act", lambda e: e.activation(out=W["sc"], in_=banks[bl][:, 0:NE], func=AF.Sigmoid), [PS(bl)], [K("r_sc")])
                V = lambda fn, r, w: A("dve", fn, r, w)
                V(lambda e: e.tensor_tensor(out=W["bi"], in0=W["sc"], in1=rb, op=ALU.add), [K("r_sc"), "rb"], [K("r_bi")])
                V(lambda e: e.tensor_reduce(out=Sm["m1"], in_=g3(W["bi"]), axis=AX.X, op=ALU.max), [K("r_bi")], [K("r_m1")])
                V(lambda e: e.tensor_tensor(out=g3(W["eq"]), in0=g3(W["bi"]), in1=b3(Sm["m1"]), op=ALU.is_equal), [K("r_bi"), K("r_m1")], [K("r_eq")])
                V(lambda e: e.scalar_tensor_tensor(out=W["b2"], in0=W["eq"], scalar=-10.0, in1=W["bi"], op0=ALU.mult, op1=ALU.add), [K("r_eq"), K("r_bi")], [K("r_b2")])
                V(lambda e: e.tensor_reduce(out=Sm["m2"], in_=g3(W["b2"]), axis=AX.X, op=ALU.max), [K("r_b2")], [K("r_m2")])
                V(lambda e: e.tensor_tensor(out=Sm["gs"], in0=Sm["m1"], in1=Sm["m2"], op=ALU.add), [K("r_m1"), K("r_m2")], [K("r_gs")])
                V(lambda e: e.max(out=Sm["t8"], in_=Sm["gs"]), [K("r_gs")], [K("r_t8")])
                V(lambda e: e.tensor_scalar(out=Sm["gsel"], in0=Sm["gs"], scalar1=Sm["t8"][:, 3:4], scalar2=None, op0=ALU.is_ge), [K("r_gs"), K("r_t8")], [K("r_gsel")])
                V(lambda e: e.tensor_scalar(out=Sm["goff"], in0=Sm["gsel"], scalar1=10.0, scalar2=-10.0, op0=ALU.mult, op1=ALU.add), [K("r_gsel")], [K("r_goff")])
                V(lambda e: e.tensor_tensor(out=g3(W["mk"]), in0=g3(W["bi"]), in1=b3(Sm["gsel"]), op=ALU.mult), [K("r_bi"), K("r_gsel")], [K("r_mk")])
                V(lambda e: e.tensor_tensor(out=g3(W["mk"]), in0=g3(W["mk"]), in1=b3(Sm["goff"]), op=ALU.add), [K("r_mk"), K("r_goff")], [K("r_mk")])
                V(lambda e: e.max(out=Sm["t8e"], in_=W["mk"]), [K("r_mk")], [K("r_t8e")])
                V(lambda e: e.tensor_scalar(out=W["sel"], in0=W["mk"], scalar1=Sm["t8e"][:, 7:8], scalar2=None, op0=ALU.is_ge), [K("r_mk"), K("r_t8e")], [K("r_sel")])
                V(lambda e: e.tensor_tensor(out=W["gw"], in0=W["sel"], in1=W["sc"], op=ALU.mult), [K("r_sel"), K("r_sc")], [K("r_gw")])
                V(lambda e: e.tensor_reduce(out=s1[:, 0:1], in_=W["gw"], axis=AX.X, op=ALU.add), [K("r_gw")], [K("r_s1")])
                V(lambda e: e.reciprocal(out=s1[:, 1:2], in_=s1[:, 0:1]), [K("r_s1")], [K("r_s2")])
                V(lambda e: e.tensor_scalar(out=W["gates"], in0=W["gw"], scalar1=s1[:, 1:2], scalar2=2.5, op0=ALU.mult, op1=ALU.mult), [K("r_gw"), K("r_s2")], [K("r_gates")])
                A("pe", lambda e: e.transpose(banks[bt][0:64, 0:128], W["gates"], ident), [K("r_gates"), "consts"], [PS(bt)])
                A("act", lambda e: e.copy(out=gT[:, tsl], in_=banks[bt][0:64, 0:128]), [PS(bt)], [("gT", tt)])
                return ops

            for g0 in range(0, S // 128, NI):
                lists = [tile_ops(tt) for tt in range(g0, g0 + NI)]
                for k in range(len(lists[0])):
                    for ol in lists:
                        eng, fn, r, w = ol[k]
                        p.op(eng, fn, reads=r, writes=w)
            p.dma("gst", gscr_d[l % 2], gT, reads=[("gT", tt) for tt in range(S // 128)], writes=[("gscr", l % 2)])

        def moe(l, cv, hooks):
            stg = {n: cv.get([128, 8, 256]) for n in ("w1", "w3")}
            stg["w2"] = cv.get([128, 2, D])
            wbf = [{"w1": cv.get([128, 8, 256], BF16), "w3": cv.get([128, 8, 256], BF16), "w2": cv.get([128, 2, D], BF16)} for _ in range(2)]
            gbc = [cv.get([128, S]) for _ in range(2)]
            sS = [[cv.get([128, TB], BF16) for f in range(2)] for _ in range(2)]
            tS = [[cv.get([128, TB], BF16) for f in range(2)] for _ in range(2)]
            hid = [[cv.get([128, TB], BF16) for f in range(2)] for _ in range(2)]
            steps = [(e, tb) for e in range(n_exp) for tb in range(NTB)]

            def load(e):
                sl = e % 2
                if e < NE:
                    srcs = {"w1": ew1_d[l, e], "w3": ew3_d[l, e], "w2": ew2_d[l, e]}
                else:
                    srcs = {"w1": sw1_d[l], "w3": sw3_d[l], "w2": sw2_d[l]}
                for n in ("w1", "w3", "w2"):
                    pat = "(kc p) f -> p kc f"
                    p.dma(("wst", n), stg[n], srcs[n].rearrange(pat, p=128), writes=[("stg", n)])
                if e < NE:
                    p.dma(("gbc", sl), gbc[sl], gscr_d[l % 2, e:e + 1, :].partition_broadcast(128), reads=[("gscr", l % 2)], writes=[("gbc", sl)])

            def cast(e):
                sl = e % 2
                for n in ("w1", "w3", "w2"):
                    if n == "w2":
                        parts = [(slice(0, 1), "act"), (slice(1, 2), "pool")]
                    else:
                        parts = [(slice(0, 3), "act"), (slice(3, 8), "pool")]
                    for (ps_, ce) in parts:
                        if ce == "act":
                            p.op("act", lambda e_, n=n, sl=sl, ps_=ps_: e_.copy(out=wbf[sl][n][:, ps_, :], in_=stg[n][:, ps_, :]), reads=[("stg", n)], writes=[("wbf", sl, n, ce)])
                        else:
                            p.op("pool", lambda e_, n=n, sl=sl, ps_=ps_: e_.tensor_copy(out=wbf[sl][n][:, ps_, :], in_=stg[n][:, ps_, :]), reads=[("stg", n)], writes=[("wbf", sl, n, ce)])

            def up(i, f):
                e, tb = steps[i]
                sl = e % 2
                for wi, n in enumerate(("w1", "w3")):
                    bk = f * 2 + wi

                    def mm(e_, n=n, bk=bk, sl=sl, tb=tb, f=f):
                        ins = None
                        for kc in range(8):
                            ins = e_.matmul(banks[bk][:, :], lhsT=wbf[sl][n][:, kc, f * 128:(f + 1) * 128], rhs=hT[:, kc, tb * TB:(tb + 1) * TB], start=(kc == 0), stop=(kc == 7))
                        return ins
                    p.op("pe", mm, reads=[("wbf", sl, n, "act"), ("wbf", sl, n, "pool")] + [("h", kc, tb) for kc in range(8)], writes=[PS(bk)])

            def gating(i, f):
                e, tb = steps[i]
                sl = e % 2
                par = i % 2
                p.op("act", lambda e_: e_.activation(out=sS[par][f], in_=banks[f * 2][:, :], func=AF.Silu), reads=[PS(f * 2)], writes=[("sS", par, f)])
                if e < NE:
                    p.op("dve", lambda e_: e_.tensor_tensor(out=tS[par][f], in0=banks[f * 2 + 1][:, :], in1=gbc[sl][:, tb * TB:(tb + 1) * TB], op=ALU.mult),
                         reads=[PS(f * 2 + 1), ("gbc", sl)], writes=[("tS", par, f)])
                    p.op("dve", lambda e_: e_.tensor_tensor(out=hid[par][f], in0=sS[par][f], in1=tS[par][f], op=ALU.mult),
                         reads=[("sS", par, f), ("tS", par, f)], writes=[("hid", par, f)])
                else:
                    p.op("dve", lambda e_: e_.tensor_tensor(out=hid[par][f], in0=banks[f * 2 + 1][:, :], in1=sS[par][f], op=ALU.mult),
                         reads=[PS(f * 2 + 1), ("sS", par, f)], writes=[("hid", par, f)])

            def down(i, dh):
                e, tb = steps[i]
                sl = e % 2
                par = i % 2
                for dq in range(4):
                    d = dh * 4 + dq
                    bk = 4 + dq

                    def mm(e_, d=d, bk=bk):
                        ins = None
                        for f in range(2):
                            ins = e_.matmul(banks[bk][:, :], lhsT=wbf[sl]["w2"][:, f, d * 128:(d + 1) * 128], rhs=hid[par][f], start=(f == 0), stop=(f == 1))
                        return ins
                    p.op("pe", mm, reads=[("wbf", sl, "w2", "act"), ("wbf", sl, "w2", "pool"), ("hid", par, 0), ("hid", par, 1)], writes=[PS(bk)])
                    p.op("dve", lambda e_, d=d, bk=bk: e_.scalar_tensor_tensor(out=xT[:, d, tb * TB:(tb + 1) * TB], in0=banks[bk][:, :], scalar=MOD(l, 5, d), in1=xT[:, d, tb * TB:(tb + 1) * TB], op0=ALU.mult, op1=ALU.add),
                         reads=[PS(bk), ("x", d, tb)] + modkeys(l), writes=[("x", d, tb)])

            load(0)
            cast(0)
            if n_exp > 1:
                load(1)
                cast(1)
            up(0, 0)
            up(0, 1)
            gating(0, 0)
            gating(0, 1)
            for i in range(len(steps)):
                e, tb = steps[i]
                if tb == 0 and i > 0 and e + 1 < n_exp:
                    load(e + 1)
                if tb == 2 and e > 0 and e + 1 < n_exp:
                    cast(e + 1)
                if tb == 1 and e in hooks:
                    hooks[e]()
                nxt = i + 1 < len(steps)
                if nxt:
                    up(i + 1, 0)
                down(i, 0)
                if nxt:
                    gating(i + 1, 0)
                    up(i + 1, 1)
                down(i, 1)
                if nxt:
                    gating(i + 1, 1)


        def ssd_units(l, cv, mark, load_win, load_wout, proj_fm, proj_tm, out_proj, conv_silu, yblk, identb, ones512b):
            cv.off = mark
            dtb = cv.get([128, 16]); alog = cv.get([128, 16]); aneg = cv.get([128, 16])
            cbuf = [cv.get([128, 3 + TB]) for _ in range(6)]
            ctmps = [cv.get([128, TB]) for _ in range(3)]
            ctmp = ctmps[0]
            xs = [cv.get([128, TB], BF16) for _ in range(4)]
            BT = cv.get([128, TB], BF16); CT = cv.get([128, TB], BF16)
            sz = [cv.get([128, TB], BF16) for _ in range(4)]
            yg = cv.get([128, 4, TB])
            dt_tm = cv.get([128, 8]); dA = cv.get([128, 8]); acs = cv.get([128, 8]); dte = cv.get([128, 8])
            w2 = cv.get([128, 8]); draw = cv.get([128, 8]); ex = cv.get([128, 8])
            dAb = cv.get([128, 8, 128])
            L = dAb
            Btmzs = [[cv.get([128, 128], BF16) for _ in range(2)] for _ in range(2)]
            decbcs = [cv.get([128, 2, 8]) for _ in range(2)]
            eD = cv.get([128, 8, 128], BF16)
            cbm = cv.get([128, 128])
            MTs = [cv.get([128, 8, 128], BF16) for _ in range(2)]
            CTss = [cv.get([128, 8, 128], BF16) for _ in range(2)]
            xdts = [cv.get([128, 8, 64], BF16) for _ in range(2)]
            xws = [cv.get([128, 8, 64], BF16) for _ in range(2)]
            pa_ctr = [0]
            Btm = cv.get([128, 128], BF16)
            S32 = cv.get([128, 8, 64])
            Sbf = [cv.get([128, 8, 64], BF16) for _ in range(2)]
            sqb = cv.get([128, TB], BF16); rs = ctmp
            b7 = banks[7][:, :].bitcast(BF16)
            MASK = consts[:, C_MASK:C_MASK + 128]
            SUF = consts[:, C_SUF:C_SUF + 128]
            p.dma("dtb", dtb, prow_d[l:l + 1, PR_DTB:PR_DTB + 16].partition_broadcast(128), writes=["dtb"])
            p.dma("alog", alog, prow_d[l:l + 1, PR_ALOG:PR_ALOG + 16].partition_broadcast(128), writes=["alog"])
            p.op("act", lambda e: e.activation(out=aneg, in_=alog, func=AF.Exp), reads=["alog"], writes=["aneg"])
            p.op("dve", lambda e: e.tensor_scalar_mul(out=aneg, in0=aneg, scalar1=-1.0), reads=["aneg"], writes=["aneg"])
            for g in range(2):
                load_win(3600 + g * 512, 512, 0)
                load_win(2576 + g * 512, 512, 512)
                load_win(4624 + g * 128, 128, 1024)
                load_win(4880 + g * 128, 128, 1152)
                load_win(5136 + g * 8, 8, 1280)
                load_wout(8 + g * 4, 4)
                for j6 in range(6):
                    p.op("pool", lambda e, j6=j6: e.memset(cbuf[j6][:, 0:3], 0.0), writes=[("cbuf", j6)])
                p.op("pool", lambda e: e.memset(S32, 0.0), writes=["s_S32"])
                p.op("pool", lambda e: e.memset(Sbf[0], 0.0), writes=[("s_Sbf", 0)])
                for pa in range(2):
                    for half in range(2):
                        p.op("pool", lambda e, half=half, pa=pa: e.memset(Btmzs[pa][half], 0.0), writes=[("s_Btmz", pa, half)])
                par = 0
                gs = slice(g * 8, g * 8 + 8)
                for tb in range(NTB):
                    def conv_chain(j6):
                        off = j6 * 128 if j6 < 4 else (1024 if j6 == 4 else 1152)
                        jc = g * 4 + j6 if j6 < 4 else (8 + g if j6 == 4 else 10 + g)
                        bank = j6
                        proj_fm(off, 128, tb, bank)
                        dst = xs[j6] if j6 < 4 else (BT if j6 == 4 else CT)
                        conv_silu(cbuf[j6], ("cbuf", j6), tb, bank, PP_SCW + jc * 4, PP_SCB + jc, ctmps[j6 % 3], ("s_ctmp", j6 % 3), dst, ("s_fm", j6), AF.Silu, eng="dve")

                    def z_chain(q):
                        bank = 6 + q % 2
                        proj_fm(512 + q * 128, 128, tb, bank)
                        p.op("act", lambda e: e.activation(out=sz[q], in_=banks[bank][:, :], func=AF.Silu), reads=[PS(bank)], writes=[("s_sz", q)])

                    replay_ops(interleave([cap_ops(conv_chain, 0), cap_ops(conv_chain, 1), cap_ops(conv_chain, 2), cap_ops(z_chain, 0), cap_ops(z_chain, 1)]))
                    replay_ops(interleave([cap_ops(conv_chain, 3), cap_ops(conv_chain, 4), cap_ops(conv_chain, 5), cap_ops(z_chain, 2), cap_ops(z_chain, 3)]))
                    def _aliases(pa):
                        return MTs[pa], CTss[pa], xdts[pa], xws[pa], Btmzs[pa], decbcs[pa]

                    def stageA(tt, pa):
                        MT, CTs, xdt, xw, Btmz, decbc = _aliases(pa)
                        KP = lambda n: (n, pa)
                        tsl = slice(tt * 128, (tt + 1) * 128)
                        proj_tm(1280, 8, tb, tt, 5)
                        p.op("dve", lambda e, gs=gs: e.tensor_tensor(out=draw, in0=banks[5][:, 0:8], in1=dtb[:, gs], op=ALU.add), reads=[PS(5), "dtb"], writes=["s_draw"])
                        p.op("act", lambda e: e.activation(out=ex, in_=draw, func=AF.Exp), reads=["s_draw"], writes=["s_ex"])
                        p.op("act", lambda e: e.activation(out=dt_tm, in_=ex, func=AF.Ln, bias=LNEPS[:, 2:3]), reads=["s_ex", "lneps"], writes=["s_dt"])
                        p.op("dve", lambda e, gs=gs: e.tensor_tensor(out=dA, in0=dt_tm, in1=aneg[:, gs], op=ALU.mult), reads=["s_dt", "aneg"], writes=["s_dA"])

                        def mm5(e):
                            e.matmul(banks[5][:, 8:16], lhsT=MASK, rhs=dA, start=True, stop=True)
                            e.matmul(banks[5][:, 16:24], lhsT=SUF, rhs=dA, start=True, stop=True)
                            e.matmul(banks[5][:, 160:168], lhsT=consts[:, C_CH0:C_CH0 + 128], rhs=dA, start=True, stop=True)
                            return e.matmul(banks[5][:, 168:176], lhsT=consts[:, C_CH1:C_CH1 + 128], rhs=dA, start=True, stop=True)
                        p.op("pe", mm5, reads=["s_dA", "consts"], writes=[PS(5)])
                        p.op("act", lambda e: e.copy(out=acs, in_=banks[5][:, 8:16]), reads=[PS(5)], writes=["s_acs"])
                        p.op("act", lambda e: e.activation(out=dte, in_=banks[5][:, 16:24], func=AF.Exp), reads=[PS(5)], writes=["s_dte"])
                        p.op("act", lambda e: e.copy(out=dAb, in_=dA.unsqueeze(2).to_broadcast([128, 8, 128])), reads=["s_dA"], writes=["s_dAb", ("s_L", 0), ("s_L", 1), "s_Lm", "s_Le"])
                        p.op("act", lambda e: e.activation(out=decbc, in_=banks[5][:, 160:176].rearrange("p (a b) -> p a b", a=2), func=AF.Exp), reads=[PS(5)], writes=[KP("s_dec")])
                        p.op("dve", lambda e: e.tensor_tensor(out=w2, in0=dt_tm, in1=dte, op=ALU.mult), reads=["s_dt", "s_dte"], writes=["s_w2"])

                        def mmD(e):
                            ins = None
                            for h in range(8):
                                ins = e.matmul(banks[2 + h // 4][:, (h % 4) * 128:(h % 4 + 1) * 128], lhsT=dAb[:, h, :], rhs=MASK, start=True, stop=True)
                            return ins
                        p.op("pe", mmD, reads=["s_dAb", "consts"], writes=[PS(2), PS(3)])
                        for k in range(2):
                            p.op("dve", lambda e, k=k: e.tensor_tensor(out=L[:, 4 * k:4 * k + 4, :], in0=banks[2 + k][:, :].rearrange("p (a b) -> p a b", a=4),
                                                                       in1=acs[:, 4 * k:4 * k + 4].unsqueeze(2).to_broadcast([128, 4, 128]), op=ALU.subtract),
                                 reads=[PS(2 + k), "s_acs"], writes=[("s_L", k)])
                            p.op("act", lambda e, k=k: e.activation(out=eD[:, 4 * k:4 * k + 4, :], in_=banks[2 + k][:, :].rearrange("p (a b) -> p a b", a=4), func=AF.Exp), reads=[PS(2 + k)], writes=[("s_eD", k)])
                        p.op("dve", lambda e: e.tensor_scalar_min(out=L, in0=L, scalar1=0.0), reads=[("s_L", 0), ("s_L", 1)], writes=["s_Lm"])
                        p.op("act", lambda e: e.activation(out=L, in_=L, func=AF.Exp), reads=["s_Lm"], writes=["s_Le"])
                        p.op("pe", lambda e, tsl=tsl: e.matmul(banks[5][:, 256:384], lhsT=BT[:, tsl], rhs=CT[:, tsl], start=True, stop=True), reads=[("s_fm", 4), ("s_fm", 5)], writes=[PS(5)])
                        p.op("dve", lambda e: e.tensor_tensor(out=cbm, in0=banks[5][:, 256:384], in1=MASK, op=ALU.mult), reads=[PS(5), "consts"], writes=["s_cbm"])
                        p.op("dve", lambda e: e.tensor_tensor(out=MT, in0=L, in1=cbm.unsqueeze(1).to_broadcast([128, 8, 128]), op=ALU.mult), reads=["s_Le", "s_cbm"], writes=[KP("s_MT")])
                        p.op("dve", lambda e, tsl=tsl: e.tensor_tensor(out=CTs, in0=eD, in1=CT[:, tsl].unsqueeze(1).to_broadcast([128, 8, 128]), op=ALU.mult), reads=[("s_eD", 0), ("s_eD", 1), ("s_fm", 5)], writes=[KP("s_CTs")])

                        def mmT(e, tsl=tsl):
                            for q in range(4):
                                e.transpose(b7[:, q * 128:(q + 1) * 128], xs[q][:, tsl], identb)
                            return e.transpose(b7[:, 512:640], BT[:, tsl], identb)
                        p.op("pe", mmT, reads=[("s_fm", j) for j in range(5)] + ["identb"], writes=[PS(7)])
                        xtm = b7[:, 0:512].rearrange("p (h k) -> p h k", h=8)
                        p.op("dve", lambda e: e.tensor_tensor(out=xdt, in0=xtm, in1=dt_tm.unsqueeze(2).to_broadcast([128, 8, 64]), op=ALU.mult), reads=[PS(7), "s_dt"], writes=[KP("s_xdt")])
                        p.op("dve", lambda e: e.tensor_tensor(out=xw, in0=xtm, in1=w2.unsqueeze(2).to_broadcast([128, 8, 64]), op=ALU.mult), reads=[PS(7), "s_w2"], writes=[KP("s_xw")])
                        for half in range(2):
                            p.op("act", lambda e, half=half: e.copy(out=Btmz[half][half * 64:(half + 1) * 64, :], in_=b7[half * 64:(half + 1) * 64, 512:640]), reads=[PS(7)], writes=[("s_Btmz", pa, half)])


                    def stageB(tt, pa, par):
                        MT, CTs, xdt, xw, Btmz, decbc = _aliases(pa)
                        KP = lambda n: (n, pa)
                        tsl = slice(tt * 128, (tt + 1) * 128)
                        def mmY(e):
                            ins = None
                            for h in range(8):
                                q, hq = h // 2, h % 2
                                ins = e.matmul(banks[4][hq * 64:(hq + 1) * 64, q * 128:(q + 1) * 128], lhsT=xdt[:, h, :], rhs=MT[:, h, :], start=True, stop=True)
                            return ins
                        p.op("pe", mmY, reads=[KP("s_xdt"), KP("s_MT")], writes=[PS(4)])
                        for half in range(2):
                            hs = slice(half * 64, (half + 1) * 64)

                            def mmO(e, half=half, par=par):
                                ins = None
                                for h in range(8):
                                    q, hq = h // 2, h % 2
                                    ins = e.matmul(banks[0][hq * 64:(hq + 1) * 64, q * 128 + half * 64:q * 128 + half * 64 + 64], lhsT=Sbf[par][:, h, :], rhs=CTs[:, h, half * 64:(half + 1) * 64], start=True, stop=True)
                                return ins
                            p.op("pe", mmO, reads=[("s_Sbf", par), KP("s_CTs")], writes=[("ps0o", half)] + ([PS(0)] if half == 0 else []))
                            p.op("pe", lambda e, half=half: e.matmul(banks[6][:, :], lhsT=Btmz[half], rhs=xw.rearrange("p h k -> p (h k)"), start=True, stop=True), reads=[("s_Btmz", pa, half), KP("s_xw")], writes=[PS(6)])
                            p.op("dve", lambda e, half=half: e.tensor_tensor(out=S32, in0=S32, in1=decbc[:, half, :].unsqueeze(2).to_broadcast([128, 8, 64]), op=ALU.mult), reads=["s_S32", KP("s_dec")], writes=["s_S32"])
                            p.op("dve", lambda e: e.tensor_tensor(out=S32, in0=S32, in1=banks[6][:, :].rearrange("p (h k) -> p h k", h=8), op=ALU.add), reads=["s_S32", PS(6)], writes=["s_S32"])
                            p.op("act", lambda e, par=par: e.copy(out=Sbf[1 - par], in_=S32), reads=["s_S32"], writes=[("s_Sbf", 1 - par)])
                            par = 1 - par
                        for q in range(4):
                            p.op("dve", lambda e, q=q, tsl=tsl, g=g: e.scalar_tensor_tensor(out=yg[:, q, tsl], in0=xs[q][:, tsl], scalar=PPc(l, PP_SD + g * 4 + q), in1=banks[4][:, q * 128:(q + 1) * 128], op0=ALU.mult, op1=ALU.add),
                                 reads=[("s_fm", q), PS(4), "pp"], writes=[("s_yg", q)])
                            p.op("dve", lambda e, q=q, tsl=tsl: e.tensor_tensor(out=yg[:, q, tsl], in0=yg[:, q, tsl], in1=banks[0][:, q * 128:(q + 1) * 128], op=ALU.add),
                                 reads=[("s_yg", q), PS(0), ("ps0o", 0), ("ps0o", 1)], writes=[("s_yg", q)])
                        return par

                    def capture(fn, *a):
                        lst = []
                        p.cap = lst
                        r = fn(*a)
                        p.cap = None
                        return lst, r

                    def replay(lst):
                        for (eng, fn, r, w) in lst:
                            p.op(eng, fn, reads=r, writes=w)

                    def merge(la, lb):
                        out = []
                        ia = ib = 0
                        na, nb = len(la), len(lb)
                        while ia < na or ib < nb:
                            if ib >= nb or (ia < na and ia * nb <= ib * na):
                                out.append(la[ia]); ia += 1
                            else:
                                out.append(lb[ib]); ib += 1
                        return out

                    lA, _ = capture(stageA, 0, pa_ctr[0] % 2)
                    replay(lA)
                    for tt in range(4):
                        pa = pa_ctr[0] % 2
                        lB, par = capture(stageB, tt, pa, par)
                        if tt + 1 < 4:
                            lA, _ = capture(stageA, tt + 1, (pa_ctr[0] + 1) % 2)
                            replay(merge(lA, lB))
                        else:
                            replay(lB)
                        pa_ctr[0] += 1
                    for q in range(4):
                        p.op("dve", lambda e, q=q: e.tensor_tensor(out=yg[:, q, :], in0=yg[:, q, :], in1=sz[q], op=ALU.mult), reads=[("s_yg", q), ("s_sz", q)], writes=[("s_yg", q)])
                    for q in range(4):
                        p.op("act", lambda e, q=q: e.activation(out=sqb, in_=yg[:, q, :], func=AF.Square), reads=[("s_yg", q)], writes=["s_sqb"])
                        p.op("pe", lambda e, q=q: e.matmul(banks[5][:, :], lhsT=ones512b, rhs=sqb, start=(q == 0), stop=(q == 3)), reads=["s_sqb", "ones512b"], writes=[PS(5)])
                    p.op("act", lambda e: e.activation(out=rs, in_=banks[5][:, :], func=AF.Ln, bias=LNEPS[:, 1:2]), reads=[PS(5), "lneps"], writes=[("s_ctmp", 0)])
                    p.op("act", lambda e: e.activation(out=rs, in_=rs, func=AF.Exp, scale=-0.5), reads=[("s_ctmp", 0)], writes=[("s_ctmp", 0)])
                    for q in range(4):
                        p.op("dve", lambda e, q=q, g=g: e.scalar_tensor_tensor(out=yblk[:, q, :], in0=yg[:, q, :], scalar=PPc(l, PP_SNORM + g * 4 + q), in1=rs, op0=ALU.mult, op1=ALU.mult),
                             reads=[("s_yg", q), ("s_ctmp", 0), "pp"], writes=[("yblk", q)])
                    out_proj(tb, 4, [0, 1])

        def mixer(l):
            cv = Carve()
            wst = [cv.get([128, 8, 128]) for _ in range(2)]
            wunit = cv.get([128, 8, 1408], BF16)
            woutst = [cv.get([128, D])] * 2
            wout = cv.get([128, 4, D], BF16)
            yblk = cv.get([128, 4, TB], BF16)
            identb = cv.get([128, 128], BF16)
            ones128b = cv.get([128, 128], BF16)
            ones512b = cv.get([128, 128], BF16)
            p.op("act", lambda e: e.copy(out=identb, in_=ident), reads=["consts"], writes=["identb"])
            p.op("pool", lambda e: e.memset(ones128b, 1.0 / 128.0), writes=["ones128b"])
            p.op("pool", lambda e: e.memset(ones512b, 1.0 / 512.0), writes=["ones512b"])
            wcnt = [0]

            def load_win(col0, ncols, dst):
                c = 0
                while c < ncols:
                    n = min(128, ncols - c)
                    k = wcnt[0] % 2
                    wcnt[0] += 1
                    p.dma(("wst", k), wst[k][:, :, 0:n], win_d[l].rearrange("(kc p) f -> p kc f", p=128)[:, :, col0 + c:col0 + c + n], writes=[("wst", k)])
                    eng = ("act", "pool")[k]
                    if eng == "act":
                        p.op("act", lambda e, k=k, n=n, c=c: e.copy(out=wunit[:, :, dst + c:dst + c + n], in_=wst[k][:, :, 0:n]), reads=[("wst", k)], writes=["wunit"])
                    else:
                        p.op("pool", lambda e, k=k, n=n, c=c: e.tensor_copy(out=wunit[:, :, dst + c:dst + c + n], in_=wst[k][:, :, 0:n]), reads=[("wst", k)], writes=["wunit"])
                    c += n

            def load_wout(ych0, n):
                for j in range(n):
                    k = 0
                    p.dma(("wost", k), woutst[k], wout_d[l, (ych0 + j) * 128:(ych0 + j + 1) * 128, :], writes=[("wost", k)])
                    p.op("pool", lambda e, k=k, j=j: e.tensor_copy(out=wout[:, j, :], in_=woutst[k]), reads=[("wost", k)], writes=["wout"])

            def proj_fm(off, ncols, tb, bank):
                def mm(e):
                    ins = None
                    for kc in range(8):
                        ins = e.matmul(banks[bank][0:ncols, :], lhsT=wunit[:, kc, off:off + ncols], rhs=hT[:, kc, tb * TB:(tb + 1) * TB], start=(kc == 0), stop=(kc == 7))
                    return ins
                p.op("pe", mm, reads=["wunit"] + [("h", kc, tb) for kc in range(8)], writes=[PS(bank)])

            def proj_tm(off, ncols, tb, tt, bank, col0=0):
                t0 = tb * TB + tt * 128

                def mm(e):
                    ins = None
                    for kc in range(8):
                        ins = e.matmul(banks[bank][:, col0:col0 + ncols], lhsT=hT[:, kc, t0:t0 + 128], rhs=wunit[:, kc, off:off + ncols], start=(kc == 0), stop=(kc == 7))
                    return ins
                p.op("pe", mm, reads=["wunit"] + [("h", kc, tb) for kc in range(8)], writes=[PS(bank)])

            def out_proj(tb, nych, bks):
                for d in range(8):
                    bk = bks[d % len(bks)]

                    def mm(e, d=d, bk=bk):
                        ins = None
                        for j in range(nych):
                            ins = e.matmul(banks[bk][:, :], lhsT=wout[:, j, d * 128:(d + 1) * 128], rhs=yblk[:, j, :], start=(j == 0), stop=(j == nych - 1))
                        return ins
                    p.op("pe", mm, reads=["wout"] + [("yblk", j) for j in range(nych)], writes=[PS(bk)])
                    p.op("dve", lambda e, d=d, bk=bk: e.scalar_tensor_tensor(out=xT[:, d, tb * TB:(tb + 1) * TB], in0=banks[bk][:, :], scalar=MOD(l, 2, d), in1=xT[:, d, tb * TB:(tb + 1) * TB], op0=ALU.mult, op1=ALU.add),
                         reads=[PS(bk), ("x", d, tb)] + modkeys(l), writes=[("x", d, tb)])

            def conv_silu(buf, key, tb, bank, wcol, bcol, tmp, tmpkey, dst, dstkey, act_func, eng="dve"):
                p.op("act", lambda e: e.copy(out=buf[:, 3:3 + TB], in_=banks[bank][:, :]), reads=[PS(bank)], writes=[key])
                p.op(eng, lambda e: e.tensor_scalar(out=tmp, in0=buf[:, 0:TB], scalar1=PPc(l, wcol), scalar2=PPc(l, bcol), op0=ALU.mult, op1=ALU.add), reads=[key, "pp"], writes=[tmpkey])
                for k in range(1, 4):
                    p.op(eng, lambda e, k=k: e.scalar_tensor_tensor(out=tmp, in0=buf[:, k:k + TB], scalar=PPc(l, wcol + k), in1=tmp, op0=ALU.mult, op1=ALU.add), reads=[key, tmpkey, "pp"], writes=[tmpkey])
                p.op("pool", lambda e: e.tensor_copy(out=buf[:, 0:3], in_=buf[:, TB:TB + 3]), reads=[key], writes=[key])
                if dst is not None:
                    p.op("act", lambda e: e.activation(out=dst, in_=tmp, func=act_func), reads=[tmpkey], writes=[dstkey])

            mark = cv.off

            if do_lru:
                cv.off = mark
                wabd = cv.get([128, 4, 128], BF16)
                wxbd = cv.get([128, 4, 128], BF16)
                nsp8 = cv.get([128, 4])
                hcar = cv.get([128, 4])
                xbuf = [cv.get([128, 3 + TB]) for _ in range(4)]
                Ts = [{n: cv.get([128, TB]) for n in ("xc", "r", "i", "om", "h", "gg")} for _ in range(4)]
                xcbs = [cv.get([128, TB], BF16) for _ in range(4)]
                for nm, src, dst in (("wa", wabd_d, wabd), ("wx", wxbd_d, wxbd)):
                    p.dma(("wost", 0), woutst[0][:, 0:512].rearrange("p (m j) -> p m j", m=4), src[l].rearrange("m i j -> i m j"), writes=[("wost", 0)])
                    p.op("pool", lambda e, dst=dst: e.tensor_copy(out=dst, in_=woutst[0][:, 0:512].rearrange("p (m j) -> p m j", m=4)), reads=[("wost", 0)], writes=[nm])
                p.op("act", lambda e: e.activation(out=nsp8, in_=pp[:, l, PP_LLAM:PP_LLAM + 4], func=AF.Exp, scale=-1.0), reads=["pp"], writes=["nsp8"])
                p.op("act", lambda e: e.activation(out=nsp8, in_=nsp8, func=AF.Ln, bias=LNEPS[:, 2:3]), reads=["nsp8", "lneps"], writes=["nsp8"])
                p.op("dve", lambda e: e.tensor_scalar_mul(out=nsp8, in0=nsp8, scalar1=-8.0), reads=["nsp8"], writes=["nsp8"])
                for m in range(4):
                    p.op("pool", lambda e, m=m: e.memset(xbuf[m][:, 0:3], 0.0), writes=[("xbuf", m)])
                load_win(0, 1024, 0)
                load_wout(0, 4)

                def lru_chain(tb, m):
                    T = Ts[m]
                    xcb = xcbs[m]
                    BA, BB = 2 * m, 2 * m + 1
                    KK = lambda n: (n, m)
                    ops = []
                    A = lambda eng, fn, r, w: ops.append((eng, fn, r, w))
                    buf = xbuf[m]
                    key = ("xbuf", m)
                    wcol, bcol = PP_LCW + m * 4, PP_LCB + m
                    sl_h = [("h", kc, tb) for kc in range(8)]

                    def mmx(e):
                        ins = None
                        for kc in range(8):
                            ins = e.matmul(banks[BA][:, :], lhsT=wunit[:, kc, m * 128:(m + 1) * 128], rhs=hT[:, kc, tb * TB:(tb + 1) * TB], start=(kc == 0), stop=(kc == 7))
                        return ins

                    def mmg(e):
                        ins = None
                        for kc in range(8):
                            ins = e.matmul(banks[BB][:, :], lhsT=wunit[:, kc, 512 + m * 128:512 + (m + 1) * 128], rhs=hT[:, kc, tb * TB:(tb + 1) * TB], start=(kc == 0), stop=(kc == 7))
                        return ins
                    A("pe", mmx, ["wunit"] + sl_h, [PS(BA)])
                    A("pe", mmg, ["wunit"] + sl_h, [PS(BB)])
                    A("act", lambda e: e.copy(out=buf[:, 3:3 + TB], in_=banks[BA][:, :]), [PS(BA)], [key])
                    A("act", lambda e: e.activation(out=T["gg"], in_=banks[BB][:, :], func=AF.Gelu_apprx_tanh), [PS(BB)], [KK("l_gg")])
                    A("dve", lambda e: e.tensor_scalar(out=T["xc"], in0=buf[:, 0:TB], scalar1=PPc(l, wcol), scalar2=PPc(l, bcol), op0=ALU.mult, op1=ALU.add), [key, "pp"], [KK("l_xc")])
                    for k in range(1, 4):
                        A("dve", lambda e, k=k: e.scalar_tensor_tensor(out=T["xc"], in0=buf[:, k:k + TB], scalar=PPc(l, wcol + k), in1=T["xc"], op0=ALU.mult, op1=ALU.add), [key, KK("l_xc"), "pp"], [KK("l_xc")])
                    A("pool", lambda e: e.tensor_copy(out=buf[:, 0:3], in_=buf[:, TB:TB + 3]), [key], [key])
                    A("act", lambda e: e.copy(out=xcb, in_=T["xc"]), [KK("l_xc")], [KK("l_xcb")])
                    A("pe", lambda e: e.matmul(banks[BA][:, :], lhsT=wabd[:, m, :], rhs=xcb, start=True, stop=True), ["wa", KK("l_xcb")], [PS(BA)])
                    A("pe", lambda e: e.matmul(banks[BB][:, :], lhsT=wxbd[:, m, :], rhs=xcb, start=True, stop=True), ["wx", KK("l_xcb")], [PS(BB)])
                    A("act", lambda e: e.activation(out=T["r"], in_=banks[BA][:, :], func=AF.Sigmoid, bias=PPc(l, PP_LBA + m)), [PS(BA), "pp"], [KK("l_r")])
                    A("act", lambda e: e.activation(out=T["i"], in_=banks[BB][:, :], func=AF.Sigmoid, bias=PPc(l, PP_LBX + m)), [PS(BB), "pp"], [KK("l_i")])
                    A("act", lambda e: e.activation(out=T["r"], in_=T["r"], func=AF.Exp, scale=nsp8[:, m:m + 1]), [KK("l_r"), "nsp8"], [KK("l_r")])
                    A("pool", lambda e: e.tensor_tensor(out=T["om"], in0=T["r"], in1=T["r"], op=ALU.mult), [KK("l_r")], [KK("l_om")])
                    A("pool", lambda e: e.tensor_scalar(out=T["om"], in0=T["om"], scalar1=-1.0, scalar2=1.0, op0=ALU.mult, op1=ALU.add), [KK("l_om")], [KK("l_om")])
                    A("act", lambda e: e.activation(out=T["om"], in_=T["om"], func=AF.Sqrt), [KK("l_om")], [KK("l_om")])
                    A("pool", lambda e: e.tensor_tensor(out=T["i"], in0=T["i"], in1=T["xc"], op=ALU.mult), [KK("l_i"), KK("l_xc")], [KK("l_i")])
                    A("dve", lambda e: e.tensor_tensor(out=T["om"], in0=T["om"], in1=T["i"], op=ALU.mult), [KK("l_om"), KK("l_i")], [KK("l_om")])
                    if tb == 0:
                        A("dve", lambda e: e.tensor_tensor_scan(out=T["h"], data0=T["r"], data1=T["om"], initial=0.0, op0=ALU.mult, op1=ALU.add), [KK("l_r"), KK("l_om")], [KK("l_h")])
                    else:
                        A("dve", lambda e: e.tensor_tensor_scan(out=T["h"], data0=T["r"], data1=T["om"], initial=hcar[:, m:m + 1], op0=ALU.mult, op1=ALU.add), [KK("l_r"), KK("l_om"), ("hcar", m)], [KK("l_h")])
                    A("pool", lambda e: e.tensor_copy(out=hcar[:, m:m + 1], in_=T["h"][:, TB - 1:TB]), [KK("l_h")], [("hcar", m)])
                    A("dve", lambda e: e.tensor_tensor(out=yblk[:, m, :], in0=T["h"], in1=T["gg"], op=ALU.mult), [KK("l_h"), KK("l_gg")], [("yblk", m)])
                    return ops

                for tb in range(NTB):
                    lists = [lru_chain(tb, m) for m in range(4)]
                    for k in range(len(lists[0])):
                        for ol in lists:
                            eng, fn, r, w = ol[k]
                            p.op(eng, fn, reads=r, writes=w)
                    out_proj(tb, 4, [0, 1, 2, 3, 4, 5, 6, 7])

            if do_gla:
                cv.off = mark
                walb = cv.get([32, 256], BF16)
                rT = cv.get([32, TB], BF16)
                e1 = cv.get([128, 128])
                l1 = cv.get([128, 128])
                ecp = cv.get([128, TB])
                ecn = cv.get([128, TB])
                esuf = [cv.get([128, 128]) for _ in range(4)]
                qd = cv.get([128, TB], BF16)
                kd = cv.get([128, TB], BF16)
                kend = [cv.get([128, 128], BF16) for _ in range(4)]
                kendz = [[cv.get([128, 128], BF16) for _ in range(2)] for _ in range(4)]
                qdz = [cv.get([128, TB], BF16) for _ in range(2)]
                CHM = [consts[:, C_CH0:C_CH0 + 128], consts[:, C_CH1:C_CH1 + 128]]
                vtm = [cv.get([128, 256], BF16) for _ in range(4)]
                sg = [cv.get([128, TB]) for _ in range(2)]
                attms = [cv.get([128, 4, 128], BF16) for _ in range(2)]
                S32 = cv.get([128, 128])
                Sbfs = [[cv.get([128, 128], BF16) for _ in range(2)] for _ in range(2)]
                sqbs = [cv.get([128, TB], BF16) for _ in range(2)]
                rss = [cv.get([128, TB]) for _ in range(2)]
                t1s = [cv.get([128, TB]) for _ in range(2)]
                p.dma(("wost", 0), woutst[0][0:32, 0:256], walpha_d[l], writes=[("wost", 0)])
                p.op("pool", lambda e: e.tensor_copy(out=walb, in_=woutst[0][0:32, 0:256]), reads=[("wost", 0)], writes=["walb"])
                p.op("pool", lambda e: e.memset(rT, 1.0), writes=["rT"])
                m64b = consts[:, C_MASK:C_MASK + 128].unsqueeze(1).to_broadcast([128, 4, 128])
                for hp in range(2):
                    load_win(1024 + hp * 128, 128, 0)
                    load_win(1280 + hp * 128, 128, 128)
                    load_win(1536 + hp * 256, 256, 256)
                    load_win(2048 + hp * 256, 256, 512)
                    load_win(2560, 16, 768)
                    load_wout(4 + hp * 2, 2)
                    p.op("pool", lambda e: e.memset(S32, 0.0), writes=[("S32", 0), ("S32", 1)])
                    for hh in range(2):
                        for pr in range(2):
                            p.op("pool", lambda e, hh=hh, pr=pr: e.memset(Sbfs[hh][pr], 0.0), writes=[("Sbf", hh, pr)])
                    if hp == 0:
                        for hh in range(2):
                            p.op("pool", lambda e, hh=hh: e.memset(qdz[hh], 0.0), writes=[("g_qdz", hh)])
                        for tt in range(4):
                            for half in range(2):
                                p.op("pool", lambda e, tt=tt, half=half: e.memset(kendz[tt][half], 0.0), writes=[("g_kendz", tt, half)])
                    par_state = {0: 0, 1: 0}
                    for tb in range(NTB):
                        proj_fm(768, 16, tb, 0)
                        p.op("act", lambda e: e.copy(out=rT[0:16, :], in_=banks[0][0:16, :]), reads=[PS(0)], writes=["rT"])
                        for tt in range(4):
                            tsl = slice(tt * 128, (tt + 1) * 128)
                            p.op("pe", lambda e, tsl=tsl, hp=hp: e.matmul(banks[5][:, 0:128], lhsT=rT[:, tsl], rhs=walb[:, hp * 128:(hp + 1) * 128], start=True, stop=True), reads=["rT", "walb"], writes=[PS(5)])
                            p.op("act", lambda e: e.activation(out=e1, in_=banks[5][:, 0:128], func=AF.Exp, scale=-1.0), reads=[PS(5)], writes=["g_e1"])
                            p.op("act", lambda e: e.activation(out=l1, in_=e1, func=AF.Ln, bias=LNEPS[:, 2:3]), reads=["g_e1", "lneps"], writes=["g_l1"])
                            p.op("pe", lambda e: e.matmul(banks[5][:, 128:256], lhsT=l1, rhs=consts[:, C_MASKG:C_MASKG + 128], start=True, stop=True), reads=["g_l1", "consts"], writes=[PS(5)])
                            p.op("pe", lambda e: e.matmul(banks[5][:, 256:384], lhsT=consts[:, C_MASKG + 128:C_MASKG + 256], rhs=l1, start=True, stop=True), reads=["g_l1", "consts"], writes=[PS(5)])
                            p.op("act", lambda e, tsl=tsl: e.activation(out=ecp[:, tsl], in_=banks[5][:, 128:256], func=AF.Exp), reads=[PS(5)], writes=["g_ecp"])
                            p.op("act", lambda e, tsl=tsl: e.activation(out=ecn[:, tsl], in_=banks[5][:, 128:256], func=AF.Exp, scale=-1.0), reads=[PS(5)], writes=["g_ecn"])
                            p.op("act", lambda e, tt=tt: e.activation(out=esuf[tt], in_=banks[5][:, 256:384], func=AF.Exp), reads=[PS(5)], writes=[("g_esuf", tt)])
                        proj_fm(0, 128, tb, 0)
                        for hh in range(2):
                            p.op("dve", lambda e, hh=hh: e.scalar_tensor_tensor(out=qdz[hh][hh * 64:(hh + 1) * 64, :], in0=banks[0][hh * 64:(hh + 1) * 64, :], scalar=0.125, in1=ecp[hh * 64:(hh + 1) * 64, :], op0=ALU.mult, op1=ALU.mult),
                                 reads=[PS(0), "g_ecp"], writes=[("g_qdz", hh)])
                        proj_fm(128, 128, tb, 1)
                        p.op("dve", lambda e: e.tensor_tensor(out=kd, in0=banks[1][:, :], in1=ecn, op=ALU.mult), reads=[PS(1), "g_ecn"], writes=["g_kd"])
                        for tt in range(4):
                            proj_tm(128, 128, tb, tt, 0)
                            for half in range(2):
                                p.op("dve", lambda e, tt=tt, half=half: e.tensor_tensor(out=kendz[tt][half][half * 64:(half + 1) * 64, :], in0=banks[0][half * 64:(half + 1) * 64, 0:128], in1=esuf[tt][half * 64:(half + 1) * 64, :], op=ALU.mult),
                                     reads=[PS(0), ("g_esuf", tt)], writes=[("g_kendz", tt, half)])
                            proj_tm(256, 256, tb, tt, 1)
                            p.op("act", lambda e, tt=tt: e.copy(out=vtm[tt], in_=banks[1][:, 0:256]), reads=[PS(1)], writes=[("g_vtm", tt)])
                        for hh in range(2):
                            proj_fm(512 + hh * 128, 128, tb, hh)
                            p.op("act", lambda e, hh=hh: e.activation(out=sg[hh], in_=banks[hh][:, :], func=AF.Silu), reads=[PS(hh)], writes=[("g_sg", hh)])
                        def head_ops(tb, hh):
                            b0 = hh * 64
                            BATT, BO, BKV = (2, 5)[hh], (3, 6)[hh], (4, 7)[hh]
                            attm, sqb, rs, t1 = attms[hh], sqbs[hh], rss[hh], t1s[hh]
                            KH = lambda n: (n, hh)

                            def att(e):
                                ins = None
                                for tt in range(4):
                                    tsl = slice(tt * 128, (tt + 1) * 128)
                                    ins = e.matmul(banks[BATT][:, tsl], lhsT=kd[:, tsl], rhs=qdz[hh][:, tsl], start=True, stop=True)
                                return ins
                            p.op("pe", att, reads=["g_kd", ("g_qdz", hh)], writes=[PS(BATT)])
                            p.op("dve", lambda e: e.tensor_tensor(out=attm, in0=banks[BATT][:, :].rearrange("p (a b) -> p a b", a=4), in1=m64b, op=ALU.mult), reads=[PS(BATT), "consts"], writes=[KH("g_attm")])
                            for c in range(8):
                                tt, half = c // 2, c % 2
                                csl = slice(c * 64, (c + 1) * 64)
                                tsl = slice(tt * 128, (tt + 1) * 128)
                                cur_par = par_state[hh]
                                if half == 0:
                                    p.op("pe", lambda e, tt=tt, tsl=tsl: e.matmul(banks[BO][:, tsl], lhsT=vtm[tt][:, hh * 128:(hh + 1) * 128], rhs=attm[:, tt, :], start=True, stop=False),
                                         reads=[("g_vtm", tt), KH("g_attm")], writes=[PS(BO)])
                                p.op("pe", lambda e, csl=csl, cur_par=cur_par, half=half: e.matmul(banks[BO][:, csl], lhsT=Sbfs[hh][cur_par], rhs=qdz[hh][:, csl], start=False, stop=(half == 1)),
                                     reads=[("Sbf", hh, cur_par), ("g_qdz", hh)], writes=[PS(BO)])
                                p.op("pe", lambda e, tt=tt, half=half: e.matmul(banks[BKV][:, 0:128], lhsT=kendz[tt][half], rhs=vtm[tt][:, hh * 128:(hh + 1) * 128], start=True, stop=True),
                                     reads=[("g_kendz", tt, half), ("g_vtm", tt)], writes=[PS(BKV)])
                                col = c * 64 + 63
                                p.op("dve", lambda e, col=col: e.scalar_tensor_tensor(out=S32[b0:b0 + 64, :], in0=S32[b0:b0 + 64, :], scalar=ecp[b0:b0 + 64, col:col + 1], in1=banks[BKV][b0:b0 + 64, 0:128], op0=ALU.mult, op1=ALU.add),
                                     reads=[("S32", hh), "g_ecp", PS(BKV)], writes=[("S32", hh)])
                                nxt = 1 - cur_par
                                p.op("act", lambda e, nxt=nxt: e.copy(out=Sbfs[hh][nxt][b0:b0 + 64, :], in_=S32[b0:b0 + 64, :]), reads=[("S32", hh)], writes=[("Sbf", hh, nxt)])
                                par_state[hh] = nxt
                            p.op("act", lambda e: e.activation(out=sqb, in_=banks[BO][:, :], func=AF.Square), reads=[PS(BO)], writes=[KH("g_sqb")])
                            p.op("pe", lambda e: e.matmul(banks[BATT][:, :], lhsT=ones128b, rhs=sqb, start=True, stop=True), reads=["ones128b", KH("g_sqb")], writes=[PS(BATT)])
                            p.op("act", lambda e: e.activation(out=rs, in_=banks[BATT][:, :], func=AF.Ln, bias=LNEPS[:, 1:2]), reads=[PS(BATT), "lneps"], writes=[KH("g_rs")])
                            p.op("act", lambda e: e.activation(out=rs, in_=rs, func=AF.Exp, scale=-0.5), reads=[KH("g_rs")], writes=[KH("g_rs")])
                            p.op("dve", lambda e: e.tensor_tensor(out=t1, in0=banks[BO][:, :], in1=rs, op=ALU.mult), reads=[PS(BO), KH("g_rs")], writes=[KH("g_t1")])
                            p.op("dve", lambda e: e.scalar_tensor_tensor(out=yblk[:, hh, :], in0=t1, scalar=PPc(l, PP_GNORM), in1=sg[hh], op0=ALU.mult, op1=ALU.mult), reads=[KH("g_t1"), ("g_sg", hh), "pp"], writes=[("yblk", hh)])

                        replay_ops(interleave([cap_ops(head_ops, tb, 0), cap_ops(head_ops, tb, 1)]))
                        out_proj(tb, 2, [0, 1])

            if do_ssd:
                ssd_units(l, cv, mark, load_win, load_wout, proj_fm, proj_tm, out_proj, conv_silu, yblk, identb, ones512b)

        cv = Carve()
        ada_bufs = ada_bufs_alloc(cv)
        for blk in range(N_ADA):
            adaln_block(0, blk, ada_bufs, blk % 4)
        adaln_finish(0, 1)

        for tb in range(NTB):
            scale_alpha(tb, engs=("pool", "dve", "act"))
        for l in range(depth):
            p.new_epoch()
            for tb in range(NTB):
                modulate(l, 1, tb)
            barrier()
            if do_lru or do_gla or do_ssd:
                mixer(l)
            barrier()
            cv = Carve()
            layernorm(l, PP_LN1G, PP_LN1B, cv, 0, 1)
            for tb in range(NTB):
                modulate(l, 2, tb)
            barrier()
            cv = Carve()
            if do_moe:
                router(l, cv, 2, 3)
            barrier()
            cv = Carve()
            hooks = {}
            if l + 1 < depth:
                ada_bufs2 = ada_bufs_alloc(cv)
                for blk in range(N_ADA):
                    hooks[2 + blk] = (lambda blk=blk: adaln_block(l + 1, blk, ada_bufs2, 4 + blk % 4))
                hooks[2 + N_ADA] = (lambda: adaln_finish(l + 1, 4))
            if do_moe:
                moe(l, cv, hooks)
            else:
                for k in sorted(hooks):
                    hooks[k]()
            barrier()
            cv = Carve()
            layernorm(l, PP_LN2G, PP_LN2B, cv, 0, 1, final=(l == depth - 1))
            barrier()

        p.dma("out", yT_d.rearrange("(c p) t -> p c t", p=128), xT[:], reads=[("x", c, tb) for c in range(8) for tb in range(NTB)])
        p.emit()
    return nc


def prep_inputs(inputs):
    f = lambda a: np.ascontiguousarray(np.asarray(a, dtype=np.float32))
    L = DEPTH
    shared = {}
    shared["consts"] = build_consts()
    pc = lambda v, n: np.asarray(v, np.float32).reshape(n, 128).T
    pp = np.zeros((L, 128, NPP), np.float32)
    prow = np.zeros((L, NPR), np.float32)
    wabd = np.zeros((L, 4, 128, 128), np.float32)
    wxbd = np.zeros((L, 4, 128, 128), np.float32)
    wal = np.zeros((L, 32, 256), np.float32)
    for l in range(L):
        pp[l, :, PP_LN1G:PP_LN1G + 8] = pc(inputs["ln1_g"][l], 8)
        pp[l, :, PP_LN1B:PP_LN1B + 8] = pc(inputs["ln1_b"][l], 8)
        pp[l, :, PP_LN2G:PP_LN2G + 8] = pc(inputs["ln2_g"][l], 8)
        pp[l, :, PP_LN2B:PP_LN2B + 8] = pc(inputs["ln2_b"][l], 8)
        for m in range(4):
            for k in range(4):
                pp[l, :, PP_LCW + m * 4 + k] = inputs["lru_conv_w"][l, k, m * 128:(m + 1) * 128]
        pp[l, :, PP_LCB:PP_LCB + 4] = pc(inputs["lru_conv_b"][l], 4)
        pp[l, :, PP_LBA:PP_LBA + 4] = pc(inputs["lru_b_a"][l], 4)
        pp[l, :, PP_LBX:PP_LBX + 4] = pc(inputs["lru_b_x"][l], 4)
        pp[l, :, PP_LLAM:PP_LLAM + 4] = pc(inputs["lru_lambda"][l], 4)
        for j in range(12):
            for k in range(4):
                pp[l, :, PP_SCW + j * 4 + k] = inputs["ssd_conv_w"][l, k, j * 128:(j + 1) * 128]
        pp[l, :, PP_SCB:PP_SCB + 12] = pc(inputs["ssd_conv_b"][l], 12)
        pp[l, :, PP_SNORM:PP_SNORM + 8] = pc(inputs["ssd_norm"][l], 8)
        pp[l, :, PP_GNORM] = inputs["gla_norm"][l]
        pp[l, :, PP_SD:PP_SD + 8] = pc(np.repeat(np.asarray(inputs["ssd_d"][l]), 64), 8)
        prow[l, PR_DTB:PR_DTB + 16] = inputs["ssd_dt_bias"][l]
        prow[l, PR_ALOG:PR_ALOG + 16] = inputs["ssd_a_log"][l]
        prow[l, PR_RB:PR_RB + NE] = inputs["router_bias"][l]
        for m in range(4):
            for q in range(2):
                wabd[l, m, q * 64:(q + 1) * 64, q * 64:(q + 1) * 64] = inputs["lru_w_a"][l, 2 * m + q]
                wxbd[l, m, q * 64:(q + 1) * 64, q * 64:(q + 1) * 64] = inputs["lru_w_x"][l, 2 * m + q]
        wal[l, 0:16] = inputs["gla_w_alpha"][l]
        wal[l, 16] = inputs["gla_b_alpha"][l]
    shared.update(pp=pp, prow=prow, lru_wa_bd=wabd, lru_wx_bd=wxbd, walpha_ext=wal)
    for k in ("w_ada", "b_ada", "w_in", "w_out", "router_w", "exp_w1", "exp_w3", "exp_w2", "shared_w1", "shared_w3", "shared_w2"):
        shared[k] = f(inputs[k])
    x = np.asarray(inputs["x"], np.float32)
    c = np.asarray(inputs["c"], np.float32)
    maps = []
    for b in range(x.shape[0]):
        m = dict(shared)
        m["xT"] = np.ascontiguousarray(x[b].T)
        m["cpc"] = np.ascontiguousarray(c[b].reshape(8, 128).T)
        maps.append(m)
    return maps


_NC_CACHE = {}


def kernel(**inputs):
    maps = prep_inputs(inputs)
    if "nc" not in _NC_CACHE:
        _NC_CACHE["nc"] = build_program()
    nc = _NC_CACHE["nc"]
    res = run_bass_kernel_spmd(nc, maps, core_ids=list(range(len(maps))))
    out = np.stack([np.ascontiguousarray(r["yT"].T) for r in res.results], axis=0)
    return out.astype(np.float32)
```

```python
from contextlib import ExitStack
import numpy as np
import concourse.bass as bass
import concourse.mybir as mybir
from concourse.bass_utils import run_bass_kernel_spmd

F32 = mybir.dt.float32
BF16 = mybir.dt.bfloat16
AF = mybir.ActivationFunctionType
ALU = mybir.AluOpType
AX = mybir.AxisListType

D = 1024
S = 2048
DEPTH = 4
NE = 64
ALPHA = (2 * DEPTH) ** 0.25
DIN = 5152
NPP = 144
TB = 512
NTB = S // TB


class Prog:
    def __init__(self, nc, stack):
        self.nc = nc
        self.stack = stack
        self.ops = []
        self.last_write = {}
        self.readers = {}
        self.epoch = 0
        self.op_epoch = []
        self.group_open = {}
        self.group_of = {}
        self.nsem = 0
        self.bar = None
        self.xeng = []
        self.cap = None

    def barrier(self, fn):
        allk = list(set(self.last_write.keys()) | set(k for k, v in self.readers.items() if v))
        oid = self._add("dve", fn, allk, allk)
        self.bar = oid
        self.last_write = {}
        self.readers = {}

    def new_sem(self, name):
        self.nsem += 1
        return self.stack.enter_context(self.nc.semaphore(f"{name}_{self.nsem}"))

    def new_epoch(self):
        self.epoch += 1

    def _add(self, eng, fn, reads, writes, chan=None):
        oid = len(self.ops)
        raw = set()
        oth = set()
        xe = set()
        for k in reads:
            w = self.last_write.get(k)
            if w is not None:
                raw.add(w)
            if isinstance(k, tuple) and k[0] in ("ps", "ps2o"):
                for r in self.readers.get(k, ()):
                    xe.add(r)
        for k in writes:
            w = self.last_write.get(k)
            if w is not None:
                oth.add(w)
            for r in self.readers.get(k, ()):
                oth.add(r)
        if self.bar is not None:
            oth.add(self.bar)
        oth -= raw
        raw.discard(oid)
        oth.discard(oid)
        xe -= raw
        xe -= oth
        xe.discard(oid)
        self.xeng.append(sorted(xe))
        self.ops.append([eng, fn, sorted(raw), sorted(oth), chan])
        self.op_epoch.append(self.epoch)
        for k in reads:
            self.readers.setdefault(k, []).append(oid)
        for k in writes:
            self.last_write[k] = oid
            self.readers[k] = []
        return oid

    def op(self, eng, fn, reads=(), writes=()):
        if self.cap is not None:
            self.cap.append((eng, fn, list(reads), list(writes)))
            return None
        return self._add(eng, fn, reads, writes)

    def dma(self, chan, out, in_, reads=(), writes=(), eng="sp", more=False, **kw):
        def fn(e, out=out, in_=in_, kw=kw):
            return e.dma_start(out=out, in_=in_, **kw)
        oid = self._add(eng, fn, reads, writes, chan=chan)
        g = self.group_open.get(chan)
        if g is None:
            g = []
            self.group_open[chan] = g
        g.append(oid)
        self.group_of[oid] = g
        if not more:
            self.group_open[chan] = None
        return oid

    def emit(self):
        nc = self.nc
        n = len(self.ops)
        is_dma = [o[4] is not None for o in self.ops]
        need_sig = [False] * n
        deps_of = []
        for i, (eng, fn, raw, oth, chan) in enumerate(self.ops):
            deps = []
            for d in raw:
                if is_dma[d] or is_dma[i] or self.ops[d][0] != eng or eng != "pe":
                    deps.append(d)
            for d in oth:
                if is_dma[d] or is_dma[i] or self.ops[d][0] != eng or eng != "pe":
                    deps.append(d)
            for d in self.xeng[i]:
                if self.ops[d][0] != eng:
                    deps.append(d)
            deps_of.append(deps)
            for d in deps:
                if not is_dma[d]:
                    need_sig[d] = True
        sig = [None] * n
        cur = {}
        chan_sem = {}
        chan_cnt = {}
        for i, (eng, fn, raw, oth, chan) in enumerate(self.ops):
            if is_dma[i]:
                if chan not in chan_sem:
                    chan_sem[chan] = self.new_sem("d")
                    chan_cnt[chan] = 0
                chan_cnt[chan] += 16
                sig[i] = (chan_sem[chan], chan_cnt[chan])
            elif need_sig[i]:
                key = (eng, self.op_epoch[i])
                if key not in cur:
                    cur[key] = [self.new_sem(eng), 0]
                cur[key][1] += 1
                sig[i] = (cur[key][0], cur[key][1])
        for i in range(n):
            if is_dma[i]:
                last = self.group_of[i][-1]
                if last != i:
                    sig[i] = (sig[i][0], sig[last][1])
        streams = {}
        for i, (eng, fn, raw, oth, chan) in enumerate(self.ops):
            streams.setdefault(eng, []).append((i, fn, [sig[d] for d in deps_of[i]]))
        final_dma = [(chan_sem[c], chan_cnt[c]) for c in chan_sem]
        self.n_waits = 0

        def run_stream(e, items, tail):
            waited = {}
            for (i, fn, waits) in items:
                best = {}
                for (s, v) in waits:
                    k = s.num
                    if waited.get(k, 0) >= v:
                        continue
                    if k not in best or best[k][1] < v:
                        best[k] = (s, v)
                for k, (s, v) in best.items():
                    e.wait_ge(s, v)
                    waited[k] = v
                    self.n_waits += 1
                ins = fn(e)
                if is_dma[i]:
                    ins.then_inc(chan_sem[self.ops[i][4]], 16)
                elif sig[i] is not None:
                    ins.then_inc(sig[i][0], 1)
            for (s, v) in tail:
                e.wait_ge(s, v)

        with nc.Block() as block:
            names = {"pe": "tensor", "act": "scalar", "dve": "vector", "pool": "gpsimd", "sp": "sync"}
            for en, attr in names.items():
                items = streams.get(en, [])
                tail = final_dma if en == "sp" else []
                if not items and not tail:
                    continue

                def body(e, items=items, tail=tail):
                    run_stream(e, items, tail)
                getattr(block, attr)(body)


C_ID = 0
C_MASK = 128
C_SUF = 256
C_CH0 = 384
C_CH1 = 512
C_M64 = 640
C_ONESD = 704
C_ONE = 832
C_SEL = 960
C_MASKG = 1984
NCONST = 2240


def build_consts():
    c = np.zeros((128, NCONST), np.float32)
    idx = np.arange(128)
    same = (idx[:, None] // 64) == (idx[None, :] // 64)
    c[:, C_ID:C_ID + 128] = np.eye(128)
    c[:, C_MASK:C_MASK + 128] = same & (idx[:, None] <= idx[None, :])
    c[:, C_SUF:C_SUF + 128] = same & (idx[:, None] > idx[None, :])
    c[:64, C_CH0:C_CH0 + 128] = 1.0
    c[64:, C_CH1:C_CH1 + 128] = 1.0
    j = idx % 64
    c[:, C_M64:C_M64 + 64] = j[:, None] <= np.arange(64)[None, :]
    c[:, C_ONESD:C_ONESD + 128] = 1.0 / 1024.0
    c[:, C_ONE:C_ONE + 128] = 1.0
    for h in range(8):
        c[h, C_SEL + h * 128:C_SEL + (h + 1) * 128] = 1.0
    c[:, C_MASKG:C_MASKG + 128] = c[:, C_MASK:C_MASK + 128] * (-1.0 / 16.0)
    c[:, C_MASKG + 128:C_MASKG + 256] = c[:, C_SUF:C_SUF + 128] * (-1.0 / 16.0)
    return c


PP_LN1G, PP_LN1B, PP_LN2G, PP_LN2B = 0, 8, 16, 24
PP_LCW, PP_LCB, PP_LBA, PP_LBX, PP_LLAM = 32, 48, 52, 56, 60
PP_SCW, PP_SCB, PP_SNORM, PP_GNORM, PP_SD = 64, 112, 124, 132, 133
PR_DTB, PR_ALOG, PR_RB = 0, 16, 32
NPR = 96


def build_program(depth=DEPTH, do_lru=True, do_gla=True, do_ssd=True, do_moe=True, n_exp=NE + 1, debug=False):
    nc = bass.Bass("TRN2", target_bir_lowering=False, dynamic_dma_scratch_size=512)
    dr = {}

    def din(name, shape):
        dr[name] = nc.dram_tensor(name, list(shape), F32, kind="ExternalInput").ap()
        return dr[name]

    xT_d = din("xT", [D, S])
    cpc_d = din("cpc", [128, 8])
    consts_d = din("consts", [128, NCONST])
    pp_d = din("pp", [DEPTH, 128, NPP])
    prow_d = din("prow", [DEPTH, NPR])
    wada_d = din("w_ada", [DEPTH, D, 6 * D])
    bada_d = din("b_ada", [DEPTH, 6 * D])
    win_d = din("w_in", [DEPTH, D, DIN])
    wout_d = din("w_out", [DEPTH, 2 * D, D])
    wabd_d = din("lru_wa_bd", [DEPTH, 4, 128, 128])
    wxbd_d = din("lru_wx_bd", [DEPTH, 4, 128, 128])
    walpha_d = din("walpha_ext", [DEPTH, 32, 256])
    rw_d = din("router_w", [DEPTH, D, NE])
    ew1_d = din("exp_w1", [DEPTH, NE, D, 256])
    ew3_d = din("exp_w3", [DEPTH, NE, D, 256])
    ew2_d = din("exp_w2", [DEPTH, NE, 256, D])
    sw1_d = din("shared_w1", [DEPTH, D, 256])
    sw3_d = din("shared_w3", [DEPTH, D, 256])
    sw2_d = din("shared_w2", [DEPTH, 256, D])
    yT_d = nc.dram_tensor("yT", [D, S], F32, kind="ExternalOutput").ap()
    gscr_d = nc.dram_tensor("gscr", [2, NE, S], F32, kind="Internal").ap()
    dbg = {}

    with ExitStack() as st:
        p = Prog(nc, st)

        def sb(name, shape, dt=F32):
            return st.enter_context(nc.sbuf_tensor(name, list(shape), dt))

        def cap_ops(fn, *a):
            lst = []
            p.cap = lst
            r = fn(*a)
            p.cap = None
            return lst

        def replay_ops(lst):
            for (eng, fn, r, w) in lst:
                p.op(eng, fn, reads=r, writes=w)

        def interleave(lists):
            out = []
            n = max(len(x) for x in lists)
            for k in range(n):
                for x in lists:
                    if k < len(x):
                        out.append(x[k])
            return out

        xT = sb("xT_sb", [128, 8, S])
        hT = sb("hT_sb", [128, 8, S], BF16)
        consts = sb("consts_sb", [128, NCONST])
        pp = sb("pp_sb", [128, DEPTH, NPP])
        mod = sb("mod_sb", [128, DEPTH, 64])
        cond = sb("cond_sb", [128, 8])
        SCRW = 29150
        scr = sb("scr_sb", [128, SCRW])
        banks = [st.enter_context(nc.psum_tensor(f"bank{i}", [128, 512], F32)) for i in range(8)]

        def PS(i):
            return ("ps", i)

        class Carve:
            def __init__(self):
                self.off = 0

            def get(self, shape, dt=F32):
                n = int(np.prod(shape[1:]))
                words = n if dt == F32 else (n + 1) // 2
                a = scr[:, self.off:self.off + words]
                self.off += words
                assert self.off <= SCRW - 1, self.off
                if dt != F32:
                    a = a.bitcast(dt)
                    if n % 2:
                        a = a[:, 0:n]
                if len(shape) == 3:
                    a = a.rearrange("p (a b) -> p a b", a=shape[1])
                elif len(shape) == 4:
                    a = a.rearrange("p (a b c) -> p a b c", a=shape[1], b=shape[2])
                if shape[0] != 128:
                    a = a[0:shape[0]]
                return a

        ident = consts[:, C_ID:C_ID + 128]
        onesD = consts[:, C_ONESD:C_ONESD + 128]

        def barrier():
            tok = scr[:, SCRW - 1:SCRW]
            p.barrier(lambda e: e.memset(tok, 0.0))

        p.dma("ld0", consts[:], consts_d[:, :], writes=["consts"])
        p.dma("ld1", pp[:], pp_d.rearrange("l p n -> p l n"), writes=["pp"])
        p.dma("ld2", cond[:], cpc_d[:, :], writes=["cond"])
        p.dma("ldx", xT[:], xT_d.rearrange("(c p) t -> p c t", p=128), writes=[("x", c, tb) for c in range(8) for tb in range(NTB)])
        p.op("act", lambda e: e.activation(out=cond[:], in_=cond[:], func=AF.Silu), reads=["cond"], writes=["cond"])

        ADA_BLK = 256
        N_ADA = 6 * D // ADA_BLK
        ada_stg = [None, None]

        def adaln_block(l, blk, cv_bufs, bank):
            stg, brow, mrow = cv_bufs[blk % 2]
            key = ("adastg", blk % 2)
            c0 = blk * ADA_BLK
            nj = ADA_BLK // 128
            p.dma(("adab", blk % 2), brow, bada_d[l:l + 1, c0:c0 + ADA_BLK], writes=[("adabrow", blk % 2)])
            p.dma(("ada", blk % 2), stg, wada_d[l].rearrange("(kc p) f -> p kc f", p=128)[:, :, c0:c0 + ADA_BLK], writes=[key])

            def mm(e, stg=stg):
                ins = None
                for kc in range(8):
                    ins = e.matmul(banks[bank][0:1, 0:ADA_BLK], lhsT=cond[:, kc:kc + 1], rhs=stg[:, kc, :], start=(kc == 0), stop=(kc == 7))
                return ins
            p.op("pe", mm, reads=[key, "cond"], writes=[PS(bank)])
            p.op("dve", lambda e: e.tensor_tensor(out=mrow, in0=banks[bank][0:1, 0:ADA_BLK], in1=brow, op=ALU.add),
                 reads=[PS(bank), ("adabrow", blk % 2)], writes=[("adamrow", blk % 2)])

            def mm2(e):
                ins = None
                for j in range(nj):
                    ins = e.matmul(banks[bank][:, 256 + j:257 + j], lhsT=mrow[0:1, j * 128:(j + 1) * 128], rhs=consts[0:1, C_ONE:C_ONE + 1], start=True, stop=True)
                return ins
            p.op("pe", mm2, reads=[("adamrow", blk % 2), "consts"], writes=[PS(bank)])
            p.op("act", lambda e: e.copy(out=mod[:, l, blk * nj:(blk + 1) * nj], in_=banks[bank][:, 256:256 + nj]), reads=[PS(bank)], writes=[("mod", l)])

        def adaln_finish(l, bank):
            p.op("dve", lambda e: e.tensor_scalar(out=mod[:, l, 48:56], in0=mod[:, l, 8:16], scalar1=1.0, scalar2=1.0 / float(ALPHA), op0=ALU.add, op1=ALU.mult), reads=[("mod", l)], writes=[("modd", l)])
            p.op("dve", lambda e: e.tensor_scalar(out=mod[:, l, 56:64], in0=mod[:, l, 32:40], scalar1=1.0, scalar2=1.0 / float(ALPHA), op0=ALU.add, op1=ALU.mult), reads=[("mod", l)], writes=[("modd2", l)])

        def ada_bufs_alloc(cv):
            return [(cv.get([128, 8, ADA_BLK]), cv.get([1, ADA_BLK]), cv.get([1, ADA_BLK])) for _ in range(2)]

        def MOD(l, j, c):
            if j < 6:
                return mod[:, l, j * 8 + c:j * 8 + c + 1]
            return mod[:, l, 48 + (j - 6) * 8 + c:48 + (j - 6) * 8 + c + 1]

        def PPc(l, col):
            return pp[:, l, col:col + 1]

        modkeys = lambda l: [("mod", l), ("modd", l), ("modd2", l)]

        def modulate(l, which, tb, engs=("dve", "pool")):
            jsc, jsh = (6, 0) if which == 1 else (7, 3)
            for c in range(8):
                eng = engs[c % len(engs)]
                p.op(eng, lambda e, c=c: e.tensor_scalar(out=hT[:, c, tb * TB:(tb + 1) * TB], in0=xT[:, c, tb * TB:(tb + 1) * TB],
                                                         scalar1=MOD(l, jsc, c), scalar2=MOD(l, jsh, c), op0=ALU.mult, op1=ALU.add),
                     reads=[("x", c, tb)] + modkeys(l), writes=[("h", c, tb)])

        def scale_alpha(tb, engs=("pool",)):
            for c in range(8):
                eng = engs[c % len(engs)]
                if eng == "act":
                    p.op(eng, lambda e, c=c: e.mul(out=xT[:, c, tb * TB:(tb + 1) * TB], in_=xT[:, c, tb * TB:(tb + 1) * TB], mul=float(ALPHA)),
                         reads=[("x", c, tb)], writes=[("x", c, tb)])
                else:
                    p.op(eng, lambda e, c=c: e.tensor_scalar_mul(out=xT[:, c, tb * TB:(tb + 1) * TB], in0=xT[:, c, tb * TB:(tb + 1) * TB], scalar1=float(ALPHA)),
                         reads=[("x", c, tb)], writes=[("x", c, tb)])

        def layernorm(l, gcol, bcol, cv, bank_m, bank_q, final=False, after_tb=None):
            def GB(col):
                return pp[:, l, col:col + 1] if final else ppA[:, l, col:col + 1]
            sq = [cv.get([128, TB]) for _ in range(2)]
            mean_sbs = [cv.get([128, TB]) for _ in range(2)]
            rstds = [cv.get([128, TB]) for _ in range(2)]
            tmp = [cv.get([128, TB]) for _ in range(2)]
            tmp2 = [cv.get([128, TB]) for _ in range(2)]
            bank_m0, bank_q0 = bank_m, bank_q
            for tb in range(NTB):
                sl = slice(tb * TB, (tb + 1) * TB)
                mean_sb = mean_sbs[tb % 2]
                rstd = rstds[tb % 2]
                bank_m = bank_m0 + 2 * (tb % 2)
                bank_q = bank_q0 + 2 * (tb % 2)
                KM = ("lnmean", tb % 2)
                KR = ("lnrstd", tb % 2)

                def mm_mean(e, sl=sl, bank_m=bank_m):
                    ins = None
                    for c in range(8):
                        ins = e.matmul(banks[bank_m][:, :], lhsT=onesD, rhs=xT[:, c, sl], start=(c == 0), stop=(c == 7))
                    return ins
                p.op("pe", mm_mean, reads=[("x", c, tb) for c in range(8)] + ["consts"], writes=[PS(bank_m)])
                for c in range(8):
                    p.op("act", lambda e, c=c, sl=sl: e.activation(out=sq[c % 2], in_=xT[:, c, sl], func=AF.Square), reads=[("x", c, tb)], writes=[("lnsq", c % 2)])
                    p.op("pe", lambda e, c=c, bank_q=bank_q: e.matmul(banks[bank_q][:, :], lhsT=onesD, rhs=sq[c % 2], start=(c == 0), stop=(c == 7)),
                         reads=[("lnsq", c % 2), "consts"], writes=[PS(bank_q)])
                p.op("act", lambda e, mean_sb=mean_sb, bank_m=bank_m: e.copy(out=mean_sb, in_=banks[bank_m][:, :]), reads=[PS(bank_m)], writes=[KM])
                p.op("dve", lambda e, rstd=rstd, mean_sb=mean_sb: e.tensor_tensor(out=rstd, in0=mean_sb, in1=mean_sb, op=ALU.mult), reads=[KM], writes=[KR])
                p.op("dve", lambda e, rstd=rstd, bank_q=bank_q: e.tensor_tensor(out=rstd, in0=banks[bank_q][:, :], in1=rstd, op=ALU.subtract), reads=[PS(bank_q), KR], writes=[KR])
                p.op("act", lambda e, rstd=rstd: e.activation(out=rstd, in_=rstd, func=AF.Ln, bias=LNEPS[:, 0:1]), reads=[KR, "lneps"], writes=[KR])
                p.op("act", lambda e, rstd=rstd: e.activation(out=rstd, in_=rstd, func=AF.Exp, scale=-0.5), reads=[KR], writes=[KR])
                for c in range(8):
                    k = c % 2
                    p.op("dve", lambda e, c=c, k=k, sl=sl, mean_sb=mean_sb: e.tensor_tensor(out=tmp[k], in0=xT[:, c, sl], in1=mean_sb, op=ALU.subtract),
                         reads=[("x", c, tb), KM], writes=[("lnt", k)])
                    p.op("pool", lambda e, k=k, rstd=rstd: e.tensor_tensor(out=tmp2[k], in0=tmp[k], in1=rstd, op=ALU.mult), reads=[("lnt", k), KR], writes=[("lnt2", k)])
                    p.op("act", lambda e, c=c, k=k, sl=sl: e.activation(out=xT[:, c, sl], in_=tmp2[k], func=AF.Identity, scale=GB(gcol + c), bias=GB(bcol + c)),
                         reads=[("lnt2", k), "pp", "ppA"], writes=[("x", c, tb)])
                if after_tb is not None:
                    after_tb(tb)

        ppA = sb("ppA_sb", [128, DEPTH, 32])
        p.op("dve", lambda e: e.tensor_scalar_mul(out=ppA[:], in0=pp[:, :, 0:32], scalar1=float(ALPHA)), reads=["pp"], writes=["ppA"])
        LNEPS = sb("lneps_sb", [128, 4])
        p.op("pool", lambda e: e.memset(LNEPS[:, 0:1], 1e-5), writes=["lneps"])
        p.op("pool", lambda e: e.memset(LNEPS[:, 1:2], 1e-6), reads=[], writes=["lneps"])
        p.op("pool", lambda e: e.memset(LNEPS[:, 2:3], 1.0), reads=[], writes=["lneps"])

        def router(l, cv, bank_l, bank_t):
            NI = 4
            rw = cv.get([128, 8, NE])
            rb = cv.get([128, NE])
            gT = cv.get([64, S])
            h32s = [cv.get([128, 8, 128]) for _ in range(NI)]
            Ws = [{n: cv.get([128, 64]) for n in ("sc", "bi", "eq", "b2", "mk", "sel", "gw", "gates")} for _ in range(NI)]
            Sms = [{n: cv.get([128, 8]) for n in ("m1", "m2", "gs", "t8", "gsel", "goff", "t8e")} for _ in range(NI)]
            s1s = [cv.get([128, 2]) for _ in range(NI)]
            p.dma("rw", rw, rw_d[l].rearrange("(kc p) e -> p kc e", p=128), writes=["rw"])
            p.dma("rb", rb, prow_d[l:l + 1, PR_RB:PR_RB + NE].partition_broadcast(128), writes=["rb"])
            g3 = lambda a: a.rearrange("p (g k) -> p g k", k=8)
            b3 = lambda a: a.unsqueeze(2).to_broadcast([128, 8, 8])

            def tile_ops(tt):
                j = tt % NI
                tb = tt // 4
                tsl = slice(tt * 128, (tt + 1) * 128)
                h32, W, Sm, s1 = h32s[j], Ws[j], Sms[j], s1s[j]
                bl, bt = 4 + j, 4 + j
                K = lambda n: (n, j)
                ops = []
                A = lambda eng, fn, r, w: ops.append((eng, fn, r, w))
                for c in range(8):
                    eng = ("dve", "pool")[c % 2]
                    A(eng, lambda e, c=c: e.tensor_scalar(out=h32[:, c, :], in0=xT[:, c, tsl], scalar1=MOD(l, 7, c), scalar2=MOD(l, 3, c), op0=ALU.mult, op1=ALU.add),
                      [("x", c, tb)] + modkeys(l), [("h32", j, c)])

                def mm(e):
                    ins = None
                    for c in range(8):
                        ins = e.matmul(banks[bl][:, 0:NE], lhsT=h32[:, c, :], rhs=rw[:, c, :], start=(c == 0), stop=(c == 7))
                    return ins
                A("pe", mm, [("h32", j, c) for c in range(8)] + ["rw"], [PS(bl)])
                A("act", lambda e: e.activation(out=W["sc"], in_=banks[bl][:, 0:NE], func=AF.Sigmoid), [PS(bl)], [K("r_sc")])
                V = lambda fn, r, w: A("dve", fn, r, w)
                V(lambda e: e.tensor_tensor(out=W["bi"], in0=W["sc"], in1=rb, op=ALU.add), [K("r_sc"), "rb"], [K("r_bi")])
                V(lambda e: e.tensor_reduce(out=Sm["m1"], in_=g3(W["bi"]), axis=AX.X, op=ALU.max), [K("r_bi")], [K("r_m1")])
                V(lambda e: e.tensor_tensor(out=g3(W["eq"]), in0=g3(W["bi"]), in1=b3(Sm["m1"]), op=ALU.is_equal), [K("r_bi"), K("r_m1")], [K("r_eq")])
                V(lambda e: e.scalar_tensor_tensor(out=W["b2"], in0=W["eq"], scalar=-10.0, in1=W["bi"], op0=ALU.mult, op1=ALU.add), [K("r_eq"), K("r_bi")], [K("r_b2")])
                V(lambda e: e.tensor_reduce(out=Sm["m2"], in_=g3(W["b2"]), axis=AX.X, op=ALU.max), [K("r_b2")], [K("r_m2")])
                V(lambda e: e.tensor_tensor(out=Sm["gs"], in0=Sm["m1"], in1=Sm["m2"], op=ALU.add), [K("r_m1"), K("r_m2")], [K("r_gs")])
                V(lambda e: e.max(out=Sm["t8"], in_=Sm["gs"]), [K("r_gs")], [K("r_t8")])
                V(lambda e: e.tensor_scalar(out=Sm["gsel"], in0=Sm["gs"], scalar1=Sm["t8"][:, 3:4], scalar2=None, op0=ALU.is_ge), [K("r_gs"), K("r_t8")], [K("r_gsel")])
                V(lambda e: e.tensor_scalar(out=Sm["goff"], in0=Sm["gsel"], scalar1=10.0, scalar2=-10.0, op0=ALU.mult, op1=ALU.add), [K("r_gsel")], [K("r_goff")])
                V(lambda e: e.tensor_tensor(out=g3(W["mk"]), in0=g3(W["bi"]), in1=b3(Sm["gsel"]), op=ALU.mult), [K("r_bi"), K("r_gsel")], [K("r_mk")])
                V(lambda e: e.tensor_tensor(out=g3(W["mk"]), in0=g3(W["mk"]), in1=b3(Sm["goff"]), op=ALU.add), [K("r_mk"), K("r_goff")], [K("r_mk")])
                V(lambda e: e.max(out=Sm["t8e"], in_=W["mk"]), [K("r_mk")], [K("r_t8e")])
                V(lambda e: e.tensor_scalar(out=W["sel"], in0=W["mk"], scalar1=Sm["t8e"][:, 7:8], scalar2=None, op0=ALU.is_ge), [K("r_mk"), K("r_t8e")], [K("r_sel")])
                V(lambda e: e.tensor_tensor(out=W["gw"], in0=W["sel"], in1=W["sc"], op=ALU.mult), [K("r_sel"), K("r_sc")], [K("r_gw")])
                V(lambda e: e.tensor_reduce(out=s1[:, 0:1], in_=W["gw"], axis=AX.X, op=ALU.add), [K("r_gw")], [K("r_s1")])
                V(lambda e: e.reciprocal(out=s1[:, 1:2], in_=s1[:, 0:1]), [K("r_s1")], [K("r_s2")])
                V(lambda e: e.tensor_scalar(out=W["gates"], in0=W["gw"], scalar1=s1[:, 1:2], scalar2=2.5, op0=ALU.mult, op1=ALU.mult), [K("r_gw"), K("r_s2")], [K("r_gates")])
                A("pe", lambda e: e.transpose(banks[bt][0:64, 0:128], W["gates"], ident), [K("r_gates"), "consts"], [PS(bt)])
                A("act", lambda e: e.copy(out=gT[:, tsl], in_=banks[bt][0:64, 0:128]), [PS(bt)], [("gT", tt)])
                return ops

            def group(tb):
                g0 = tb * NI
                lists = [tile_ops(tt) for tt in range(g0, g0 + NI)]
                for k in range(len(lists[0])):
                    for ol in lists:
                        eng, fn, r, w = ol[k]
                        p.op(eng, fn, reads=r, writes=w)

            def finish():
                p.dma("gst", gscr_d[l % 2], gT, reads=[("gT", tt) for tt in range(S // 128)], writes=[("gscr", l % 2)])
            return group, finish

        def moe(l, cv, hooks):
            stg = {n: cv.get([128, 8, 256]) for n in ("w1", "w3")}
            stg["w2"] = cv.get([128, 2, D])
            wbf = [{"w1": cv.get([128, 8, 256], BF16), "w3": cv.get([128, 8, 256], BF16), "w2": cv.get([128, 2, D], BF16)} for _ in range(2)]
            gbc = [cv.get([128, S]) for _ in range(2)]
            sS = [[cv.get([128, TB], BF16) for f in range(2)] for _ in range(2)]
            tS = [[cv.get([128, TB], BF16) for f in range(2)] for _ in range(2)]
            hid = [[cv.get([128, TB], BF16) for f in range(2)] for _ in range(2)]
            steps = [(e, tb) for e in range(n_exp) for tb in range(NTB)]

            def load(e):
                sl = e % 2
                if e < NE:
                    srcs = {"w1": ew1_d[l, e], "w3": ew3_d[l, e], "w2": ew2_d[l, e]}
                else:
                    srcs = {"w1": sw1_d[l], "w3": sw3_d[l], "w2": sw2_d[l]}
                for n in ("w1", "w3", "w2"):
                    pat = "(kc p) f -> p kc f"
                    p.dma(("wst", n), stg[n], srcs[n].rearrange(pat, p=128), writes=[("stg", n)])
                if e < NE:
                    p.dma(("gbc", sl), gbc[sl], gscr_d[l % 2, e:e + 1, :].partition_broadcast(128), reads=[("gscr", l % 2)], writes=[("gbc", sl)])

            def cast(e):
                sl = e % 2
                for n in ("w1", "w3", "w2"):
                    if n == "w2":
                        parts = [(slice(0, 1), "act"), (slice(1, 2), "pool")]
                    else:
                        parts = [(slice(0, 3), "act"), (slice(3, 8), "pool")]
                    for (ps_, ce) in parts:
                        if ce == "act":
                            p.op("act", lambda e_, n=n, sl=sl, ps_=ps_: e_.copy(out=wbf[sl][n][:, ps_, :], in_=stg[n][:, ps_, :]), reads=[("stg", n)], writes=[("wbf", sl, n, ce)])
                        else:
                            p.op("pool", lambda e_, n=n, sl=sl, ps_=ps_: e_.tensor_copy(out=wbf[sl][n][:, ps_, :], in_=stg[n][:, ps_, :]), reads=[("stg", n)], writes=[("wbf", sl, n, ce)])

            def up(i, f):
                e, tb = steps[i]
                sl = e % 2
                for wi, n in enumerate(("w1", "w3")):
                    bk = f * 2 + wi

                    def mm(e_, n=n, bk=bk, sl=sl, tb=tb, f=f):
                        ins = None
                        for kc in range(8):
                            ins = e_.matmul(banks[bk][:, :], lhsT=wbf[sl][n][:, kc, f * 128:(f + 1) * 128], rhs=hT[:, kc, tb * TB:(tb + 1) * TB], start=(kc == 0), stop=(kc == 7))
                        return ins
                    p.op("pe", mm, reads=[("wbf", sl, n, "act"), ("wbf", sl, n, "pool")] + [("h", kc, tb) for kc in range(8)], writes=[PS(bk)])

            def gating(i, f):
                e, tb = steps[i]
                sl = e % 2
                par = i % 2
                p.op("act", lambda e_: e_.activation(out=sS[par][f], in_=banks[f * 2][:, :], func=AF.Silu), reads=[PS(f * 2)], writes=[("sS", par, f)])
                if e < NE:
                    p.op("dve", lambda e_: e_.tensor_tensor(out=tS[par][f], in0=banks[f * 2 + 1][:, :], in1=gbc[sl][:, tb * TB:(tb + 1) * TB], op=ALU.mult),
                         reads=[PS(f * 2 + 1), ("gbc", sl)], writes=[("tS", par, f)])
                    p.op("dve", lambda e_: e_.tensor_tensor(out=hid[par][f], in0=sS[par][f], in1=tS[par][f], op=ALU.mult),
                         reads=[("sS", par, f), ("tS", par, f)], writes=[("hid", par, f)])
                else:
                    p.op("dve", lambda e_: e_.tensor_tensor(out=hid[par][f], in0=banks[f * 2 + 1][:, :], in1=sS[par][f], op=ALU.mult),
                         reads=[PS(f * 2 + 1), ("sS", par, f)], writes=[("hid", par, f)])

            def down(i, dh):
                e, tb = steps[i]
                sl = e % 2
                par = i % 2
                for dq in range(4):
                    d = dh * 4 + dq
                    bk = 4 + dq

                    def mm(e_, d=d, bk=bk):
                        ins = None
                        for f in range(2):
                            ins = e_.matmul(banks[bk][:, :], lhsT=wbf[sl]["w2"][:, f, d * 128:(d + 1) * 128], rhs=hid[par][f], start=(f == 0), stop=(f == 1))
                        return ins
                    p.op("pe", mm, reads=[("wbf", sl, "w2", "act"), ("wbf", sl, "w2", "pool"), ("hid", par, 0), ("hid", par, 1)], writes=[PS(bk)])
                    p.op("dve", lambda e_, d=d, bk=bk: e_.scalar_tensor_tensor(out=xT[:, d, tb * TB:(tb + 1) * TB], in0=banks[bk][:, :], scalar=MOD(l, 5, d), in1=xT[:, d, tb * TB:(tb + 1) * TB], op0=ALU.mult, op1=ALU.add),
                         reads=[PS(bk), ("x", d, tb)] + modkeys(l), writes=[("x", d, tb)])

            load(0)
            cast(0)
            if n_exp > 1:
                load(1)
                cast(1)
            up(0, 0)
            up(0, 1)
            gating(0, 0)
            gating(0, 1)
            for i in range(len(steps)):
                e, tb = steps[i]
                if tb == 0 and i > 0 and e + 1 < n_exp:
                    load(e + 1)
                if tb == 2 and e > 0 and e + 1 < n_exp:
                    cast(e + 1)
                if tb == 1 and e in hooks:
                    hooks[e]()
                nxt = i + 1 < len(steps)
                if nxt:
                    up(i + 1, 0)
                down(i, 0)
                if nxt:
                    gating(i + 1, 0)
                    up(i + 1, 1)
                down(i, 1)
                if nxt:
                    gating(i + 1, 1)


        def ssd_units(l, cv, mark, load_win, load_wout, proj_fm, proj_tm, out_proj, conv_silu, yblk, identb, ones512b):
            cv.off = mark
            dtb = cv.get([128, 16]); alog = cv.get([128, 16]); aneg = cv.get([128, 16])
            cbuf = [cv.get([128, 3 + TB]) for _ in range(6)]
            ctmps = [cv.get([128, TB]) for _ in range(3)]
            ctmp = ctmps[0]
            xs = [cv.get([128, TB], BF16) for _ in range(4)]
            BT = cv.get([128, TB], BF16); CT = cv.get([128, TB], BF16)
            sz = [cv.get([128, TB], BF16) for _ in range(4)]
            yg = cv.get([128, 4, TB])
            dt_tm = cv.get([128, 8]); dA = cv.get([128, 8]); acs = cv.get([128, 8]); dte = cv.get([128, 8])
            w2 = cv.get([128, 8]); draw = cv.get([128, 8]); ex = cv.get([128, 8])
            dAb = cv.get([128, 8, 128])
            L = dAb
            Btmzs = [[cv.get([128, 128], BF16) for _ in range(2)] for _ in range(2)]
            decbcs = [cv.get([128, 2, 8]) for _ in range(2)]
            eD = cv.get([128, 8, 128], BF16)
            cbm = cv.get([128, 128])
            MTs = [cv.get([128, 8, 128], BF16) for _ in range(2)]
            CTss = [cv.get([128, 8, 128], BF16) for _ in range(2)]
            xdts = [cv.get([128, 8, 64], BF16) for _ in range(2)]
            xws = [cv.get([128, 8, 64], BF16) for _ in range(2)]
            pa_ctr = [0]
            Btm = cv.get([128, 128], BF16)
            S32 = cv.get([128, 8, 64])
            Sbf = [cv.get([128, 8, 64], BF16) for _ in range(2)]
            sqb = cv.get([128, TB], BF16); rs = ctmp
            b7 = banks[7][:, :].bitcast(BF16)
            MASK = consts[:, C_MASK:C_MASK + 128]
            SUF = consts[:, C_SUF:C_SUF + 128]
            p.dma("dtb", dtb, prow_d[l:l + 1, PR_DTB:PR_DTB + 16].partition_broadcast(128), writes=["dtb"])
            p.dma("alog", alog, prow_d[l:l + 1, PR_ALOG:PR_ALOG + 16].partition_broadcast(128), writes=["alog"])
            p.op("act", lambda e: e.activation(out=aneg, in_=alog, func=AF.Exp), reads=["alog"], writes=["aneg"])
            p.op("dve", lambda e: e.tensor_scalar_mul(out=aneg, in0=aneg, scalar1=-1.0), reads=["aneg"], writes=["aneg"])
            for g in range(2):
                load_win(3600 + g * 512, 512, 0)
                load_win(2576 + g * 512, 512, 512)
                load_win(4624 + g * 128, 128, 1024)
                load_win(4880 + g * 128, 128, 1152)
                load_win(5136 + g * 8, 8, 1280)
                load_wout(8 + g * 4, 4)
                for j6 in range(6):
                    p.op("pool", lambda e, j6=j6: e.memset(cbuf[j6][:, 0:3], 0.0), writes=[("cbuf", j6)])
                p.op("pool", lambda e: e.memset(S32, 0.0), writes=["s_S32"])
                p.op("pool", lambda e: e.memset(Sbf[0], 0.0), writes=[("s_Sbf", 0)])
                for pa in range(2):
                    for half in range(2):
                        p.op("pool", lambda e, half=half, pa=pa: e.memset(Btmzs[pa][half], 0.0), writes=[("s_Btmz", pa, half)])
                par = 0
                gs = slice(g * 8, g * 8 + 8)
                for tb in range(NTB):
                    def conv_chain(j6):
                        off = j6 * 128 if j6 < 4 else (1024 if j6 == 4 else 1152)
                        jc = g * 4 + j6 if j6 < 4 else (8 + g if j6 == 4 else 10 + g)
                        bank = j6
                        proj_fm(off, 128, tb, bank)
                        dst = xs[j6] if j6 < 4 else (BT if j6 == 4 else CT)
                        conv_silu(cbuf[j6], ("cbuf", j6), tb, bank, PP_SCW + jc * 4, PP_SCB + jc, ctmps[j6 % 3], ("s_ctmp", j6 % 3), dst, ("s_fm", j6), AF.Silu, eng="dve")

                    def z_chain(q):
                        bank = 6 + q % 2
                        proj_fm(512 + q * 128, 128, tb, bank)
                        p.op("act", lambda e: e.activation(out=sz[q], in_=banks[bank][:, :], func=AF.Silu), reads=[PS(bank)], writes=[("s_sz", q)])

                    replay_ops(interleave([cap_ops(conv_chain, 0), cap_ops(conv_chain, 1), cap_ops(conv_chain, 2), cap_ops(z_chain, 0), cap_ops(z_chain, 1)]))
                    replay_ops(interleave([cap_ops(conv_chain, 3), cap_ops(conv_chain, 4), cap_ops(conv_chain, 5), cap_ops(z_chain, 2), cap_ops(z_chain, 3)]))
                    def _aliases(pa):
                        return MTs[pa], CTss[pa], xdts[pa], xws[pa], Btmzs[pa], decbcs[pa]

                    def stageA(tt, pa):
                        MT, CTs, xdt, xw, Btmz, decbc = _aliases(pa)
                        KP = lambda n: (n, pa)
                        tsl = slice(tt * 128, (tt + 1) * 128)
                        proj_tm(1280, 8, tb, tt, 5)
                        p.op("dve", lambda e, gs=gs: e.tensor_tensor(out=draw, in0=banks[5][:, 0:8], in1=dtb[:, gs], op=ALU.add), reads=[PS(5), "dtb"], writes=["s_draw"])
                        p.op("act", lambda e: e.activation(out=ex, in_=draw, func=AF.Exp), reads=["s_draw"], writes=["s_ex"])
                        p.op("act", lambda e: e.activation(out=dt_tm, in_=ex, func=AF.Ln, bias=LNEPS[:, 2:3]), reads=["s_ex", "lneps"], writes=["s_dt"])
                        p.op("dve", lambda e, gs=gs: e.tensor_tensor(out=dA, in0=dt_tm, in1=aneg[:, gs], op=ALU.mult), reads=["s_dt", "aneg"], writes=["s_dA"])

                        def mm5(e):
                            e.matmul(banks[5][:, 8:16], lhsT=MASK, rhs=dA, start=True, stop=True)
                            e.matmul(banks[5][:, 16:24], lhsT=SUF, rhs=dA, start=True, stop=True)
                            e.matmul(banks[5][:, 160:168], lhsT=consts[:, C_CH0:C_CH0 + 128], rhs=dA, start=True, stop=True)
                            return e.matmul(banks[5][:, 168:176], lhsT=consts[:, C_CH1:C_CH1 + 128], rhs=dA, start=True, stop=True)
                        p.op("pe", mm5, reads=["s_dA", "consts"], writes=[PS(5)])
                        p.op("act", lambda e: e.copy(out=acs, in_=banks[5][:, 8:16]), reads=[PS(5)], writes=["s_acs"])
                        p.op("act", lambda e: e.activation(out=dte, in_=banks[5][:, 16:24], func=AF.Exp), reads=[PS(5)], writes=["s_dte"])
                        p.op("act", lambda e: e.copy(out=dAb, in_=dA.unsqueeze(2).to_broadcast([128, 8, 128])), reads=["s_dA"], writes=["s_dAb", ("s_L", 0), ("s_L", 1), "s_Lm", "s_Le"])
                        p.op("act", lambda e: e.activation(out=decbc, in_=banks[5][:, 160:176].rearrange("p (a b) -> p a b", a=2), func=AF.Exp), reads=[PS(5)], writes=[KP("s_dec")])
                        p.op("dve", lambda e: e.tensor_tensor(out=w2, in0=dt_tm, in1=dte, op=ALU.mult), reads=["s_dt", "s_dte"], writes=["s_w2"])

                        def mmD(e):
                            ins = None
                            for h in range(8):
                                ins = e.matmul(banks[2 + h // 4][:, (h % 4) * 128:(h % 4 + 1) * 128], lhsT=dAb[:, h, :], rhs=MASK, start=True, stop=True)
                            return ins
                        p.op("pe", mmD, reads=["s_dAb", "consts"], writes=[PS(2), PS(3)])
                        for k in range(2):
                            p.op("dve", lambda e, k=k: e.tensor_tensor(out=L[:, 4 * k:4 * k + 4, :], in0=banks[2 + k][:, :].rearrange("p (a b) -> p a b", a=4),
                                                                       in1=acs[:, 4 * k:4 * k + 4].unsqueeze(2).to_broadcast([128, 4, 128]), op=ALU.subtract),
                                 reads=[PS(2 + k), "s_acs"], writes=[("s_L", k)])
                            p.op("act", lambda e, k=k: e.activation(out=eD[:, 4 * k:4 * k + 4, :], in_=banks[2 + k][:, :].rearrange("p (a b) -> p a b", a=4), func=AF.Exp), reads=[PS(2 + k)], writes=[("s_eD", k)])
                        p.op("dve", lambda e: e.tensor_scalar_min(out=L, in0=L, scalar1=0.0), reads=[("s_L", 0), ("s_L", 1)], writes=["s_Lm"])
                        p.op("act", lambda e: e.activation(out=L, in_=L, func=AF.Exp), reads=["s_Lm"], writes=["s_Le"])
                        p.op("pe", lambda e, tsl=tsl: e.matmul(banks[5][:, 256:384], lhsT=BT[:, tsl], rhs=CT[:, tsl], start=True, stop=True), reads=[("s_fm", 4), ("s_fm", 5)], writes=[PS(5)])
                        p.op("dve", lambda e: e.tensor_tensor(out=cbm, in0=banks[5][:, 256:384], in1=MASK, op=ALU.mult), reads=[PS(5), "consts"], writes=["s_cbm"])
                        p.op("dve", lambda e: e.tensor_tensor(out=MT, in0=L, in1=cbm.unsqueeze(1).to_broadcast([128, 8, 128]), op=ALU.mult), reads=["s_Le", "s_cbm"], writes=[KP("s_MT")])
                        p.op("dve", lambda e, tsl=tsl: e.tensor_tensor(out=CTs, in0=eD, in1=CT[:, tsl].unsqueeze(1).to_broadcast([128, 8, 128]), op=ALU.mult), reads=[("s_eD", 0), ("s_eD", 1), ("s_fm", 5)], writes=[KP("s_CTs")])

                        def mmT(e, tsl=tsl):
                            for q in range(4):
                                e.transpose(b7[:, q * 128:(q + 1) * 128], xs[q][:, tsl], identb)
                            return e.transpose(b7[:, 512:640], BT[:, tsl], identb)
                        p.op("pe", mmT, reads=[("s_fm", j) for j in range(5)] + ["identb"], writes=[PS(7)])
                        xtm = b7[:, 0:512].rearrange("p (h k) -> p h k", h=8)
                        p.op("dve", lambda e: e.tensor_tensor(out=xdt, in0=xtm, in1=dt_tm.unsqueeze(2).to_broadcast([128, 8, 64]), op=ALU.mult), reads=[PS(7), "s_dt"], writes=[KP("s_xdt")])
                        p.op("dve", lambda e: e.tensor_tensor(out=xw, in0=xtm, in1=w2.unsqueeze(2).to_broadcast([128, 8, 64]), op=ALU.mult), reads=[PS(7), "s_w2"], writes=[KP("s_xw")])
                        for half in range(2):
                            p.op("act", lambda e, half=half: e.copy(out=Btmz[half][half * 64:(half + 1) * 64, :], in_=b7[half * 64:(half + 1) * 64, 512:640]), reads=[PS(7)], writes=[("s_Btmz", pa, half)])


                    def stageB(tt, pa, par):
                        MT, CTs, xdt, xw, Btmz, decbc = _aliases(pa)
                        KP = lambda n: (n, pa)
                        tsl = slice(tt * 128, (tt + 1) * 128)
                        def mmY(e):
                            ins = None
                            for h in range(8):
                                q, hq = h // 2, h % 2
                                ins = e.matmul(banks[4][hq * 64:(hq + 1) * 64, q * 128:(q + 1) * 128], lhsT=xdt[:, h, :], rhs=MT[:, h, :], start=True, stop=True)
                            return ins
                        p.op("pe", mmY, reads=[KP("s_xdt"), KP("s_MT")], writes=[PS(4)])
                        for half in range(2):
                            hs = slice(half * 64, (half + 1) * 64)

                            def mmO(e, half=half, par=par):
                                ins = None
                                for h in range(8):
                                    q, hq = h // 2, h % 2
                                    ins = e.matmul(banks[0][hq * 64:(hq + 1) * 64, q * 128 + half * 64:q * 128 + half * 64 + 64], lhsT=Sbf[par][:, h, :], rhs=CTs[:, h, half * 64:(half + 1) * 64], start=True, stop=True)
                                return ins
                            p.op("pe", mmO, reads=[("s_Sbf", par), KP("s_CTs")], writes=[("ps0o", half)] + ([PS(0)] if half == 0 else []))
                            p.op("pe", lambda e, half=half: e.matmul(banks[6][:, :], lhsT=Btmz[half], rhs=xw.rearrange("p h k -> p (h k)"), start=True, stop=True), reads=[("s_Btmz", pa, half), KP("s_xw")], writes=[PS(6)])
                            p.op("dve", lambda e, half=half: e.tensor_tensor(out=S32, in0=S32, in1=decbc[:, half, :].unsqueeze(2).to_broadcast([128, 8, 64]), op=ALU.mult), reads=["s_S32", KP("s_dec")], writes=["s_S32"])
                            p.op("dve", lambda e: e.tensor_tensor(out=S32, in0=S32, in1=banks[6][:, :].rearrange("p (h k) -> p h k", h=8), op=ALU.add), reads=["s_S32", PS(6)], writes=["s_S32"])
                            p.op("act", lambda e, par=par: e.copy(out=Sbf[1 - par], in_=S32), reads=["s_S32"], writes=[("s_Sbf", 1 - par)])
                            par = 1 - par
                        for q in range(4):
                            p.op("dve", lambda e, q=q, tsl=tsl, g=g: e.scalar_tensor_tensor(out=yg[:, q, tsl], in0=xs[q][:, tsl], scalar=PPc(l, PP_SD + g * 4 + q), in1=banks[4][:, q * 128:(q + 1) * 128], op0=ALU.mult, op1=ALU.add),
                                 reads=[("s_fm", q), PS(4), "pp"], writes=[("s_yg", q)])
                            p.op("dve", lambda e, q=q, tsl=tsl: e.tensor_tensor(out=yg[:, q, tsl], in0=yg[:, q, tsl], in1=banks[0][:, q * 128:(q + 1) * 128], op=ALU.add),
                                 reads=[("s_yg", q), PS(0), ("ps0o", 0), ("ps0o", 1)], writes=[("s_yg", q)])
                        return par

                    def capture(fn, *a):
                        lst = []
                        p.cap = lst
                        r = fn(*a)
                        p.cap = None
                        return lst, r

                    def replay(lst):
                        for (eng, fn, r, w) in lst:
                            p.op(eng, fn, reads=r, writes=w)

                    def merge(la, lb):
                        out = []
                        ia = ib = 0
                        na, nb = len(la), len(lb)
                        while ia < na or ib < nb:
                            if ib >= nb or (ia < na and ia * nb <= ib * na):
                                out.append(la[ia]); ia += 1
                            else:
                                out.append(lb[ib]); ib += 1
                        return out

                    lA, _ = capture(stageA, 0, pa_ctr[0] % 2)
                    replay(lA)
                    for tt in range(4):
                        pa = pa_ctr[0] % 2
                        lB, par = capture(stageB, tt, pa, par)
                        if tt + 1 < 4:
                            lA, _ = capture(stageA, tt + 1, (pa_ctr[0] + 1) % 2)
                            replay(merge(lA, lB))
                        else:
                            replay(lB)
                        pa_ctr[0] += 1
                    for q in range(4):
                        p.op("dve", lambda e, q=q: e.tensor_tensor(out=yg[:, q, :], in0=yg[:, q, :], in1=sz[q], op=ALU.mult), reads=[("s_yg", q), ("s_sz", q)], writes=[("s_yg", q)])
                    for q in range(4):
                        p.op("act", lambda e, q=q: e.activation(out=sqb, in_=yg[:, q, :], func=AF.Square), reads=[("s_yg", q)], writes=["s_sqb"])
                        p.op("pe", lambda e, q=q: e.matmul(banks[5][:, :], lhsT=ones512b, rhs=sqb, start=(q == 0), stop=(q == 3)), reads=["s_sqb", "ones512b"], writes=[PS(5)])
                    p.op("act", lambda e: e.activation(out=rs, in_=banks[5][:, :], func=AF.Ln, bias=LNEPS[:, 1:2]), reads=[PS(5), "lneps"], writes=[("s_ctmp", 0)])
                    p.op("act", lambda e: e.activation(out=rs, in_=rs, func=AF.Exp, scale=-0.5), reads=[("s_ctmp", 0)], writes=[("s_ctmp", 0)])
                    for q in range(4):
                        p.op("dve", lambda e, q=q, g=g: e.scalar_tensor_tensor(out=yblk[:, q, :], in0=yg[:, q, :], scalar=PPc(l, PP_SNORM + g * 4 + q), in1=rs, op0=ALU.mult, op1=ALU.mult),
                             reads=[("s_yg", q), ("s_ctmp", 0), "pp"], writes=[("yblk", q)])
                    out_proj(tb, 4, [0, 1])

        def mixer(l):
            cv = Carve()
            wst = [cv.get([128, 8, 128]) for _ in range(2)]
            wunit = cv.get([128, 8, 1408], BF16)
            woutst = [cv.get([128, D])] * 2
            wout = cv.get([128, 4, D], BF16)
            yblk = cv.get([128, 4, TB], BF16)
            identb = cv.get([128, 128], BF16)
            ones128b = cv.get([128, 128], BF16)
            ones512b = cv.get([128, 128], BF16)
            p.op("act", lambda e: e.copy(out=identb, in_=ident), reads=["consts"], writes=["identb"])
            p.op("pool", lambda e: e.memset(ones128b, 1.0 / 128.0), writes=["ones128b"])
            p.op("pool", lambda e: e.memset(ones512b, 1.0 / 512.0), writes=["ones512b"])
            wcnt = [0]

            def load_win(col0, ncols, dst):
                c = 0
                while c < ncols:
                    n = min(128, ncols - c)
                    k = wcnt[0] % 2
                    wcnt[0] += 1
                    p.dma(("wst", k), wst[k][:, :, 0:n], win_d[l].rearrange("(kc p) f -> p kc f", p=128)[:, :, col0 + c:col0 + c + n], writes=[("wst", k)])
                    eng = ("act", "pool")[k]
                    if eng == "act":
                        p.op("act", lambda e, k=k, n=n, c=c: e.copy(out=wunit[:, :, dst + c:dst + c + n], in_=wst[k][:, :, 0:n]), reads=[("wst", k)], writes=["wunit"])
                    else:
                        p.op("pool", lambda e, k=k, n=n, c=c: e.tensor_copy(out=wunit[:, :, dst + c:dst + c + n], in_=wst[k][:, :, 0:n]), reads=[("wst", k)], writes=["wunit"])
                    c += n

            def load_wout(ych0, n):
                for j in range(n):
                    k = 0
                    p.dma(("wost", k), woutst[k], wout_d[l, (ych0 + j) * 128:(ych0 + j + 1) * 128, :], writes=[("wost", k)])
                    p.op("pool", lambda e, k=k, j=j: e.tensor_copy(out=wout[:, j, :], in_=woutst[k]), reads=[("wost", k)], writes=["wout"])

            def proj_fm(off, ncols, tb, bank):
                def mm(e):
                    ins = None
                    for kc in range(8):
                        ins = e.matmul(banks[bank][0:ncols, :], lhsT=wunit[:, kc, off:off + ncols], rhs=hT[:, kc, tb * TB:(tb + 1) * TB], start=(kc == 0), stop=(kc == 7))
                    return ins
                p.op("pe", mm, reads=["wunit"] + [("h", kc, tb) for kc in range(8)], writes=[PS(bank)])

            def proj_tm(off, ncols, tb, tt, bank, col0=0):
                t0 = tb * TB + tt * 128

                def mm(e):
                    ins = None
                    for kc in range(8):
                        ins = e.matmul(banks[bank][:, col0:col0 + ncols], lhsT=hT[:, kc, t0:t0 + 128], rhs=wunit[:, kc, off:off + ncols], start=(kc == 0), stop=(kc == 7))
                    return ins
                p.op("pe", mm, reads=["wunit"] + [("h", kc, tb) for kc in range(8)], writes=[PS(bank)])

            def out_proj(tb, nych, bks):
                for d in range(8):
                    bk = bks[d % len(bks)]

                    def mm(e, d=d, bk=bk):
                        ins = None
                        for j in range(nych):
                            ins = e.matmul(banks[bk][:, :], lhsT=wout[:, j, d * 128:(d + 1) * 128], rhs=yblk[:, j, :], start=(j == 0), stop=(j == nych - 1))
                        return ins
                    p.op("pe", mm, reads=["wout"] + [("yblk", j) for j in range(nych)], writes=[PS(bk)])
                    p.op("dve", lambda e, d=d, bk=bk: e.scalar_tensor_tensor(out=xT[:, d, tb * TB:(tb + 1) * TB], in0=banks[bk][:, :], scalar=MOD(l, 2, d), in1=xT[:, d, tb * TB:(tb + 1) * TB], op0=ALU.mult, op1=ALU.add),
                         reads=[PS(bk), ("x", d, tb)] + modkeys(l), writes=[("x", d, tb)])

            def conv_silu(buf, key, tb, bank, wcol, bcol, tmp, tmpkey, dst, dstkey, act_func, eng="dve"):
                p.op("act", lambda e: e.copy(out=buf[:, 3:3 + TB], in_=banks[bank][:, :]), reads=[PS(bank)], writes=[key])
                p.op(eng, lambda e: e.tensor_scalar(out=tmp, in0=buf[:, 0:TB], scalar1=PPc(l, wcol), scalar2=PPc(l, bcol), op0=ALU.mult, op1=ALU.add), reads=[key, "pp"], writes=[tmpkey])
                for k in range(1, 4):
                    p.op(eng, lambda e, k=k: e.scalar_tensor_tensor(out=tmp, in0=buf[:, k:k + TB], scalar=PPc(l, wcol + k), in1=tmp, op0=ALU.mult, op1=ALU.add), reads=[key, tmpkey, "pp"], writes=[tmpkey])
                p.op("pool", lambda e: e.tensor_copy(out=buf[:, 0:3], in_=buf[:, TB:TB + 3]), reads=[key], writes=[key])
                if dst is not None:
                    p.op("act", lambda e: e.activation(out=dst, in_=tmp, func=act_func), reads=[tmpkey], writes=[dstkey])

            mark = cv.off

            if do_lru:
                cv.off = mark
                wabd = cv.get([128, 4, 128], BF16)
                wxbd = cv.get([128, 4, 128], BF16)
                nsp8 = cv.get([128, 4])
                hcar = cv.get([128, 4])
                xbuf = [cv.get([128, 3 + TB]) for _ in range(4)]
                Ts = [{n: cv.get([128, TB]) for n in ("xc", "r", "i", "om", "h", "gg")} for _ in range(4)]
                xcbs = [cv.get([128, TB], BF16) for _ in range(4)]
                for nm, src, dst in (("wa", wabd_d, wabd), ("wx", wxbd_d, wxbd)):
                    p.dma(("wost", 0), woutst[0][:, 0:512].rearrange("p (m j) -> p m j", m=4), src[l].rearrange("m i j -> i m j"), writes=[("wost", 0)])
                    p.op("pool", lambda e, dst=dst: e.tensor_copy(out=dst, in_=woutst[0][:, 0:512].rearrange("p (m j) -> p m j", m=4)), reads=[("wost", 0)], writes=[nm])
                p.op("act", lambda e: e.activation(out=nsp8, in_=pp[:, l, PP_LLAM:PP_LLAM + 4], func=AF.Exp, scale=-1.0), reads=["pp"], writes=["nsp8"])
                p.op("act", lambda e: e.activation(out=nsp8, in_=nsp8, func=AF.Ln, bias=LNEPS[:, 2:3]), reads=["nsp8", "lneps"], writes=["nsp8"])
                p.op("dve", lambda e: e.tensor_scalar_mul(out=nsp8, in0=nsp8, scalar1=-8.0), reads=["nsp8"], writes=["nsp8"])
                for m in range(4):
                    p.op("pool", lambda e, m=m: e.memset(xbuf[m][:, 0:3], 0.0), writes=[("xbuf", m)])
                load_win(0, 1024, 0)
                load_wout(0, 4)

                def lru_chain(tb, m):
                    T = Ts[m]
                    xcb = xcbs[m]
                    BA, BB = 2 * m, 2 * m + 1
                    KK = lambda n: (n, m)
                    ops = []
                    A = lambda eng, fn, r, w: ops.append((eng, fn, r, w))
                    buf = xbuf[m]
                    key = ("xbuf", m)
                    wcol, bcol = PP_LCW + m * 4, PP_LCB + m
                    sl_h = [("h", kc, tb) for kc in range(8)]

                    def mmx(e):
                        ins = None
                        for kc in range(8):
                            ins = e.matmul(banks[BA][:, :], lhsT=wunit[:, kc, m * 128:(m + 1) * 128], rhs=hT[:, kc, tb * TB:(tb + 1) * TB], start=(kc == 0), stop=(kc == 7))
                        return ins

                    def mmg(e):
                        ins = None
                        for kc in range(8):
                            ins = e.matmul(banks[BB][:, :], lhsT=wunit[:, kc, 512 + m * 128:512 + (m + 1) * 128], rhs=hT[:, kc, tb * TB:(tb + 1) * TB], start=(kc == 0), stop=(kc == 7))
                        return ins
                    A("pe", mmx, ["wunit"] + sl_h, [PS(BA)])
                    A("pe", mmg, ["wunit"] + sl_h, [PS(BB)])
                    A("act", lambda e: e.copy(out=buf[:, 3:3 + TB], in_=banks[BA][:, :]), [PS(BA)], [key])
                    A("act", lambda e: e.activation(out=T["gg"], in_=banks[BB][:, :], func=AF.Gelu_apprx_tanh), [PS(BB)], [KK("l_gg")])
                    A("dve", lambda e: e.tensor_scalar(out=T["xc"], in0=buf[:, 0:TB], scalar1=PPc(l, wcol), scalar2=PPc(l, bcol), op0=ALU.mult, op1=ALU.add), [key, "pp"], [KK("l_xc")])
                    for k in range(1, 4):
                        A("dve", lambda e, k=k: e.scalar_tensor_tensor(out=T["xc"], in0=buf[:, k:k + TB], scalar=PPc(l, wcol + k), in1=T["xc"], op0=ALU.mult, op1=ALU.add), [key, KK("l_xc"), "pp"], [KK("l_xc")])
                    A("pool", lambda e: e.tensor_copy(out=buf[:, 0:3], in_=buf[:, TB:TB + 3]), [key], [key])
                    A("act", lambda e: e.copy(out=xcb, in_=T["xc"]), [KK("l_xc")], [KK("l_xcb")])
                    A("pe", lambda e: e.matmul(banks[BA][:, :], lhsT=wabd[:, m, :], rhs=xcb, start=True, stop=True), ["wa", KK("l_xcb")], [PS(BA)])
                    A("pe", lambda e: e.matmul(banks[BB][:, :], lhsT=wxbd[:, m, :], rhs=xcb, start=True, stop=True), ["wx", KK("l_xcb")], [PS(BB)])
                    A("act", lambda e: e.activation(out=T["r"], in_=banks[BA][:, :], func=AF.Sigmoid, bias=PPc(l, PP_LBA + m)), [PS(BA), "pp"], [KK("l_r")])
                    A("act", lambda e: e.activation(out=T["i"], in_=banks[BB][:, :], func=AF.Sigmoid, bias=PPc(l, PP_LBX + m)), [PS(BB), "pp"], [KK("l_i")])
                    A("act", lambda e: e.activation(out=T["r"], in_=T["r"], func=AF.Exp, scale=nsp8[:, m:m + 1]), [KK("l_r"), "nsp8"], [KK("l_r")])
                    A("pool", lambda e: e.tensor_tensor(out=T["om"], in0=T["r"], in1=T["r"], op=ALU.mult), [KK("l_r")], [KK("l_om")])
                    A("pool", lambda e: e.tensor_scalar(out=T["om"], in0=T["om"], scalar1=-1.0, scalar2=1.0, op0=ALU.mult, op1=ALU.add), [KK("l_om")], [KK("l_om")])
                    A("act", lambda e: e.activation(out=T["om"], in_=T["om"], func=AF.Sqrt), [KK("l_om")], [KK("l_om")])
                    A("pool", lambda e: e.tensor_tensor(out=T["i"], in0=T["i"], in1=T["xc"], op=ALU.mult), [KK("l_i"), KK("l_xc")], [KK("l_i")])
                    A("dve", lambda e: e.tensor_tensor(out=T["om"], in0=T["om"], in1=T["i"], op=ALU.mult), [KK("l_om"), KK("l_i")], [KK("l_om")])
                    if tb == 0:
                        A("dve", lambda e: e.tensor_tensor_scan(out=T["h"], data0=T["r"], data1=T["om"], initial=0.0, op0=ALU.mult, op1=ALU.add), [KK("l_r"), KK("l_om")], [KK("l_h")])
                    else:
                        A("dve", lambda e: e.tensor_tensor_scan(out=T["h"], data0=T["r"], data1=T["om"], initial=hcar[:, m:m + 1], op0=ALU.mult, op1=ALU.add), [KK("l_r"), KK("l_om"), ("hcar", m)], [KK("l_h")])
                    A("pool", lambda e: e.tensor_copy(out=hcar[:, m:m + 1], in_=T["h"][:, TB - 1:TB]), [KK("l_h")], [("hcar", m)])
                    A("dve", lambda e: e.tensor_tensor(out=yblk[:, m, :], in0=T["h"], in1=T["gg"], op=ALU.mult), [KK("l_h"), KK("l_gg")], [("yblk", m)])
                    return ops

                for tb in range(NTB):
                    lists = [lru_chain(tb, m) for m in range(4)]
                    for k in range(len(lists[0])):
                        for ol in lists:
                            eng, fn, r, w = ol[k]
                            p.op(eng, fn, reads=r, writes=w)
                    out_proj(tb, 4, [0, 1, 2, 3, 4, 5, 6, 7])

            if do_gla:
                cv.off = mark
                walb = cv.get([32, 256], BF16)
                rT = cv.get([32, TB], BF16)
                e1 = cv.get([128, 128])
                l1 = cv.get([128, 128])
                ecp = cv.get([128, TB])
                ecn = cv.get([128, TB])
                esuf = [cv.get([128, 128]) for _ in range(4)]
                qd = cv.get([128, TB], BF16)
                kd = cv.get([128, TB], BF16)
                kend = [cv.get([128, 128], BF16) for _ in range(4)]
                kendz = [[cv.get([128, 128], BF16) for _ in range(2)] for _ in range(4)]
                qdz = [cv.get([128, TB], BF16) for _ in range(2)]
                CHM = [consts[:, C_CH0:C_CH0 + 128], consts[:, C_CH1:C_CH1 + 128]]
                vtm = [cv.get([128, 256], BF16) for _ in range(4)]
                sg = [cv.get([128, TB]) for _ in range(2)]
                attms = [cv.get([128, 4, 128], BF16) for _ in range(2)]
                S32 = cv.get([128, 128])
                Sbfs = [[cv.get([128, 128], BF16) for _ in range(2)] for _ in range(2)]
                sqbs = [cv.get([128, TB], BF16) for _ in range(2)]
                rss = [cv.get([128, TB]) for _ in range(2)]
                t1s = [cv.get([128, TB]) for _ in range(2)]
                p.dma(("wost", 0), woutst[0][0:32, 0:256], walpha_d[l], writes=[("wost", 0)])
                p.op("pool", lambda e: e.tensor_copy(out=walb, in_=woutst[0][0:32, 0:256]), reads=[("wost", 0)], writes=["walb"])
                p.op("pool", lambda e: e.memset(rT, 1.0), writes=["rT"])
                m64b = consts[:, C_MASK:C_MASK + 128].unsqueeze(1).to_broadcast([128, 4, 128])
                for hp in range(2):
                    load_win(1024 + hp * 128, 128, 0)
                    load_win(1280 + hp * 128, 128, 128)
                    load_win(1536 + hp * 256, 256, 256)
                    load_win(2048 + hp * 256, 256, 512)
                    load_win(2560, 16, 768)
                    load_wout(4 + hp * 2, 2)
                    p.op("pool", lambda e: e.memset(S32, 0.0), writes=[("S32", 0), ("S32", 1)])
                    for hh in range(2):
                        for pr in range(2):
                            p.op("pool", lambda e, hh=hh, pr=pr: e.memset(Sbfs[hh][pr], 0.0), writes=[("Sbf", hh, pr)])
                    if hp == 0:
                        for hh in range(2):
                            p.op("pool", lambda e, hh=hh: e.memset(qdz[hh], 0.0), writes=[("g_qdz", hh)])
                        for tt in range(4):
                            for half in range(2):
                                p.op("pool", lambda e, tt=tt, half=half: e.memset(kendz[tt][half], 0.0), writes=[("g_kendz", tt, half)])
                    par_state = {0: 0, 1: 0}
                    for tb in range(NTB):
                        proj_fm(768, 16, tb, 0)
                        p.op("act", lambda e: e.copy(out=rT[0:16, :], in_=banks[0][0:16, :]), reads=[PS(0)], writes=["rT"])
                        for tt in range(4):
                            tsl = slice(tt * 128, (tt + 1) * 128)
                            p.op("pe", lambda e, tsl=tsl, hp=hp: e.matmul(banks[5][:, 0:128], lhsT=rT[:, tsl], rhs=walb[:, hp * 128:(hp + 1) * 128], start=True, stop=True), reads=["rT", "walb"], writes=[PS(5)])
                            p.op("act", lambda e: e.activation(out=e1, in_=banks[5][:, 0:128], func=AF.Exp, scale=-1.0), reads=[PS(5)], writes=["g_e1"])
                            p.op("act", lambda e: e.activation(out=l1, in_=e1, func=AF.Ln, bias=LNEPS[:, 2:3]), reads=["g_e1", "lneps"], writes=["g_l1"])
                            p.op("pe", lambda e: e.matmul(banks[5][:, 128:256], lhsT=l1, rhs=consts[:, C_MASKG:C_MASKG + 128], start=True, stop=True), reads=["g_l1", "consts"], writes=[PS(5)])
                            p.op("pe", lambda e: e.matmul(banks[5][:, 256:384], lhsT=consts[:, C_MASKG + 128:C_MASKG + 256], rhs=l1, start=True, stop=True), reads=["g_l1", "consts"], writes=[PS(5)])
                            p.op("act", lambda e, tsl=tsl: e.activation(out=ecp[:, tsl], in_=banks[5][:, 128:256], func=AF.Exp), reads=[PS(5)], writes=["g_ecp"])
                            p.op("act", lambda e, tsl=tsl: e.activation(out=ecn[:, tsl], in_=banks[5][:, 128:256], func=AF.Exp, scale=-1.0), reads=[PS(5)], writes=["g_ecn"])
                            p.op("act", lambda e, tt=tt: e.activation(out=esuf[tt], in_=banks[5][:, 256:384], func=AF.Exp), reads=[PS(5)], writes=[("g_esuf", tt)])
                        proj_fm(0, 128, tb, 0)
                        for hh in range(2):
                            p.op("dve", lambda e, hh=hh: e.scalar_tensor_tensor(out=qdz[hh][hh * 64:(hh + 1) * 64, :], in0=banks[0][hh * 64:(hh + 1) * 64, :], scalar=0.125, in1=ecp[hh * 64:(hh + 1) * 64, :], op0=ALU.mult, op1=ALU.mult),
                                 reads=[PS(0), "g_ecp"], writes=[("g_qdz", hh)])
                        proj_fm(128, 128, tb, 1)
                        p.op("dve", lambda e: e.tensor_tensor(out=kd, in0=banks[1][:, :], in1=ecn, op=ALU.mult), reads=[PS(1), "g_ecn"], writes=["g_kd"])
                        for tt in range(4):
                            proj_tm(128, 128, tb, tt, 0)
                            for half in range(2):
                                p.op("dve", lambda e, tt=tt, half=half: e.tensor_tensor(out=kendz[tt][half][half * 64:(half + 1) * 64, :], in0=banks[0][half * 64:(half + 1) * 64, 0:128], in1=esuf[tt][half * 64:(half + 1) * 64, :], op=ALU.mult),
                                     reads=[PS(0), ("g_esuf", tt)], writes=[("g_kendz", tt, half)])
                            proj_tm(256, 256, tb, tt, 1)
                            p.op("act", lambda e, tt=tt: e.copy(out=vtm[tt], in_=banks[1][:, 0:256]), reads=[PS(1)], writes=[("g_vtm", tt)])
                        for hh in range(2):
                            proj_fm(512 + hh * 128, 128, tb, hh)
                            p.op("act", lambda e, hh=hh: e.activation(out=sg[hh], in_=banks[hh][:, :], func=AF.Silu), reads=[PS(hh)], writes=[("g_sg", hh)])
                        def head_ops(tb, hh):
                            b0 = hh * 64
                            BATT, BO, BKV = (2, 5)[hh], (3, 6)[hh], (4, 7)[hh]
                            attm, sqb, rs, t1 = attms[hh], sqbs[hh], rss[hh], t1s[hh]
                            KH = lambda n: (n, hh)

                            def att(e):
                                ins = None
                                for tt in range(4):
                                    tsl = slice(tt * 128, (tt + 1) * 128)
                                    ins = e.matmul(banks[BATT][:, tsl], lhsT=kd[:, tsl], rhs=qdz[hh][:, tsl], start=True, stop=True)
                                return ins
                            p.op("pe", att, reads=["g_kd", ("g_qdz", hh)], writes=[PS(BATT)])
                            p.op("dve", lambda e: e.tensor_tensor(out=attm, in0=banks[BATT][:, :].rearrange("p (a b) -> p a b", a=4), in1=m64b, op=ALU.mult), reads=[PS(BATT), "consts"], writes=[KH("g_attm")])
                            for c in range(8):
                                tt, half = c // 2, c % 2
                                csl = slice(c * 64, (c + 1) * 64)
                                tsl = slice(tt * 128, (tt + 1) * 128)
                                cur_par = par_state[hh]
                                if half == 0:
                                    p.op("pe", lambda e, tt=tt, tsl=tsl: e.matmul(banks[BO][:, tsl], lhsT=vtm[tt][:, hh * 128:(hh + 1) * 128], rhs=attm[:, tt, :], start=True, stop=False),
                                         reads=[("g_vtm", tt), KH("g_attm")], writes=[PS(BO)])
                                p.op("pe", lambda e, csl=csl, cur_par=cur_par, half=half: e.matmul(banks[BO][:, csl], lhsT=Sbfs[hh][cur_par], rhs=qdz[hh][:, csl], start=False, stop=(half == 1)),
                                     reads=[("Sbf", hh, cur_par), ("g_qdz", hh)], writes=[PS(BO)])
                                p.op("pe", lambda e, tt=tt, half=half: e.matmul(banks[BKV][:, 0:128], lhsT=kendz[tt][half], rhs=vtm[tt][:, hh * 128:(hh + 1) * 128], start=True, stop=True),
                                     reads=[("g_kendz", tt, half), ("g_vtm", tt)], writes=[PS(BKV)])
                                col = c * 64 + 63
                                p.op("dve", lambda e, col=col: e.scalar_tensor_tensor(out=S32[b0:b0 + 64, :], in0=S32[b0:b0 + 64, :], scalar=ecp[b0:b0 + 64, col:col + 1], in1=banks[BKV][b0:b0 + 64, 0:128], op0=ALU.mult, op1=ALU.add),
                                     reads=[("S32", hh), "g_ecp", PS(BKV)], writes=[("S32", hh)])
                                nxt = 1 - cur_par
                                p.op("act", lambda e, nxt=nxt: e.copy(out=Sbfs[hh][nxt][b0:b0 + 64, :], in_=S32[b0:b0 + 64, :]), reads=[("S32", hh)], writes=[("Sbf", hh, nxt)])
                                par_state[hh] = nxt
                            p.op("act", lambda e: e.activation(out=sqb, in_=banks[BO][:, :], func=AF.Square), reads=[PS(BO)], writes=[KH("g_sqb")])
                            p.op("pe", lambda e: e.matmul(banks[BATT][:, :], lhsT=ones128b, rhs=sqb, start=True, stop=True), reads=["ones128b", KH("g_sqb")], writes=[PS(BATT)])
                            p.op("act", lambda e: e.activation(out=rs, in_=banks[BATT][:, :], func=AF.Ln, bias=LNEPS[:, 1:2]), reads=[PS(BATT), "lneps"], writes=[KH("g_rs")])
                            p.op("act", lambda e: e.activation(out=rs, in_=rs, func=AF.Exp, scale=-0.5), reads=[KH("g_rs")], writes=[KH("g_rs")])
                            p.op("dve", lambda e: e.tensor_tensor(out=t1, in0=banks[BO][:, :], in1=rs, op=ALU.mult), reads=[PS(BO), KH("g_rs")], writes=[KH("g_t1")])
                            p.op("dve", lambda e: e.scalar_tensor_tensor(out=yblk[:, hh, :], in0=t1, scalar=PPc(l, PP_GNORM), in1=sg[hh], op0=ALU.mult, op1=ALU.mult), reads=[KH("g_t1"), ("g_sg", hh), "pp"], writes=[("yblk", hh)])

                        replay_ops(interleave([cap_ops(head_ops, tb, 0), cap_ops(head_ops, tb, 1)]))
                        out_proj(tb, 2, [0, 1])

            if do_ssd:
                ssd_units(l, cv, mark, load_win, load_wout, proj_fm, proj_tm, out_proj, conv_silu, yblk, identb, ones512b)

        cv = Carve()
        ada_bufs = ada_bufs_alloc(cv)
        for blk in range(N_ADA):
            adaln_block(0, blk, ada_bufs, blk % 4)
        adaln_finish(0, 1)

        for tb in range(NTB):
            scale_alpha(tb, engs=("pool", "dve", "act"))
        for l in range(depth):
            p.new_epoch()
            for tb in range(NTB):
                modulate(l, 1, tb)
            barrier()
            if do_lru or do_gla or do_ssd:
                mixer(l)
            barrier()
            cv = Carve()
            if do_moe:
                rgroup, rfinish = router(l, cv, 2, 3)

                def after_tb(tb):
                    modulate(l, 2, tb)
                    rgroup(tb)
                layernorm(l, PP_LN1G, PP_LN1B, cv, 0, 1, after_tb=after_tb)
                rfinish()
            else:
                layernorm(l, PP_LN1G, PP_LN1B, cv, 0, 1, after_tb=lambda tb: modulate(l, 2, tb))
            barrier()
            cv = Carve()
            hooks = {}
            if l + 1 < depth:
                ada_bufs2 = ada_bufs_alloc(cv)
                for blk in range(N_ADA):
                    hooks[2 + blk] = (lambda blk=blk: adaln_block(l + 1, blk, ada_bufs2, 4 + blk % 4))
                hooks[2 + N_ADA] = (lambda: adaln_finish(l + 1, 4))
            if do_moe:
                moe(l, cv, hooks)
            else:
                for k in sorted(hooks):
                    hooks[k]()
            barrier()
            cv = Carve()
            layernorm(l, PP_LN2G, PP_LN2B, cv, 0, 1, final=(l == depth - 1))
            barrier()

        p.dma("out", yT_d.rearrange("(c p) t -> p c t", p=128), xT[:], reads=[("x", c, tb) for c in range(8) for tb in range(NTB)])
        p.emit()
    return nc


def prep_inputs(inputs):
    f = lambda a: np.ascontiguousarray(np.asarray(a, dtype=np.float32))
    L = DEPTH
    shared = {}
    shared["consts"] = build_consts()
    pc = lambda v, n: np.asarray(v, np.float32).reshape(n, 128).T
    pp = np.zeros((L, 128, NPP), np.float32)
    prow = np.zeros((L, NPR), np.float32)
    wabd = np.zeros((L, 4, 128, 128), np.float32)
    wxbd = np.zeros((L, 4, 128, 128), np.float32)
    wal = np.zeros((L, 32, 256), np.float32)
    for l in range(L):
        pp[l, :, PP_LN1G:PP_LN1G + 8] = pc(inputs["ln1_g"][l], 8)
        pp[l, :, PP_LN1B:PP_LN1B + 8] = pc(inputs["ln1_b"][l], 8)
        pp[l, :, PP_LN2G:PP_LN2G + 8] = pc(inputs["ln2_g"][l], 8)
        pp[l, :, PP_LN2B:PP_LN2B + 8] = pc(inputs["ln2_b"][l], 8)
        for m in range(4):
            for k in range(4):
                pp[l, :, PP_LCW + m * 4 + k] = inputs["lru_conv_w"][l, k, m * 128:(m + 1) * 128]
        pp[l, :, PP_LCB:PP_LCB + 4] = pc(inputs["lru_conv_b"][l], 4)
        pp[l, :, PP_LBA:PP_LBA + 4] = pc(inputs["lru_b_a"][l], 4)
        pp[l, :, PP_LBX:PP_LBX + 4] = pc(inputs["lru_b_x"][l], 4)
        pp[l, :, PP_LLAM:PP_LLAM + 4] = pc(inputs["lru_lambda"][l], 4)
        for j in range(12):
            for k in range(4):
                pp[l, :, PP_SCW + j * 4 + k] = inputs["ssd_conv_w"][l, k, j * 128:(j + 1) * 128]
        pp[l, :, PP_SCB:PP_SCB + 12] = pc(inputs["ssd_conv_b"][l], 12)
        pp[l, :, PP_SNORM:PP_SNORM + 8] = pc(inputs["ssd_norm"][l], 8)
        pp[l, :, PP_GNORM] = inputs["gla_norm"][l]
        pp[l, :, PP_SD:PP_SD + 8] = pc(np.repeat(np.asarray(inputs["ssd_d"][l]), 64), 8)
        prow[l, PR_DTB:PR_DTB + 16] = inputs["ssd_dt_bias"][l]
        prow[l, PR_ALOG:PR_ALOG + 16] = inputs["ssd_a_log"][l]
        prow[l, PR_RB:PR_RB + NE] = inputs["router_bias"][l]
        for m in range(4):
            for q in range(2):
                wabd[l, m, q * 64:(q + 1) * 64, q * 64:(q + 1) * 64] = inputs["lru_w_a"][l, 2 * m + q]
                wxbd[l, m, q * 64:(q + 1) * 64, q * 64:(q + 1) * 64] = inputs["lru_w_x"][l, 2 * m + q]
        wal[l, 0:16] = inputs["gla_w_alpha"][l]
        wal[l, 16] = inputs["gla_b_alpha"][l]
    shared.update(pp=pp, prow=prow, lru_wa_bd=wabd, lru_wx_bd=wxbd, walpha_ext=wal)
    for k in ("w_ada", "b_ada", "w_in", "w_out", "router_w", "exp_w1", "exp_w3", "exp_w2", "shared_w1", "shared_w3", "shared_w2"):
        shared[k] = f(inputs[k])
    x = np.asarray(inputs["x"], np.float32)
    c = np.asarray(inputs["c"], np.float32)
    maps = []
    for b in range(x.shape[0]):
        m = dict(shared)
        m["xT"] = np.ascontiguousarray(x[b].T)
        m["cpc"] = np.ascontiguousarray(c[b].reshape(8, 128).T)
        maps.append(m)
    return maps


_NC_CACHE = {}


def kernel(**inputs):
    maps = prep_inputs(inputs)
    if "nc" not in _NC_CACHE:
        _NC_CACHE["nc"] = build_program()
    nc = _NC_CACHE["nc"]
    res = run_bass_kernel_spmd(nc, maps, core_ids=list(range(len(maps))))
    out = np.stack([np.ascontiguousarray(r["yT"].T) for r in res.results], axis=0)
    return out.astype(np.float32)
```

```python
from contextlib import ExitStack
import numpy as np
import concourse.bass as bass
import concourse.mybir as mybir
from concourse.bass_utils import run_bass_kernel_spmd

F32 = mybir.dt.float32
BF16 = mybir.dt.bfloat16
AF = mybir.ActivationFunctionType
ALU = mybir.AluOpType
AX = mybir.AxisListType

D = 1024
S = 2048
DEPTH = 4
NE = 64
ALPHA = (2 * DEPTH) ** 0.25
DIN = 5152
NPP = 144
TB = 512
NTB = S // TB


class Prog:
    def __init__(self, nc, stack):
        self.nc = nc
        self.stack = stack
        self.ops = []
        self.last_write = {}
        self.readers = {}
        self.epoch = 0
        self.op_epoch = []
        self.group_open = {}
        self.group_of = {}
        self.nsem = 0
        self.bar = None
        self.xeng = []
        self.cap = None

    def barrier(self, fn):
        allk = list(set(self.last_write.keys()) | set(k for k, v in self.readers.items() if v))
        oid = self._add("dve", fn, allk, allk)
        self.bar = oid
        self.last_write = {}
        self.readers = {}

    def new_sem(self, name):
        self.nsem += 1
        return self.stack.enter_context(self.nc.semaphore(f"{name}_{self.nsem}"))

    def new_epoch(self):
        self.epoch += 1

    def _add(self, eng, fn, reads, writes, chan=None):
        oid = len(self.ops)
        raw = set()
        oth = set()
        xe = set()
        for k in reads:
            w = self.last_write.get(k)
            if w is not None:
                raw.add(w)
            if isinstance(k, tuple) and k[0] in ("ps", "ps2o"):
                for r in self.readers.get(k, ()):
                    xe.add(r)
        for k in writes:
            w = self.last_write.get(k)
            if w is not None:
                oth.add(w)
            for r in self.readers.get(k, ()):
                oth.add(r)
        if self.bar is not None:
            oth.add(self.bar)
        oth -= raw
        raw.discard(oid)
        oth.discard(oid)
        xe -= raw
        xe -= oth
        xe.discard(oid)
        self.xeng.append(sorted(xe))
        self.ops.append([eng, fn, sorted(raw), sorted(oth), chan])
        self.op_epoch.append(self.epoch)
        for k in reads:
            self.readers.setdefault(k, []).append(oid)
        for k in writes:
            self.last_write[k] = oid
            self.readers[k] = []
        return oid

    def op(self, eng, fn, reads=(), writes=()):
        if self.cap is not None:
            self.cap.append((eng, fn, list(reads), list(writes)))
            return None
        return self._add(eng, fn, reads, writes)

    def dma(self, chan, out, in_, reads=(), writes=(), eng="sp", more=False, **kw):
        def fn(e, out=out, in_=in_, kw=kw):
            return e.dma_start(out=out, in_=in_, **kw)
        oid = self._add(eng, fn, reads, writes, chan=chan)
        g = self.group_open.get(chan)
        if g is None:
            g = []
            self.group_open[chan] = g
        g.append(oid)
        self.group_of[oid] = g
        if not more:
            self.group_open[chan] = None
        return oid

    def emit(self):
        nc = self.nc
        n = len(self.ops)
        is_dma = [o[4] is not None for o in self.ops]
        need_sig = [False] * n
        deps_of = []
        for i, (eng, fn, raw, oth, chan) in enumerate(self.ops):
            deps = []
            for d in raw:
                if is_dma[d] or is_dma[i] or self.ops[d][0] != eng or eng != "pe":
                    deps.append(d)
            for d in oth:
                if is_dma[d] or is_dma[i] or self.ops[d][0] != eng or eng != "pe":
                    deps.append(d)
            for d in self.xeng[i]:
                if self.ops[d][0] != eng:
                    deps.append(d)
            deps_of.append(deps)
            for d in deps:
                if not is_dma[d]:
                    need_sig[d] = True
        sig = [None] * n
        cur = {}
        chan_sem = {}
        chan_cnt = {}
        for i, (eng, fn, raw, oth, chan) in enumerate(self.ops):
            if is_dma[i]:
                if chan not in chan_sem:
                    chan_sem[chan] = self.new_sem("d")
                    chan_cnt[chan] = 0
                chan_cnt[chan] += 16
                sig[i] = (chan_sem[chan], chan_cnt[chan])
            elif need_sig[i]:
                key = (eng, self.op_epoch[i])
                if key not in cur:
                    cur[key] = [self.new_sem(eng), 0]
                cur[key][1] += 1
                sig[i] = (cur[key][0], cur[key][1])
        for i in range(n):
            if is_dma[i]:
                last = self.group_of[i][-1]
                if last != i:
                    sig[i] = (sig[i][0], sig[last][1])
        streams = {}
        for i, (eng, fn, raw, oth, chan) in enumerate(self.ops):
            streams.setdefault(eng, []).append((i, fn, [sig[d] for d in deps_of[i]]))
        final_dma = [(chan_sem[c], chan_cnt[c]) for c in chan_sem]
        self.n_waits = 0

        def run_stream(e, items, tail):
            waited = {}
            for (i, fn, waits) in items:
                best = {}
                for (s, v) in waits:
                    k = s.num
                    if waited.get(k, 0) >= v:
                        continue
                    if k not in best or best[k][1] < v:
                        best[k] = (s, v)
                for k, (s, v) in best.items():
                    e.wait_ge(s, v)
                    waited[k] = v
                    self.n_waits += 1
                ins = fn(e)
                if is_dma[i]:
                    ins.then_inc(chan_sem[self.ops[i][4]], 16)
                elif sig[i] is not None:
                    ins.then_inc(sig[i][0], 1)
            for (s, v) in tail:
                e.wait_ge(s, v)

        with nc.Block() as block:
            names = {"pe": "tensor", "act": "scalar", "dve": "vector", "pool": "gpsimd", "sp": "sync"}
            for en, attr in names.items():
                items = streams.get(en, [])
                tail = final_dma if en == "sp" else []
                if not items and not tail:
                    continue

                def body(e, items=items, tail=tail):
                    run_stream(e, items, tail)
                getattr(block, attr)(body)


C_ID = 0
C_MASK = 128
C_SUF = 256
C_CH0 = 384
C_CH1 = 512
C_M64 = 640
C_ONESD = 704
C_ONE = 832
C_SEL = 960
C_MASKG = 1984
NCONST = 2240


def build_consts():
    c = np.zeros((128, NCONST), np.float32)
    idx = np.arange(128)
    same = (idx[:, None] // 64) == (idx[None, :] // 64)
    c[:, C_ID:C_ID + 128] = np.eye(128)
    c[:, C_MASK:C_MASK + 128] = same & (idx[:, None] <= idx[None, :])
    c[:, C_SUF:C_SUF + 128] = same & (idx[:, None] > idx[None, :])
    c[:64, C_CH0:C_CH0 + 128] = 1.0
    c[64:, C_CH1:C_CH1 + 128] = 1.0
    j = idx % 64
    c[:, C_M64:C_M64 + 64] = j[:, None] <= np.arange(64)[None, :]
    c[:, C_ONESD:C_ONESD + 128] = 1.0 / 1024.0
    c[:, C_ONE:C_ONE + 128] = 1.0
    for h in range(8):
        c[h, C_SEL + h * 128:C_SEL + (h + 1) * 128] = 1.0
    c[:, C_MASKG:C_MASKG + 128] = c[:, C_MASK:C_MASK + 128] * (-1.0 / 16.0)
    c[:, C_MASKG + 128:C_MASKG + 256] = c[:, C_SUF:C_SUF + 128] * (-1.0 / 16.0)
    return c


PP_LN1G, PP_LN1B, PP_LN2G, PP_LN2B = 0, 8, 16, 24
PP_LCW, PP_LCB, PP_LBA, PP_LBX, PP_LLAM = 32, 48, 52, 56, 60
PP_SCW, PP_SCB, PP_SNORM, PP_GNORM, PP_SD = 64, 112, 124, 132, 133
PR_DTB, PR_ALOG, PR_RB = 0, 16, 32
NPR = 96


def build_program(depth=DEPTH, do_lru=True, do_gla=True, do_ssd=True, do_moe=True, n_exp=NE + 1, debug=False):
    nc = bass.Bass("TRN2", target_bir_lowering=False, dynamic_dma_scratch_size=512)
    dr = {}

    def din(name, shape):
        dr[name] = nc.dram_tensor(name, list(shape), F32, kind="ExternalInput").ap()
        return dr[name]

    xT_d = din("xT", [D, S])
    cpc_d = din("cpc", [128, 8])
    consts_d = din("consts", [128, NCONST])
    pp_d = din("pp", [DEPTH, 128, NPP])
    prow_d = din("prow", [DEPTH, NPR])
    wada_d = din("w_ada", [DEPTH, D, 6 * D])
    bada_d = din("b_ada", [DEPTH, 6 * D])
    win_d = din("w_in", [DEPTH, D, DIN])
    wout_d = din("w_out", [DEPTH, 2 * D, D])
    wabd_d = din("lru_wa_bd", [DEPTH, 4, 128, 128])
    wxbd_d = din("lru_wx_bd", [DEPTH, 4, 128, 128])
    walpha_d = din("walpha_ext", [DEPTH, 32, 256])
    rw_d = din("router_w", [DEPTH, D, NE])
    ew1_d = din("exp_w1", [DEPTH, NE, D, 256])
    ew3_d = din("exp_w3", [DEPTH, NE, D, 256])
    ew2_d = din("exp_w2", [DEPTH, NE, 256, D])
    sw1_d = din("shared_w1", [DEPTH, D, 256])
    sw3_d = din("shared_w3", [DEPTH, D, 256])
    sw2_d = din("shared_w2", [DEPTH, 256, D])
    yT_d = nc.dram_tensor("yT", [D, S], F32, kind="ExternalOutput").ap()
    gscr_d = nc.dram_tensor("gscr", [2, NE, S], F32, kind="Internal").ap()
    dbg = {}

    with ExitStack() as st:
        p = Prog(nc, st)

        def sb(name, shape, dt=F32):
            return st.enter_context(nc.sbuf_tensor(name, list(shape), dt))

        def cap_ops(fn, *a):
            lst = []
            p.cap = lst
            r = fn(*a)
            p.cap = None
            return lst

        def replay_ops(lst):
            for (eng, fn, r, w) in lst:
                p.op(eng, fn, reads=r, writes=w)

        def interleave(lists):
            out = []
            n = max(len(x) for x in lists)
            for k in range(n):
                for x in lists:
                    if k < len(x):
                        out.append(x[k])
            return out

        xT = sb("xT_sb", [128, 8, S])
        hT = sb("hT_sb", [128, 8, S], BF16)
        consts = sb("consts_sb", [128, NCONST])
        pp = sb("pp_sb", [128, DEPTH, NPP])
        mod = sb("mod_sb", [128, DEPTH, 64])
        cond = sb("cond_sb", [128, 8])
        SCRW = 29150
        scr = sb("scr_sb", [128, SCRW])
        banks = [st.enter_context(nc.psum_tensor(f"bank{i}", [128, 512], F32)) for i in range(8)]

        def PS(i):
            return ("ps", i)

        class Carve:
            def __init__(self):
                self.off = 0

            def get(self, shape, dt=F32):
                n = int(np.prod(shape[1:]))
                words = n if dt == F32 else (n + 1) // 2
                a = scr[:, self.off:self.off + words]
                self.off += words
                assert self.off <= SCRW - 1, self.off
                if dt != F32:
                    a = a.bitcast(dt)
                    if n % 2:
                        a = a[:, 0:n]
                if len(shape) == 3:
                    a = a.rearrange("p (a b) -> p a b", a=shape[1])
                elif len(shape) == 4:
                    a = a.rearrange("p (a b c) -> p a b c", a=shape[1], b=shape[2])
                if shape[0] != 128:
                    a = a[0:shape[0]]
                return a

        ident = consts[:, C_ID:C_ID + 128]
        onesD = consts[:, C_ONESD:C_ONESD + 128]

        def barrier():
            tok = scr[:, SCRW - 1:SCRW]
            p.barrier(lambda e: e.memset(tok, 0.0))

        p.dma("ld0", consts[:], consts_d[:, :], writes=["consts"])
        p.dma("ld1", pp[:], pp_d.rearrange("l p n -> p l n"), writes=["pp"])
        p.dma("ld2", cond[:], cpc_d[:, :], writes=["cond"])
        p.dma("ldx", xT[:], xT_d.rearrange("(c p) t -> p c t", p=128), writes=[("x", c, tb) for c in range(8) for tb in range(NTB)])
        p.op("act", lambda e: e.activation(out=cond[:], in_=cond[:], func=AF.Silu), reads=["cond"], writes=["cond"])

        ADA_BLK = 256
        N_ADA = 6 * D // ADA_BLK
        ada_stg = [None, None]

        def adaln_block(l, blk, cv_bufs, bank):
            stg, brow, mrow = cv_bufs[blk % 2]
            key = ("adastg", blk % 2)
            c0 = blk * ADA_BLK
            nj = ADA_BLK // 128
            p.dma(("adab", blk % 2), brow, bada_d[l:l + 1, c0:c0 + ADA_BLK], writes=[("adabrow", blk % 2)])
            p.dma(("ada", blk % 2), stg, wada_d[l].rearrange("(kc p) f -> p kc f", p=128)[:, :, c0:c0 + ADA_BLK], writes=[key])

            def mm(e, stg=stg):
                ins = None
                for kc in range(8):
                    ins = e.matmul(banks[bank][0:1, 0:ADA_BLK], lhsT=cond[:, kc:kc + 1], rhs=stg[:, kc, :], start=(kc == 0), stop=(kc == 7))
                return ins
            p.op("pe", mm, reads=[key, "cond"], writes=[PS(bank)])
            p.op("dve", lambda e: e.tensor_tensor(out=mrow, in0=banks[bank][0:1, 0:ADA_BLK], in1=brow, op=ALU.add),
                 reads=[PS(bank), ("adabrow", blk % 2)], writes=[("adamrow", blk % 2)])

            def mm2(e):
                ins = None
                for j in range(nj):
                    ins = e.matmul(banks[bank][:, 256 + j:257 + j], lhsT=mrow[0:1, j * 128:(j + 1) * 128], rhs=consts[0:1, C_ONE:C_ONE + 1], start=True, stop=True)
                return ins
            p.op("pe", mm2, reads=[("adamrow", blk % 2), "consts"], writes=[PS(bank)])
            p.op("act", lambda e: e.copy(out=mod[:, l, blk * nj:(blk + 1) * nj], in_=banks[bank][:, 256:256 + nj]), reads=[PS(bank)], writes=[("mod", l)])

        def adaln_finish(l, bank):
            p.op("dve", lambda e: e.tensor_scalar(out=mod[:, l, 48:56], in0=mod[:, l, 8:16], scalar1=1.0, scalar2=1.0 / float(ALPHA), op0=ALU.add, op1=ALU.mult), reads=[("mod", l)], writes=[("modd", l)])
            p.op("dve", lambda e: e.tensor_scalar(out=mod[:, l, 56:64], in0=mod[:, l, 32:40], scalar1=1.0, scalar2=1.0 / float(ALPHA), op0=ALU.add, op1=ALU.mult), reads=[("mod", l)], writes=[("modd2", l)])

        def ada_bufs_alloc(cv):
            return [(cv.get([128, 8, ADA_BLK]), cv.get([1, ADA_BLK]), cv.get([1, ADA_BLK])) for _ in range(2)]

        def MOD(l, j, c):
            if j < 6:
                return mod[:, l, j * 8 + c:j * 8 + c + 1]
            return mod[:, l, 48 + (j - 6) * 8 + c:48 + (j - 6) * 8 + c + 1]

        def PPc(l, col):
            return pp[:, l, col:col + 1]

        modkeys = lambda l: [("mod", l), ("modd", l), ("modd2", l)]

        def modulate(l, which, tb, engs=("dve", "pool")):
            jsc, jsh = (6, 0) if which == 1 else (7, 3)
            for c in range(8):
                eng = engs[c % len(engs)]
                p.op(eng, lambda e, c=c: e.tensor_scalar(out=hT[:, c, tb * TB:(tb + 1) * TB], in0=xT[:, c, tb * TB:(tb + 1) * TB],
                                                         scalar1=MOD(l, jsc, c), scalar2=MOD(l, jsh, c), op0=ALU.mult, op1=ALU.add),
                     reads=[("x", c, tb)] + modkeys(l), writes=[("h", c, tb)])

        def scale_alpha(tb, engs=("pool",)):
            for c in range(8):
                eng = engs[c % len(engs)]
                if eng == "act":
                    p.op(eng, lambda e, c=c: e.mul(out=xT[:, c, tb * TB:(tb + 1) * TB], in_=xT[:, c, tb * TB:(tb + 1) * TB], mul=float(ALPHA)),
                         reads=[("x", c, tb)], writes=[("x", c, tb)])
                else:
                    p.op(eng, lambda e, c=c: e.tensor_scalar_mul(out=xT[:, c, tb * TB:(tb + 1) * TB], in0=xT[:, c, tb * TB:(tb + 1) * TB], scalar1=float(ALPHA)),
                         reads=[("x", c, tb)], writes=[("x", c, tb)])

        def layernorm(l, gcol, bcol, cv, bank_m, bank_q, final=False, after_tb=None):
            def GB(col):
                return pp[:, l, col:col + 1] if final else ppA[:, l, col:col + 1]
            sq = [cv.get([128, TB]) for _ in range(2)]
            mean_sbs = [cv.get([128, TB]) for _ in range(2)]
            rstds = [cv.get([128, TB]) for _ in range(2)]
            tmp = [cv.get([128, TB]) for _ in range(2)]
            tmp2 = [cv.get([128, TB]) for _ in range(2)]
            bank_m0, bank_q0 = bank_m, bank_q
            for tb in range(NTB):
                sl = slice(tb * TB, (tb + 1) * TB)
                mean_sb = mean_sbs[tb % 2]
                rstd = rstds[tb % 2]
                bank_m = bank_m0 + 2 * (tb % 2)
                bank_q = bank_q0 + 2 * (tb % 2)
                KM = ("lnmean", tb % 2)
                KR = ("lnrstd", tb % 2)

                def mm_mean(e, sl=sl, bank_m=bank_m):
                    ins = None
                    for c in range(8):
                        ins = e.matmul(banks[bank_m][:, :], lhsT=onesD, rhs=xT[:, c, sl], start=(c == 0), stop=(c == 7))
                    return ins
                p.op("pe", mm_mean, reads=[("x", c, tb) for c in range(8)] + ["consts"], writes=[PS(bank_m)])
                for c in range(8):
                    p.op("act", lambda e, c=c, sl=sl: e.activation(out=sq[c % 2], in_=xT[:, c, sl], func=AF.Square), reads=[("x", c, tb)], writes=[("lnsq", c % 2)])
                    p.op("pe", lambda e, c=c, bank_q=bank_q: e.matmul(banks[bank_q][:, :], lhsT=onesD, rhs=sq[c % 2], start=(c == 0), stop=(c == 7)),
                         reads=[("lnsq", c % 2), "consts"], writes=[PS(bank_q)])
                p.op("act", lambda e, mean_sb=mean_sb, bank_m=bank_m: e.copy(out=mean_sb, in_=banks[bank_m][:, :]), reads=[PS(bank_m)], writes=[KM])
                p.op("dve", lambda e, rstd=rstd, mean_sb=mean_sb: e.tensor_tensor(out=rstd, in0=mean_sb, in1=mean_sb, op=ALU.mult), reads=[KM], writes=[KR])
                p.op("dve", lambda e, rstd=rstd, bank_q=bank_q: e.tensor_tensor(out=rstd, in0=banks[bank_q][:, :], in1=rstd, op=ALU.subtract), reads=[PS(bank_q), KR], writes=[KR])
                p.op("act", lambda e, rstd=rstd: e.activation(out=rstd, in_=rstd, func=AF.Ln, bias=LNEPS[:, 0:1]), reads=[KR, "lneps"], writes=[KR])
                p.op("act", lambda e, rstd=rstd: e.activation(out=rstd, in_=rstd, func=AF.Exp, scale=-0.5), reads=[KR], writes=[KR])
                for c in range(8):
                    k = c % 2
                    p.op("dve", lambda e, c=c, k=k, sl=sl, mean_sb=mean_sb: e.tensor_tensor(out=tmp[k], in0=xT[:, c, sl], in1=mean_sb, op=ALU.subtract),
                         reads=[("x", c, tb), KM], writes=[("lnt", k)])
                    p.op("pool", lambda e, k=k, rstd=rstd: e.tensor_tensor(out=tmp2[k], in0=tmp[k], in1=rstd, op=ALU.mult), reads=[("lnt", k), KR], writes=[("lnt2", k)])
                    p.op("act", lambda e, c=c, k=k, sl=sl: e.activation(out=xT[:, c, sl], in_=tmp2[k], func=AF.Identity, scale=GB(gcol + c), bias=GB(bcol + c)),
                         reads=[("lnt2", k), "pp", "ppA"], writes=[("x", c, tb)])
                if after_tb is not None:
                    after_tb(tb)

        ppA = sb("ppA_sb", [128, DEPTH, 32])
        p.op("dve", lambda e: e.tensor_scalar_mul(out=ppA[:], in0=pp[:, :, 0:32], scalar1=float(ALPHA)), reads=["pp"], writes=["ppA"])
        LNEPS = sb("lneps_sb", [128, 4])
        p.op("pool", lambda e: e.memset(LNEPS[:, 0:1], 1e-5), writes=["lneps"])
        p.op("pool", lambda e: e.memset(LNEPS[:, 1:2], 1e-6), reads=[], writes=["lneps"])
        p.op("pool", lambda e: e.memset(LNEPS[:, 2:3], 1.0), reads=[], writes=["lneps"])

        def router(l, cv, bank_l, bank_t):
            NI = 4
            rw = cv.get([128, 8, NE])
            rb = cv.get([128, NE])
            gT = cv.get([64, S])
            h32s = [cv.get([128, 8, 128]) for _ in range(NI)]
            Ws = [{n: cv.get([128, 64]) for n in ("sc", "bi", "eq", "b2", "mk", "sel", "gw", "gates")} for _ in range(NI)]
            Sms = [{n: cv.get([128, 8]) for n in ("m1", "m2", "gs", "t8", "gsel", "goff", "t8e")} for _ in range(NI)]
            s1s = [cv.get([128, 2]) for _ in range(NI)]
            p.dma("rw", rw, rw_d[l].rearrange("(kc p) e -> p kc e", p=128), writes=["rw"])
            p.dma("rb", rb, prow_d[l:l + 1, PR_RB:PR_RB + NE].partition_broadcast(128), writes=["rb"])
            g3 = lambda a: a.rearrange("p (g k) -> p g k", k=8)
            b3 = lambda a: a.unsqueeze(2).to_broadcast([128, 8, 8])

            def tile_ops(tt):
                j = tt % NI
                tb = tt // 4
                tsl = slice(tt * 128, (tt + 1) * 128)
                h32, W, Sm, s1 = h32s[j], Ws[j], Sms[j], s1s[j]
                bl, bt = 4 + j, 4 + j
                K = lambda n: (n, j)
                ops = []
                A = lambda eng, fn, r, w: ops.append((eng, fn, r, w))
                for c in range(8):
                    eng = ("dve", "pool")[c % 2]
                    A(eng, lambda e, c=c: e.tensor_scalar(out=h32[:, c, :], in0=xT[:, c, tsl], scalar1=MOD(l, 7, c), scalar2=MOD(l, 3, c), op0=ALU.mult, op1=ALU.add),
                      [("x", c, tb)] + modkeys(l), [("h32", j, c)])

                def mm(e):
                    ins = None
                    for c in range(8):
                        ins = e.matmul(banks[bl][:, 0:NE], lhsT=h32[:, c, :], rhs=rw[:, c, :], start=(c == 0), stop=(c == 7))
                    return ins
                A("pe", mm, [("h32", j, c) for c in range(8)] + ["rw"], [PS(bl)])
                A("act", lambda e: e.activation(out=W["sc"], in_=banks[bl][:, 0:NE], func=AF.Sigmoid), [PS(bl)], [K("r_sc")])
                V = lambda fn, r, w: A("dve", fn, r, w)
                V(lambda e: e.tensor_tensor(out=W["bi"], in0=W["sc"], in1=rb, op=ALU.add), [K("r_sc"), "rb"], [K("r_bi")])
                V(lambda e: e.tensor_reduce(out=Sm["m1"], in_=g3(W["bi"]), axis=AX.X, op=ALU.max), [K("r_bi")], [K("r_m1")])
                V(lambda e: e.tensor_tensor(out=g3(W["eq"]), in0=g3(W["bi"]), in1=b3(Sm["m1"]), op=ALU.is_equal), [K("r_bi"), K("r_m1")], [K("r_eq")])
                V(lambda e: e.scalar_tensor_tensor(out=W["b2"], in0=W["eq"], scalar=-10.0, in1=W["bi"], op0=ALU.mult, op1=ALU.add), [K("r_eq"), K("r_bi")], [K("r_b2")])
                V(lambda e: e.tensor_reduce(out=Sm["m2"], in_=g3(W["b2"]), axis=AX.X, op=ALU.max), [K("r_b2")], [K("r_m2")])
                V(lambda e: e.tensor_tensor(out=Sm["gs"], in0=Sm["m1"], in1=Sm["m2"], op=ALU.add), [K("r_m1"), K("r_m2")], [K("r_gs")])
                V(lambda e: e.max(out=Sm["t8"], in_=Sm["gs"]), [K("r_gs")], [K("r_t8")])
                V(lambda e: e.tensor_scalar(out=Sm["gsel"], in0=Sm["gs"], scalar1=Sm["t8"][:, 3:4], scalar2=None, op0=ALU.is_ge), [K("r_gs"), K("r_t8")], [K("r_gsel")])
                V(lambda e: e.tensor_scalar(out=Sm["goff"], in0=Sm["gsel"], scalar1=10.0, scalar2=-10.0, op0=ALU.mult, op1=ALU.add), [K("r_gsel")], [K("r_goff")])
                V(lambda e: e.tensor_tensor(out=g3(W["mk"]), in0=g3(W["bi"]), in1=b3(Sm["gsel"]), op=ALU.mult), [K("r_bi"), K("r_gsel")], [K("r_mk")])
                V(lambda e: e.tensor_tensor(out=g3(W["mk"]), in0=g3(W["mk"]), in1=b3(Sm["goff"]), op=ALU.add), [K("r_mk"), K("r_goff")], [K("r_mk")])
                V(lambda e: e.max(out=Sm["t8e"], in_=W["mk"]), [K("r_mk")], [K("r_t8e")])
                V(lambda e: e.tensor_scalar(out=W["sel"], in0=W["mk"], scalar1=Sm["t8e"][:, 7:8], scalar2=None, op0=ALU.is_ge), [K("r_mk"), K("r_t8e")], [K("r_sel")])
                V(lambda e: e.tensor_tensor(out=W["gw"], in0=W["sel"], in1=W["sc"], op=ALU.mult), [K("r_sel"), K("r_sc")], [K("r_gw")])
                V(lambda e: e.tensor_reduce(out=s1[:, 0:1], in_=W["gw"], axis=AX.X, op=ALU.add), [K("r_gw")], [K("r_s1")])
                V(lambda e: e.reciprocal(out=s1[:, 1:2], in_=s1[:, 0:1]), [K("r_s1")], [K("r_s2")])
                V(lambda e: e.tensor_scalar(out=W["gates"], in0=W["gw"], scalar1=s1[:, 1:2], scalar2=2.5, op0=ALU.mult, op1=ALU.mult), [K("r_gw"), K("r_s2")], [K("r_gates")])
                A("pe", lambda e: e.transpose(banks[bt][0:64, 0:128], W["gates"], ident), [K("r_gates"), "consts"], [PS(bt)])
                A("act", lambda e: e.copy(out=gT[:, tsl], in_=banks[bt][0:64, 0:128]), [PS(bt)], [("gT", tt)])
                return ops

            def group(tb):
                g0 = tb * NI
                lists = [tile_ops(tt) for tt in range(g0, g0 + NI)]
                for k in range(len(lists[0])):
                    for ol in lists:
                        eng, fn, r, w = ol[k]
                        p.op(eng, fn, reads=r, writes=w)

            def finish():
                p.dma("gst", gscr_d[l % 2], gT, reads=[("gT", tt) for tt in range(S // 128)], writes=[("gscr", l % 2)])
            return group, finish

        def moe(l, cv, hooks):
            stg = {n: cv.get([128, 8, 256]) for n in ("w1", "w3")}
            stg["w2"] = cv.get([128, 2, D])
            wbf = [{"w1": cv.get([128, 8, 256], BF16), "w3": cv.get([128, 8, 256], BF16), "w2": cv.get([128, 2, D], BF16)} for _ in range(2)]
            gbc = [cv.get([128, S]) for _ in range(2)]
            sS = [[cv.get([128, TB], BF16) for f in range(2)] for _ in range(2)]
            tS = [[cv.get([128, TB], BF16) for f in range(2)] for _ in range(2)]
            hid = [[cv.get([128, TB], BF16) for f in range(2)] for _ in range(2)]
            steps = [(e, tb) for e in range(n_exp) for tb in range(NTB)]

            def load(e):
                sl = e % 2
                if e < NE:
                    srcs = {"w1": ew1_d[l, e], "w3": ew3_d[l, e], "w2": ew2_d[l, e]}
                else:
                    srcs = {"w1": sw1_d[l], "w3": sw3_d[l], "w2": sw2_d[l]}
                for n in ("w1", "w3", "w2"):
                    pat = "(kc p) f -> p kc f"
                    p.dma(("wst", n), stg[n], srcs[n].rearrange(pat, p=128), writes=[("stg", n)])
                if e < NE:
                    p.dma(("gbc", sl), gbc[sl], gscr_d[l % 2, e:e + 1, :].partition_broadcast(128), reads=[("gscr", l % 2)], writes=[("gbc", sl)])

            def cast(e):
                sl = e % 2
                for n in ("w1", "w3", "w2"):
                    if n == "w2":
                        parts = [(slice(0, 1), "act"), (slice(1, 2), "pool")]
                    else:
                        parts = [(slice(0, 3), "act"), (slice(3, 8), "pool")]
                    for (ps_, ce) in parts:
                        if ce == "act":
                            p.op("act", lambda e_, n=n, sl=sl, ps_=ps_: e_.copy(out=wbf[sl][n][:, ps_, :], in_=stg[n][:, ps_, :]), reads=[("stg", n)], writes=[("wbf", sl, n, ce)])
                        else:
                            p.op("pool", lambda e_, n=n, sl=sl, ps_=ps_: e_.tensor_copy(out=wbf[sl][n][:, ps_, :], in_=stg[n][:, ps_, :]), reads=[("stg", n)], writes=[("wbf", sl, n, ce)])

            def up(i, f):
                e, tb = steps[i]
                sl = e % 2
                for wi, n in enumerate(("w1", "w3")):
                    bk = f * 2 + wi

                    def mm(e_, n=n, bk=bk, sl=sl, tb=tb, f=f):
                        ins = None
                        for kc in range(8):
                            ins = e_.matmul(banks[bk][:, :], lhsT=wbf[sl][n][:, kc, f * 128:(f + 1) * 128], rhs=hT[:, kc, tb * TB:(tb + 1) * TB], start=(kc == 0), stop=(kc == 7))
                        return ins
                    p.op("pe", mm, reads=[("wbf", sl, n, "act"), ("wbf", sl, n, "pool")] + [("h", kc, tb) for kc in range(8)], writes=[PS(bk)])

            def gating(i, f):
                e, tb = steps[i]
                sl = e % 2
                par = i % 2
                p.op("act", lambda e_: e_.activation(out=sS[par][f], in_=banks[f * 2][:, :], func=AF.Silu), reads=[PS(f * 2)], writes=[("sS", par, f)])
                if e < NE:
                    p.op("dve", lambda e_: e_.tensor_tensor(out=tS[par][f], in0=banks[f * 2 + 1][:, :], in1=gbc[sl][:, tb * TB:(tb + 1) * TB], op=ALU.mult),
                         reads=[PS(f * 2 + 1), ("gbc", sl)], writes=[("tS", par, f)])
                    p.op("dve", lambda e_: e_.tensor_tensor(out=hid[par][f], in0=sS[par][f], in1=tS[par][f], op=ALU.mult),
                         reads=[("sS", par, f), ("tS", par, f)], writes=[("hid", par, f)])
                else:
                    p.op("dve", lambda e_: e_.tensor_tensor(out=hid[par][f], in0=banks[f * 2 + 1][:, :], in1=sS[par][f], op=ALU.mult),
                         reads=[PS(f * 2 + 1), ("sS", par, f)], writes=[("hid", par, f)])

            def down(i, dh):
                e, tb = steps[i]
                sl = e % 2
                par = i % 2
                for dq in range(4):
                    d = dh * 4 + dq
                    bk = 4 + dq

                    def mm(e_, d=d, bk=bk):
                        ins = None
                        for f in range(2):
                            ins = e_.matmul(banks[bk][:, :], lhsT=wbf[sl]["w2"][:, f, d * 128:(d + 1) * 128], rhs=hid[par][f], start=(f == 0), stop=(f == 1))
                        return ins
                    p.op("pe", mm, reads=[("wbf", sl, "w2", "act"), ("wbf", sl, "w2", "pool"), ("hid", par, 0), ("hid", par, 1)], writes=[PS(bk)])
                    p.op("dve", lambda e_, d=d, bk=bk: e_.scalar_tensor_tensor(out=xT[:, d, tb * TB:(tb + 1) * TB], in0=banks[bk][:, :], scalar=MOD(l, 5, d), in1=xT[:, d, tb * TB:(tb + 1) * TB], op0=ALU.mult, op1=ALU.add),
                         reads=[PS(bk), ("x", d, tb)] + modkeys(l), writes=[("x", d, tb)])

            load(0)
            cast(0)
            if n_exp > 1:
                load(1)
                cast(1)
            up(0, 0)
            up(0, 1)
            gating(0, 0)
            gating(0, 1)
            for i in range(len(steps)):
                e, tb = steps[i]
                if tb == 0 and i > 0 and e + 1 < n_exp:
                    load(e + 1)
                if tb == 2 and e > 0 and e + 1 < n_exp:
                    cast(e + 1)
                if tb == 1 and e in hooks:
                    hooks[e]()
                nxt = i + 1 < len(steps)
                if nxt:
                    up(i + 1, 0)
                down(i, 0)
                if nxt:
                    gating(i + 1, 0)
                    up(i + 1, 1)
                down(i, 1)
                if nxt:
                    gating(i + 1, 1)


        def ssd_units(l, cv, mark, load_win, load_wout, proj_fm, proj_tm, out_proj, conv_silu, yblk, identb, ones512b):
            cv.off = mark
            dtb = cv.get([128, 16]); alog = cv.get([128, 16]); aneg = cv.get([128, 16])
            cbuf = [cv.get([128, 3 + TB]) for _ in range(6)]
            ctmps = [cv.get([128, TB]) for _ in range(3)]
            ctmp = ctmps[0]
            xs = [cv.get([128, TB], BF16) for _ in range(4)]
            BT = cv.get([128, TB], BF16); CT = cv.get([128, TB], BF16)
            sz = [cv.get([128, TB], BF16) for _ in range(4)]
            yg = cv.get([128, 4, TB])
            dt_tm = cv.get([128, 8]); dA = cv.get([128, 8]); acs = cv.get([128, 8]); dte = cv.get([128, 8])
            w2 = cv.get([128, 8]); draw = cv.get([128, 8]); ex = cv.get([128, 8])
            dAb = cv.get([128, 8, 128])
            L = dAb
            Btmzs = [[cv.get([128, 128], BF16) for _ in range(2)] for _ in range(2)]
            decbcs = [cv.get([128, 2, 8]) for _ in range(2)]
            eD = cv.get([128, 8, 128], BF16)
            cbm = cv.get([128, 128])
            MTs = [cv.get([128, 8, 128], BF16) for _ in range(2)]
            CTss = [cv.get([128, 8, 128], BF16) for _ in range(2)]
            xdts = [cv.get([128, 8, 64], BF16) for _ in range(2)]
            xws = [cv.get([128, 8, 64], BF16) for _ in range(2)]
            pa_ctr = [0]
            Btm = cv.get([128, 128], BF16)
            S32 = cv.get([128, 8, 64])
            Sbf = [cv.get([128, 8, 64], BF16) for _ in range(2)]
            sqb = cv.get([128, TB], BF16); rs = ctmp
            b7 = banks[7][:, :].bitcast(BF16)
            MASK = consts[:, C_MASK:C_MASK + 128]
            SUF = consts[:, C_SUF:C_SUF + 128]
            p.dma("dtb", dtb, prow_d[l:l + 1, PR_DTB:PR_DTB + 16].partition_broadcast(128), writes=["dtb"])
            p.dma("alog", alog, prow_d[l:l + 1, PR_ALOG:PR_ALOG + 16].partition_broadcast(128), writes=["alog"])
            p.op("act", lambda e: e.activation(out=aneg, in_=alog, func=AF.Exp), reads=["alog"], writes=["aneg"])
            p.op("dve", lambda e: e.tensor_scalar_mul(out=aneg, in0=aneg, scalar1=-1.0), reads=["aneg"], writes=["aneg"])
            for g in range(2):
                load_win(3600 + g * 512, 512, 0)
                load_win(2576 + g * 512, 512, 512)
                load_win(4624 + g * 128, 128, 1024)
                load_win(4880 + g * 128, 128, 1152)
                load_win(5136 + g * 8, 8, 1280)
                load_wout(8 + g * 4, 4)
                for j6 in range(6):
                    p.op("pool", lambda e, j6=j6: e.memset(cbuf[j6][:, 0:3], 0.0), writes=[("cbuf", j6)])
                p.op("pool", lambda e: e.memset(S32, 0.0), writes=["s_S32"])
                p.op("pool", lambda e: e.memset(Sbf[0], 0.0), writes=[("s_Sbf", 0)])
                for pa in range(2):
                    for half in range(2):
                        p.op("pool", lambda e, half=half, pa=pa: e.memset(Btmzs[pa][half], 0.0), writes=[("s_Btmz", pa, half)])
                par = 0
                gs = slice(g * 8, g * 8 + 8)
                for tb in range(NTB):
                    def conv_chain(j6):
                        off = j6 * 128 if j6 < 4 else (1024 if j6 == 4 else 1152)
                        jc = g * 4 + j6 if j6 < 4 else (8 + g if j6 == 4 else 10 + g)
                        bank = j6
                        proj_fm(off, 128, tb, bank)
                        dst = xs[j6] if j6 < 4 else (BT if j6 == 4 else CT)
                        conv_silu(cbuf[j6], ("cbuf", j6), tb, bank, PP_SCW + jc * 4, PP_SCB + jc, ctmps[j6 % 3], ("s_ctmp", j6 % 3), dst, ("s_fm", j6), AF.Silu, eng="dve")

                    def z_chain(q):
                        bank = 6 + q % 2
                        proj_fm(512 + q * 128, 128, tb, bank)
                        p.op("act", lambda e: e.activation(out=sz[q], in_=banks[bank][:, :], func=AF.Silu), reads=[PS(bank)], writes=[("s_sz", q)])

                    replay_ops(interleave([cap_ops(conv_chain, 0), cap_ops(conv_chain, 1), cap_ops(conv_chain, 2), cap_ops(z_chain, 0), cap_ops(z_chain, 1)]))
                    replay_ops(interleave([cap_ops(conv_chain, 3), cap_ops(conv_chain, 4), cap_ops(conv_chain, 5), cap_ops(z_chain, 2), cap_ops(z_chain, 3)]))
                    def _aliases(pa):
                        return MTs[pa], CTss[pa], xdts[pa], xws[pa], Btmzs[pa], decbcs[pa]

                    def stageA(tt, pa):
                        MT, CTs, xdt, xw, Btmz, decbc = _aliases(pa)
                        KP = lambda n: (n, pa)
                        tsl = slice(tt * 128, (tt + 1) * 128)
                        proj_tm(1280, 8, tb, tt, 5)
                        p.op("dve", lambda e, gs=gs: e.tensor_tensor(out=draw, in0=banks[5][:, 0:8], in1=dtb[:, gs], op=ALU.add), reads=[PS(5), "dtb"], writes=["s_draw"])
                        p.op("act", lambda e: e.activation(out=ex, in_=draw, func=AF.Exp), reads=["s_draw"], writes=["s_ex"])
                        p.op("act", lambda e: e.activation(out=dt_tm, in_=ex, func=AF.Ln, bias=LNEPS[:, 2:3]), reads=["s_ex", "lneps"], writes=["s_dt"])
                        p.op("dve", lambda e, gs=gs: e.tensor_tensor(out=dA, in0=dt_tm, in1=aneg[:, gs], op=ALU.mult), reads=["s_dt", "aneg"], writes=["s_dA"])

                        def mm5(e):
                            e.matmul(banks[5][:, 8:16], lhsT=MASK, rhs=dA, start=True, stop=True)
                            e.matmul(banks[5][:, 16:24], lhsT=SUF, rhs=dA, start=True, stop=True)
                            e.matmul(banks[5][:, 160:168], lhsT=consts[:, C_CH0:C_CH0 + 128], rhs=dA, start=True, stop=True)
                            return e.matmul(banks[5][:, 168:176], lhsT=consts[:, C_CH1:C_CH1 + 128], rhs=dA, start=True, stop=True)
                        p.op("pe", mm5, reads=["s_dA", "consts"], writes=[PS(5)])
                        p.op("act", lambda e: e.copy(out=acs, in_=banks[5][:, 8:16]), reads=[PS(5)], writes=["s_acs"])
                        p.op("act", lambda e: e.activation(out=dte, in_=banks[5][:, 16:24], func=AF.Exp), reads=[PS(5)], writes=["s_dte"])
                        p.op("act", lambda e: e.copy(out=dAb, in_=dA.unsqueeze(2).to_broadcast([128, 8, 128])), reads=["s_dA"], writes=["s_dAb", ("s_L", 0), ("s_L", 1), "s_Lm", "s_Le"])
                        p.op("act", lambda e: e.activation(out=decbc, in_=banks[5][:, 160:176].rearrange("p (a b) -> p a b", a=2), func=AF.Exp), reads=[PS(5)], writes=[KP("s_dec")])
                        p.op("dve", lambda e: e.tensor_tensor(out=w2, in0=dt_tm, in1=dte, op=ALU.mult), reads=["s_dt", "s_dte"], writes=["s_w2"])

                        def mmD(e):
                            ins = None
                            for h in range(8):
                                ins = e.matmul(banks[2 + h // 4][:, (h % 4) * 128:(h % 4 + 1) * 128], lhsT=dAb[:, h, :], rhs=MASK, start=True, stop=True)
                            return ins
                        p.op("pe", mmD, reads=["s_dAb", "consts"], writes=[PS(2), PS(3)])
                        for k in range(2):
                            p.op("dve", lambda e, k=k: e.tensor_tensor(out=L[:, 4 * k:4 * k + 4, :], in0=banks[2 + k][:, :].rearrange("p (a b) -> p a b", a=4),
                                                                       in1=acs[:, 4 * k:4 * k + 4].unsqueeze(2).to_broadcast([128, 4, 128]), op=ALU.subtract),
                                 reads=[PS(2 + k), "s_acs"], writes=[("s_L", k)])
                            p.op("act", lambda e, k=k: e.activation(out=eD[:, 4 * k:4 * k + 4, :], in_=banks[2 + k][:, :].rearrange("p (a b) -> p a b", a=4), func=AF.Exp), reads=[PS(2 + k)], writes=[("s_eD", k)])
                        p.op("dve", lambda e: e.tensor_scalar_min(out=L, in0=L, scalar1=0.0), reads=[("s_L", 0), ("s_L", 1)], writes=["s_Lm"])
                        p.op("act", lambda e: e.activation(out=L, in_=L, func=AF.Exp), reads=["s_Lm"], writes=["s_Le"])
                        p.op("pe", lambda e, tsl=tsl: e.matmul(banks[5][:, 256:384], lhsT=BT[:, tsl], rhs=CT[:, tsl], start=True, stop=True), reads=[("s_fm", 4), ("s_fm", 5)], writes=[PS(5)])
                        p.op("dve", lambda e: e.tensor_tensor(out=cbm, in0=banks[5][:, 256:384], in1=MASK, op=ALU.mult), reads=[PS(5), "consts"], writes=["s_cbm"])
                        p.op("dve", lambda e: e.tensor_tensor(out=MT, in0=L, in1=cbm.unsqueeze(1).to_broadcast([128, 8, 128]), op=ALU.mult), reads=["s_Le", "s_cbm"], writes=[KP("s_MT")])
                        p.op("dve", lambda e, tsl=tsl: e.tensor_tensor(out=CTs, in0=eD, in1=CT[:, tsl].unsqueeze(1).to_broadcast([128, 8, 128]), op=ALU.mult), reads=[("s_eD", 0), ("s_eD", 1), ("s_fm", 5)], writes=[KP("s_CTs")])

                        def mmT(e, tsl=tsl):
                            for q in range(4):
                                e.transpose(b7[:, q * 128:(q + 1) * 128], xs[q][:, tsl], identb)
                            return e.transpose(b7[:, 512:640], BT[:, tsl], identb)
                        p.op("pe", mmT, reads=[("s_fm", j) for j in range(5)] + ["identb"], writes=[PS(7)])
                        xtm = b7[:, 0:512].rearrange("p (h k) -> p h k", h=8)
                        p.op("dve", lambda e: e.tensor_tensor(out=xdt, in0=xtm, in1=dt_tm.unsqueeze(2).to_broadcast([128, 8, 64]), op=ALU.mult), reads=[PS(7), "s_dt"], writes=[KP("s_xdt")])
                        p.op("dve", lambda e: e.tensor_tensor(out=xw, in0=xtm, in1=w2.unsqueeze(2).to_broadcast([128, 8, 64]), op=ALU.mult), reads=[PS(7), "s_w2"], writes=[KP("s_xw")])
                        for half in range(2):
                            p.op("act", lambda e, half=half: e.copy(out=Btmz[half][half * 64:(half + 1) * 64, :], in_=b7[half * 64:(half + 1) * 64, 512:640]), reads=[PS(7)], writes=[("s_Btmz", pa, half)])


                    def stageB(tt, pa, par):
                        MT, CTs, xdt, xw, Btmz, decbc = _aliases(pa)
                        KP = lambda n: (n, pa)
                        tsl = slice(tt * 128, (tt + 1) * 128)
                        def mmY(e):
                            ins = None
                            for h in range(8):
                                q, hq = h // 2, h % 2
                                ins = e.matmul(banks[4][hq * 64:(hq + 1) * 64, q * 128:(q + 1) * 128], lhsT=xdt[:, h, :], rhs=MT[:, h, :], start=True, stop=True)
                            return ins
                        p.op("pe", mmY, reads=[KP("s_xdt"), KP("s_MT")], writes=[PS(4)])
                        for half in range(2):
                            hs = slice(half * 64, (half + 1) * 64)

                            def mmO(e, half=half, par=par):
                                ins = None
                                for h in range(8):
                                    q, hq = h // 2, h % 2
                                    ins = e.matmul(banks[0][hq * 64:(hq + 1) * 64, q * 128 + half * 64:q * 128 + half * 64 + 64], lhsT=Sbf[par][:, h, :], rhs=CTs[:, h, half * 64:(half + 1) * 64], start=True, stop=True)
                                return ins
                            p.op("pe", mmO, reads=[("s_Sbf", par), KP("s_CTs")], writes=[("ps0o", half)] + ([PS(0)] if half == 0 else []))
                            p.op("pe", lambda e, half=half: e.matmul(banks[6][:, :], lhsT=Btmz[half], rhs=xw.rearrange("p h k -> p (h k)"), start=True, stop=True), reads=[("s_Btmz", pa, half), KP("s_xw")], writes=[PS(6)])
                            p.op("dve", lambda e, half=half: e.tensor_tensor(out=S32, in0=S32, in1=decbc[:, half, :].unsqueeze(2).to_broadcast([128, 8, 64]), op=ALU.mult), reads=["s_S32", KP("s_dec")], writes=["s_S32"])
                            p.op("dve", lambda e: e.tensor_tensor(out=S32, in0=S32, in1=banks[6][:, :].rearrange("p (h k) -> p h k", h=8), op=ALU.add), reads=["s_S32", PS(6)], writes=["s_S32"])
                            p.op("act", lambda e, par=par: e.copy(out=Sbf[1 - par], in_=S32), reads=["s_S32"], writes=[("s_Sbf", 1 - par)])
                            par = 1 - par
                        for q in range(4):
                            p.op("dve", lambda e, q=q, tsl=tsl, g=g: e.scalar_tensor_tensor(out=yg[:, q, tsl], in0=xs[q][:, tsl], scalar=PPc(l, PP_SD + g * 4 + q), in1=banks[4][:, q * 128:(q + 1) * 128], op0=ALU.mult, op1=ALU.add),
                                 reads=[("s_fm", q), PS(4), "pp"], writes=[("s_yg", q)])
                            p.op("dve", lambda e, q=q, tsl=tsl: e.tensor_tensor(out=yg[:, q, tsl], in0=yg[:, q, tsl], in1=banks[0][:, q * 128:(q + 1) * 128], op=ALU.add),
                                 reads=[("s_yg", q), PS(0), ("ps0o", 0), ("ps0o", 1)], writes=[("s_yg", q)])
                        return par

                    def capture(fn, *a):
                        lst = []
                        p.cap = lst
                        r = fn(*a)
                        p.cap = None
                        return lst, r

                    def replay(lst):
                        for (eng, fn, r, w) in lst:
                            p.op(eng, fn, reads=r, writes=w)

                    def merge(la, lb):
                        out = []
                        ia = ib = 0
                        na, nb = len(la), len(lb)
                        while ia < na or ib < nb:
                            if ib >= nb or (ia < na and ia * nb <= ib * na):
                                out.append(la[ia]); ia += 1
                            else:
                                out.append(lb[ib]); ib += 1
                        return out

                    lA, _ = capture(stageA, 0, pa_ctr[0] % 2)
                    replay(lA)
                    for tt in range(4):
                        pa = pa_ctr[0] % 2
                        lB, par = capture(stageB, tt, pa, par)
                        if tt + 1 < 4:
                            lA, _ = capture(stageA, tt + 1, (pa_ctr[0] + 1) % 2)
                            replay(merge(lA, lB))
                        else:
                            replay(lB)
                        pa_ctr[0] += 1
                    for q in range(4):
                        p.op("dve", lambda e, q=q: e.tensor_tensor(out=yg[:, q, :], in0=yg[:, q, :], in1=sz[q], op=ALU.mult), reads=[("s_yg", q), ("s_sz", q)], writes=[("s_yg", q)])
                    for q in range(4):
                        p.op("act", lambda e, q=q: e.activation(out=sqb, in_=yg[:, q, :], func=AF.Square), reads=[("s_yg", q)], writes=["s_sqb"])
                        p.op("pe", lambda e, q=q: e.matmul(banks[5][:, :], lhsT=ones512b, rhs=sqb, start=(q == 0), stop=(q == 3)), reads=["s_sqb", "ones512b"], writes=[PS(5)])
                    p.op("act", lambda e: e.activation(out=rs, in_=banks[5][:, :], func=AF.Ln, bias=LNEPS[:, 1:2]), reads=[PS(5), "lneps"], writes=[("s_ctmp", 0)])
                    p.op("act", lambda e: e.activation(out=rs, in_=rs, func=AF.Exp, scale=-0.5), reads=[("s_ctmp", 0)], writes=[("s_ctmp", 0)])
                    for q in range(4):
                        p.op("dve", lambda e, q=q, g=g: e.scalar_tensor_tensor(out=yblk[:, q, :], in0=yg[:, q, :], scalar=PPc(l, PP_SNORM + g * 4 + q), in1=rs, op0=ALU.mult, op1=ALU.mult),
                             reads=[("s_yg", q), ("s_ctmp", 0), "pp"], writes=[("yblk", q)])
                    out_proj(tb, 4, [0, 1])

        def mixer(l, ada_next=None):
            cv = Carve()
            wst = [cv.get([128, 8, 128]) for _ in range(2)]
            wunit = cv.get([128, 8, 1408], BF16)
            woutst = [cv.get([128, D])] * 2
            wout = cv.get([128, 4, D], BF16)
            yblk = cv.get([128, 4, TB], BF16)
            identb = cv.get([128, 128], BF16)
            ones128b = cv.get([128, 128], BF16)
            ones512b = cv.get([128, 128], BF16)
            p.op("act", lambda e: e.copy(out=identb, in_=ident), reads=["consts"], writes=["identb"])
            p.op("pool", lambda e: e.memset(ones128b, 1.0 / 128.0), writes=["ones128b"])
            p.op("pool", lambda e: e.memset(ones512b, 1.0 / 512.0), writes=["ones512b"])
            wcnt = [0]

            def load_win(col0, ncols, dst):
                c = 0
                while c < ncols:
                    n = min(128, ncols - c)
                    k = wcnt[0] % 2
                    wcnt[0] += 1
                    p.dma(("wst", k), wst[k][:, :, 0:n], win_d[l].rearrange("(kc p) f -> p kc f", p=128)[:, :, col0 + c:col0 + c + n], writes=[("wst", k)])
                    eng = ("act", "pool")[k]
                    if eng == "act":
                        p.op("act", lambda e, k=k, n=n, c=c: e.copy(out=wunit[:, :, dst + c:dst + c + n], in_=wst[k][:, :, 0:n]), reads=[("wst", k)], writes=["wunit"])
                    else:
                        p.op("pool", lambda e, k=k, n=n, c=c: e.tensor_copy(out=wunit[:, :, dst + c:dst + c + n], in_=wst[k][:, :, 0:n]), reads=[("wst", k)], writes=["wunit"])
                    c += n

            def load_wout(ych0, n):
                for j in range(n):
                    k = 0
                    p.dma(("wost", k), woutst[k], wout_d[l, (ych0 + j) * 128:(ych0 + j + 1) * 128, :], writes=[("wost", k)])
                    p.op("pool", lambda e, k=k, j=j: e.tensor_copy(out=wout[:, j, :], in_=woutst[k]), reads=[("wost", k)], writes=["wout"])

            def proj_fm(off, ncols, tb, bank):
                def mm(e):
                    ins = None
                    for kc in range(8):
                        ins = e.matmul(banks[bank][0:ncols, :], lhsT=wunit[:, kc, off:off + ncols], rhs=hT[:, kc, tb * TB:(tb + 1) * TB], start=(kc == 0), stop=(kc == 7))
                    return ins
                p.op("pe", mm, reads=["wunit"] + [("h", kc, tb) for kc in range(8)], writes=[PS(bank)])

            def proj_tm(off, ncols, tb, tt, bank, col0=0):
                t0 = tb * TB + tt * 128

                def mm(e):
                    ins = None
                    for kc in range(8):
                        ins = e.matmul(banks[bank][:, col0:col0 + ncols], lhsT=hT[:, kc, t0:t0 + 128], rhs=wunit[:, kc, off:off + ncols], start=(kc == 0), stop=(kc == 7))
                    return ins
                p.op("pe", mm, reads=["wunit"] + [("h", kc, tb) for kc in range(8)], writes=[PS(bank)])

            def out_proj(tb, nych, bks):
                for d in range(8):
                    bk = bks[d % len(bks)]

                    def mm(e, d=d, bk=bk):
                        ins = None
                        for j in range(nych):
                            ins = e.matmul(banks[bk][:, :], lhsT=wout[:, j, d * 128:(d + 1) * 128], rhs=yblk[:, j, :], start=(j == 0), stop=(j == nych - 1))
                        return ins
                    p.op("pe", mm, reads=["wout"] + [("yblk", j) for j in range(nych)], writes=[PS(bk)])
                    p.op("dve", lambda e, d=d, bk=bk: e.scalar_tensor_tensor(out=xT[:, d, tb * TB:(tb + 1) * TB], in0=banks[bk][:, :], scalar=MOD(l, 2, d), in1=xT[:, d, tb * TB:(tb + 1) * TB], op0=ALU.mult, op1=ALU.add),
                         reads=[PS(bk), ("x", d, tb)] + modkeys(l), writes=[("x", d, tb)])

            def conv_silu(buf, key, tb, bank, wcol, bcol, tmp, tmpkey, dst, dstkey, act_func, eng="dve"):
                p.op("act", lambda e: e.copy(out=buf[:, 3:3 + TB], in_=banks[bank][:, :]), reads=[PS(bank)], writes=[key])
                p.op(eng, lambda e: e.tensor_scalar(out=tmp, in0=buf[:, 0:TB], scalar1=PPc(l, wcol), scalar2=PPc(l, bcol), op0=ALU.mult, op1=ALU.add), reads=[key, "pp"], writes=[tmpkey])
                for k in range(1, 4):
                    p.op(eng, lambda e, k=k: e.scalar_tensor_tensor(out=tmp, in0=buf[:, k:k + TB], scalar=PPc(l, wcol + k), in1=tmp, op0=ALU.mult, op1=ALU.add), reads=[key, tmpkey, "pp"], writes=[tmpkey])
                p.op("pool", lambda e: e.tensor_copy(out=buf[:, 0:3], in_=buf[:, TB:TB + 3]), reads=[key], writes=[key])
                if dst is not None:
                    p.op("act", lambda e: e.activation(out=dst, in_=tmp, func=act_func), reads=[tmpkey], writes=[dstkey])

            mark = cv.off

            if do_lru:
                cv.off = mark
                wabd = cv.get([128, 4, 128], BF16)
                wxbd = cv.get([128, 4, 128], BF16)
                nsp8 = cv.get([128, 4])
                hcar = cv.get([128, 4])
                xbuf = [cv.get([128, 3 + TB]) for _ in range(4)]
                Ts = [{n: cv.get([128, TB]) for n in ("xc", "r", "i", "om", "h", "gg")} for _ in range(4)]
                xcbs = [cv.get([128, TB], BF16) for _ in range(4)]
                for nm, src, dst in (("wa", wabd_d, wabd), ("wx", wxbd_d, wxbd)):
                    p.dma(("wost", 0), woutst[0][:, 0:512].rearrange("p (m j) -> p m j", m=4), src[l].rearrange("m i j -> i m j"), writes=[("wost", 0)])
                    p.op("pool", lambda e, dst=dst: e.tensor_copy(out=dst, in_=woutst[0][:, 0:512].rearrange("p (m j) -> p m j", m=4)), reads=[("wost", 0)], writes=[nm])
                p.op("act", lambda e: e.activation(out=nsp8, in_=pp[:, l, PP_LLAM:PP_LLAM + 4], func=AF.Exp, scale=-1.0), reads=["pp"], writes=["nsp8"])
                p.op("act", lambda e: e.activation(out=nsp8, in_=nsp8, func=AF.Ln, bias=LNEPS[:, 2:3]), reads=["nsp8", "lneps"], writes=["nsp8"])
                p.op("dve", lambda e: e.tensor_scalar_mul(out=nsp8, in0=nsp8, scalar1=-8.0), reads=["nsp8"], writes=["nsp8"])
                for m in range(4):
                    p.op("pool", lambda e, m=m: e.memset(xbuf[m][:, 0:3], 0.0), writes=[("xbuf", m)])
                load_win(0, 1024, 0)
                load_wout(0, 4)

                def lru_chain(tb, m):
                    T = Ts[m]
                    xcb = xcbs[m]
                    BA, BB = 2 * m, 2 * m + 1
                    KK = lambda n: (n, m)
                    ops = []
                    A = lambda eng, fn, r, w: ops.append((eng, fn, r, w))
                    buf = xbuf[m]
                    key = ("xbuf", m)
                    wcol, bcol = PP_LCW + m * 4, PP_LCB + m
                    sl_h = [("h", kc, tb) for kc in range(8)]

                    def mmx(e):
                        ins = None
                        for kc in range(8):
                            ins = e.matmul(banks[BA][:, :], lhsT=wunit[:, kc, m * 128:(m + 1) * 128], rhs=hT[:, kc, tb * TB:(tb + 1) * TB], start=(kc == 0), stop=(kc == 7))
                        return ins

                    def mmg(e):
                        ins = None
                        for kc in range(8):
                            ins = e.matmul(banks[BB][:, :], lhsT=wunit[:, kc, 512 + m * 128:512 + (m + 1) * 128], rhs=hT[:, kc, tb * TB:(tb + 1) * TB], start=(kc == 0), stop=(kc == 7))
                        return ins
                    A("pe", mmx, ["wunit"] + sl_h, [PS(BA)])
                    A("pe", mmg, ["wunit"] + sl_h, [PS(BB)])
                    A("act", lambda e: e.copy(out=buf[:, 3:3 + TB], in_=banks[BA][:, :]), [PS(BA)], [key])
                    A("act", lambda e: e.activation(out=T["gg"], in_=banks[BB][:, :], func=AF.Gelu_apprx_tanh), [PS(BB)], [KK("l_gg")])
                    A("dve", lambda e: e.tensor_scalar(out=T["xc"], in0=buf[:, 0:TB], scalar1=PPc(l, wcol), scalar2=PPc(l, bcol), op0=ALU.mult, op1=ALU.add), [key, "pp"], [KK("l_xc")])
                    for k in range(1, 4):
                        A("dve", lambda e, k=k: e.scalar_tensor_tensor(out=T["xc"], in0=buf[:, k:k + TB], scalar=PPc(l, wcol + k), in1=T["xc"], op0=ALU.mult, op1=ALU.add), [key, KK("l_xc"), "pp"], [KK("l_xc")])
                    A("pool", lambda e: e.tensor_copy(out=buf[:, 0:3], in_=buf[:, TB:TB + 3]), [key], [key])
                    A("act", lambda e: e.copy(out=xcb, in_=T["xc"]), [KK("l_xc")], [KK("l_xcb")])
                    A("pe", lambda e: e.matmul(banks[BA][:, :], lhsT=wabd[:, m, :], rhs=xcb, start=True, stop=True), ["wa", KK("l_xcb")], [PS(BA)])
                    A("pe", lambda e: e.matmul(banks[BB][:, :], lhsT=wxbd[:, m, :], rhs=xcb, start=True, stop=True), ["wx", KK("l_xcb")], [PS(BB)])
                    A("act", lambda e: e.activation(out=T["r"], in_=banks[BA][:, :], func=AF.Sigmoid, bias=PPc(l, PP_LBA + m)), [PS(BA), "pp"], [KK("l_r")])
                    A("act", lambda e: e.activation(out=T["i"], in_=banks[BB][:, :], func=AF.Sigmoid, bias=PPc(l, PP_LBX + m)), [PS(BB), "pp"], [KK("l_i")])
                    A("act", lambda e: e.activation(out=T["r"], in_=T["r"], func=AF.Exp, scale=nsp8[:, m:m + 1]), [KK("l_r"), "nsp8"], [KK("l_r")])
                    A("pool", lambda e: e.tensor_tensor(out=T["om"], in0=T["r"], in1=T["r"], op=ALU.mult), [KK("l_r")], [KK("l_om")])
                    A("pool", lambda e: e.tensor_scalar(out=T["om"], in0=T["om"], scalar1=-1.0, scalar2=1.0, op0=ALU.mult, op1=ALU.add), [KK("l_om")], [KK("l_om")])
                    A("act", lambda e: e.activation(out=T["om"], in_=T["om"], func=AF.Sqrt), [KK("l_om")], [KK("l_om")])
                    A("pool", lambda e: e.tensor_tensor(out=T["i"], in0=T["i"], in1=T["xc"], op=ALU.mult), [KK("l_i"), KK("l_xc")], [KK("l_i")])
                    A("dve", lambda e: e.tensor_tensor(out=T["om"], in0=T["om"], in1=T["i"], op=ALU.mult), [KK("l_om"), KK("l_i")], [KK("l_om")])
                    if tb == 0:
                        A("dve", lambda e: e.tensor_tensor_scan(out=T["h"], data0=T["r"], data1=T["om"], initial=0.0, op0=ALU.mult, op1=ALU.add), [KK("l_r"), KK("l_om")], [KK("l_h")])
                    else:
                        A("dve", lambda e: e.tensor_tensor_scan(out=T["h"], data0=T["r"], data1=T["om"], initial=hcar[:, m:m + 1], op0=ALU.mult, op1=ALU.add), [KK("l_r"), KK("l_om"), ("hcar", m)], [KK("l_h")])
                    A("pool", lambda e: e.tensor_copy(out=hcar[:, m:m + 1], in_=T["h"][:, TB - 1:TB]), [KK("l_h")], [("hcar", m)])
                    A("dve", lambda e: e.tensor_tensor(out=yblk[:, m, :], in0=T["h"], in1=T["gg"], op=ALU.mult), [KK("l_h"), KK("l_gg")], [("yblk", m)])
                    return ops

                for tb in range(NTB):
                    lists = [lru_chain(tb, m) for m in range(4)]
                    for k in range(len(lists[0])):
                        for ol in lists:
                            eng, fn, r, w = ol[k]
                            p.op(eng, fn, reads=r, writes=w)
                    out_proj(tb, 4, [0, 1, 2, 3, 4, 5, 6, 7])

            if do_gla:
                cv.off = mark
                walb = cv.get([32, 256], BF16)
                rT = cv.get([32, TB], BF16)
                e1 = cv.get([128, 128])
                l1 = cv.get([128, 128])
                ecp = cv.get([128, TB])
                ecn = cv.get([128, TB])
                esuf = [cv.get([128, 128]) for _ in range(4)]
                qd = cv.get([128, TB], BF16)
                kd = cv.get([128, TB], BF16)
                kend = [cv.get([128, 128], BF16) for _ in range(4)]
                kendz = [[cv.get([128, 128], BF16) for _ in range(2)] for _ in range(4)]
                qdz = [cv.get([128, TB], BF16) for _ in range(2)]
                CHM = [consts[:, C_CH0:C_CH0 + 128], consts[:, C_CH1:C_CH1 + 128]]
                vtm = [cv.get([128, 256], BF16) for _ in range(4)]
                sg = [cv.get([128, TB]) for _ in range(2)]
                attms = [cv.get([128, 4, 128], BF16) for _ in range(2)]
                S32 = cv.get([128, 128])
                Sbfs = [[cv.get([128, 128], BF16) for _ in range(2)] for _ in range(2)]
                sqbs = [cv.get([128, TB], BF16) for _ in range(2)]
                rss = [cv.get([128, TB]) for _ in range(2)]
                t1s = [cv.get([128, TB]) for _ in range(2)]
                ada_bufs_m = ada_bufs_alloc(cv) if ada_next is not None else None
                p.dma(("wost", 0), woutst[0][0:32, 0:256], walpha_d[l], writes=[("wost", 0)])
                p.op("pool", lambda e: e.tensor_copy(out=walb, in_=woutst[0][0:32, 0:256]), reads=[("wost", 0)], writes=["walb"])
                p.op("pool", lambda e: e.memset(rT, 1.0), writes=["rT"])
                m64b = consts[:, C_MASK:C_MASK + 128].unsqueeze(1).to_broadcast([128, 4, 128])
                for hp in range(2):
                    load_win(1024 + hp * 128, 128, 0)
                    load_win(1280 + hp * 128, 128, 128)
                    load_win(1536 + hp * 256, 256, 256)
                    load_win(2048 + hp * 256, 256, 512)
                    load_win(2560, 16, 768)
                    load_wout(4 + hp * 2, 2)
                    p.op("pool", lambda e: e.memset(S32, 0.0), writes=[("S32", 0), ("S32", 1)])
                    for hh in range(2):
                        for pr in range(2):
                            p.op("pool", lambda e, hh=hh, pr=pr: e.memset(Sbfs[hh][pr], 0.0), writes=[("Sbf", hh, pr)])
                    if hp == 0:
                        for hh in range(2):
                            p.op("pool", lambda e, hh=hh: e.memset(qdz[hh], 0.0), writes=[("g_qdz", hh)])
                        for tt in range(4):
                            for half in range(2):
                                p.op("pool", lambda e, tt=tt, half=half: e.memset(kendz[tt][half], 0.0), writes=[("g_kendz", tt, half)])
                    par_state = {0: 0, 1: 0}
                    for tb in range(NTB):
                        proj_fm(768, 16, tb, 0)
                        p.op("act", lambda e: e.copy(out=rT[0:16, :], in_=banks[0][0:16, :]), reads=[PS(0)], writes=["rT"])
                        for tt in range(4):
                            tsl = slice(tt * 128, (tt + 1) * 128)
                            p.op("pe", lambda e, tsl=tsl, hp=hp: e.matmul(banks[5][:, 0:128], lhsT=rT[:, tsl], rhs=walb[:, hp * 128:(hp + 1) * 128], start=True, stop=True), reads=["rT", "walb"], writes=[PS(5)])
                            p.op("act", lambda e: e.activation(out=e1, in_=banks[5][:, 0:128], func=AF.Exp, scale=-1.0), reads=[PS(5)], writes=["g_e1"])
                            p.op("act", lambda e: e.activation(out=l1, in_=e1, func=AF.Ln, bias=LNEPS[:, 2:3]), reads=["g_e1", "lneps"], writes=["g_l1"])
                            p.op("pe", lambda e: e.matmul(banks[5][:, 128:256], lhsT=l1, rhs=consts[:, C_MASKG:C_MASKG + 128], start=True, stop=True), reads=["g_l1", "consts"], writes=[PS(5)])
                            p.op("pe", lambda e: e.matmul(banks[5][:, 256:384], lhsT=consts[:, C_MASKG + 128:C_MASKG + 256], rhs=l1, start=True, stop=True), reads=["g_l1", "consts"], writes=[PS(5)])
                            p.op("act", lambda e, tsl=tsl: e.activation(out=ecp[:, tsl], in_=banks[5][:, 128:256], func=AF.Exp), reads=[PS(5)], writes=["g_ecp"])
                            p.op("act", lambda e, tsl=tsl: e.activation(out=ecn[:, tsl], in_=banks[5][:, 128:256], func=AF.Exp, scale=-1.0), reads=[PS(5)], writes=["g_ecn"])
                            p.op("act", lambda e, tt=tt: e.activation(out=esuf[tt], in_=banks[5][:, 256:384], func=AF.Exp), reads=[PS(5)], writes=[("g_esuf", tt)])
                        proj_fm(0, 128, tb, 0)
                        for hh in range(2):
                            p.op("dve", lambda e, hh=hh: e.scalar_tensor_tensor(out=qdz[hh][hh * 64:(hh + 1) * 64, :], in0=banks[0][hh * 64:(hh + 1) * 64, :], scalar=0.125, in1=ecp[hh * 64:(hh + 1) * 64, :], op0=ALU.mult, op1=ALU.mult),
                                 reads=[PS(0), "g_ecp"], writes=[("g_qdz", hh)])
                        proj_fm(128, 128, tb, 1)
                        p.op("dve", lambda e: e.tensor_tensor(out=kd, in0=banks[1][:, :], in1=ecn, op=ALU.mult), reads=[PS(1), "g_ecn"], writes=["g_kd"])
                        for tt in range(4):
                            proj_tm(128, 128, tb, tt, 0)
                            for half in range(2):
                                p.op("dve", lambda e, tt=tt, half=half: e.tensor_tensor(out=kendz[tt][half][half * 64:(half + 1) * 64, :], in0=banks[0][half * 64:(half + 1) * 64, 0:128], in1=esuf[tt][half * 64:(half + 1) * 64, :], op=ALU.mult),
                                     reads=[PS(0), ("g_esuf", tt)], writes=[("g_kendz", tt, half)])
                            proj_tm(256, 256, tb, tt, 1)
                            p.op("act", lambda e, tt=tt: e.copy(out=vtm[tt], in_=banks[1][:, 0:256]), reads=[PS(1)], writes=[("g_vtm", tt)])
                        for hh in range(2):
                            proj_fm(512 + hh * 128, 128, tb, hh)
                            p.op("act", lambda e, hh=hh: e.activation(out=sg[hh], in_=banks[hh][:, :], func=AF.Silu), reads=[PS(hh)], writes=[("g_sg", hh)])
                        def head_ops(tb, hh):
                            b0 = hh * 64
                            BATT, BO, BKV = (2, 5)[hh], (3, 6)[hh], (4, 7)[hh]
                            attm, sqb, rs, t1 = attms[hh], sqbs[hh], rss[hh], t1s[hh]
                            KH = lambda n: (n, hh)

                            def att(e):
                                ins = None
                                for tt in range(4):
                                    tsl = slice(tt * 128, (tt + 1) * 128)
                                    ins = e.matmul(banks[BATT][:, tsl], lhsT=kd[:, tsl], rhs=qdz[hh][:, tsl], start=True, stop=True)
                                return ins
                            p.op("pe", att, reads=["g_kd", ("g_qdz", hh)], writes=[PS(BATT)])
                            p.op("dve", lambda e: e.tensor_tensor(out=attm, in0=banks[BATT][:, :].rearrange("p (a b) -> p a b", a=4), in1=m64b, op=ALU.mult), reads=[PS(BATT), "consts"], writes=[KH("g_attm")])
                            for c in range(8):
                                tt, half = c // 2, c % 2
                                csl = slice(c * 64, (c + 1) * 64)
                                tsl = slice(tt * 128, (tt + 1) * 128)
                                cur_par = par_state[hh]
                                if half == 0:
                                    p.op("pe", lambda e, tt=tt, tsl=tsl: e.matmul(banks[BO][:, tsl], lhsT=vtm[tt][:, hh * 128:(hh + 1) * 128], rhs=attm[:, tt, :], start=True, stop=False),
                                         reads=[("g_vtm", tt), KH("g_attm")], writes=[PS(BO)])
                                p.op("pe", lambda e, csl=csl, cur_par=cur_par, half=half: e.matmul(banks[BO][:, csl], lhsT=Sbfs[hh][cur_par], rhs=qdz[hh][:, csl], start=False, stop=(half == 1)),
                                     reads=[("Sbf", hh, cur_par), ("g_qdz", hh)], writes=[PS(BO)])
                                p.op("pe", lambda e, tt=tt, half=half: e.matmul(banks[BKV][:, 0:128], lhsT=kendz[tt][half], rhs=vtm[tt][:, hh * 128:(hh + 1) * 128], start=True, stop=True),
                                     reads=[("g_kendz", tt, half), ("g_vtm", tt)], writes=[PS(BKV)])
                                col = c * 64 + 63
                                p.op("dve", lambda e, col=col: e.scalar_tensor_tensor(out=S32[b0:b0 + 64, :], in0=S32[b0:b0 + 64, :], scalar=ecp[b0:b0 + 64, col:col + 1], in1=banks[BKV][b0:b0 + 64, 0:128], op0=ALU.mult, op1=ALU.add),
                                     reads=[("S32", hh), "g_ecp", PS(BKV)], writes=[("S32", hh)])
                                nxt = 1 - cur_par
                                p.op("act", lambda e, nxt=nxt: e.copy(out=Sbfs[hh][nxt][b0:b0 + 64, :], in_=S32[b0:b0 + 64, :]), reads=[("S32", hh)], writes=[("Sbf", hh, nxt)])
                                par_state[hh] = nxt
                            p.op("act", lambda e: e.activation(out=sqb, in_=banks[BO][:, :], func=AF.Square), reads=[PS(BO)], writes=[KH("g_sqb")])
                            p.op("pe", lambda e: e.matmul(banks[BATT][:, :], lhsT=ones128b, rhs=sqb, start=True, stop=True), reads=["ones128b", KH("g_sqb")], writes=[PS(BATT)])
                            p.op("act", lambda e: e.activation(out=rs, in_=banks[BATT][:, :], func=AF.Ln, bias=LNEPS[:, 1:2]), reads=[PS(BATT), "lneps"], writes=[KH("g_rs")])
                            p.op("act", lambda e: e.activation(out=rs, in_=rs, func=AF.Exp, scale=-0.5), reads=[KH("g_rs")], writes=[KH("g_rs")])
                            p.op("dve", lambda e: e.tensor_tensor(out=t1, in0=banks[BO][:, :], in1=rs, op=ALU.mult), reads=[PS(BO), KH("g_rs")], writes=[KH("g_t1")])
                            p.op("dve", lambda e: e.scalar_tensor_tensor(out=yblk[:, hh, :], in0=t1, scalar=PPc(l, PP_GNORM), in1=sg[hh], op0=ALU.mult, op1=ALU.mult), reads=[KH("g_t1"), ("g_sg", hh), "pp"], writes=[("yblk", hh)])

                        replay_ops(interleave([cap_ops(head_ops, tb, 0), cap_ops(head_ops, tb, 1)]))
                        if ada_next is not None:
                            slot = hp * NTB + tb
                            for blk in range(slot * 3, slot * 3 + 3):
                                adaln_block(ada_next, blk, ada_bufs_m, 2 + (blk % 2) * 3)
                            if slot == 2 * NTB - 1:
                                adaln_finish(ada_next, 0)
                        out_proj(tb, 2, [0, 1])

            if do_ssd:
                ssd_units(l, cv, mark, load_win, load_wout, proj_fm, proj_tm, out_proj, conv_silu, yblk, identb, ones512b)

        cv = Carve()
        ada_bufs = ada_bufs_alloc(cv)
        for blk in range(N_ADA):
            adaln_block(0, blk, ada_bufs, blk % 4)
        adaln_finish(0, 1)

        for tb in range(NTB):
            scale_alpha(tb, engs=("pool", "dve", "act"))
        for l in range(depth):
            p.new_epoch()
            for tb in range(NTB):
                modulate(l, 1, tb)
            barrier()
            ada_in_mixer = do_gla and (l + 1 < depth)
            if do_lru or do_gla or do_ssd:
                mixer(l, ada_next=(l + 1 if ada_in_mixer else None))
            barrier()
            cv = Carve()
            if do_moe:
                rgroup, rfinish = router(l, cv, 2, 3)

                def after_tb(tb):
                    modulate(l, 2, tb)
                    rgroup(tb)
                layernorm(l, PP_LN1G, PP_LN1B, cv, 0, 1, after_tb=after_tb)
                rfinish()
            else:
                layernorm(l, PP_LN1G, PP_LN1B, cv, 0, 1, after_tb=lambda tb: modulate(l, 2, tb))
            barrier()
            cv = Carve()
            hooks = {}
            if l + 1 < depth and not ada_in_mixer:
                ada_bufs2 = ada_bufs_alloc(cv)
                for blk in range(N_ADA):
                    hooks[2 + blk] = (lambda blk=blk: adaln_block(l + 1, blk, ada_bufs2, 4 + blk % 4))
                hooks[2 + N_ADA] = (lambda: adaln_finish(l + 1, 4))
            if do_moe:
                moe(l, cv, hooks)
            else:
                for k in sorted(hooks):
                    hooks[k]()
            barrier()
            cv = Carve()
            if l == depth - 1:
                yv = yT_d.rearrange("(c p) t -> p c t", p=128)

                def out_tb(tb):
                    p.dma(("out", tb), yv[:, :, tb * TB:(tb + 1) * TB], xT[:, :, tb * TB:(tb + 1) * TB], reads=[("x", c, tb) for c in range(8)])
                layernorm(l, PP_LN2G, PP_LN2B, cv, 0, 1, final=True, after_tb=out_tb)
            else:
                layernorm(l, PP_LN2G, PP_LN2B, cv, 0, 1)
            barrier()

        p.emit()
    return nc


def prep_inputs(inputs):
    f = lambda a: np.ascontiguousarray(np.asarray(a, dtype=np.float32))
    L = DEPTH
    shared = {}
    shared["consts"] = build_consts()
    pc = lambda v, n: np.asarray(v, np.float32).reshape(n, 128).T
    pp = np.zeros((L, 128, NPP), np.float32)
    prow = np.zeros((L, NPR), np.float32)
    wabd = np.zeros((L, 4, 128, 128), np.float32)
    wxbd = np.zeros((L, 4, 128, 128), np.float32)
    wal = np.zeros((L, 32, 256), np.float32)
    for l in range(L):
        pp[l, :, PP_LN1G:PP_LN1G + 8] = pc(inputs["ln1_g"][l], 8)
        pp[l, :, PP_LN1B:PP_LN1B + 8] = pc(inputs["ln1_b"][l], 8)
        pp[l, :, PP_LN2G:PP_LN2G + 8] = pc(inputs["ln2_g"][l], 8)
        pp[l, :, PP_LN2B:PP_LN2B + 8] = pc(inputs["ln2_b"][l], 8)
        for m in range(4):
            for k in range(4):
                pp[l, :, PP_LCW + m * 4 + k] = inputs["lru_conv_w"][l, k, m * 128:(m + 1) * 128]
        pp[l, :, PP_LCB:PP_LCB + 4] = pc(inputs["lru_conv_b"][l], 4)
        pp[l, :, PP_LBA:PP_LBA + 4] = pc(inputs["lru_b_a"][l], 4)
        pp[l, :, PP_LBX:PP_LBX + 4] = pc(inputs["lru_b_x"][l], 4)
        pp[l, :, PP_LLAM:PP_LLAM + 4] = pc(inputs["lru_lambda"][l], 4)
        for j in range(12):
            for k in range(4):
                pp[l, :, PP_SCW + j * 4 + k] = inputs["ssd_conv_w"][l, k, j * 128:(j + 1) * 128]
        pp[l, :, PP_SCB:PP_SCB + 12] = pc(inputs["ssd_conv_b"][l], 12)
        pp[l, :, PP_SNORM:PP_SNORM + 8] = pc(inputs["ssd_norm"][l], 8)
        pp[l, :, PP_GNORM] = inputs["gla_norm"][l]
        pp[l, :, PP_SD:PP_SD + 8] = pc(np.repeat(np.asarray(inputs["ssd_d"][l]), 64), 8)
        prow[l, PR_DTB:PR_DTB + 16] = inputs["ssd_dt_bias"][l]
        prow[l, PR_ALOG:PR_ALOG + 16] = inputs["ssd_a_log"][l]
        prow[l, PR_RB:PR_RB + NE] = inputs["router_bias"][l]
        for m in range(4):
            for q in range(2):
                wabd[l, m, q * 64:(q + 1) * 64, q * 64:(q + 1) * 64] = inputs["lru_w_a"][l, 2 * m + q]
                wxbd[l, m, q * 64:(q + 1) * 64, q * 64:(q + 1) * 64] = inputs["lru_w_x"][l, 2 * m + q]
        wal[l, 0:16] = inputs["gla_w_alpha"][l]
        wal[l, 16] = inputs["gla_b_alpha"][l]
    shared.update(pp=pp, prow=prow, lru_wa_bd=wabd, lru_wx_bd=wxbd, walpha_ext=wal)
    for k in ("w_ada", "b_ada", "w_in", "w_out", "router_w", "exp_w1", "exp_w3", "exp_w2", "shared_w1", "shared_w3", "shared_w2"):
        shared[k] = f(inputs[k])
    x = np.asarray(inputs["x"], np.float32)
    c = np.asarray(inputs["c"], np.float32)
    maps = []
    for b in range(x.shape[0]):
        m = dict(shared)
        m["xT"] = np.ascontiguousarray(x[b].T)
        m["cpc"] = np.ascontiguousarray(c[b].reshape(8, 128).T)
        maps.append(m)
    return maps


_NC_CACHE = {}


def kernel(**inputs):
    maps = prep_inputs(inputs)
    if "nc" not in _NC_CACHE:
        _NC_CACHE["nc"] = build_program()
    nc = _NC_CACHE["nc"]
    res = run_bass_kernel_spmd(nc, maps, core_ids=list(range(len(maps))))
    out = np.stack([np.ascontiguousarray(r["yT"].T) for r in res.results], axis=0)
    return out.astype(np.float32)
```

```python
from contextlib import ExitStack
import numpy as np
import concourse.bass as bass
import concourse.mybir as mybir
from concourse.bass_utils import run_bass_kernel_spmd

F32 = mybir.dt.float32
BF16 = mybir.dt.bfloat16
AF = mybir.ActivationFunctionType
ALU = mybir.AluOpType
AX = mybir.AxisListType

D = 1024
S = 2048
DEPTH = 4
NE = 64
ALPHA = (2 * DEPTH) ** 0.25
DIN = 5152
NPP = 144
TB = 512
NTB = S // TB


class Prog:
    def __init__(self, nc, stack):
        self.nc = nc
        self.stack = stack
        self.ops = []
        self.last_write = {}
        self.readers = {}
        self.epoch = 0
        self.op_epoch = []
        self.group_open = {}
        self.group_of = {}
        self.nsem = 0
        self.bar = None
        self.xeng = []
        self.cap = None

    def barrier(self, fn):
        allk = list(set(self.last_write.keys()) | set(k for k, v in self.readers.items() if v))
        oid = self._add("dve", fn, allk, allk)
        self.bar = oid
        self.last_write = {}
        self.readers = {}

    def new_sem(self, name):
        self.nsem += 1
        return self.stack.enter_context(self.nc.semaphore(f"{name}_{self.nsem}"))

    def new_epoch(self):
        self.epoch += 1

    def _add(self, eng, fn, reads, writes, chan=None):
        oid = len(self.ops)
        raw = set()
        oth = set()
        xe = set()
        for k in reads:
            w = self.last_write.get(k)
            if w is not None:
                raw.add(w)
            if isinstance(k, tuple) and k[0] in ("ps", "ps2o"):
                for r in self.readers.get(k, ()):
                    xe.add(r)
        for k in writes:
            w = self.last_write.get(k)
            if w is not None:
                oth.add(w)
            for r in self.readers.get(k, ()):
                oth.add(r)
        if self.bar is not None:
            oth.add(self.bar)
        oth -= raw
        raw.discard(oid)
        oth.discard(oid)
        xe -= raw
        xe -= oth
        xe.discard(oid)
        self.xeng.append(sorted(xe))
        self.ops.append([eng, fn, sorted(raw), sorted(oth), chan])
        self.op_epoch.append(self.epoch)
        for k in reads:
            self.readers.setdefault(k, []).append(oid)
        for k in writes:
            self.last_write[k] = oid
            self.readers[k] = []
        return oid

    def op(self, eng, fn, reads=(), writes=()):
        if self.cap is not None:
            self.cap.append((eng, fn, list(reads), list(writes)))
            return None
        return self._add(eng, fn, reads, writes)

    def dma(self, chan, out, in_, reads=(), writes=(), eng="sp", more=False, **kw):
        def fn(e, out=out, in_=in_, kw=kw):
            return e.dma_start(out=out, in_=in_, **kw)
        oid = self._add(eng, fn, reads, writes, chan=chan)
        g = self.group_open.get(chan)
        if g is None:
            g = []
            self.group_open[chan] = g
        g.append(oid)
        self.group_of[oid] = g
        if not more:
            self.group_open[chan] = None
        return oid

    def emit(self):
        nc = self.nc
        n = len(self.ops)
        is_dma = [o[4] is not None for o in self.ops]
        need_sig = [False] * n
        deps_of = []
        for i, (eng, fn, raw, oth, chan) in enumerate(self.ops):
            deps = []
            for d in raw:
                if is_dma[d] or is_dma[i] or self.ops[d][0] != eng or eng != "pe":
                    deps.append(d)
            for d in oth:
                if is_dma[d] or is_dma[i] or self.ops[d][0] != eng or eng != "pe":
                    deps.append(d)
            for d in self.xeng[i]:
                if self.ops[d][0] != eng:
                    deps.append(d)
            deps_of.append(deps)
            for d in deps:
                if not is_dma[d]:
                    need_sig[d] = True
        sig = [None] * n
        cur = {}
        chan_sem = {}
        chan_cnt = {}
        for i, (eng, fn, raw, oth, chan) in enumerate(self.ops):
            if is_dma[i]:
                if chan not in chan_sem:
                    chan_sem[chan] = self.new_sem("d")
                    chan_cnt[chan] = 0
                chan_cnt[chan] += 16
                sig[i] = (chan_sem[chan], chan_cnt[chan])
            elif need_sig[i]:
                key = (eng, self.op_epoch[i])
                if key not in cur:
                    cur[key] = [self.new_sem(eng), 0]
                cur[key][1] += 1
                sig[i] = (cur[key][0], cur[key][1])
        for i in range(n):
            if is_dma[i]:
                last = self.group_of[i][-1]
                if last != i:
                    sig[i] = (sig[i][0], sig[last][1])
        streams = {}
        for i, (eng, fn, raw, oth, chan) in enumerate(self.ops):
            streams.setdefault(eng, []).append((i, fn, [sig[d] for d in deps_of[i]]))
        final_dma = [(chan_sem[c], chan_cnt[c]) for c in chan_sem]
        self.n_waits = 0

        def run_stream(e, items, tail):
            waited = {}
            for (i, fn, waits) in items:
                best = {}
                for (s, v) in waits:
                    k = s.num
                    if waited.get(k, 0) >= v:
                        continue
                    if k not in best or best[k][1] < v:
                        best[k] = (s, v)
                for k, (s, v) in best.items():
                    e.wait_ge(s, v)
                    waited[k] = v
                    self.n_waits += 1
                ins = fn(e)
                if is_dma[i]:
                    ins.then_inc(chan_sem[self.ops[i][4]], 16)
                elif sig[i] is not None:
                    ins.then_inc(sig[i][0], 1)
            for (s, v) in tail:
                e.wait_ge(s, v)

        with nc.Block() as block:
            names = {"pe": "tensor", "act": "scalar", "dve": "vector", "pool": "gpsimd", "sp": "sync"}
            for en, attr in names.items():
                items = streams.get(en, [])
                tail = final_dma if en == "sp" else []
                if not items and not tail:
                    continue

                def body(e, items=items, tail=tail):
                    run_stream(e, items, tail)
                getattr(block, attr)(body)


C_ID = 0
C_MASK = 128
C_SUF = 256
C_CH0 = 384
C_CH1 = 512
C_M64 = 640
C_ONESD = 704
C_ONE = 832
C_SEL = 960
C_MASKG = 1984
NCONST = 2240


def build_consts():
    c = np.zeros((128, NCONST), np.float32)
    idx = np.arange(128)
    same = (idx[:, None] // 64) == (idx[None, :] // 64)
    c[:, C_ID:C_ID + 128] = np.eye(128)
    c[:, C_MASK:C_MASK + 128] = same & (idx[:, None] <= idx[None, :])
    c[:, C_SUF:C_SUF + 128] = same & (idx[:, None] > idx[None, :])
    c[:64, C_CH0:C_CH0 + 128] = 1.0
    c[64:, C_CH1:C_CH1 + 128] = 1.0
    j = idx % 64
    c[:, C_M64:C_M64 + 64] = j[:, None] <= np.arange(64)[None, :]
    c[:, C_ONESD:C_ONESD + 128] = 1.0 / 1024.0
    c[:, C_ONE:C_ONE + 128] = 1.0
    for h in range(8):
        c[h, C_SEL + h * 128:C_SEL + (h + 1) * 128] = 1.0
    c[:, C_MASKG:C_MASKG + 128] = c[:, C_MASK:C_MASK + 128] * (-1.0 / 16.0)
    c[:, C_MASKG + 128:C_MASKG + 256] = c[:, C_SUF:C_SUF + 128] * (-1.0 / 16.0)
    return c


PP_LN1G, PP_LN1B, PP_LN2G, PP_LN2B = 0, 8, 16, 24
PP_LCW, PP_LCB, PP_LBA, PP_LBX, PP_LLAM = 32, 48, 52, 56, 60
PP_SCW, PP_SCB, PP_SNORM, PP_GNORM, PP_SD = 64, 112, 124, 132, 133
PR_DTB, PR_ALOG, PR_RB = 0, 16, 32
NPR = 96


def build_program(depth=DEPTH, do_lru=True, do_gla=True, do_ssd=True, do_moe=True, n_exp=NE + 1, debug=False):
    nc = bass.Bass("TRN2", target_bir_lowering=False, dynamic_dma_scratch_size=512)
    dr = {}

    def din(name, shape):
        dr[name] = nc.dram_tensor(name, list(shape), F32, kind="ExternalInput").ap()
        return dr[name]

    xT_d = din("xT", [D, S])
    cpc_d = din("cpc", [128, 8])
    consts_d = din("consts", [128, NCONST])
    pp_d = din("pp", [DEPTH, 128, NPP])
    prow_d = din("prow", [DEPTH, NPR])
    wada_d = din("w_ada", [DEPTH, D, 6 * D])
    bada_d = din("b_ada", [DEPTH, 6 * D])
    win_d = din("w_in", [DEPTH, D, DIN])
    wout_d = din("w_out", [DEPTH, 2 * D, D])
    wabd_d = din("lru_wa_bd", [DEPTH, 4, 128, 128])
    wxbd_d = din("lru_wx_bd", [DEPTH, 4, 128, 128])
    walpha_d = din("walpha_ext", [DEPTH, 32, 256])
    rw_d = din("router_w", [DEPTH, D, NE])
    ew1_d = din("exp_w1", [DEPTH, NE, D, 256])
    ew3_d = din("exp_w3", [DEPTH, NE, D, 256])
    ew2_d = din("exp_w2", [DEPTH, NE, 256, D])
    sw1_d = din("shared_w1", [DEPTH, D, 256])
    sw3_d = din("shared_w3", [DEPTH, D, 256])
    sw2_d = din("shared_w2", [DEPTH, 256, D])
    yT_d = nc.dram_tensor("yT", [D, S], F32, kind="ExternalOutput").ap()
    gscr_d = nc.dram_tensor("gscr", [2, NE, S], F32, kind="Internal").ap()
    dbg = {}

    with ExitStack() as st:
        p = Prog(nc, st)

        def sb(name, shape, dt=F32):
            return st.enter_context(nc.sbuf_tensor(name, list(shape), dt))

        def cap_ops(fn, *a):
            lst = []
            p.cap = lst
            r = fn(*a)
            p.cap = None
            return lst

        def replay_ops(lst):
            for (eng, fn, r, w) in lst:
                p.op(eng, fn, reads=r, writes=w)

        def interleave(lists):
            out = []
            n = max(len(x) for x in lists)
            for k in range(n):
                for x in lists:
                    if k < len(x):
                        out.append(x[k])
            return out

        xT = sb("xT_sb", [128, 8, S])
        hT = sb("hT_sb", [128, 8, S], BF16)
        consts = sb("consts_sb", [128, NCONST])
        pp = sb("pp_sb", [128, DEPTH, NPP])
        mod = sb("mod_sb", [128, DEPTH, 64])
        cond = sb("cond_sb", [128, 8])
        SCRW = 29150
        scr = sb("scr_sb", [128, SCRW])
        banks = [st.enter_context(nc.psum_tensor(f"bank{i}", [128, 512], F32)) for i in range(8)]

        def PS(i):
            return ("ps", i)

        class Carve:
            def __init__(self):
                self.off = 0

            def get(self, shape, dt=F32):
                n = int(np.prod(shape[1:]))
                words = n if dt == F32 else (n + 1) // 2
                a = scr[:, self.off:self.off + words]
                self.off += words
                assert self.off <= SCRW - 1, self.off
                if dt != F32:
                    a = a.bitcast(dt)
                    if n % 2:
                        a = a[:, 0:n]
                if len(shape) == 3:
                    a = a.rearrange("p (a b) -> p a b", a=shape[1])
                elif len(shape) == 4:
                    a = a.rearrange("p (a b c) -> p a b c", a=shape[1], b=shape[2])
                if shape[0] != 128:
                    a = a[0:shape[0]]
                return a

        ident = consts[:, C_ID:C_ID + 128]
        onesD = consts[:, C_ONESD:C_ONESD + 128]

        def barrier():
            tok = scr[:, SCRW - 1:SCRW]
            p.barrier(lambda e: e.memset(tok, 0.0))

        p.dma("ld0", consts[:], consts_d[:, :], writes=["consts"])
        p.dma("ld1", pp[:], pp_d.rearrange("l p n -> p l n"), writes=["pp"])
        p.dma("ld2", cond[:], cpc_d[:, :], writes=["cond"])
        p.dma("ldx", xT[:], xT_d.rearrange("(c p) t -> p c t", p=128), writes=[("x", c, tb) for c in range(8) for tb in range(NTB)])
        p.op("act", lambda e: e.activation(out=cond[:], in_=cond[:], func=AF.Silu), reads=["cond"], writes=["cond"])

        ADA_BLK = 256
        N_ADA = 6 * D // ADA_BLK
        ada_stg = [None, None]

        def adaln_block(l, blk, cv_bufs, bank):
            stg, brow, mrow = cv_bufs[blk % 2]
            key = ("adastg", blk % 2)
            c0 = blk * ADA_BLK
            nj = ADA_BLK // 128
            p.dma(("adab", blk % 2), brow, bada_d[l:l + 1, c0:c0 + ADA_BLK], writes=[("adabrow", blk % 2)])
            p.dma(("ada", blk % 2), stg, wada_d[l].rearrange("(kc p) f -> p kc f", p=128)[:, :, c0:c0 + ADA_BLK], writes=[key])

            def mm(e, stg=stg):
                ins = None
                for kc in range(8):
                    ins = e.matmul(banks[bank][0:1, 0:ADA_BLK], lhsT=cond[:, kc:kc + 1], rhs=stg[:, kc, :], start=(kc == 0), stop=(kc == 7))
                return ins
            p.op("pe", mm, reads=[key, "cond"], writes=[PS(bank)])
            p.op("dve", lambda e: e.tensor_tensor(out=mrow, in0=banks[bank][0:1, 0:ADA_BLK], in1=brow, op=ALU.add),
                 reads=[PS(bank), ("adabrow", blk % 2)], writes=[("adamrow", blk % 2)])

            def mm2(e):
                ins = None
                for j in range(nj):
                    ins = e.matmul(banks[bank][:, 256 + j:257 + j], lhsT=mrow[0:1, j * 128:(j + 1) * 128], rhs=consts[0:1, C_ONE:C_ONE + 1], start=True, stop=True)
                return ins
            p.op("pe", mm2, reads=[("adamrow", blk % 2), "consts"], writes=[PS(bank)])
            p.op("act", lambda e: e.copy(out=mod[:, l, blk * nj:(blk + 1) * nj], in_=banks[bank][:, 256:256 + nj]), reads=[PS(bank)], writes=[("mod", l)])

        def adaln_finish(l, bank):
            p.op("dve", lambda e: e.tensor_scalar(out=mod[:, l, 48:56], in0=mod[:, l, 8:16], scalar1=1.0, scalar2=1.0 / float(ALPHA), op0=ALU.add, op1=ALU.mult), reads=[("mod", l)], writes=[("modd", l)])
            p.op("dve", lambda e: e.tensor_scalar(out=mod[:, l, 56:64], in0=mod[:, l, 32:40], scalar1=1.0, scalar2=1.0 / float(ALPHA), op0=ALU.add, op1=ALU.mult), reads=[("mod", l)], writes=[("modd2", l)])

        def ada_bufs_alloc(cv):
            return [(cv.get([128, 8, ADA_BLK]), cv.get([1, ADA_BLK]), cv.get([1, ADA_BLK])) for _ in range(2)]

        def MOD(l, j, c):
            if j < 6:
                return mod[:, l, j * 8 + c:j * 8 + c + 1]
            return mod[:, l, 48 + (j - 6) * 8 + c:48 + (j - 6) * 8 + c + 1]

        def PPc(l, col):
            return pp[:, l, col:col + 1]

        modkeys = lambda l: [("mod", l), ("modd", l), ("modd2", l)]

        def modulate(l, which, tb, engs=("dve", "pool")):
            jsc, jsh = (6, 0) if which == 1 else (7, 3)
            for c in range(8):
                eng = engs[c % len(engs)]
                p.op(eng, lambda e, c=c: e.tensor_scalar(out=hT[:, c, tb * TB:(tb + 1) * TB], in0=xT[:, c, tb * TB:(tb + 1) * TB],
                                                         scalar1=MOD(l, jsc, c), scalar2=MOD(l, jsh, c), op0=ALU.mult, op1=ALU.add),
                     reads=[("x", c, tb)] + modkeys(l), writes=[("h", c, tb)])

        def scale_alpha(tb, engs=("pool",)):
            for c in range(8):
                eng = engs[c % len(engs)]
                if eng == "act":
                    p.op(eng, lambda e, c=c: e.mul(out=xT[:, c, tb * TB:(tb + 1) * TB], in_=xT[:, c, tb * TB:(tb + 1) * TB], mul=float(ALPHA)),
                         reads=[("x", c, tb)], writes=[("x", c, tb)])
                else:
                    p.op(eng, lambda e, c=c: e.tensor_scalar_mul(out=xT[:, c, tb * TB:(tb + 1) * TB], in0=xT[:, c, tb * TB:(tb + 1) * TB], scalar1=float(ALPHA)),
                         reads=[("x", c, tb)], writes=[("x", c, tb)])

        def layernorm(l, gcol, bcol, cv, bank_m, bank_q, final=False, after_tb=None):
            def GB(col):
                return pp[:, l, col:col + 1] if final else ppA[:, l, col:col + 1]
            sq = [cv.get([128, TB]) for _ in range(2)]
            mean_sbs = [cv.get([128, TB]) for _ in range(2)]
            rstds = [cv.get([128, TB]) for _ in range(2)]
            tmp = [cv.get([128, TB]) for _ in range(2)]
            tmp2 = [cv.get([128, TB]) for _ in range(2)]
            bank_m0, bank_q0 = bank_m, bank_q
            for tb in range(NTB):
                sl = slice(tb * TB, (tb + 1) * TB)
                mean_sb = mean_sbs[tb % 2]
                rstd = rstds[tb % 2]
                bank_m = bank_m0 + 2 * (tb % 2)
                bank_q = bank_q0 + 2 * (tb % 2)
                KM = ("lnmean", tb % 2)
                KR = ("lnrstd", tb % 2)

                def mm_mean(e, sl=sl, bank_m=bank_m):
                    ins = None
                    for c in range(8):
                        ins = e.matmul(banks[bank_m][:, :], lhsT=onesD, rhs=xT[:, c, sl], start=(c == 0), stop=(c == 7))
                    return ins
                p.op("pe", mm_mean, reads=[("x", c, tb) for c in range(8)] + ["consts"], writes=[PS(bank_m)])
                for c in range(8):
                    p.op("act", lambda e, c=c, sl=sl: e.activation(out=sq[c % 2], in_=xT[:, c, sl], func=AF.Square), reads=[("x", c, tb)], writes=[("lnsq", c % 2)])
                    p.op("pe", lambda e, c=c, bank_q=bank_q: e.matmul(banks[bank_q][:, :], lhsT=onesD, rhs=sq[c % 2], start=(c == 0), stop=(c == 7)),
                         reads=[("lnsq", c % 2), "consts"], writes=[PS(bank_q)])
                p.op("act", lambda e, mean_sb=mean_sb, bank_m=bank_m: e.copy(out=mean_sb, in_=banks[bank_m][:, :]), reads=[PS(bank_m)], writes=[KM])
                p.op("dve", lambda e, rstd=rstd, mean_sb=mean_sb: e.tensor_tensor(out=rstd, in0=mean_sb, in1=mean_sb, op=ALU.mult), reads=[KM], writes=[KR])
                p.op("dve", lambda e, rstd=rstd, bank_q=bank_q: e.tensor_tensor(out=rstd, in0=banks[bank_q][:, :], in1=rstd, op=ALU.subtract), reads=[PS(bank_q), KR], writes=[KR])
                p.op("act", lambda e, rstd=rstd: e.activation(out=rstd, in_=rstd, func=AF.Ln, bias=LNEPS[:, 0:1]), reads=[KR, "lneps"], writes=[KR])
                p.op("act", lambda e, rstd=rstd: e.activation(out=rstd, in_=rstd, func=AF.Exp, scale=-0.5), reads=[KR], writes=[KR])
                for c in range(8):
                    k = c % 2
                    p.op("dve", lambda e, c=c, k=k, sl=sl, mean_sb=mean_sb: e.tensor_tensor(out=tmp[k], in0=xT[:, c, sl], in1=mean_sb, op=ALU.subtract),
                         reads=[("x", c, tb), KM], writes=[("lnt", k)])
                    p.op("pool", lambda e, k=k, rstd=rstd: e.tensor_tensor(out=tmp2[k], in0=tmp[k], in1=rstd, op=ALU.mult), reads=[("lnt", k), KR], writes=[("lnt2", k)])
                    p.op("act", lambda e, c=c, k=k, sl=sl: e.activation(out=xT[:, c, sl], in_=tmp2[k], func=AF.Identity, scale=GB(gcol + c), bias=GB(bcol + c)),
                         reads=[("lnt2", k), "pp", "ppA"], writes=[("x", c, tb)])
                if after_tb is not None:
                    after_tb(tb)

        ppA = sb("ppA_sb", [128, DEPTH, 32])
        p.op("dve", lambda e: e.tensor_scalar_mul(out=ppA[:], in0=pp[:, :, 0:32], scalar1=float(ALPHA)), reads=["pp"], writes=["ppA"])
        LNEPS = sb("lneps_sb", [128, 4])
        p.op("pool", lambda e: e.memset(LNEPS[:, 0:1], 1e-5), writes=["lneps"])
        p.op("pool", lambda e: e.memset(LNEPS[:, 1:2], 1e-6), reads=[], writes=["lneps"])
        p.op("pool", lambda e: e.memset(LNEPS[:, 2:3], 1.0), reads=[], writes=["lneps"])

        def router(l, cv, bank_l, bank_t):
            NI = 4
            rw = cv.get([128, 8, NE])
            rb = cv.get([128, NE])
            gT = cv.get([64, S])
            h32s = [cv.get([128, 8, 128]) for _ in range(NI)]
            Ws = [{n: cv.get([128, 64]) for n in ("sc", "bi", "eq", "b2", "mk", "sel", "gw", "gates")} for _ in range(NI)]
            Sms = [{n: cv.get([128, 8]) for n in ("m1", "m2", "gs", "t8", "gsel", "goff", "t8e")} for _ in range(NI)]
            s1s = [cv.get([128, 2]) for _ in range(NI)]
            p.dma("rw", rw, rw_d[l].rearrange("(kc p) e -> p kc e", p=128), writes=["rw"])
            p.dma("rb", rb, prow_d[l:l + 1, PR_RB:PR_RB + NE].partition_broadcast(128), writes=["rb"])
            g3 = lambda a: a.rearrange("p (g k) -> p g k", k=8)
            b3 = lambda a: a.unsqueeze(2).to_broadcast([128, 8, 8])

            def tile_ops(tt):
                j = tt % NI
                tb = tt // 4
                tsl = slice(tt * 128, (tt + 1) * 128)
                h32, W, Sm, s1 = h32s[j], Ws[j], Sms[j], s1s[j]
                bl, bt = 4 + j, 4 + j
                K = lambda n: (n, j)
                ops = []
                A = lambda eng, fn, r, w: ops.append((eng, fn, r, w))
                for c in range(8):
                    eng = ("dve", "pool")[c % 2]
                    A(eng, lambda e, c=c: e.tensor_scalar(out=h32[:, c, :], in0=xT[:, c, tsl], scalar1=MOD(l, 7, c), scalar2=MOD(l, 3, c), op0=ALU.mult, op1=ALU.add),
                      [("x", c, tb)] + modkeys(l), [("h32", j, c)])

                def mm(e):
                    ins = None
                    for c in range(8):
                        ins = e.matmul(banks[bl][:, 0:NE], lhsT=h32[:, c, :], rhs=rw[:, c, :], start=(c == 0), stop=(c == 7))
                    return ins
                A("pe", mm, [("h32", j, c) for c in range(8)] + ["rw"], [PS(bl)])
                A("act", lambda e: e.activation(out=W["sc"], in_=banks[bl][:, 0:NE], func=AF.Sigmoid), [PS(bl)], [K("r_sc")])
                V = lambda fn, r, w: A("dve", fn, r, w)
                V(lambda e: e.tensor_tensor(out=W["bi"], in0=W["sc"], in1=rb, op=ALU.add), [K("r_sc"), "rb"], [K("r_bi")])
                V(lambda e: e.tensor_reduce(out=Sm["m1"], in_=g3(W["bi"]), axis=AX.X, op=ALU.max), [K("r_bi")], [K("r_m1")])
                V(lambda e: e.tensor_tensor(out=g3(W["eq"]), in0=g3(W["bi"]), in1=b3(Sm["m1"]), op=ALU.is_equal), [K("r_bi"), K("r_m1")], [K("r_eq")])
                V(lambda e: e.scalar_tensor_tensor(out=W["b2"], in0=W["eq"], scalar=-10.0, in1=W["bi"], op0=ALU.mult, op1=ALU.add), [K("r_eq"), K("r_bi")], [K("r_b2")])
                V(lambda e: e.tensor_reduce(out=Sm["m2"], in_=g3(W["b2"]), axis=AX.X, op=ALU.max), [K("r_b2")], [K("r_m2")])
                V(lambda e: e.tensor_tensor(out=Sm["gs"], in0=Sm["m1"], in1=Sm["m2"], op=ALU.add), [K("r_m1"), K("r_m2")], [K("r_gs")])
                V(lambda e: e.max(out=Sm["t8"], in_=Sm["gs"]), [K("r_gs")], [K("r_t8")])
                V(lambda e: e.tensor_scalar(out=Sm["gsel"], in0=Sm["gs"], scalar1=Sm["t8"][:, 3:4], scalar2=None, op0=ALU.is_ge), [K("r_gs"), K("r_t8")], [K("r_gsel")])
                V(lambda e: e.tensor_scalar(out=Sm["goff"], in0=Sm["gsel"], scalar1=10.0, scalar2=-10.0, op0=ALU.mult, op1=ALU.add), [K("r_gsel")], [K("r_goff")])
                V(lambda e: e.tensor_tensor(out=g3(W["mk"]), in0=g3(W["bi"]), in1=b3(Sm["gsel"]), op=ALU.mult), [K("r_bi"), K("r_gsel")], [K("r_mk")])
                V(lambda e: e.tensor_tensor(out=g3(W["mk"]), in0=g3(W["mk"]), in1=b3(Sm["goff"]), op=ALU.add), [K("r_mk"), K("r_goff")], [K("r_mk")])
                V(lambda e: e.max(out=Sm["t8e"], in_=W["mk"]), [K("r_mk")], [K("r_t8e")])
                V(lambda e: e.tensor_scalar(out=W["sel"], in0=W["mk"], scalar1=Sm["t8e"][:, 7:8], scalar2=None, op0=ALU.is_ge), [K("r_mk"), K("r_t8e")], [K("r_sel")])
                V(lambda e: e.tensor_tensor(out=W["gw"], in0=W["sel"], in1=W["sc"], op=ALU.mult), [K("r_sel"), K("r_sc")], [K("r_gw")])
                V(lambda e: e.tensor_reduce(out=s1[:, 0:1], in_=W["gw"], axis=AX.X, op=ALU.add), [K("r_gw")], [K("r_s1")])
                V(lambda e: e.reciprocal(out=s1[:, 1:2], in_=s1[:, 0:1]), [K("r_s1")], [K("r_s2")])
                V(lambda e: e.tensor_scalar(out=W["gates"], in0=W["gw"], scalar1=s1[:, 1:2], scalar2=2.5, op0=ALU.mult, op1=ALU.mult), [K("r_gw"), K("r_s2")], [K("r_gates")])
                A("pe", lambda e: e.transpose(banks[bt][0:64, 0:128], W["gates"], ident), [K("r_gates"), "consts"], [PS(bt)])
                A("act", lambda e: e.copy(out=gT[:, tsl], in_=banks[bt][0:64, 0:128]), [PS(bt)], [("gT", tt)])
                return ops

            def group(tb):
                g0 = tb * NI
                lists = [tile_ops(tt) for tt in range(g0, g0 + NI)]
                for k in range(len(lists[0])):
                    for ol in lists:
                        eng, fn, r, w = ol[k]
                        p.op(eng, fn, reads=r, writes=w)

            def finish():
                p.dma("gst", gscr_d[l % 2], gT, reads=[("gT", tt) for tt in range(S // 128)], writes=[("gscr", l % 2)])
            return group, finish

        def moe(l, cv, hooks):
            stg = {n: cv.get([128, 8, 256]) for n in ("w1", "w3")}
            stg["w2"] = cv.get([128, 2, D])
            wbf = [{"w1": cv.get([128, 8, 256], BF16), "w3": cv.get([128, 8, 256], BF16), "w2": cv.get([128, 2, D], BF16)} for _ in range(2)]
            gbc = [cv.get([128, S]) for _ in range(2)]
            sS = [[cv.get([128, TB], BF16) for f in range(2)] for _ in range(2)]
            tS = [[cv.get([128, TB], BF16) for f in range(2)] for _ in range(2)]
            hid = [[cv.get([128, TB], BF16) for f in range(2)] for _ in range(2)]
            steps = [(e, tb) for e in range(n_exp) for tb in range(NTB)]

            def load(e):
                sl = e % 2
                if e < NE:
                    srcs = {"w1": ew1_d[l, e], "w3": ew3_d[l, e], "w2": ew2_d[l, e]}
                else:
                    srcs = {"w1": sw1_d[l], "w3": sw3_d[l], "w2": sw2_d[l]}
                for n in ("w1", "w3", "w2"):
                    pat = "(kc p) f -> p kc f"
                    p.dma(("wst", n), stg[n], srcs[n].rearrange(pat, p=128), writes=[("stg", n)])
                if e < NE:
                    p.dma(("gbc", sl), gbc[sl], gscr_d[l % 2, e:e + 1, :].partition_broadcast(128), reads=[("gscr", l % 2)], writes=[("gbc", sl)])

            def cast(e):
                sl = e % 2
                for n in ("w1", "w3", "w2"):
                    if n == "w2":
                        parts = [(slice(0, 1), "act"), (slice(1, 2), "pool")]
                    else:
                        parts = [(slice(0, 3), "act"), (slice(3, 8), "pool")]
                    for (ps_, ce) in parts:
                        if ce == "act":
                            p.op("act", lambda e_, n=n, sl=sl, ps_=ps_: e_.copy(out=wbf[sl][n][:, ps_, :], in_=stg[n][:, ps_, :]), reads=[("stg", n)], writes=[("wbf", sl, n, ce)])
                        else:
                            p.op("pool", lambda e_, n=n, sl=sl, ps_=ps_: e_.tensor_copy(out=wbf[sl][n][:, ps_, :], in_=stg[n][:, ps_, :]), reads=[("stg", n)], writes=[("wbf", sl, n, ce)])

            def up(i, f):
                e, tb = steps[i]
                sl = e % 2
                for wi, n in enumerate(("w1", "w3")):
                    bk = f * 2 + wi

                    def mm(e_, n=n, bk=bk, sl=sl, tb=tb, f=f):
                        ins = None
                        for kc in range(8):
                            ins = e_.matmul(banks[bk][:, :], lhsT=wbf[sl][n][:, kc, f * 128:(f + 1) * 128], rhs=hT[:, kc, tb * TB:(tb + 1) * TB], start=(kc == 0), stop=(kc == 7))
                        return ins
                    p.op("pe", mm, reads=[("wbf", sl, n, "act"), ("wbf", sl, n, "pool")] + [("h", kc, tb) for kc in range(8)], writes=[PS(bk)])

            def gating(i, f):
                e, tb = steps[i]
                sl = e % 2
                par = i % 2
                p.op("act", lambda e_: e_.activation(out=sS[par][f], in_=banks[f * 2][:, :], func=AF.Silu), reads=[PS(f * 2)], writes=[("sS", par, f)])
                if e < NE:
                    p.op("dve", lambda e_: e_.tensor_tensor(out=tS[par][f], in0=banks[f * 2 + 1][:, :], in1=gbc[sl][:, tb * TB:(tb + 1) * TB], op=ALU.mult),
                         reads=[PS(f * 2 + 1), ("gbc", sl)], writes=[("tS", par, f)])
                    p.op("dve", lambda e_: e_.tensor_tensor(out=hid[par][f], in0=sS[par][f], in1=tS[par][f], op=ALU.mult),
                         reads=[("sS", par, f), ("tS", par, f)], writes=[("hid", par, f)])
                else:
                    p.op("dve", lambda e_: e_.tensor_tensor(out=hid[par][f], in0=banks[f * 2 + 1][:, :], in1=sS[par][f], op=ALU.mult),
                         reads=[PS(f * 2 + 1), ("sS", par, f)], writes=[("hid", par, f)])

            def down(i, dh):
                e, tb = steps[i]
                sl = e % 2
                par = i % 2
                for dq in range(4):
                    d = dh * 4 + dq
                    bk = 4 + dq

                    def mm(e_, d=d, bk=bk):
                        ins = None
                        for f in range(2):
                            ins = e_.matmul(banks[bk][:, :], lhsT=wbf[sl]["w2"][:, f, d * 128:(d + 1) * 128], rhs=hid[par][f], start=(f == 0), stop=(f == 1))
                        return ins
                    p.op("pe", mm, reads=[("wbf", sl, "w2", "act"), ("wbf", sl, "w2", "pool"), ("hid", par, 0), ("hid", par, 1)], writes=[PS(bk)])
                    p.op("dve", lambda e_, d=d, bk=bk: e_.scalar_tensor_tensor(out=xT[:, d, tb * TB:(tb + 1) * TB], in0=banks[bk][:, :], scalar=MOD(l, 5, d), in1=xT[:, d, tb * TB:(tb + 1) * TB], op0=ALU.mult, op1=ALU.add),
                         reads=[PS(bk), ("x", d, tb)] + modkeys(l), writes=[("x", d, tb)])

            load(0)
            cast(0)
            if n_exp > 1:
                load(1)
                cast(1)
            up(0, 0)
            up(0, 1)
            gating(0, 0)
            gating(0, 1)
            for i in range(len(steps)):
                e, tb = steps[i]
                if tb == 0 and i > 0 and e + 1 < n_exp:
                    load(e + 1)
                if tb == 2 and e > 0 and e + 1 < n_exp:
                    cast(e + 1)
                if tb == 1 and e in hooks:
                    hooks[e]()
                nxt = i + 1 < len(steps)
                if nxt:
                    up(i + 1, 0)
                down(i, 0)
                if nxt:
                    gating(i + 1, 0)
                    up(i + 1, 1)
                down(i, 1)
                if nxt:
                    gating(i + 1, 1)


        def ssd_units(l, cv, mark, load_win, load_wout, proj_fm, proj_tm, out_proj, conv_silu, yblk, identb, ones512b):
            cv.off = mark
            dtb = cv.get([128, 16]); alog = cv.get([128, 16]); aneg = cv.get([128, 16])
            cbuf = [cv.get([128, 3 + TB]) for _ in range(6)]
            ctmps = [cv.get([128, TB]) for _ in range(3)]
            ctmp = ctmps[0]
            xs = [cv.get([128, TB], BF16) for _ in range(4)]
            BT = cv.get([128, TB], BF16); CT = cv.get([128, TB], BF16)
            sz = [cv.get([128, TB], BF16) for _ in range(4)]
            yg = cv.get([128, 4, TB])
            dt_tm = cv.get([128, 8]); dA = cv.get([128, 8]); acs = cv.get([128, 8]); dte = cv.get([128, 8])
            w2 = cv.get([128, 8]); draw = cv.get([128, 8]); ex = cv.get([128, 8])
            dAb = cv.get([128, 8, 128])
            L = dAb
            Btmzs = [[cv.get([128, 128], BF16) for _ in range(2)] for _ in range(2)]
            decbcs = [cv.get([128, 2, 8]) for _ in range(2)]
            eD = cv.get([128, 8, 128], BF16)
            cbm = cv.get([128, 128])
            MTs = [cv.get([128, 8, 128], BF16) for _ in range(2)]
            CTss = [cv.get([128, 8, 128], BF16) for _ in range(2)]
            xdts = [cv.get([128, 8, 64], BF16) for _ in range(2)]
            xws = [cv.get([128, 8, 64], BF16) for _ in range(2)]
            pa_ctr = [0]
            Btm = cv.get([128, 128], BF16)
            S32 = cv.get([128, 8, 64])
            Sbf = [cv.get([128, 8, 64], BF16) for _ in range(2)]
            sqb = cv.get([128, TB], BF16); rs = ctmp
            b7 = banks[7][:, :].bitcast(BF16)
            MASK = consts[:, C_MASK:C_MASK + 128]
            SUF = consts[:, C_SUF:C_SUF + 128]
            p.dma("dtb", dtb, prow_d[l:l + 1, PR_DTB:PR_DTB + 16].partition_broadcast(128), writes=["dtb"])
            p.dma("alog", alog, prow_d[l:l + 1, PR_ALOG:PR_ALOG + 16].partition_broadcast(128), writes=["alog"])
            p.op("act", lambda e: e.activation(out=aneg, in_=alog, func=AF.Exp), reads=["alog"], writes=["aneg"])
            p.op("dve", lambda e: e.tensor_scalar_mul(out=aneg, in0=aneg, scalar1=-1.0), reads=["aneg"], writes=["aneg"])
            for g in range(2):
                load_win(3600 + g * 512, 512, 0)
                load_win(2576 + g * 512, 512, 512)
                load_win(4624 + g * 128, 128, 1024)
                load_win(4880 + g * 128, 128, 1152)
                load_win(5136 + g * 8, 8, 1280)
                load_wout(8 + g * 4, 4)
                for j6 in range(6):
                    p.op("pool", lambda e, j6=j6: e.memset(cbuf[j6][:, 0:3], 0.0), writes=[("cbuf", j6)])
                p.op("pool", lambda e: e.memset(S32, 0.0), writes=["s_S32"])
                p.op("pool", lambda e: e.memset(Sbf[0], 0.0), writes=[("s_Sbf", 0)])
                for pa in range(2):
                    for half in range(2):
                        p.op("pool", lambda e, half=half, pa=pa: e.memset(Btmzs[pa][half], 0.0), writes=[("s_Btmz", pa, half)])
                par = 0
                gs = slice(g * 8, g * 8 + 8)
                for tb in range(NTB):
                    def conv_chain(j6):
                        off = j6 * 128 if j6 < 4 else (1024 if j6 == 4 else 1152)
                        jc = g * 4 + j6 if j6 < 4 else (8 + g if j6 == 4 else 10 + g)
                        bank = j6
                        proj_fm(off, 128, tb, bank)
                        dst = xs[j6] if j6 < 4 else (BT if j6 == 4 else CT)
                        conv_silu(cbuf[j6], ("cbuf", j6), tb, bank, PP_SCW + jc * 4, PP_SCB + jc, ctmps[j6 % 3], ("s_ctmp", j6 % 3), dst, ("s_fm", j6), AF.Silu, eng="dve")

                    def z_chain(q):
                        bank = 6 + q % 2
                        proj_fm(512 + q * 128, 128, tb, bank)
                        p.op("act", lambda e: e.activation(out=sz[q], in_=banks[bank][:, :], func=AF.Silu), reads=[PS(bank)], writes=[("s_sz", q)])

                    replay_ops(interleave([cap_ops(conv_chain, 0), cap_ops(conv_chain, 1), cap_ops(conv_chain, 2), cap_ops(z_chain, 0), cap_ops(z_chain, 1)]))
                    replay_ops(interleave([cap_ops(conv_chain, 3), cap_ops(conv_chain, 4), cap_ops(conv_chain, 5), cap_ops(z_chain, 2), cap_ops(z_chain, 3)]))
                    def _aliases(pa):
                        return MTs[pa], CTss[pa], xdts[pa], xws[pa], Btmzs[pa], decbcs[pa]

                    def stageA(tt, pa):
                        MT, CTs, xdt, xw, Btmz, decbc = _aliases(pa)
                        KP = lambda n: (n, pa)
                        tsl = slice(tt * 128, (tt + 1) * 128)
                        proj_tm(1280, 8, tb, tt, 5)
                        p.op("dve", lambda e, gs=gs: e.tensor_tensor(out=draw, in0=banks[5][:, 0:8], in1=dtb[:, gs], op=ALU.add), reads=[PS(5), "dtb"], writes=["s_draw"])
                        p.op("act", lambda e: e.activation(out=ex, in_=draw, func=AF.Exp), reads=["s_draw"], writes=["s_ex"])
                        p.op("act", lambda e: e.activation(out=dt_tm, in_=ex, func=AF.Ln, bias=LNEPS[:, 2:3]), reads=["s_ex", "lneps"], writes=["s_dt"])
                        p.op("dve", lambda e, gs=gs: e.tensor_tensor(out=dA, in0=dt_tm, in1=aneg[:, gs], op=ALU.mult), reads=["s_dt", "aneg"], writes=["s_dA"])

                        def mm5(e):
                            e.matmul(banks[5][:, 8:16], lhsT=MASK, rhs=dA, start=True, stop=True)
                            e.matmul(banks[5][:, 16:24], lhsT=SUF, rhs=dA, start=True, stop=True)
                            e.matmul(banks[5][:, 160:168], lhsT=consts[:, C_CH0:C_CH0 + 128], rhs=dA, start=True, stop=True)
                            return e.matmul(banks[5][:, 168:176], lhsT=consts[:, C_CH1:C_CH1 + 128], rhs=dA, start=True, stop=True)
                        p.op("pe", mm5, reads=["s_dA", "consts"], writes=[PS(5)])
                        p.op("act", lambda e: e.copy(out=acs, in_=banks[5][:, 8:16]), reads=[PS(5)], writes=["s_acs"])
                        p.op("act", lambda e: e.activation(out=dte, in_=banks[5][:, 16:24], func=AF.Exp), reads=[PS(5)], writes=["s_dte"])
                        p.op("act", lambda e: e.copy(out=dAb, in_=dA.unsqueeze(2).to_broadcast([128, 8, 128])), reads=["s_dA"], writes=["s_dAb", ("s_L", 0), ("s_L", 1), "s_Lm", "s_Le"])
                        p.op("act", lambda e: e.activation(out=decbc, in_=banks[5][:, 160:176].rearrange("p (a b) -> p a b", a=2), func=AF.Exp), reads=[PS(5)], writes=[KP("s_dec")])
                        p.op("dve", lambda e: e.tensor_tensor(out=w2, in0=dt_tm, in1=dte, op=ALU.mult), reads=["s_dt", "s_dte"], writes=["s_w2"])

                        def mmD(e):
                            ins = None
                            for h in range(8):
                                ins = e.matmul(banks[2 + h // 4][:, (h % 4) * 128:(h % 4 + 1) * 128], lhsT=dAb[:, h, :], rhs=MASK, start=True, stop=True)
                            return ins
                        p.op("pe", mmD, reads=["s_dAb", "consts"], writes=[PS(2), PS(3)])
                        for k in range(2):
                            p.op("dve", lambda e, k=k: e.tensor_tensor(out=L[:, 4 * k:4 * k + 4, :], in0=banks[2 + k][:, :].rearrange("p (a b) -> p a b", a=4),
                                                                       in1=acs[:, 4 * k:4 * k + 4].unsqueeze(2).to_broadcast([128, 4, 128]), op=ALU.subtract),
                                 reads=[PS(2 + k), "s_acs"], writes=[("s_L", k)])
                            p.op("act", lambda e, k=k: e.activation(out=eD[:, 4 * k:4 * k + 4, :], in_=banks[2 + k][:, :].rearrange("p (a b) -> p a b", a=4), func=AF.Exp), reads=[PS(2 + k)], writes=[("s_eD", k)])
                        p.op("dve", lambda e: e.tensor_scalar_min(out=L, in0=L, scalar1=0.0), reads=[("s_L", 0), ("s_L", 1)], writes=["s_Lm"])
                        p.op("act", lambda e: e.activation(out=L, in_=L, func=AF.Exp), reads=["s_Lm"], writes=["s_Le"])
                        p.op("pe", lambda e, tsl=tsl: e.matmul(banks[5][:, 256:384], lhsT=BT[:, tsl], rhs=CT[:, tsl], start=True, stop=True), reads=[("s_fm", 4), ("s_fm", 5)], writes=[PS(5)])
                        p.op("dve", lambda e: e.tensor_tensor(out=cbm, in0=banks[5][:, 256:384], in1=MASK, op=ALU.mult), reads=[PS(5), "consts"], writes=["s_cbm"])
                        p.op("dve", lambda e: e.tensor_tensor(out=MT, in0=L, in1=cbm.unsqueeze(1).to_broadcast([128, 8, 128]), op=ALU.mult), reads=["s_Le", "s_cbm"], writes=[KP("s_MT")])
                        p.op("dve", lambda e, tsl=tsl: e.tensor_tensor(out=CTs, in0=eD, in1=CT[:, tsl].unsqueeze(1).to_broadcast([128, 8, 128]), op=ALU.mult), reads=[("s_eD", 0), ("s_eD", 1), ("s_fm", 5)], writes=[KP("s_CTs")])

                        def mmT(e, tsl=tsl):
                            for q in range(4):
                                e.transpose(b7[:, q * 128:(q + 1) * 128], xs[q][:, tsl], identb)
                            return e.transpose(b7[:, 512:640], BT[:, tsl], identb)
                        p.op("pe", mmT, reads=[("s_fm", j) for j in range(5)] + ["identb"], writes=[PS(7)])
                        xtm = b7[:, 0:512].rearrange("p (h k) -> p h k", h=8)
                        p.op("dve", lambda e: e.tensor_tensor(out=xdt, in0=xtm, in1=dt_tm.unsqueeze(2).to_broadcast([128, 8, 64]), op=ALU.mult), reads=[PS(7), "s_dt"], writes=[KP("s_xdt")])
                        p.op("dve", lambda e: e.tensor_tensor(out=xw, in0=xtm, in1=w2.unsqueeze(2).to_broadcast([128, 8, 64]), op=ALU.mult), reads=[PS(7), "s_w2"], writes=[KP("s_xw")])
                        for half in range(2):
                            p.op("act", lambda e, half=half: e.copy(out=Btmz[half][half * 64:(half + 1) * 64, :], in_=b7[half * 64:(half + 1) * 64, 512:640]), reads=[PS(7)], writes=[("s_Btmz", pa, half)])


                    def stageB(tt, pa, par):
                        MT, CTs, xdt, xw, Btmz, decbc = _aliases(pa)
                        KP = lambda n: (n, pa)
                        tsl = slice(tt * 128, (tt + 1) * 128)
                        def mmY(e):
                            ins = None
                            for h in range(8):
                                q, hq = h // 2, h % 2
                                ins = e.matmul(banks[4][hq * 64:(hq + 1) * 64, q * 128:(q + 1) * 128], lhsT=xdt[:, h, :], rhs=MT[:, h, :], start=True, stop=True)
                            return ins
                        p.op("pe", mmY, reads=[KP("s_xdt"), KP("s_MT")], writes=[PS(4)])
                        for half in range(2):
                            hs = slice(half * 64, (half + 1) * 64)

                            def mmO(e, half=half, par=par):
                                ins = None
                                for h in range(8):
                                    q, hq = h // 2, h % 2
                                    ins = e.matmul(banks[0][hq * 64:(hq + 1) * 64, q * 128 + half * 64:q * 128 + half * 64 + 64], lhsT=Sbf[par][:, h, :], rhs=CTs[:, h, half * 64:(half + 1) * 64], start=True, stop=True)
                                return ins
                            p.op("pe", mmO, reads=[("s_Sbf", par), KP("s_CTs")], writes=[("ps0o", half)] + ([PS(0)] if half == 0 else []))
                            p.op("pe", lambda e, half=half: e.matmul(banks[6][:, :], lhsT=Btmz[half], rhs=xw.rearrange("p h k -> p (h k)"), start=True, stop=True), reads=[("s_Btmz", pa, half), KP("s_xw")], writes=[PS(6)])
                            p.op("dve", lambda e, half=half: e.tensor_tensor(out=S32, in0=S32, in1=decbc[:, half, :].unsqueeze(2).to_broadcast([128, 8, 64]), op=ALU.mult), reads=["s_S32", KP("s_dec")], writes=["s_S32"])
                            p.op("dve", lambda e: e.tensor_tensor(out=S32, in0=S32, in1=banks[6][:, :].rearrange("p (h k) -> p h k", h=8), op=ALU.add), reads=["s_S32", PS(6)], writes=["s_S32"])
                            p.op("act", lambda e, par=par: e.copy(out=Sbf[1 - par], in_=S32), reads=["s_S32"], writes=[("s_Sbf", 1 - par)])
                            par = 1 - par
                        for q in range(4):
                            p.op("dve", lambda e, q=q, tsl=tsl, g=g: e.scalar_tensor_tensor(out=yg[:, q, tsl], in0=xs[q][:, tsl], scalar=PPc(l, PP_SD + g * 4 + q), in1=banks[4][:, q * 128:(q + 1) * 128], op0=ALU.mult, op1=ALU.add),
                                 reads=[("s_fm", q), PS(4), "pp"], writes=[("s_yg", q)])
                            p.op("dve", lambda e, q=q, tsl=tsl: e.tensor_tensor(out=yg[:, q, tsl], in0=yg[:, q, tsl], in1=banks[0][:, q * 128:(q + 1) * 128], op=ALU.add),
                                 reads=[("s_yg", q), PS(0), ("ps0o", 0), ("ps0o", 1)], writes=[("s_yg", q)])
                        return par

                    def capture(fn, *a):
                        lst = []
                        p.cap = lst
                        r = fn(*a)
                        p.cap = None
                        return lst, r

                    def replay(lst):
                        for (eng, fn, r, w) in lst:
                            p.op(eng, fn, reads=r, writes=w)

                    def merge(la, lb):
                        out = []
                        ia = ib = 0
                        na, nb = len(la), len(lb)
                        while ia < na or ib < nb:
                            if ib >= nb or (ia < na and ia * nb <= ib * na):
                                out.append(la[ia]); ia += 1
                            else:
                                out.append(lb[ib]); ib += 1
                        return out

                    lA, _ = capture(stageA, 0, pa_ctr[0] % 2)
                    replay(lA)
                    for tt in range(4):
                        pa = pa_ctr[0] % 2
                        lB, par = capture(stageB, tt, pa, par)
                        if tt + 1 < 4:
                            lA, _ = capture(stageA, tt + 1, (pa_ctr[0] + 1) % 2)
                            replay(merge(lA, lB))
                        else:
                            replay(lB)
                        pa_ctr[0] += 1
                    for q in range(4):
                        p.op("dve", lambda e, q=q: e.tensor_tensor(out=yg[:, q, :], in0=yg[:, q, :], in1=sz[q], op=ALU.mult), reads=[("s_yg", q), ("s_sz", q)], writes=[("s_yg", q)])
                    for q in range(4):
                        p.op("act", lambda e, q=q: e.activation(out=sqb, in_=yg[:, q, :], func=AF.Square), reads=[("s_yg", q)], writes=["s_sqb"])
                        p.op("pe", lambda e, q=q: e.matmul(banks[5][:, :], lhsT=ones512b, rhs=sqb, start=(q == 0), stop=(q == 3)), reads=["s_sqb", "ones512b"], writes=[PS(5)])
                    p.op("act", lambda e: e.activation(out=rs, in_=banks[5][:, :], func=AF.Ln, bias=LNEPS[:, 1:2]), reads=[PS(5), "lneps"], writes=[("s_ctmp", 0)])
                    p.op("act", lambda e: e.activation(out=rs, in_=rs, func=AF.Exp, scale=-0.5), reads=[("s_ctmp", 0)], writes=[("s_ctmp", 0)])
                    for q in range(4):
                        p.op("dve", lambda e, q=q, g=g: e.scalar_tensor_tensor(out=yblk[:, q, :], in0=yg[:, q, :], scalar=PPc(l, PP_SNORM + g * 4 + q), in1=rs, op0=ALU.mult, op1=ALU.mult),
                             reads=[("s_yg", q), ("s_ctmp", 0), "pp"], writes=[("yblk", q)])
                    out_proj(tb, 4, [0, 1])

        def mixer(l, ada_next=None):
            cv = Carve()
            wst = [cv.get([128, 8, 128]) for _ in range(2)]
            wunit = cv.get([128, 8, 1408], BF16)
            woutst = [cv.get([128, D])] * 2
            wout = cv.get([128, 4, D], BF16)
            yblk = cv.get([128, 4, TB], BF16)
            identb = cv.get([128, 128], BF16)
            ones128b = cv.get([128, 128], BF16)
            ones512b = cv.get([128, 128], BF16)
            p.op("act", lambda e: e.copy(out=identb, in_=ident), reads=["consts"], writes=["identb"])
            p.op("pool", lambda e: e.memset(ones128b, 1.0 / 128.0), writes=["ones128b"])
            p.op("pool", lambda e: e.memset(ones512b, 1.0 / 512.0), writes=["ones512b"])
            wcnt = [0]

            def load_win(col0, ncols, dst):
                c = 0
                while c < ncols:
                    n = min(128, ncols - c)
                    k = wcnt[0] % 2
                    wcnt[0] += 1
                    p.dma(("wst", k), wst[k][:, :, 0:n], win_d[l].rearrange("(kc p) f -> p kc f", p=128)[:, :, col0 + c:col0 + c + n], writes=[("wst", k)])
                    p.op("pool", lambda e, k=k, n=n, c=c: e.tensor_copy(out=wunit[:, :, dst + c:dst + c + n], in_=wst[k][:, :, 0:n]), reads=[("wst", k)], writes=[("wunit", (dst + c) // 128)])
                    c += n

            def load_wout(ych0, n):
                for j in range(n):
                    k = 0
                    p.dma(("wost", k), woutst[k], wout_d[l, (ych0 + j) * 128:(ych0 + j + 1) * 128, :], writes=[("wost", k)])
                    p.op("pool", lambda e, k=k, j=j: e.tensor_copy(out=wout[:, j, :], in_=woutst[k]), reads=[("wost", k)], writes=[("wout", j)])

            def proj_fm(off, ncols, tb, bank):
                def mm(e):
                    ins = None
                    for kc in range(8):
                        ins = e.matmul(banks[bank][0:ncols, :], lhsT=wunit[:, kc, off:off + ncols], rhs=hT[:, kc, tb * TB:(tb + 1) * TB], start=(kc == 0), stop=(kc == 7))
                    return ins
                p.op("pe", mm, reads=[("wunit", b) for b in range(off // 128, (off + ncols - 1) // 128 + 1)] + [("h", kc, tb) for kc in range(8)], writes=[PS(bank)])

            def proj_tm(off, ncols, tb, tt, bank, col0=0):
                t0 = tb * TB + tt * 128

                def mm(e):
                    ins = None
                    for kc in range(8):
                        ins = e.matmul(banks[bank][:, col0:col0 + ncols], lhsT=hT[:, kc, t0:t0 + 128], rhs=wunit[:, kc, off:off + ncols], start=(kc == 0), stop=(kc == 7))
                    return ins
                p.op("pe", mm, reads=[("wunit", b) for b in range(off // 128, (off + ncols - 1) // 128 + 1)] + [("h", kc, tb) for kc in range(8)], writes=[PS(bank)])

            def out_proj(tb, nych, bks):
                for d in range(8):
                    bk = bks[d % len(bks)]

                    def mm(e, d=d, bk=bk):
                        ins = None
                        for j in range(nych):
                            ins = e.matmul(banks[bk][:, :], lhsT=wout[:, j, d * 128:(d + 1) * 128], rhs=yblk[:, j, :], start=(j == 0), stop=(j == nych - 1))
                        return ins
                    p.op("pe", mm, reads=[("wout", j) for j in range(nych)] + [("yblk", j) for j in range(nych)], writes=[PS(bk)])
                    p.op("dve", lambda e, d=d, bk=bk: e.scalar_tensor_tensor(out=xT[:, d, tb * TB:(tb + 1) * TB], in0=banks[bk][:, :], scalar=MOD(l, 2, d), in1=xT[:, d, tb * TB:(tb + 1) * TB], op0=ALU.mult, op1=ALU.add),
                         reads=[PS(bk), ("x", d, tb)] + modkeys(l), writes=[("x", d, tb)])

            def conv_silu(buf, key, tb, bank, wcol, bcol, tmp, tmpkey, dst, dstkey, act_func, eng="dve"):
                p.op("act", lambda e: e.copy(out=buf[:, 3:3 + TB], in_=banks[bank][:, :]), reads=[PS(bank)], writes=[key])
                p.op(eng, lambda e: e.tensor_scalar(out=tmp, in0=buf[:, 0:TB], scalar1=PPc(l, wcol), scalar2=PPc(l, bcol), op0=ALU.mult, op1=ALU.add), reads=[key, "pp"], writes=[tmpkey])
                for k in range(1, 4):
                    p.op(eng, lambda e, k=k: e.scalar_tensor_tensor(out=tmp, in0=buf[:, k:k + TB], scalar=PPc(l, wcol + k), in1=tmp, op0=ALU.mult, op1=ALU.add), reads=[key, tmpkey, "pp"], writes=[tmpkey])
                p.op("pool", lambda e: e.tensor_copy(out=buf[:, 0:3], in_=buf[:, TB:TB + 3]), reads=[key], writes=[key])
                if dst is not None:
                    p.op("act", lambda e: e.activation(out=dst, in_=tmp, func=act_func), reads=[tmpkey], writes=[dstkey])

            mark = cv.off

            if do_lru:
                cv.off = mark
                wabd = cv.get([128, 4, 128], BF16)
                wxbd = cv.get([128, 4, 128], BF16)
                nsp8 = cv.get([128, 4])
                hcar = cv.get([128, 4])
                xbuf = [cv.get([128, 3 + TB]) for _ in range(4)]
                Ts = [{n: cv.get([128, TB]) for n in ("xc", "r", "i", "om", "h", "gg")} for _ in range(4)]
                xcbs = [cv.get([128, TB], BF16) for _ in range(4)]
                for nm, src, dst in (("wa", wabd_d, wabd), ("wx", wxbd_d, wxbd)):
                    p.dma(("wost", 0), woutst[0][:, 0:512].rearrange("p (m j) -> p m j", m=4), src[l].rearrange("m i j -> i m j"), writes=[("wost", 0)])
                    p.op("pool", lambda e, dst=dst: e.tensor_copy(out=dst, in_=woutst[0][:, 0:512].rearrange("p (m j) -> p m j", m=4)), reads=[("wost", 0)], writes=[nm])
                p.op("act", lambda e: e.activation(out=nsp8, in_=pp[:, l, PP_LLAM:PP_LLAM + 4], func=AF.Exp, scale=-1.0), reads=["pp"], writes=["nsp8"])
                p.op("act", lambda e: e.activation(out=nsp8, in_=nsp8, func=AF.Ln, bias=LNEPS[:, 2:3]), reads=["nsp8", "lneps"], writes=["nsp8"])
                p.op("dve", lambda e: e.tensor_scalar_mul(out=nsp8, in0=nsp8, scalar1=-8.0), reads=["nsp8"], writes=["nsp8"])
                for m in range(4):
                    p.op("pool", lambda e, m=m: e.memset(xbuf[m][:, 0:3], 0.0), writes=[("xbuf", m)])
                load_win(0, 1024, 0)
                load_wout(0, 4)

                def lru_chain(tb, m):
                    T = Ts[m]
                    xcb = xcbs[m]
                    BA, BB = 2 * m, 2 * m + 1
                    KK = lambda n: (n, m)
                    ops = []
                    A = lambda eng, fn, r, w: ops.append((eng, fn, r, w))
                    buf = xbuf[m]
                    key = ("xbuf", m)
                    wcol, bcol = PP_LCW + m * 4, PP_LCB + m
                    sl_h = [("h", kc, tb) for kc in range(8)]

                    def mmx(e):
                        ins = None
                        for kc in range(8):
                            ins = e.matmul(banks[BA][:, :], lhsT=wunit[:, kc, m * 128:(m + 1) * 128], rhs=hT[:, kc, tb * TB:(tb + 1) * TB], start=(kc == 0), stop=(kc == 7))
                        return ins

                    def mmg(e):
                        ins = None
                        for kc in range(8):
                            ins = e.matmul(banks[BB][:, :], lhsT=wunit[:, kc, 512 + m * 128:512 + (m + 1) * 128], rhs=hT[:, kc, tb * TB:(tb + 1) * TB], start=(kc == 0), stop=(kc == 7))
                        return ins
                    A("pe", mmx, [("wunit", m)] + sl_h, [PS(BA)])
                    A("pe", mmg, [("wunit", 4 + m)] + sl_h, [PS(BB)])
                    A("act", lambda e: e.copy(out=buf[:, 3:3 + TB], in_=banks[BA][:, :]), [PS(BA)], [key])
                    A("act", lambda e: e.activation(out=T["gg"], in_=banks[BB][:, :], func=AF.Gelu_apprx_tanh), [PS(BB)], [KK("l_gg")])
                    A("dve", lambda e: e.tensor_scalar(out=T["xc"], in0=buf[:, 0:TB], scalar1=PPc(l, wcol), scalar2=PPc(l, bcol), op0=ALU.mult, op1=ALU.add), [key, "pp"], [KK("l_xc")])
                    for k in range(1, 4):
                        A("dve", lambda e, k=k: e.scalar_tensor_tensor(out=T["xc"], in0=buf[:, k:k + TB], scalar=PPc(l, wcol + k), in1=T["xc"], op0=ALU.mult, op1=ALU.add), [key, KK("l_xc"), "pp"], [KK("l_xc")])
                    A("pool", lambda e: e.tensor_copy(out=buf[:, 0:3], in_=buf[:, TB:TB + 3]), [key], [key])
                    A("act", lambda e: e.copy(out=xcb, in_=T["xc"]), [KK("l_xc")], [KK("l_xcb")])
                    A("pe", lambda e: e.matmul(banks[BA][:, :], lhsT=wabd[:, m, :], rhs=xcb, start=True, stop=True), ["wa", KK("l_xcb")], [PS(BA)])
                    A("pe", lambda e: e.matmul(banks[BB][:, :], lhsT=wxbd[:, m, :], rhs=xcb, start=True, stop=True), ["wx", KK("l_xcb")], [PS(BB)])
                    A("act", lambda e: e.activation(out=T["r"], in_=banks[BA][:, :], func=AF.Sigmoid, bias=PPc(l, PP_LBA + m)), [PS(BA), "pp"], [KK("l_r")])
                    A("act", lambda e: e.activation(out=T["i"], in_=banks[BB][:, :], func=AF.Sigmoid, bias=PPc(l, PP_LBX + m)), [PS(BB), "pp"], [KK("l_i")])
                    A("act", lambda e: e.activation(out=T["r"], in_=T["r"], func=AF.Exp, scale=nsp8[:, m:m + 1]), [KK("l_r"), "nsp8"], [KK("l_r")])
                    A("pool", lambda e: e.tensor_tensor(out=T["om"], in0=T["r"], in1=T["r"], op=ALU.mult), [KK("l_r")], [KK("l_om")])
                    A("pool", lambda e: e.tensor_scalar(out=T["om"], in0=T["om"], scalar1=-1.0, scalar2=1.0, op0=ALU.mult, op1=ALU.add), [KK("l_om")], [KK("l_om")])
                    A("act", lambda e: e.activation(out=T["om"], in_=T["om"], func=AF.Sqrt), [KK("l_om")], [KK("l_om")])
                    A("pool", lambda e: e.tensor_tensor(out=T["i"], in0=T["i"], in1=T["xc"], op=ALU.mult), [KK("l_i"), KK("l_xc")], [KK("l_i")])
                    A("dve", lambda e: e.tensor_tensor(out=T["om"], in0=T["om"], in1=T["i"], op=ALU.mult), [KK("l_om"), KK("l_i")], [KK("l_om")])
                    if tb == 0:
                        A("dve", lambda e: e.tensor_tensor_scan(out=T["h"], data0=T["r"], data1=T["om"], initial=0.0, op0=ALU.mult, op1=ALU.add), [KK("l_r"), KK("l_om")], [KK("l_h")])
                    else:
                        A("dve", lambda e: e.tensor_tensor_scan(out=T["h"], data0=T["r"], data1=T["om"], initial=hcar[:, m:m + 1], op0=ALU.mult, op1=ALU.add), [KK("l_r"), KK("l_om"), ("hcar", m)], [KK("l_h")])
                    A("pool", lambda e: e.tensor_copy(out=hcar[:, m:m + 1], in_=T["h"][:, TB - 1:TB]), [KK("l_h")], [("hcar", m)])
                    A("dve", lambda e: e.tensor_tensor(out=yblk[:, m, :], in0=T["h"], in1=T["gg"], op=ALU.mult), [KK("l_h"), KK("l_gg")], [("yblk", m)])
                    return ops

                for tb in range(NTB):
                    lists = [lru_chain(tb, m) for m in range(4)]
                    for k in range(len(lists[0])):
                        for ol in lists:
                            eng, fn, r, w = ol[k]
                            p.op(eng, fn, reads=r, writes=w)
                    out_proj(tb, 4, [0, 1, 2, 3, 4, 5, 6, 7])

            if do_gla:
                cv.off = mark
                walb = cv.get([32, 256], BF16)
                rT = cv.get([32, TB], BF16)
                e1 = cv.get([128, 128])
                l1 = cv.get([128, 128])
                ecp = cv.get([128, TB])
                ecn = cv.get([128, TB])
                esuf = [cv.get([128, 128]) for _ in range(4)]
                qd = cv.get([128, TB], BF16)
                kd = cv.get([128, TB], BF16)
                kend = [cv.get([128, 128], BF16) for _ in range(4)]
                kendz = [[cv.get([128, 128], BF16) for _ in range(2)] for _ in range(4)]
                qdz = [cv.get([128, TB], BF16) for _ in range(2)]
                CHM = [consts[:, C_CH0:C_CH0 + 128], consts[:, C_CH1:C_CH1 + 128]]
                vtm = [cv.get([128, 256], BF16) for _ in range(4)]
                sg = [cv.get([128, TB]) for _ in range(2)]
                attms = [cv.get([128, 4, 128], BF16) for _ in range(2)]
                S32 = cv.get([128, 128])
                Sbfs = [[cv.get([128, 128], BF16) for _ in range(2)] for _ in range(2)]
                sqbs = [cv.get([128, TB], BF16) for _ in range(2)]
                rss = [cv.get([128, TB]) for _ in range(2)]
                t1s = [cv.get([128, TB]) for _ in range(2)]
                ada_bufs_m = ada_bufs_alloc(cv) if ada_next is not None else None
                p.dma(("wost", 0), woutst[0][0:32, 0:256], walpha_d[l], writes=[("wost", 0)])
                p.op("pool", lambda e: e.tensor_copy(out=walb, in_=woutst[0][0:32, 0:256]), reads=[("wost", 0)], writes=["walb"])
                p.op("pool", lambda e: e.memset(rT, 1.0), writes=["rT"])
                m64b = consts[:, C_MASK:C_MASK + 128].unsqueeze(1).to_broadcast([128, 4, 128])
                for hp in range(2):
                    load_win(1024 + hp * 128, 128, 0)
                    load_win(1280 + hp * 128, 128, 128)
                    load_win(1536 + hp * 256, 256, 256)
                    load_win(2048 + hp * 256, 256, 512)
                    load_win(2560, 16, 768)
                    load_wout(4 + hp * 2, 2)
                    p.op("pool", lambda e: e.memset(S32, 0.0), writes=[("S32", 0), ("S32", 1)])
                    for hh in range(2):
                        for pr in range(2):
                            p.op("pool", lambda e, hh=hh, pr=pr: e.memset(Sbfs[hh][pr], 0.0), writes=[("Sbf", hh, pr)])
                    if hp == 0:
                        for hh in range(2):
                            p.op("pool", lambda e, hh=hh: e.memset(qdz[hh], 0.0), writes=[("g_qdz", hh)])
                        for tt in range(4):
                            for half in range(2):
                                p.op("pool", lambda e, tt=tt, half=half: e.memset(kendz[tt][half], 0.0), writes=[("g_kendz", tt, half)])
                    par_state = {0: 0, 1: 0}
                    for tb in range(NTB):
                        proj_fm(768, 16, tb, 0)
                        p.op("act", lambda e: e.copy(out=rT[0:16, :], in_=banks[0][0:16, :]), reads=[PS(0)], writes=["rT"])
                        for tt in range(4):
                            tsl = slice(tt * 128, (tt + 1) * 128)
                            p.op("pe", lambda e, tsl=tsl, hp=hp: e.matmul(banks[5][:, 0:128], lhsT=rT[:, tsl], rhs=walb[:, hp * 128:(hp + 1) * 128], start=True, stop=True), reads=["rT", "walb"], writes=[PS(5)])
                            p.op("act", lambda e: e.activation(out=e1, in_=banks[5][:, 0:128], func=AF.Exp, scale=-1.0), reads=[PS(5)], writes=["g_e1"])
                            p.op("act", lambda e: e.activation(out=l1, in_=e1, func=AF.Ln, bias=LNEPS[:, 2:3]), reads=["g_e1", "lneps"], writes=["g_l1"])
                            p.op("pe", lambda e: e.matmul(banks[5][:, 128:256], lhsT=l1, rhs=consts[:, C_MASKG:C_MASKG + 128], start=True, stop=True), reads=["g_l1", "consts"], writes=[PS(5)])
                            p.op("pe", lambda e: e.matmul(banks[5][:, 256:384], lhsT=consts[:, C_MASKG + 128:C_MASKG + 256], rhs=l1, start=True, stop=True), reads=["g_l1", "consts"], writes=[PS(5)])
                            p.op("act", lambda e, tsl=tsl: e.activation(out=ecp[:, tsl], in_=banks[5][:, 128:256], func=AF.Exp), reads=[PS(5)], writes=["g_ecp"])
                            p.op("act", lambda e, tsl=tsl: e.activation(out=ecn[:, tsl], in_=banks[5][:, 128:256], func=AF.Exp, scale=-1.0), reads=[PS(5)], writes=["g_ecn"])
                            p.op("act", lambda e, tt=tt: e.activation(out=esuf[tt], in_=banks[5][:, 256:384], func=AF.Exp), reads=[PS(5)], writes=[("g_esuf", tt)])
                        proj_fm(0, 128, tb, 0)
                        for hh in range(2):
                            p.op("dve", lambda e, hh=hh: e.scalar_tensor_tensor(out=qdz[hh][hh * 64:(hh + 1) * 64, :], in0=banks[0][hh * 64:(hh + 1) * 64, :], scalar=0.125, in1=ecp[hh * 64:(hh + 1) * 64, :], op0=ALU.mult, op1=ALU.mult),
                                 reads=[PS(0), "g_ecp"], writes=[("g_qdz", hh)])
                        proj_fm(128, 128, tb, 1)
                        p.op("dve", lambda e: e.tensor_tensor(out=kd, in0=banks[1][:, :], in1=ecn, op=ALU.mult), reads=[PS(1), "g_ecn"], writes=["g_kd"])
                        for tt in range(4):
                            proj_tm(128, 128, tb, tt, 0)
                            for half in range(2):
                                p.op("dve", lambda e, tt=tt, half=half: e.tensor_tensor(out=kendz[tt][half][half * 64:(half + 1) * 64, :], in0=banks[0][half * 64:(half + 1) * 64, 0:128], in1=esuf[tt][half * 64:(half + 1) * 64, :], op=ALU.mult),
                                     reads=[PS(0), ("g_esuf", tt)], writes=[("g_kendz", tt, half)])
                            proj_tm(256, 256, tb, tt, 1)
                            p.op("act", lambda e, tt=tt: e.copy(out=vtm[tt], in_=banks[1][:, 0:256]), reads=[PS(1)], writes=[("g_vtm", tt)])
                        for hh in range(2):
                            proj_fm(512 + hh * 128, 128, tb, hh)
                            p.op("act", lambda e, hh=hh: e.activation(out=sg[hh], in_=banks[hh][:, :], func=AF.Silu), reads=[PS(hh)], writes=[("g_sg", hh)])
                        def head_ops(tb, hh):
                            b0 = hh * 64
                            BATT, BO, BKV = (2, 5)[hh], (3, 6)[hh], (4, 7)[hh]
                            attm, sqb, rs, t1 = attms[hh], sqbs[hh], rss[hh], t1s[hh]
                            KH = lambda n: (n, hh)

                            def att(e):
                                ins = None
                                for tt in range(4):
                                    tsl = slice(tt * 128, (tt + 1) * 128)
                                    ins = e.matmul(banks[BATT][:, tsl], lhsT=kd[:, tsl], rhs=qdz[hh][:, tsl], start=True, stop=True)
                                return ins
                            p.op("pe", att, reads=["g_kd", ("g_qdz", hh)], writes=[PS(BATT)])
                            p.op("dve", lambda e: e.tensor_tensor(out=attm, in0=banks[BATT][:, :].rearrange("p (a b) -> p a b", a=4), in1=m64b, op=ALU.mult), reads=[PS(BATT), "consts"], writes=[KH("g_attm")])
                            for c in range(8):
                                tt, half = c // 2, c % 2
                                csl = slice(c * 64, (c + 1) * 64)
                                tsl = slice(tt * 128, (tt + 1) * 128)
                                cur_par = par_state[hh]
                                if half == 0:
                                    p.op("pe", lambda e, tt=tt, tsl=tsl: e.matmul(banks[BO][:, tsl], lhsT=vtm[tt][:, hh * 128:(hh + 1) * 128], rhs=attm[:, tt, :], start=True, stop=False),
                                         reads=[("g_vtm", tt), KH("g_attm")], writes=[PS(BO)])
                                p.op("pe", lambda e, csl=csl, cur_par=cur_par, half=half: e.matmul(banks[BO][:, csl], lhsT=Sbfs[hh][cur_par], rhs=qdz[hh][:, csl], start=False, stop=(half == 1)),
                                     reads=[("Sbf", hh, cur_par), ("g_qdz", hh)], writes=[PS(BO)])
                                p.op("pe", lambda e, tt=tt, half=half: e.matmul(banks[BKV][:, 0:128], lhsT=kendz[tt][half], rhs=vtm[tt][:, hh * 128:(hh + 1) * 128], start=True, stop=True),
                                     reads=[("g_kendz", tt, half), ("g_vtm", tt)], writes=[PS(BKV)])
                                col = c * 64 + 63
                                p.op("dve", lambda e, col=col: e.scalar_tensor_tensor(out=S32[b0:b0 + 64, :], in0=S32[b0:b0 + 64, :], scalar=ecp[b0:b0 + 64, col:col + 1], in1=banks[BKV][b0:b0 + 64, 0:128], op0=ALU.mult, op1=ALU.add),
                                     reads=[("S32", hh), "g_ecp", PS(BKV)], writes=[("S32", hh)])
                                nxt = 1 - cur_par
                                p.op("act", lambda e, nxt=nxt: e.copy(out=Sbfs[hh][nxt][b0:b0 + 64, :], in_=S32[b0:b0 + 64, :]), reads=[("S32", hh)], writes=[("Sbf", hh, nxt)])
                                par_state[hh] = nxt
                            p.op("act", lambda e: e.activation(out=sqb, in_=banks[BO][:, :], func=AF.Square), reads=[PS(BO)], writes=[KH("g_sqb")])
                            p.op("pe", lambda e: e.matmul(banks[BATT][:, :], lhsT=ones128b, rhs=sqb, start=True, stop=True), reads=["ones128b", KH("g_sqb")], writes=[PS(BATT)])
                            p.op("act", lambda e: e.activation(out=rs, in_=banks[BATT][:, :], func=AF.Ln, bias=LNEPS[:, 1:2]), reads=[PS(BATT), "lneps"], writes=[KH("g_rs")])
                            p.op("act", lambda e: e.activation(out=rs, in_=rs, func=AF.Exp, scale=-0.5), reads=[KH("g_rs")], writes=[KH("g_rs")])
                            p.op("dve", lambda e: e.tensor_tensor(out=t1, in0=banks[BO][:, :], in1=rs, op=ALU.mult), reads=[PS(BO), KH("g_rs")], writes=[KH("g_t1")])
                            p.op("dve", lambda e: e.scalar_tensor_tensor(out=yblk[:, hh, :], in0=t1, scalar=PPc(l, PP_GNORM), in1=sg[hh], op0=ALU.mult, op1=ALU.mult), reads=[KH("g_t1"), ("g_sg", hh), "pp"], writes=[("yblk", hh)])

                        replay_ops(interleave([cap_ops(head_ops, tb, 0), cap_ops(head_ops, tb, 1)]))
                        if ada_next is not None:
                            slot = hp * NTB + tb
                            for blk in range(slot * 3, slot * 3 + 3):
                                adaln_block(ada_next, blk, ada_bufs_m, 2 + (blk % 2) * 3)
                            if slot == 2 * NTB - 1:
                                adaln_finish(ada_next, 0)
                        out_proj(tb, 2, [0, 1])

            if do_ssd:
                ssd_units(l, cv, mark, load_win, load_wout, proj_fm, proj_tm, out_proj, conv_silu, yblk, identb, ones512b)

        cv = Carve()
        ada_bufs = ada_bufs_alloc(cv)
        for blk in range(N_ADA):
            adaln_block(0, blk, ada_bufs, blk % 4)
        adaln_finish(0, 1)

        for tb in range(NTB):
            scale_alpha(tb, engs=("pool", "dve", "act"))
        for l in range(depth):
            p.new_epoch()
            for tb in range(NTB):
                modulate(l, 1, tb)
            barrier()
            ada_in_mixer = do_gla and (l + 1 < depth)
            if do_lru or do_gla or do_ssd:
                mixer(l, ada_next=(l + 1 if ada_in_mixer else None))
            barrier()
            cv = Carve()
            if do_moe:
                rgroup, rfinish = router(l, cv, 2, 3)

                def after_tb(tb):
                    modulate(l, 2, tb)
                    rgroup(tb)
                layernorm(l, PP_LN1G, PP_LN1B, cv, 0, 1, after_tb=after_tb)
                rfinish()
            else:
                layernorm(l, PP_LN1G, PP_LN1B, cv, 0, 1, after_tb=lambda tb: modulate(l, 2, tb))
            barrier()
            cv = Carve()
            hooks = {}
            if l + 1 < depth and not ada_in_mixer:
                ada_bufs2 = ada_bufs_alloc(cv)
                for blk in range(N_ADA):
                    hooks[2 + blk] = (lambda blk=blk: adaln_block(l + 1, blk, ada_bufs2, 4 + blk % 4))
                hooks[2 + N_ADA] = (lambda: adaln_finish(l + 1, 4))
            if do_moe:
                moe(l, cv, hooks)
            else:
                for k in sorted(hooks):
                    hooks[k]()
            barrier()
            cv = Carve()
            if l == depth - 1:
                yv = yT_d.rearrange("(c p) t -> p c t", p=128)

                def out_tb(tb):
                    p.dma(("out", tb), yv[:, :, tb * TB:(tb + 1) * TB], xT[:, :, tb * TB:(tb + 1) * TB], reads=[("x", c, tb) for c in range(8)])
                layernorm(l, PP_LN2G, PP_LN2B, cv, 0, 1, final=True, after_tb=out_tb)
            else:
                layernorm(l, PP_LN2G, PP_LN2B, cv, 0, 1)
            barrier()

        p.emit()
    return nc


def prep_inputs(inputs):
    f = lambda a: np.ascontiguousarray(np.asarray(a, dtype=np.float32))
    L = DEPTH
    shared = {}
    shared["consts"] = build_consts()
    pc = lambda v, n: np.asarray(v, np.float32).reshape(n, 128).T
    pp = np.zeros((L, 128, NPP), np.float32)
    prow = np.zeros((L, NPR), np.float32)
    wabd = np.zeros((L, 4, 128, 128), np.float32)
    wxbd = np.zeros((L, 4, 128, 128), np.float32)
    wal = np.zeros((L, 32, 256), np.float32)
    for l in range(L):
        pp[l, :, PP_LN1G:PP_LN1G + 8] = pc(inputs["ln1_g"][l], 8)
        pp[l, :, PP_LN1B:PP_LN1B + 8] = pc(inputs["ln1_b"][l], 8)
        pp[l, :, PP_LN2G:PP_LN2G + 8] = pc(inputs["ln2_g"][l], 8)
        pp[l, :, PP_LN2B:PP_LN2B + 8] = pc(inputs["ln2_b"][l], 8)
        for m in range(4):
            for k in range(4):
                pp[l, :, PP_LCW + m * 4 + k] = inputs["lru_conv_w"][l, k, m * 128:(m + 1) * 128]
        pp[l, :, PP_LCB:PP_LCB + 4] = pc(inputs["lru_conv_b"][l], 4)
        pp[l, :, PP_LBA:PP_LBA + 4] = pc(inputs["lru_b_a"][l], 4)
        pp[l, :, PP_LBX:PP_LBX + 4] = pc(inputs["lru_b_x"][l], 4)
        pp[l, :, PP_LLAM:PP_LLAM + 4] = pc(inputs["lru_lambda"][l], 4)
        for j in range(12):
            for k in range(4):
                pp[l, :, PP_SCW + j * 4 + k] = inputs["ssd_conv_w"][l, k, j * 128:(j + 1) * 128]
        pp[l, :, PP_SCB:PP_SCB + 12] = pc(inputs["ssd_conv_b"][l], 12)
        pp[l, :, PP_SNORM:PP_SNORM + 8] = pc(inputs["ssd_norm"][l], 8)
        pp[l, :, PP_GNORM] = inputs["gla_norm"][l]
        pp[l, :, PP_SD:PP_SD + 8] = pc(np.repeat(np.asarray(inputs["ssd_d"][l]), 64), 8)
        prow[l, PR_DTB:PR_DTB + 16] = inputs["ssd_dt_bias"][l]
        prow[l, PR_ALOG:PR_ALOG + 16] = inputs["ssd_a_log"][l]
        prow[l, PR_RB:PR_RB + NE] = inputs["router_bias"][l]
        for m in range(4):
            for q in range(2):
                wabd[l, m, q * 64:(q + 1) * 64, q * 64:(q + 1) * 64] = inputs["lru_w_a"][l, 2 * m + q]
                wxbd[l, m, q * 64:(q + 1) * 64, q * 64:(q + 1) * 64] = inputs["lru_w_x"][l, 2 * m + q]
        wal[l, 0:16] = inputs["gla_w_alpha"][l]
        wal[l, 16] = inputs["gla_b_alpha"][l]
    shared.update(pp=pp, prow=prow, lru_wa_bd=wabd, lru_wx_bd=wxbd, walpha_ext=wal)
    for k in ("w_ada", "b_ada", "w_in", "w_out", "router_w", "exp_w1", "exp_w3", "exp_w2", "shared_w1", "shared_w3", "shared_w2"):
        shared[k] = f(inputs[k])
    x = np.asarray(inputs["x"], np.float32)
    c = np.asarray(inputs["c"], np.float32)
    maps = []
    for b in range(x.shape[0]):
        m = dict(shared)
        m["xT"] = np.ascontiguousarray(x[b].T)
        m["cpc"] = np.ascontiguousarray(c[b].reshape(8, 128).T)
        maps.append(m)
    return maps


_NC_CACHE = {}


def kernel(**inputs):
    maps = prep_inputs(inputs)
    if "nc" not in _NC_CACHE:
        _NC_CACHE["nc"] = build_program()
    nc = _NC_CACHE["nc"]
    res = run_bass_kernel_spmd(nc, maps, core_ids=list(range(len(maps))))
    out = np.stack([np.ascontiguousarray(r["yT"].T) for r in res.results], axis=0)
    return out.astype(np.float32)
```

```python
from contextlib import ExitStack
import numpy as np
import concourse.bass as bass
import concourse.mybir as mybir
from concourse.bass_utils import run_bass_kernel_spmd

F32 = mybir.dt.float32
BF16 = mybir.dt.bfloat16
AF = mybir.ActivationFunctionType
ALU = mybir.AluOpType
AX = mybir.AxisListType

D = 1024
S = 2048
DEPTH = 4
NE = 64
ALPHA = (2 * DEPTH) ** 0.25
DIN = 5152
NPP = 144
TB = 512
NTB = S // TB


class Prog:
    def __init__(self, nc, stack):
        self.nc = nc
        self.stack = stack
        self.ops = []
        self.last_write = {}
        self.readers = {}
        self.epoch = 0
        self.op_epoch = []
        self.group_open = {}
        self.group_of = {}
        self.nsem = 0
        self.bar = None
        self.xeng = []
        self.cap = None

    def barrier(self, fn):
        allk = list(set(self.last_write.keys()) | set(k for k, v in self.readers.items() if v))
        oid = self._add("dve", fn, allk, allk)
        self.bar = oid
        self.last_write = {}
        self.readers = {}

    def new_sem(self, name):
        self.nsem += 1
        return self.stack.enter_context(self.nc.semaphore(f"{name}_{self.nsem}"))

    def new_epoch(self):
        self.epoch += 1

    def _add(self, eng, fn, reads, writes, chan=None):
        oid = len(self.ops)
        raw = set()
        oth = set()
        xe = set()
        for k in reads:
            w = self.last_write.get(k)
            if w is not None:
                raw.add(w)
            if isinstance(k, tuple) and k[0] in ("ps", "ps2o"):
                for r in self.readers.get(k, ()):
                    xe.add(r)
        for k in writes:
            w = self.last_write.get(k)
            if w is not None:
                oth.add(w)
            for r in self.readers.get(k, ()):
                oth.add(r)
        if self.bar is not None:
            oth.add(self.bar)
        oth -= raw
        raw.discard(oid)
        oth.discard(oid)
        xe -= raw
        xe -= oth
        xe.discard(oid)
        self.xeng.append(sorted(xe))
        self.ops.append([eng, fn, sorted(raw), sorted(oth), chan])
        self.op_epoch.append(self.epoch)
        for k in reads:
            self.readers.setdefault(k, []).append(oid)
        for k in writes:
            self.last_write[k] = oid
            self.readers[k] = []
        return oid

    def op(self, eng, fn, reads=(), writes=()):
        if self.cap is not None:
            self.cap.append((eng, fn, list(reads), list(writes)))
            return None
        return self._add(eng, fn, reads, writes)

    def dma(self, chan, out, in_, reads=(), writes=(), eng="sp", more=False, **kw):
        def fn(e, out=out, in_=in_, kw=kw):
            return e.dma_start(out=out, in_=in_, **kw)
        oid = self._add(eng, fn, reads, writes, chan=chan)
        g = self.group_open.get(chan)
        if g is None:
            g = []
            self.group_open[chan] = g
        g.append(oid)
        self.group_of[oid] = g
        if not more:
            self.group_open[chan] = None
        return oid

    def emit(self):
        nc = self.nc
        n = len(self.ops)
        is_dma = [o[4] is not None for o in self.ops]
        need_sig = [False] * n
        deps_of = []
        for i, (eng, fn, raw, oth, chan) in enumerate(self.ops):
            deps = []
            for d in raw:
                if is_dma[d] or is_dma[i] or self.ops[d][0] != eng or eng != "pe":
                    deps.append(d)
            for d in oth:
                if is_dma[d] or is_dma[i] or self.ops[d][0] != eng or eng != "pe":
                    deps.append(d)
            for d in self.xeng[i]:
                if self.ops[d][0] != eng:
                    deps.append(d)
            deps_of.append(deps)
            for d in deps:
                if not is_dma[d]:
                    need_sig[d] = True
        sig = [None] * n
        cur = {}
        chan_sem = {}
        chan_cnt = {}
        for i, (eng, fn, raw, oth, chan) in enumerate(self.ops):
            if is_dma[i]:
                if chan not in chan_sem:
                    chan_sem[chan] = self.new_sem("d")
                    chan_cnt[chan] = 0
                chan_cnt[chan] += 16
                sig[i] = (chan_sem[chan], chan_cnt[chan])
            elif need_sig[i]:
                key = (eng, self.op_epoch[i])
                if key not in cur:
                    cur[key] = [self.new_sem(eng), 0]
                cur[key][1] += 1
                sig[i] = (cur[key][0], cur[key][1])
        for i in range(n):
            if is_dma[i]:
                last = self.group_of[i][-1]
                if last != i:
                    sig[i] = (sig[i][0], sig[last][1])
        streams = {}
        for i, (eng, fn, raw, oth, chan) in enumerate(self.ops):
            streams.setdefault(eng, []).append((i, fn, [sig[d] for d in deps_of[i]]))
        final_dma = [(chan_sem[c], chan_cnt[c]) for c in chan_sem]
        self.n_waits = 0

        def run_stream(e, items, tail):
            waited = {}
            for (i, fn, waits) in items:
                best = {}
                for (s, v) in waits:
                    k = s.num
                    if waited.get(k, 0) >= v:
                        continue
                    if k not in best or best[k][1] < v:
                        best[k] = (s, v)
                for k, (s, v) in best.items():
                    e.wait_ge(s, v)
                    waited[k] = v
                    self.n_waits += 1
                ins = fn(e)
                if is_dma[i]:
                    ins.then_inc(chan_sem[self.ops[i][4]], 16)
                elif sig[i] is not None:
                    ins.then_inc(sig[i][0], 1)
            for (s, v) in tail:
                e.wait_ge(s, v)

        with nc.Block() as block:
            names = {"pe": "tensor", "act": "scalar", "dve": "vector", "pool": "gpsimd", "sp": "sync"}
            for en, attr in names.items():
                items = streams.get(en, [])
                tail = final_dma if en == "sp" else []
                if not items and not tail:
                    continue

                def body(e, items=items, tail=tail):
                    run_stream(e, items, tail)
                getattr(block, attr)(body)


C_ID = 0
C_MASK = 128
C_SUF = 256
C_CH0 = 384
C_CH1 = 512
C_M64 = 640
C_ONESD = 704
C_ONE = 832
C_SEL = 960
C_MASKG = 1984
NCONST = 2240


def build_consts():
    c = np.zeros((128, NCONST), np.float32)
    idx = np.arange(128)
    same = (idx[:, None] // 64) == (idx[None, :] // 64)
    c[:, C_ID:C_ID + 128] = np.eye(128)
    c[:, C_MASK:C_MASK + 128] = same & (idx[:, None] <= idx[None, :])
    c[:, C_SUF:C_SUF + 128] = same & (idx[:, None] > idx[None, :])
    c[:64, C_CH0:C_CH0 + 128] = 1.0
    c[64:, C_CH1:C_CH1 + 128] = 1.0
    j = idx % 64
    c[:, C_M64:C_M64 + 64] = j[:, None] <= np.arange(64)[None, :]
    c[:, C_ONESD:C_ONESD + 128] = 1.0 / 1024.0
    c[:, C_ONE:C_ONE + 128] = 1.0
    for h in range(8):
        c[h, C_SEL + h * 128:C_SEL + (h + 1) * 128] = 1.0
    c[:, C_MASKG:C_MASKG + 128] = c[:, C_MASK:C_MASK + 128] * (-1.0 / 16.0)
    c[:, C_MASKG + 128:C_MASKG + 256] = c[:, C_SUF:C_SUF + 128] * (-1.0 / 16.0)
    return c


PP_LN1G, PP_LN1B, PP_LN2G, PP_LN2B = 0, 8, 16, 24
PP_LCW, PP_LCB, PP_LBA, PP_LBX, PP_LLAM = 32, 48, 52, 56, 60
PP_SCW, PP_SCB, PP_SNORM, PP_GNORM, PP_SD = 64, 112, 124, 132, 133
PR_DTB, PR_ALOG, PR_RB = 0, 16, 32
NPR = 96


def build_program(depth=DEPTH, do_lru=True, do_gla=True, do_ssd=True, do_moe=True, n_exp=NE + 1, debug=False):
    nc = bass.Bass("TRN2", target_bir_lowering=False, dynamic_dma_scratch_size=512)
    dr = {}

    def din(name, shape):
        dr[name] = nc.dram_tensor(name, list(shape), F32, kind="ExternalInput").ap()
        return dr[name]

    xT_d = din("xT", [D, S])
    cpc_d = din("cpc", [128, 8])
    consts_d = din("consts", [128, NCONST])
    pp_d = din("pp", [DEPTH, 128, NPP])
    prow_d = din("prow", [DEPTH, NPR])
    wada_d = din("w_ada", [DEPTH, D, 6 * D])
    bada_d = din("b_ada", [DEPTH, 6 * D])
    win_d = din("w_in", [DEPTH, D, DIN])
    wout_d = din("w_out", [DEPTH, 2 * D, D])
    wabd_d = din("lru_wa_bd", [DEPTH, 4, 128, 128])
    wxbd_d = din("lru_wx_bd", [DEPTH, 4, 128, 128])
    walpha_d = din("walpha_ext", [DEPTH, 32, 256])
    rw_d = din("router_w", [DEPTH, D, NE])
    ew1_d = din("exp_w1", [DEPTH, NE, D, 256])
    ew3_d = din("exp_w3", [DEPTH, NE, D, 256])
    ew2_d = din("exp_w2", [DEPTH, NE, 256, D])
    sw1_d = din("shared_w1", [DEPTH, D, 256])
    sw3_d = din("shared_w3", [DEPTH, D, 256])
    sw2_d = din("shared_w2", [DEPTH, 256, D])
    yT_d = nc.dram_tensor("yT", [D, S], F32, kind="ExternalOutput").ap()
    gscr_d = nc.dram_tensor("gscr", [2, NE, S], F32, kind="Internal").ap()
    dbg = {}

    with ExitStack() as st:
        p = Prog(nc, st)

        def sb(name, shape, dt=F32):
            return st.enter_context(nc.sbuf_tensor(name, list(shape), dt))

        def cap_ops(fn, *a):
            lst = []
            p.cap = lst
            r = fn(*a)
            p.cap = None
            return lst

        def replay_ops(lst):
            for (eng, fn, r, w) in lst:
                p.op(eng, fn, reads=r, writes=w)

        def interleave(lists):
            out = []
            n = max(len(x) for x in lists)
            for k in range(n):
                for x in lists:
                    if k < len(x):
                        out.append(x[k])
            return out

        xT = sb("xT_sb", [128, 8, S])
        hT = sb("hT_sb", [128, 8, S], BF16)
        consts = sb("consts_sb", [128, NCONST])
        pp = sb("pp_sb", [128, DEPTH, NPP])
        mod = sb("mod_sb", [128, DEPTH, 64])
        cond = sb("cond_sb", [128, 8])
        SCRW = 29150
        scr = sb("scr_sb", [128, SCRW])
        banks = [st.enter_context(nc.psum_tensor(f"bank{i}", [128, 512], F32)) for i in range(8)]

        def PS(i):
            return ("ps", i)

        class Carve:
            def __init__(self):
                self.off = 0

            def get(self, shape, dt=F32):
                n = int(np.prod(shape[1:]))
                words = n if dt == F32 else (n + 1) // 2
                a = scr[:, self.off:self.off + words]
                self.off += words
                assert self.off <= SCRW - 1, self.off
                if dt != F32:
                    a = a.bitcast(dt)
                    if n % 2:
                        a = a[:, 0:n]
                if len(shape) == 3:
                    a = a.rearrange("p (a b) -> p a b", a=shape[1])
                elif len(shape) == 4:
                    a = a.rearrange("p (a b c) -> p a b c", a=shape[1], b=shape[2])
                if shape[0] != 128:
                    a = a[0:shape[0]]
                return a

        ident = consts[:, C_ID:C_ID + 128]
        onesD = consts[:, C_ONESD:C_ONESD + 128]

        def barrier():
            tok = scr[:, SCRW - 1:SCRW]
            p.barrier(lambda e: e.memset(tok, 0.0))

        p.dma("ld0", consts[:], consts_d[:, :], writes=["consts"])
        p.dma("ld1", pp[:], pp_d.rearrange("l p n -> p l n"), writes=["pp"])
        p.dma("ld2", cond[:], cpc_d[:, :], writes=["cond"])
        p.dma("ldx", xT[:], xT_d.rearrange("(c p) t -> p c t", p=128), writes=[("x", c, tb) for c in range(8) for tb in range(NTB)])
        p.op("act", lambda e: e.activation(out=cond[:], in_=cond[:], func=AF.Silu), reads=["cond"], writes=["cond"])

        ADA_BLK = 256
        N_ADA = 6 * D // ADA_BLK
        ada_stg = [None, None]

        def adaln_block(l, blk, cv_bufs, bank):
            stg, brow, mrow = cv_bufs[blk % 2]
            key = ("adastg", blk % 2)
            c0 = blk * ADA_BLK
            nj = ADA_BLK // 128
            p.dma(("adab", blk % 2), brow, bada_d[l:l + 1, c0:c0 + ADA_BLK], writes=[("adabrow", blk % 2)])
            p.dma(("ada", blk % 2), stg, wada_d[l].rearrange("(kc p) f -> p kc f", p=128)[:, :, c0:c0 + ADA_BLK], writes=[key])

            def mm(e, stg=stg):
                ins = None
                for kc in range(8):
                    ins = e.matmul(banks[bank][0:1, 0:ADA_BLK], lhsT=cond[:, kc:kc + 1], rhs=stg[:, kc, :], start=(kc == 0), stop=(kc == 7))
                return ins
            p.op("pe", mm, reads=[key, "cond"], writes=[PS(bank)])
            p.op("dve", lambda e: e.tensor_tensor(out=mrow, in0=banks[bank][0:1, 0:ADA_BLK], in1=brow, op=ALU.add),
                 reads=[PS(bank), ("adabrow", blk % 2)], writes=[("adamrow", blk % 2)])

            def mm2(e):
                ins = None
                for j in range(nj):
                    ins = e.matmul(banks[bank][:, 256 + j:257 + j], lhsT=mrow[0:1, j * 128:(j + 1) * 128], rhs=consts[0:1, C_ONE:C_ONE + 1], start=True, stop=True)
                return ins
            p.op("pe", mm2, reads=[("adamrow", blk % 2), "consts"], writes=[PS(bank)])
            p.op("act", lambda e: e.copy(out=mod[:, l, blk * nj:(blk + 1) * nj], in_=banks[bank][:, 256:256 + nj]), reads=[PS(bank)], writes=[("mod", l)])

        def adaln_finish(l, bank):
            p.op("dve", lambda e: e.tensor_scalar(out=mod[:, l, 48:56], in0=mod[:, l, 8:16], scalar1=1.0, scalar2=1.0 / float(ALPHA), op0=ALU.add, op1=ALU.mult), reads=[("mod", l)], writes=[("modd", l)])
            p.op("dve", lambda e: e.tensor_scalar(out=mod[:, l, 56:64], in0=mod[:, l, 32:40], scalar1=1.0, scalar2=1.0 / float(ALPHA), op0=ALU.add, op1=ALU.mult), reads=[("mod", l)], writes=[("modd2", l)])

        def ada_bufs_alloc(cv):
            return [(cv.get([128, 8, ADA_BLK]), cv.get([1, ADA_BLK]), cv.get([1, ADA_BLK])) for _ in range(2)]

        def MOD(l, j, c):
            if j < 6:
                return mod[:, l, j * 8 + c:j * 8 + c + 1]
            return mod[:, l, 48 + (j - 6) * 8 + c:48 + (j - 6) * 8 + c + 1]

        def PPc(l, col):
            return pp[:, l, col:col + 1]

        modkeys = lambda l: [("mod", l), ("modd", l), ("modd2", l)]

        def modulate(l, which, tb, engs=("dve", "pool")):
            jsc, jsh = (6, 0) if which == 1 else (7, 3)
            for c in range(8):
                eng = engs[c % len(engs)]
                p.op(eng, lambda e, c=c: e.tensor_scalar(out=hT[:, c, tb * TB:(tb + 1) * TB], in0=xT[:, c, tb * TB:(tb + 1) * TB],
                                                         scalar1=MOD(l, jsc, c), scalar2=MOD(l, jsh, c), op0=ALU.mult, op1=ALU.add),
                     reads=[("x", c, tb)] + modkeys(l), writes=[("h", c, tb)])

        def scale_alpha(tb, engs=("pool",)):
            for c in range(8):
                eng = engs[c % len(engs)]
                if eng == "act":
                    p.op(eng, lambda e, c=c: e.mul(out=xT[:, c, tb * TB:(tb + 1) * TB], in_=xT[:, c, tb * TB:(tb + 1) * TB], mul=float(ALPHA)),
                         reads=[("x", c, tb)], writes=[("x", c, tb)])
                else:
                    p.op(eng, lambda e, c=c: e.tensor_scalar_mul(out=xT[:, c, tb * TB:(tb + 1) * TB], in0=xT[:, c, tb * TB:(tb + 1) * TB], scalar1=float(ALPHA)),
                         reads=[("x", c, tb)], writes=[("x", c, tb)])

        def layernorm(l, gcol, bcol, cv, bank_m, bank_q, final=False, after_tb=None):
            def GB(col):
                return pp[:, l, col:col + 1] if final else ppA[:, l, col:col + 1]
            sq = [cv.get([128, TB]) for _ in range(2)]
            mean_sbs = [cv.get([128, TB]) for _ in range(2)]
            rstds = [cv.get([128, TB]) for _ in range(2)]
            tmp = [cv.get([128, TB]) for _ in range(2)]
            tmp2 = [cv.get([128, TB]) for _ in range(2)]
            bank_m0, bank_q0 = bank_m, bank_q
            for tb in range(NTB):
                sl = slice(tb * TB, (tb + 1) * TB)
                mean_sb = mean_sbs[tb % 2]
                rstd = rstds[tb % 2]
                bank_m = bank_m0 + 2 * (tb % 2)
                bank_q = bank_q0 + 2 * (tb % 2)
                KM = ("lnmean", tb % 2)
                KR = ("lnrstd", tb % 2)

                def mm_mean(e, sl=sl, bank_m=bank_m):
                    ins = None
                    for c in range(8):
                        ins = e.matmul(banks[bank_m][:, :], lhsT=onesD, rhs=xT[:, c, sl], start=(c == 0), stop=(c == 7))
                    return ins
                p.op("pe", mm_mean, reads=[("x", c, tb) for c in range(8)] + ["consts"], writes=[PS(bank_m)])
                for c in range(8):
                    p.op("act", lambda e, c=c, sl=sl: e.activation(out=sq[c % 2], in_=xT[:, c, sl], func=AF.Square), reads=[("x", c, tb)], writes=[("lnsq", c % 2)])
                    p.op("pe", lambda e, c=c, bank_q=bank_q: e.matmul(banks[bank_q][:, :], lhsT=onesD, rhs=sq[c % 2], start=(c == 0), stop=(c == 7)),
                         reads=[("lnsq", c % 2), "consts"], writes=[PS(bank_q)])
                p.op("act", lambda e, mean_sb=mean_sb, bank_m=bank_m: e.copy(out=mean_sb, in_=banks[bank_m][:, :]), reads=[PS(bank_m)], writes=[KM])
                p.op("dve", lambda e, rstd=rstd, mean_sb=mean_sb: e.tensor_tensor(out=rstd, in0=mean_sb, in1=mean_sb, op=ALU.mult), reads=[KM], writes=[KR])
                p.op("dve", lambda e, rstd=rstd, bank_q=bank_q: e.tensor_tensor(out=rstd, in0=banks[bank_q][:, :], in1=rstd, op=ALU.subtract), reads=[PS(bank_q), KR], writes=[KR])
                p.op("act", lambda e, rstd=rstd: e.activation(out=rstd, in_=rstd, func=AF.Ln, bias=LNEPS[:, 0:1]), reads=[KR, "lneps"], writes=[KR])
                p.op("act", lambda e, rstd=rstd: e.activation(out=rstd, in_=rstd, func=AF.Exp, scale=-0.5), reads=[KR], writes=[KR])
                for c in range(8):
                    k = c % 2
                    p.op("dve", lambda e, c=c, k=k, sl=sl, mean_sb=mean_sb: e.tensor_tensor(out=tmp[k], in0=xT[:, c, sl], in1=mean_sb, op=ALU.subtract),
                         reads=[("x", c, tb), KM], writes=[("lnt", k)])
                    p.op(("pool", "dve")[c % 2], lambda e, k=k, rstd=rstd: e.tensor_tensor(out=tmp2[k], in0=tmp[k], in1=rstd, op=ALU.mult), reads=[("lnt", k), KR], writes=[("lnt2", k)])
                    p.op("act", lambda e, c=c, k=k, sl=sl: e.activation(out=xT[:, c, sl], in_=tmp2[k], func=AF.Identity, scale=GB(gcol + c), bias=GB(bcol + c)),
                         reads=[("lnt2", k), "pp", "ppA"], writes=[("x", c, tb)])
                if after_tb is not None:
                    after_tb(tb)

        ppA = sb("ppA_sb", [128, DEPTH, 32])
        p.op("dve", lambda e: e.tensor_scalar_mul(out=ppA[:], in0=pp[:, :, 0:32], scalar1=float(ALPHA)), reads=["pp"], writes=["ppA"])
        LNEPS = sb("lneps_sb", [128, 4])
        p.op("pool", lambda e: e.memset(LNEPS[:, 0:1], 1e-5), writes=["lneps"])
        p.op("pool", lambda e: e.memset(LNEPS[:, 1:2], 1e-6), reads=[], writes=["lneps"])
        p.op("pool", lambda e: e.memset(LNEPS[:, 2:3], 1.0), reads=[], writes=["lneps"])

        def router(l, cv, bank_l, bank_t):
            NI = 4
            rw = cv.get([128, 8, NE])
            rb = cv.get([128, NE])
            gT = cv.get([64, S])
            h32s = [cv.get([128, 8, 128]) for _ in range(NI)]
            Ws = [{n: cv.get([128, 64]) for n in ("sc", "bi", "eq", "b2", "mk", "sel", "gw", "gates")} for _ in range(NI)]
            Sms = [{n: cv.get([128, 8]) for n in ("m1", "m2", "gs", "t8", "gsel", "goff", "t8e")} for _ in range(NI)]
            s1s = [cv.get([128, 2]) for _ in range(NI)]
            p.dma("rw", rw, rw_d[l].rearrange("(kc p) e -> p kc e", p=128), writes=["rw"])
            p.dma("rb", rb, prow_d[l:l + 1, PR_RB:PR_RB + NE].partition_broadcast(128), writes=["rb"])
            g3 = lambda a: a.rearrange("p (g k) -> p g k", k=8)
            b3 = lambda a: a.unsqueeze(2).to_broadcast([128, 8, 8])

            def tile_ops(tt):
                j = tt % NI
                tb = tt // 4
                tsl = slice(tt * 128, (tt + 1) * 128)
                h32, W, Sm, s1 = h32s[j], Ws[j], Sms[j], s1s[j]
                bl, bt = 4 + j, 4 + j
                K = lambda n: (n, j)
                ops = []
                A = lambda eng, fn, r, w: ops.append((eng, fn, r, w))
                for c in range(8):
                    eng = ("dve", "pool")[c % 2]
                    A(eng, lambda e, c=c: e.tensor_scalar(out=h32[:, c, :], in0=xT[:, c, tsl], scalar1=MOD(l, 7, c), scalar2=MOD(l, 3, c), op0=ALU.mult, op1=ALU.add),
                      [("x", c, tb)] + modkeys(l), [("h32", j, c)])

                def mm(e):
                    ins = None
                    for c in range(8):
                        ins = e.matmul(banks[bl][:, 0:NE], lhsT=h32[:, c, :], rhs=rw[:, c, :], start=(c == 0), stop=(c == 7))
                    return ins
                A("pe", mm, [("h32", j, c) for c in range(8)] + ["rw"], [PS(bl)])
                A("act", lambda e: e.activation(out=W["sc"], in_=banks[bl][:, 0:NE], func=AF.Sigmoid), [PS(bl)], [K("r_sc")])
                V = lambda fn, r, w: A("dve", fn, r, w)
                V(lambda e: e.tensor_tensor(out=W["bi"], in0=W["sc"], in1=rb, op=ALU.add), [K("r_sc"), "rb"], [K("r_bi")])
                V(lambda e: e.tensor_reduce(out=Sm["m1"], in_=g3(W["bi"]), axis=AX.X, op=ALU.max), [K("r_bi")], [K("r_m1")])
                V(lambda e: e.tensor_tensor(out=g3(W["eq"]), in0=g3(W["bi"]), in1=b3(Sm["m1"]), op=ALU.is_equal), [K("r_bi"), K("r_m1")], [K("r_eq")])
                V(lambda e: e.scalar_tensor_tensor(out=W["b2"], in0=W["eq"], scalar=-10.0, in1=W["bi"], op0=ALU.mult, op1=ALU.add), [K("r_eq"), K("r_bi")], [K("r_b2")])
                V(lambda e: e.tensor_reduce(out=Sm["m2"], in_=g3(W["b2"]), axis=AX.X, op=ALU.max), [K("r_b2")], [K("r_m2")])
                V(lambda e: e.tensor_tensor(out=Sm["gs"], in0=Sm["m1"], in1=Sm["m2"], op=ALU.add), [K("r_m1"), K("r_m2")], [K("r_gs")])
                V(lambda e: e.max(out=Sm["t8"], in_=Sm["gs"]), [K("r_gs")], [K("r_t8")])
                V(lambda e: e.tensor_scalar(out=Sm["gsel"], in0=Sm["gs"], scalar1=Sm["t8"][:, 3:4], scalar2=None, op0=ALU.is_ge), [K("r_gs"), K("r_t8")], [K("r_gsel")])
                V(lambda e: e.tensor_scalar(out=Sm["goff"], in0=Sm["gsel"], scalar1=10.0, scalar2=-10.0, op0=ALU.mult, op1=ALU.add), [K("r_gsel")], [K("r_goff")])
                V(lambda e: e.tensor_tensor(out=g3(W["mk"]), in0=g3(W["bi"]), in1=b3(Sm["gsel"]), op=ALU.mult), [K("r_bi"), K("r_gsel")], [K("r_mk")])
                V(lambda e: e.tensor_tensor(out=g3(W["mk"]), in0=g3(W["mk"]), in1=b3(Sm["goff"]), op=ALU.add), [K("r_mk"), K("r_goff")], [K("r_mk")])
                V(lambda e: e.max(out=Sm["t8e"], in_=W["mk"]), [K("r_mk")], [K("r_t8e")])
                V(lambda e: e.tensor_scalar(out=W["sel"], in0=W["mk"], scalar1=Sm["t8e"][:, 7:8], scalar2=None, op0=ALU.is_ge), [K("r_mk"), K("r_t8e")], [K("r_sel")])
                V(lambda e: e.tensor_tensor(out=W["gw"], in0=W["sel"], in1=W["sc"], op=ALU.mult), [K("r_sel"), K("r_sc")], [K("r_gw")])
                V(lambda e: e.tensor_reduce(out=s1[:, 0:1], in_=W["gw"], axis=AX.X, op=ALU.add), [K("r_gw")], [K("r_s1")])
                V(lambda e: e.reciprocal(out=s1[:, 1:2], in_=s1[:, 0:1]), [K("r_s1")], [K("r_s2")])
                V(lambda e: e.tensor_scalar(out=W["gates"], in0=W["gw"], scalar1=s1[:, 1:2], scalar2=2.5, op0=ALU.mult, op1=ALU.mult), [K("r_gw"), K("r_s2")], [K("r_gates")])
                A("pe", lambda e: e.transpose(banks[bt][0:64, 0:128], W["gates"], ident), [K("r_gates"), "consts"], [PS(bt)])
                A("act", lambda e: e.copy(out=gT[:, tsl], in_=banks[bt][0:64, 0:128]), [PS(bt)], [("gT", tt)])
                return ops

            def group(tb):
                g0 = tb * NI
                lists = [tile_ops(tt) for tt in range(g0, g0 + NI)]
                for k in range(len(lists[0])):
                    for ol in lists:
                        eng, fn, r, w = ol[k]
                        p.op(eng, fn, reads=r, writes=w)

            def finish():
                p.dma("gst", gscr_d[l % 2], gT, reads=[("gT", tt) for tt in range(S // 128)], writes=[("gscr", l % 2)])
            return group, finish

        def moe(l, cv, hooks):
            stg = {n: cv.get([128, 8, 256]) for n in ("w1", "w3")}
            stg["w2"] = cv.get([128, 2, D])
            wbf = [{"w1": cv.get([128, 8, 256], BF16), "w3": cv.get([128, 8, 256], BF16), "w2": cv.get([128, 2, D], BF16)} for _ in range(2)]
            gbc = [cv.get([128, S]) for _ in range(2)]
            sS = [[cv.get([128, TB], BF16) for f in range(2)] for _ in range(2)]
            tS = [[cv.get([128, TB], BF16) for f in range(2)] for _ in range(2)]
            hid = [[cv.get([128, TB], BF16) for f in range(2)] for _ in range(2)]
            steps = [(e, tb) for e in range(n_exp) for tb in range(NTB)]

            def load(e):
                sl = e % 2
                if e < NE:
                    srcs = {"w1": ew1_d[l, e], "w3": ew3_d[l, e], "w2": ew2_d[l, e]}
                else:
                    srcs = {"w1": sw1_d[l], "w3": sw3_d[l], "w2": sw2_d[l]}
                for n in ("w1", "w3", "w2"):
                    pat = "(kc p) f -> p kc f"
                    p.dma(("wst", n), stg[n], srcs[n].rearrange(pat, p=128), writes=[("stg", n)])
                if e < NE:
                    p.dma(("gbc", sl), gbc[sl], gscr_d[l % 2, e:e + 1, :].partition_broadcast(128), reads=[("gscr", l % 2)], writes=[("gbc", sl)])

            def cast(e):
                sl = e % 2
                for n in ("w1", "w3", "w2"):
                    if n == "w2":
                        parts = [(slice(0, 1), "act"), (slice(1, 2), "pool")]
                    else:
                        parts = [(slice(0, 3), "act"), (slice(3, 8), "pool")]
                    for (ps_, ce) in parts:
                        if ce == "act":
                            p.op("act", lambda e_, n=n, sl=sl, ps_=ps_: e_.copy(out=wbf[sl][n][:, ps_, :], in_=stg[n][:, ps_, :]), reads=[("stg", n)], writes=[("wbf", sl, n, ce)])
                        else:
                            p.op("pool", lambda e_, n=n, sl=sl, ps_=ps_: e_.tensor_copy(out=wbf[sl][n][:, ps_, :], in_=stg[n][:, ps_, :]), reads=[("stg", n)], writes=[("wbf", sl, n, ce)])

            def up(i, f):
                e, tb = steps[i]
                sl = e % 2
                for wi, n in enumerate(("w1", "w3")):
                    bk = f * 2 + wi

                    def mm(e_, n=n, bk=bk, sl=sl, tb=tb, f=f):
                        ins = None
                        for kc in range(8):
                            ins = e_.matmul(banks[bk][:, :], lhsT=wbf[sl][n][:, kc, f * 128:(f + 1) * 128], rhs=hT[:, kc, tb * TB:(tb + 1) * TB], start=(kc == 0), stop=(kc == 7))
                        return ins
                    p.op("pe", mm, reads=[("wbf", sl, n, "act"), ("wbf", sl, n, "pool")] + [("h", kc, tb) for kc in range(8)], writes=[PS(bk)])

            def gating(i, f):
                e, tb = steps[i]
                sl = e % 2
                par = i % 2
                p.op("act", lambda e_: e_.activation(out=sS[par][f], in_=banks[f * 2][:, :], func=AF.Silu), reads=[PS(f * 2)], writes=[("sS", par, f)])
                if e < NE:
                    p.op("dve", lambda e_: e_.tensor_tensor(out=tS[par][f], in0=banks[f * 2 + 1][:, :], in1=gbc[sl][:, tb * TB:(tb + 1) * TB], op=ALU.mult),
                         reads=[PS(f * 2 + 1), ("gbc", sl)], writes=[("tS", par, f)])
                    p.op("dve", lambda e_: e_.tensor_tensor(out=hid[par][f], in0=sS[par][f], in1=tS[par][f], op=ALU.mult),
                         reads=[("sS", par, f), ("tS", par, f)], writes=[("hid", par, f)])
                else:
                    p.op("dve", lambda e_: e_.tensor_tensor(out=hid[par][f], in0=banks[f * 2 + 1][:, :], in1=sS[par][f], op=ALU.mult),
                         reads=[PS(f * 2 + 1), ("sS", par, f)], writes=[("hid", par, f)])

            def down(i, dh):
                e, tb = steps[i]
                sl = e % 2
                par = i % 2
                for dq in range(4):
                    d = dh * 4 + dq
                    bk = 4 + dq

                    def mm(e_, d=d, bk=bk):
                        ins = None
                        for f in range(2):
                            ins = e_.matmul(banks[bk][:, :], lhsT=wbf[sl]["w2"][:, f, d * 128:(d + 1) * 128], rhs=hid[par][f], start=(f == 0), stop=(f == 1))
                        return ins
                    p.op("pe", mm, reads=[("wbf", sl, "w2", "act"), ("wbf", sl, "w2", "pool"), ("hid", par, 0), ("hid", par, 1)], writes=[PS(bk)])
                    p.op("dve", lambda e_, d=d, bk=bk: e_.scalar_tensor_tensor(out=xT[:, d, tb * TB:(tb + 1) * TB], in0=banks[bk][:, :], scalar=MOD(l, 5, d), in1=xT[:, d, tb * TB:(tb + 1) * TB], op0=ALU.mult, op1=ALU.add),
                         reads=[PS(bk), ("x", d, tb)] + modkeys(l), writes=[("x", d, tb)])

            load(0)
            cast(0)
            if n_exp > 1:
                load(1)
                cast(1)
            up(0, 0)
            up(0, 1)
            gating(0, 0)
            gating(0, 1)
            for i in range(len(steps)):
                e, tb = steps[i]
                if tb == 0 and i > 0 and e + 1 < n_exp:
                    load(e + 1)
                if tb == 2 and e > 0 and e + 1 < n_exp:
                    cast(e + 1)
                if tb == 1 and e in hooks:
                    hooks[e]()
                nxt = i + 1 < len(steps)
                if nxt:
                    up(i + 1, 0)
                down(i, 0)
                if nxt:
                    gating(i + 1, 0)
                    up(i + 1, 1)
                down(i, 1)
                if nxt:
                    gating(i + 1, 1)


        def ssd_units(l, cv, mark, load_win, load_wout, proj_fm, proj_tm, out_proj, conv_silu, yblk, identb, ones512b):
            cv.off = mark
            dtb = cv.get([128, 16]); alog = cv.get([128, 16]); aneg = cv.get([128, 16])
            cbuf = [cv.get([128, 3 + TB]) for _ in range(6)]
            ctmps = [cv.get([128, TB]) for _ in range(3)]
            ctmp = ctmps[0]
            xs = [cv.get([128, TB], BF16) for _ in range(4)]
            BT = cv.get([128, TB], BF16); CT = cv.get([128, TB], BF16)
            sz = [cv.get([128, TB], BF16) for _ in range(4)]
            yg = cv.get([128, 4, TB])
            dt_tm = cv.get([128, 8]); dA = cv.get([128, 8]); acs = cv.get([128, 8]); dte = cv.get([128, 8])
            w2 = cv.get([128, 8]); draw = cv.get([128, 8]); ex = cv.get([128, 8])
            dAb = cv.get([128, 8, 128])
            L = dAb
            Btmzs = [[cv.get([128, 128], BF16) for _ in range(2)] for _ in range(2)]
            decbcs = [cv.get([128, 2, 8]) for _ in range(2)]
            eD = cv.get([128, 8, 128], BF16)
            cbm = cv.get([128, 128])
            MTs = [cv.get([128, 8, 128], BF16) for _ in range(2)]
            CTss = [cv.get([128, 8, 128], BF16) for _ in range(2)]
            xdts = [cv.get([128, 8, 64], BF16) for _ in range(2)]
            xws = [cv.get([128, 8, 64], BF16) for _ in range(2)]
            pa_ctr = [0]
            Btm = cv.get([128, 128], BF16)
            S32 = cv.get([128, 8, 64])
            Sbf = [cv.get([128, 8, 64], BF16) for _ in range(2)]
            sqb = cv.get([128, TB], BF16); rs = ctmp
            b7 = banks[7][:, :].bitcast(BF16)
            MASK = consts[:, C_MASK:C_MASK + 128]
            SUF = consts[:, C_SUF:C_SUF + 128]
            p.dma("dtb", dtb, prow_d[l:l + 1, PR_DTB:PR_DTB + 16].partition_broadcast(128), writes=["dtb"])
            p.dma("alog", alog, prow_d[l:l + 1, PR_ALOG:PR_ALOG + 16].partition_broadcast(128), writes=["alog"])
            p.op("act", lambda e: e.activation(out=aneg, in_=alog, func=AF.Exp), reads=["alog"], writes=["aneg"])
            p.op("dve", lambda e: e.tensor_scalar_mul(out=aneg, in0=aneg, scalar1=-1.0), reads=["aneg"], writes=["aneg"])
            for g in range(2):
                load_win(3600 + g * 512, 512, 0)
                load_win(2576 + g * 512, 512, 512)
                load_win(4624 + g * 128, 128, 1024)
                load_win(4880 + g * 128, 128, 1152)
                load_win(5136 + g * 8, 8, 1280)
                load_wout(8 + g * 4, 4)
                for j6 in range(6):
                    p.op("pool", lambda e, j6=j6: e.memset(cbuf[j6][:, 0:3], 0.0), writes=[("cbuf", j6)])
                p.op("pool", lambda e: e.memset(S32, 0.0), writes=["s_S32"])
                p.op("pool", lambda e: e.memset(Sbf[0], 0.0), writes=[("s_Sbf", 0)])
                for pa in range(2):
                    for half in range(2):
                        p.op("pool", lambda e, half=half, pa=pa: e.memset(Btmzs[pa][half], 0.0), writes=[("s_Btmz", pa, half)])
                par = 0
                gs = slice(g * 8, g * 8 + 8)
                for tb in range(NTB):
                    def conv_chain(j6):
                        off = j6 * 128 if j6 < 4 else (1024 if j6 == 4 else 1152)
                        jc = g * 4 + j6 if j6 < 4 else (8 + g if j6 == 4 else 10 + g)
                        bank = j6
                        proj_fm(off, 128, tb, bank)
                        dst = xs[j6] if j6 < 4 else (BT if j6 == 4 else CT)
                        conv_silu(cbuf[j6], ("cbuf", j6), tb, bank, PP_SCW + jc * 4, PP_SCB + jc, ctmps[j6 % 3], ("s_ctmp", j6 % 3), dst, ("s_fm", j6), AF.Silu, eng="dve")

                    def z_chain(q):
                        bank = 6 + q % 2
                        proj_fm(512 + q * 128, 128, tb, bank)
                        p.op("act", lambda e: e.activation(out=sz[q], in_=banks[bank][:, :], func=AF.Silu), reads=[PS(bank)], writes=[("s_sz", q)])

                    replay_ops(interleave([cap_ops(conv_chain, 0), cap_ops(conv_chain, 1), cap_ops(conv_chain, 2), cap_ops(z_chain, 0), cap_ops(z_chain, 1)]))
                    replay_ops(interleave([cap_ops(conv_chain, 3), cap_ops(conv_chain, 4), cap_ops(conv_chain, 5), cap_ops(z_chain, 2), cap_ops(z_chain, 3)]))
                    def _aliases(pa):
                        return MTs[pa], CTss[pa], xdts[pa], xws[pa], Btmzs[pa], decbcs[pa]

                    def stageA(tt, pa):
                        MT, CTs, xdt, xw, Btmz, decbc = _aliases(pa)
                        KP = lambda n: (n, pa)
                        tsl = slice(tt * 128, (tt + 1) * 128)
                        proj_tm(1280, 8, tb, tt, 5)
                        p.op("dve", lambda e, gs=gs: e.tensor_tensor(out=draw, in0=banks[5][:, 0:8], in1=dtb[:, gs], op=ALU.add), reads=[PS(5), "dtb"], writes=["s_draw"])
                        p.op("act", lambda e: e.activation(out=ex, in_=draw, func=AF.Exp), reads=["s_draw"], writes=["s_ex"])
                        p.op("act", lambda e: e.activation(out=dt_tm, in_=ex, func=AF.Ln, bias=LNEPS[:, 2:3]), reads=["s_ex", "lneps"], writes=["s_dt"])
                        p.op("dve", lambda e, gs=gs: e.tensor_tensor(out=dA, in0=dt_tm, in1=aneg[:, gs], op=ALU.mult), reads=["s_dt", "aneg"], writes=["s_dA"])

                        def mm5(e):
                            e.matmul(banks[5][:, 8:16], lhsT=MASK, rhs=dA, start=True, stop=True)
                            e.matmul(banks[5][:, 16:24], lhsT=SUF, rhs=dA, start=True, stop=True)
                            e.matmul(banks[5][:, 160:168], lhsT=consts[:, C_CH0:C_CH0 + 128], rhs=dA, start=True, stop=True)
                            return e.matmul(banks[5][:, 168:176], lhsT=consts[:, C_CH1:C_CH1 + 128], rhs=dA, start=True, stop=True)
                        p.op("pe", mm5, reads=["s_dA", "consts"], writes=[PS(5)])
                        p.op("act", lambda e: e.copy(out=acs, in_=banks[5][:, 8:16]), reads=[PS(5)], writes=["s_acs"])
                        p.op("act", lambda e: e.activation(out=dte, in_=banks[5][:, 16:24], func=AF.Exp), reads=[PS(5)], writes=["s_dte"])
                        p.op("act", lambda e: e.copy(out=dAb, in_=dA.unsqueeze(2).to_broadcast([128, 8, 128])), reads=["s_dA"], writes=["s_dAb", ("s_L", 0), ("s_L", 1), "s_Lm", "s_Le"])
                        p.op("act", lambda e: e.activation(out=decbc, in_=banks[5][:, 160:176].rearrange("p (a b) -> p a b", a=2), func=AF.Exp), reads=[PS(5)], writes=[KP("s_dec")])
                        p.op("dve", lambda e: e.tensor_tensor(out=w2, in0=dt_tm, in1=dte, op=ALU.mult), reads=["s_dt", "s_dte"], writes=["s_w2"])

                        def mmD(e):
                            ins = None
                            for h in range(8):
                                ins = e.matmul(banks[2 + h // 4][:, (h % 4) * 128:(h % 4 + 1) * 128], lhsT=dAb[:, h, :], rhs=MASK, start=True, stop=True)
                            return ins
                        p.op("pe", mmD, reads=["s_dAb", "consts"], writes=[PS(2), PS(3)])
                        for k in range(2):
                            p.op("dve", lambda e, k=k: e.tensor_tensor(out=L[:, 4 * k:4 * k + 4, :], in0=banks[2 + k][:, :].rearrange("p (a b) -> p a b", a=4),
                                                                       in1=acs[:, 4 * k:4 * k + 4].unsqueeze(2).to_broadcast([128, 4, 128]), op=ALU.subtract),
                                 reads=[PS(2 + k), "s_acs"], writes=[("s_L", k)])
                            p.op("act", lambda e, k=k: e.activation(out=eD[:, 4 * k:4 * k + 4, :], in_=banks[2 + k][:, :].rearrange("p (a b) -> p a b", a=4), func=AF.Exp), reads=[PS(2 + k)], writes=[("s_eD", k)])
                        p.op("dve", lambda e: e.tensor_scalar_min(out=L, in0=L, scalar1=0.0), reads=[("s_L", 0), ("s_L", 1)], writes=["s_Lm"])
                        p.op("act", lambda e: e.activation(out=L, in_=L, func=AF.Exp), reads=["s_Lm"], writes=["s_Le"])
                        p.op("pe", lambda e, tsl=tsl: e.matmul(banks[5][:, 256:384], lhsT=BT[:, tsl], rhs=CT[:, tsl], start=True, stop=True), reads=[("s_fm", 4), ("s_fm", 5)], writes=[PS(5)])
                        p.op("dve", lambda e: e.tensor_tensor(out=cbm, in0=banks[5][:, 256:384], in1=MASK, op=ALU.mult), reads=[PS(5), "consts"], writes=["s_cbm"])
                        p.op("dve", lambda e: e.tensor_tensor(out=MT, in0=L, in1=cbm.unsqueeze(1).to_broadcast([128, 8, 128]), op=ALU.mult), reads=["s_Le", "s_cbm"], writes=[KP("s_MT")])
                        p.op("dve", lambda e, tsl=tsl: e.tensor_tensor(out=CTs, in0=eD, in1=CT[:, tsl].unsqueeze(1).to_broadcast([128, 8, 128]), op=ALU.mult), reads=[("s_eD", 0), ("s_eD", 1), ("s_fm", 5)], writes=[KP("s_CTs")])

                        def mmT(e, tsl=tsl):
                            for q in range(4):
                                e.transpose(b7[:, q * 128:(q + 1) * 128], xs[q][:, tsl], identb)
                            return e.transpose(b7[:, 512:640], BT[:, tsl], identb)
                        p.op("pe", mmT, reads=[("s_fm", j) for j in range(5)] + ["identb"], writes=[PS(7)])
                        xtm = b7[:, 0:512].rearrange("p (h k) -> p h k", h=8)
                        p.op("dve", lambda e: e.tensor_tensor(out=xdt, in0=xtm, in1=dt_tm.unsqueeze(2).to_broadcast([128, 8, 64]), op=ALU.mult), reads=[PS(7), "s_dt"], writes=[KP("s_xdt")])
                        p.op("dve", lambda e: e.tensor_tensor(out=xw, in0=xtm, in1=w2.unsqueeze(2).to_broadcast([128, 8, 64]), op=ALU.mult), reads=[PS(7), "s_w2"], writes=[KP("s_xw")])
                        for half in range(2):
                            p.op("act", lambda e, half=half: e.copy(out=Btmz[half][half * 64:(half + 1) * 64, :], in_=b7[half * 64:(half + 1) * 64, 512:640]), reads=[PS(7)], writes=[("s_Btmz", pa, half)])


                    def stageB(tt, pa, par):
                        MT, CTs, xdt, xw, Btmz, decbc = _aliases(pa)
                        KP = lambda n: (n, pa)
                        tsl = slice(tt * 128, (tt + 1) * 128)
                        def mmY(e):
                            ins = None
                            for h in range(8):
                                q, hq = h // 2, h % 2
                                ins = e.matmul(banks[4][hq * 64:(hq + 1) * 64, q * 128:(q + 1) * 128], lhsT=xdt[:, h, :], rhs=MT[:, h, :], start=True, stop=True)
                            return ins
                        p.op("pe", mmY, reads=[KP("s_xdt"), KP("s_MT")], writes=[PS(4)])
                        for half in range(2):
                            hs = slice(half * 64, (half + 1) * 64)

                            def mmO(e, half=half, par=par):
                                ins = None
                                for h in range(8):
                                    q, hq = h // 2, h % 2
                                    ins = e.matmul(banks[0][hq * 64:(hq + 1) * 64, q * 128 + half * 64:q * 128 + half * 64 + 64], lhsT=Sbf[par][:, h, :], rhs=CTs[:, h, half * 64:(half + 1) * 64], start=True, stop=True)
                                return ins
                            p.op("pe", mmO, reads=[("s_Sbf", par), KP("s_CTs")], writes=[("ps0o", half)] + ([PS(0)] if half == 0 else []))
                            p.op("pe", lambda e, half=half: e.matmul(banks[6][:, :], lhsT=Btmz[half], rhs=xw.rearrange("p h k -> p (h k)"), start=True, stop=True), reads=[("s_Btmz", pa, half), KP("s_xw")], writes=[PS(6)])
                            p.op("dve", lambda e, half=half: e.tensor_tensor(out=S32, in0=S32, in1=decbc[:, half, :].unsqueeze(2).to_broadcast([128, 8, 64]), op=ALU.mult), reads=["s_S32", KP("s_dec")], writes=["s_S32"])
                            p.op("dve", lambda e: e.tensor_tensor(out=S32, in0=S32, in1=banks[6][:, :].rearrange("p (h k) -> p h k", h=8), op=ALU.add), reads=["s_S32", PS(6)], writes=["s_S32"])
                            p.op("act", lambda e, par=par: e.copy(out=Sbf[1 - par], in_=S32), reads=["s_S32"], writes=[("s_Sbf", 1 - par)])
                            par = 1 - par
                        for q in range(4):
                            p.op("dve", lambda e, q=q, tsl=tsl, g=g: e.scalar_tensor_tensor(out=yg[:, q, tsl], in0=xs[q][:, tsl], scalar=PPc(l, PP_SD + g * 4 + q), in1=banks[4][:, q * 128:(q + 1) * 128], op0=ALU.mult, op1=ALU.add),
                                 reads=[("s_fm", q), PS(4), "pp"], writes=[("s_yg", q)])
                            p.op("dve", lambda e, q=q, tsl=tsl: e.tensor_tensor(out=yg[:, q, tsl], in0=yg[:, q, tsl], in1=banks[0][:, q * 128:(q + 1) * 128], op=ALU.add),
                                 reads=[("s_yg", q), PS(0), ("ps0o", 0), ("ps0o", 1)], writes=[("s_yg", q)])
                        return par

                    def capture(fn, *a):
                        lst = []
                        p.cap = lst
                        r = fn(*a)
                        p.cap = None
                        return lst, r

                    def replay(lst):
                        for (eng, fn, r, w) in lst:
                            p.op(eng, fn, reads=r, writes=w)

                    def merge(la, lb):
                        out = []
                        ia = ib = 0
                        na, nb = len(la), len(lb)
                        while ia < na or ib < nb:
                            if ib >= nb or (ia < na and ia * nb <= ib * na):
                                out.append(la[ia]); ia += 1
                            else:
                                out.append(lb[ib]); ib += 1
                        return out

                    lA, _ = capture(stageA, 0, pa_ctr[0] % 2)
                    replay(lA)
                    for tt in range(4):
                        pa = pa_ctr[0] % 2
                        lB, par = capture(stageB, tt, pa, par)
                        if tt + 1 < 4:
                            lA, _ = capture(stageA, tt + 1, (pa_ctr[0] + 1) % 2)
                            replay(merge(lA, lB))
                        else:
                            replay(lB)
                        pa_ctr[0] += 1
                    for q in range(4):
                        p.op("dve", lambda e, q=q: e.tensor_tensor(out=yg[:, q, :], in0=yg[:, q, :], in1=sz[q], op=ALU.mult), reads=[("s_yg", q), ("s_sz", q)], writes=[("s_yg", q)])
                    for q in range(4):
                        p.op("act", lambda e, q=q: e.activation(out=sqb, in_=yg[:, q, :], func=AF.Square), reads=[("s_yg", q)], writes=["s_sqb"])
                        p.op("pe", lambda e, q=q: e.matmul(banks[5][:, :], lhsT=ones512b, rhs=sqb, start=(q == 0), stop=(q == 3)), reads=["s_sqb", "ones512b"], writes=[PS(5)])
                    p.op("act", lambda e: e.activation(out=rs, in_=banks[5][:, :], func=AF.Ln, bias=LNEPS[:, 1:2]), reads=[PS(5), "lneps"], writes=[("s_ctmp", 0)])
                    p.op("act", lambda e: e.activation(out=rs, in_=rs, func=AF.Exp, scale=-0.5), reads=[("s_ctmp", 0)], writes=[("s_ctmp", 0)])
                    for q in range(4):
                        p.op("dve", lambda e, q=q, g=g: e.scalar_tensor_tensor(out=yblk[:, q, :], in0=yg[:, q, :], scalar=PPc(l, PP_SNORM + g * 4 + q), in1=rs, op0=ALU.mult, op1=ALU.mult),
                             reads=[("s_yg", q), ("s_ctmp", 0), "pp"], writes=[("yblk", q)])
                    out_proj(tb, 4, [0, 1])

        def mixer(l, ada_next=None):
            cv = Carve()
            wst = [cv.get([128, 8, 128]) for _ in range(2)]
            wunit = cv.get([128, 8, 1408], BF16)
            woutst = [cv.get([128, D])] * 2
            wout = cv.get([128, 4, D], BF16)
            yblk = cv.get([128, 4, TB], BF16)
            identb = cv.get([128, 128], BF16)
            ones128b = cv.get([128, 128], BF16)
            ones512b = cv.get([128, 128], BF16)
            p.op("act", lambda e: e.copy(out=identb, in_=ident), reads=["consts"], writes=["identb"])
            p.op("pool", lambda e: e.memset(ones128b, 1.0 / 128.0), writes=["ones128b"])
            p.op("pool", lambda e: e.memset(ones512b, 1.0 / 512.0), writes=["ones512b"])
            wcnt = [0]

            def load_win(col0, ncols, dst):
                c = 0
                while c < ncols:
                    n = min(128, ncols - c)
                    k = wcnt[0] % 2
                    wcnt[0] += 1
                    p.dma(("wst", k), wst[k][:, :, 0:n], win_d[l].rearrange("(kc p) f -> p kc f", p=128)[:, :, col0 + c:col0 + c + n], writes=[("wst", k)])
                    p.op("pool", lambda e, k=k, n=n, c=c: e.tensor_copy(out=wunit[:, :, dst + c:dst + c + n], in_=wst[k][:, :, 0:n]), reads=[("wst", k)], writes=[("wunit", (dst + c) // 128)])
                    c += n

            def load_wout(ych0, n):
                for j in range(n):
                    k = 0
                    p.dma(("wost", k), woutst[k], wout_d[l, (ych0 + j) * 128:(ych0 + j + 1) * 128, :], writes=[("wost", k)])
                    p.op("pool", lambda e, k=k, j=j: e.tensor_copy(out=wout[:, j, :], in_=woutst[k]), reads=[("wost", k)], writes=[("wout", j)])

            def proj_fm(off, ncols, tb, bank):
                def mm(e):
                    ins = None
                    for kc in range(8):
                        ins = e.matmul(banks[bank][0:ncols, :], lhsT=wunit[:, kc, off:off + ncols], rhs=hT[:, kc, tb * TB:(tb + 1) * TB], start=(kc == 0), stop=(kc == 7))
                    return ins
                p.op("pe", mm, reads=[("wunit", b) for b in range(off // 128, (off + ncols - 1) // 128 + 1)] + [("h", kc, tb) for kc in range(8)], writes=[PS(bank)])

            def proj_tm(off, ncols, tb, tt, bank, col0=0):
                t0 = tb * TB + tt * 128

                def mm(e):
                    ins = None
                    for kc in range(8):
                        ins = e.matmul(banks[bank][:, col0:col0 + ncols], lhsT=hT[:, kc, t0:t0 + 128], rhs=wunit[:, kc, off:off + ncols], start=(kc == 0), stop=(kc == 7))
                    return ins
                p.op("pe", mm, reads=[("wunit", b) for b in range(off // 128, (off + ncols - 1) // 128 + 1)] + [("h", kc, tb) for kc in range(8)], writes=[PS(bank)])

            def out_proj(tb, nych, bks):
                for d in range(8):
                    bk = bks[d % len(bks)]

                    def mm(e, d=d, bk=bk):
                        ins = None
                        for j in range(nych):
                            ins = e.matmul(banks[bk][:, :], lhsT=wout[:, j, d * 128:(d + 1) * 128], rhs=yblk[:, j, :], start=(j == 0), stop=(j == nych - 1))
                        return ins
                    p.op("pe", mm, reads=[("wout", j) for j in range(nych)] + [("yblk", j) for j in range(nych)], writes=[PS(bk)])
                    p.op("dve", lambda e, d=d, bk=bk: e.scalar_tensor_tensor(out=xT[:, d, tb * TB:(tb + 1) * TB], in0=banks[bk][:, :], scalar=MOD(l, 2, d), in1=xT[:, d, tb * TB:(tb + 1) * TB], op0=ALU.mult, op1=ALU.add),
                         reads=[PS(bk), ("x", d, tb)] + modkeys(l), writes=[("x", d, tb)])

            def conv_silu(buf, key, tb, bank, wcol, bcol, tmp, tmpkey, dst, dstkey, act_func, eng="dve"):
                p.op("act", lambda e: e.copy(out=buf[:, 3:3 + TB], in_=banks[bank][:, :]), reads=[PS(bank)], writes=[key])
                p.op(eng, lambda e: e.tensor_scalar(out=tmp, in0=buf[:, 0:TB], scalar1=PPc(l, wcol), scalar2=PPc(l, bcol), op0=ALU.mult, op1=ALU.add), reads=[key, "pp"], writes=[tmpkey])
                for k in range(1, 4):
                    p.op(eng, lambda e, k=k: e.scalar_tensor_tensor(out=tmp, in0=buf[:, k:k + TB], scalar=PPc(l, wcol + k), in1=tmp, op0=ALU.mult, op1=ALU.add), reads=[key, tmpkey, "pp"], writes=[tmpkey])
                p.op("pool", lambda e: e.tensor_copy(out=buf[:, 0:3], in_=buf[:, TB:TB + 3]), reads=[key], writes=[key])
                if dst is not None:
                    p.op("act", lambda e: e.activation(out=dst, in_=tmp, func=act_func), reads=[tmpkey], writes=[dstkey])

            mark = cv.off

            if do_lru:
                cv.off = mark
                wabd = cv.get([128, 4, 128], BF16)
                wxbd = cv.get([128, 4, 128], BF16)
                nsp8 = cv.get([128, 4])
                hcar = cv.get([128, 4])
                xbuf = [cv.get([128, 3 + TB]) for _ in range(4)]
                Ts = [{n: cv.get([128, TB]) for n in ("xc", "r", "i", "om", "h", "gg")} for _ in range(4)]
                xcbs = [cv.get([128, TB], BF16) for _ in range(4)]
                for nm, src, dst in (("wa", wabd_d, wabd), ("wx", wxbd_d, wxbd)):
                    p.dma(("wost", 0), woutst[0][:, 0:512].rearrange("p (m j) -> p m j", m=4), src[l].rearrange("m i j -> i m j"), writes=[("wost", 0)])
                    p.op("pool", lambda e, dst=dst: e.tensor_copy(out=dst, in_=woutst[0][:, 0:512].rearrange("p (m j) -> p m j", m=4)), reads=[("wost", 0)], writes=[nm])
                p.op("act", lambda e: e.activation(out=nsp8, in_=pp[:, l, PP_LLAM:PP_LLAM + 4], func=AF.Exp, scale=-1.0), reads=["pp"], writes=["nsp8"])
                p.op("act", lambda e: e.activation(out=nsp8, in_=nsp8, func=AF.Ln, bias=LNEPS[:, 2:3]), reads=["nsp8", "lneps"], writes=["nsp8"])
                p.op("dve", lambda e: e.tensor_scalar_mul(out=nsp8, in0=nsp8, scalar1=-8.0), reads=["nsp8"], writes=["nsp8"])
                for m in range(4):
                    p.op("pool", lambda e, m=m: e.memset(xbuf[m][:, 0:3], 0.0), writes=[("xbuf", m)])
                load_win(0, 1024, 0)
                load_wout(0, 4)

                def lru_chain(tb, m):
                    T = Ts[m]
                    xcb = xcbs[m]
                    BA, BB = 2 * m, 2 * m + 1
                    KK = lambda n: (n, m)
                    ops = []
                    A = lambda eng, fn, r, w: ops.append((eng, fn, r, w))
                    buf = xbuf[m]
                    key = ("xbuf", m)
                    wcol, bcol = PP_LCW + m * 4, PP_LCB + m
                    sl_h = [("h", kc, tb) for kc in range(8)]

                    def mmx(e):
                        ins = None
                        for kc in range(8):
                            ins = e.matmul(banks[BA][:, :], lhsT=wunit[:, kc, m * 128:(m + 1) * 128], rhs=hT[:, kc, tb * TB:(tb + 1) * TB], start=(kc == 0), stop=(kc == 7))
                        return ins

                    def mmg(e):
                        ins = None
                        for kc in range(8):
                            ins = e.matmul(banks[BB][:, :], lhsT=wunit[:, kc, 512 + m * 128:512 + (m + 1) * 128], rhs=hT[:, kc, tb * TB:(tb + 1) * TB], start=(kc == 0), stop=(kc == 7))
                        return ins
                    A("pe", mmx, [("wunit", m)] + sl_h, [PS(BA)])
                    A("pe", mmg, [("wunit", 4 + m)] + sl_h, [PS(BB)])
                    A("act", lambda e: e.copy(out=buf[:, 3:3 + TB], in_=banks[BA][:, :]), [PS(BA)], [key])
                    A("act", lambda e: e.activation(out=T["gg"], in_=banks[BB][:, :], func=AF.Gelu_apprx_tanh), [PS(BB)], [KK("l_gg")])
                    A("dve", lambda e: e.tensor_scalar(out=T["xc"], in0=buf[:, 0:TB], scalar1=PPc(l, wcol), scalar2=PPc(l, bcol), op0=ALU.mult, op1=ALU.add), [key, "pp"], [KK("l_xc")])
                    for k in range(1, 4):
                        A("dve", lambda e, k=k: e.scalar_tensor_tensor(out=T["xc"], in0=buf[:, k:k + TB], scalar=PPc(l, wcol + k), in1=T["xc"], op0=ALU.mult, op1=ALU.add), [key, KK("l_xc"), "pp"], [KK("l_xc")])
                    A("pool", lambda e: e.tensor_copy(out=buf[:, 0:3], in_=buf[:, TB:TB + 3]), [key], [key])
                    A("act", lambda e: e.copy(out=xcb, in_=T["xc"]), [KK("l_xc")], [KK("l_xcb")])
                    A("pe", lambda e: e.matmul(banks[BA][:, :], lhsT=wabd[:, m, :], rhs=xcb, start=True, stop=True), ["wa", KK("l_xcb")], [PS(BA)])
                    A("pe", lambda e: e.matmul(banks[BB][:, :], lhsT=wxbd[:, m, :], rhs=xcb, start=True, stop=True), ["wx", KK("l_xcb")], [PS(BB)])
                    A("act", lambda e: e.activation(out=T["r"], in_=banks[BA][:, :], func=AF.Sigmoid, bias=PPc(l, PP_LBA + m)), [PS(BA), "pp"], [KK("l_r")])
                    A("act", lambda e: e.activation(out=T["i"], in_=banks[BB][:, :], func=AF.Sigmoid, bias=PPc(l, PP_LBX + m)), [PS(BB), "pp"], [KK("l_i")])
                    A("act", lambda e: e.activation(out=T["r"], in_=T["r"], func=AF.Exp, scale=nsp8[:, m:m + 1]), [KK("l_r"), "nsp8"], [KK("l_r")])
                    A("pool", lambda e: e.tensor_tensor(out=T["om"], in0=T["r"], in1=T["r"], op=ALU.mult), [KK("l_r")], [KK("l_om")])
                    A("pool", lambda e: e.tensor_scalar(out=T["om"], in0=T["om"], scalar1=-1.0, scalar2=1.0, op0=ALU.mult, op1=ALU.add), [KK("l_om")], [KK("l_om")])
                    A("act", lambda e: e.activation(out=T["om"], in_=T["om"], func=AF.Sqrt), [KK("l_om")], [KK("l_om")])
                    A("pool", lambda e: e.tensor_tensor(out=T["i"], in0=T["i"], in1=T["xc"], op=ALU.mult), [KK("l_i"), KK("l_xc")], [KK("l_i")])
                    A("dve", lambda e: e.tensor_tensor(out=T["om"], in0=T["om"], in1=T["i"], op=ALU.mult), [KK("l_om"), KK("l_i")], [KK("l_om")])
                    if tb == 0:
                        A("dve", lambda e: e.tensor_tensor_scan(out=T["h"], data0=T["r"], data1=T["om"], initial=0.0, op0=ALU.mult, op1=ALU.add), [KK("l_r"), KK("l_om")], [KK("l_h")])
                    else:
                        A("dve", lambda e: e.tensor_tensor_scan(out=T["h"], data0=T["r"], data1=T["om"], initial=hcar[:, m:m + 1], op0=ALU.mult, op1=ALU.add), [KK("l_r"), KK("l_om"), ("hcar", m)], [KK("l_h")])
                    A("pool", lambda e: e.tensor_copy(out=hcar[:, m:m + 1], in_=T["h"][:, TB - 1:TB]), [KK("l_h")], [("hcar", m)])
                    A("dve", lambda e: e.tensor_tensor(out=yblk[:, m, :], in0=T["h"], in1=T["gg"], op=ALU.mult), [KK("l_h"), KK("l_gg")], [("yblk", m)])
                    return ops

                for tb in range(NTB):
                    lists = [lru_chain(tb, m) for m in range(4)]
                    for k in range(len(lists[0])):
                        for ol in lists:
                            eng, fn, r, w = ol[k]
                            p.op(eng, fn, reads=r, writes=w)
                    out_proj(tb, 4, [0, 1, 2, 3, 4, 5, 6, 7])

            if do_gla:
                cv.off = mark
                walb = cv.get([32, 256], BF16)
                rT = cv.get([32, TB], BF16)
                e1 = cv.get([128, 128])
                l1 = cv.get([128, 128])
                ecp = cv.get([128, TB])
                ecn = cv.get([128, TB])
                esuf = [cv.get([128, 128]) for _ in range(4)]
                qd = cv.get([128, TB], BF16)
                kd = cv.get([128, TB], BF16)
                kend = [cv.get([128, 128], BF16) for _ in range(4)]
                kendz = [[cv.get([128, 128], BF16) for _ in range(2)] for _ in range(4)]
                qdz = [cv.get([128, TB], BF16) for _ in range(2)]
                CHM = [consts[:, C_CH0:C_CH0 + 128], consts[:, C_CH1:C_CH1 + 128]]
                vtm = [cv.get([128, 256], BF16) for _ in range(4)]
                sg = [cv.get([128, TB]) for _ in range(2)]
                attms = [cv.get([128, 4, 128], BF16) for _ in range(2)]
                S32 = cv.get([128, 128])
                Sbfs = [[cv.get([128, 128], BF16) for _ in range(2)] for _ in range(2)]
                sqbs = [cv.get([128, TB], BF16) for _ in range(2)]
                rss = [cv.get([128, TB]) for _ in range(2)]
                t1s = [cv.get([128, TB]) for _ in range(2)]
                ada_bufs_m = ada_bufs_alloc(cv) if ada_next is not None else None
                p.dma(("wost", 0), woutst[0][0:32, 0:256], walpha_d[l], writes=[("wost", 0)])
                p.op("pool", lambda e: e.tensor_copy(out=walb, in_=woutst[0][0:32, 0:256]), reads=[("wost", 0)], writes=["walb"])
                p.op("pool", lambda e: e.memset(rT, 1.0), writes=["rT"])
                m64b = consts[:, C_MASK:C_MASK + 128].unsqueeze(1).to_broadcast([128, 4, 128])
                for hp in range(2):
                    load_win(1024 + hp * 128, 128, 0)
                    load_win(1280 + hp * 128, 128, 128)
                    load_win(1536 + hp * 256, 256, 256)
                    load_win(2048 + hp * 256, 256, 512)
                    load_win(2560, 16, 768)
                    load_wout(4 + hp * 2, 2)
                    p.op("pool", lambda e: e.memset(S32, 0.0), writes=[("S32", 0), ("S32", 1)])
                    for hh in range(2):
                        for pr in range(2):
                            p.op("pool", lambda e, hh=hh, pr=pr: e.memset(Sbfs[hh][pr], 0.0), writes=[("Sbf", hh, pr)])
                    if hp == 0:
                        for hh in range(2):
                            p.op("pool", lambda e, hh=hh: e.memset(qdz[hh], 0.0), writes=[("g_qdz", hh)])
                        for tt in range(4):
                            for half in range(2):
                                p.op("pool", lambda e, tt=tt, half=half: e.memset(kendz[tt][half], 0.0), writes=[("g_kendz", tt, half)])
                    par_state = {0: 0, 1: 0}
                    for tb in range(NTB):
                        proj_fm(768, 16, tb, 0)
                        p.op("act", lambda e: e.copy(out=rT[0:16, :], in_=banks[0][0:16, :]), reads=[PS(0)], writes=["rT"])
                        for tt in range(4):
                            tsl = slice(tt * 128, (tt + 1) * 128)
                            p.op("pe", lambda e, tsl=tsl, hp=hp: e.matmul(banks[5][:, 0:128], lhsT=rT[:, tsl], rhs=walb[:, hp * 128:(hp + 1) * 128], start=True, stop=True), reads=["rT", "walb"], writes=[PS(5)])
                            p.op("act", lambda e: e.activation(out=e1, in_=banks[5][:, 0:128], func=AF.Exp, scale=-1.0), reads=[PS(5)], writes=["g_e1"])
                            p.op("act", lambda e: e.activation(out=l1, in_=e1, func=AF.Ln, bias=LNEPS[:, 2:3]), reads=["g_e1", "lneps"], writes=["g_l1"])
                            p.op("pe", lambda e: e.matmul(banks[5][:, 128:256], lhsT=l1, rhs=consts[:, C_MASKG:C_MASKG + 128], start=True, stop=True), reads=["g_l1", "consts"], writes=[PS(5)])
                            p.op("pe", lambda e: e.matmul(banks[5][:, 256:384], lhsT=consts[:, C_MASKG + 128:C_MASKG + 256], rhs=l1, start=True, stop=True), reads=["g_l1", "consts"], writes=[PS(5)])
                            p.op("act", lambda e, tsl=tsl: e.activation(out=ecp[:, tsl], in_=banks[5][:, 128:256], func=AF.Exp), reads=[PS(5)], writes=["g_ecp"])
                            p.op("act", lambda e, tsl=tsl: e.activation(out=ecn[:, tsl], in_=banks[5][:, 128:256], func=AF.Exp, scale=-1.0), reads=[PS(5)], writes=["g_ecn"])
                            p.op("act", lambda e, tt=tt: e.activation(out=esuf[tt], in_=banks[5][:, 256:384], func=AF.Exp), reads=[PS(5)], writes=[("g_esuf", tt)])
                        proj_fm(0, 128, tb, 0)
                        for hh in range(2):
                            p.op("dve", lambda e, hh=hh: e.scalar_tensor_tensor(out=qdz[hh][hh * 64:(hh + 1) * 64, :], in0=banks[0][hh * 64:(hh + 1) * 64, :], scalar=0.125, in1=ecp[hh * 64:(hh + 1) * 64, :], op0=ALU.mult, op1=ALU.mult),
                                 reads=[PS(0), "g_ecp"], writes=[("g_qdz", hh)])
                        proj_fm(128, 128, tb, 1)
                        p.op("dve", lambda e: e.tensor_tensor(out=kd, in0=banks[1][:, :], in1=ecn, op=ALU.mult), reads=[PS(1), "g_ecn"], writes=["g_kd"])
                        for tt in range(4):
                            proj_tm(128, 128, tb, tt, 0)
                            for half in range(2):
                                p.op("dve", lambda e, tt=tt, half=half: e.tensor_tensor(out=kendz[tt][half][half * 64:(half + 1) * 64, :], in0=banks[0][half * 64:(half + 1) * 64, 0:128], in1=esuf[tt][half * 64:(half + 1) * 64, :], op=ALU.mult),
                                     reads=[PS(0), ("g_esuf", tt)], writes=[("g_kendz", tt, half)])
                            proj_tm(256, 256, tb, tt, 1)
                            p.op("act", lambda e, tt=tt: e.copy(out=vtm[tt], in_=banks[1][:, 0:256]), reads=[PS(1)], writes=[("g_vtm", tt)])
                        for hh in range(2):
                            proj_fm(512 + hh * 128, 128, tb, hh)
                            p.op("act", lambda e, hh=hh: e.activation(out=sg[hh], in_=banks[hh][:, :], func=AF.Silu), reads=[PS(hh)], writes=[("g_sg", hh)])
                        def head_ops(tb, hh):
                            b0 = hh * 64
                            BATT, BO, BKV = (2, 5)[hh], (3, 6)[hh], (4, 7)[hh]
                            attm, sqb, rs, t1 = attms[hh], sqbs[hh], rss[hh], t1s[hh]
                            KH = lambda n: (n, hh)

                            def att(e):
                                ins = None
                                for tt in range(4):
                                    tsl = slice(tt * 128, (tt + 1) * 128)
                                    ins = e.matmul(banks[BATT][:, tsl], lhsT=kd[:, tsl], rhs=qdz[hh][:, tsl], start=True, stop=True)
                                return ins
                            p.op("pe", att, reads=["g_kd", ("g_qdz", hh)], writes=[PS(BATT)])
                            p.op("dve", lambda e: e.tensor_tensor(out=attm, in0=banks[BATT][:, :].rearrange("p (a b) -> p a b", a=4), in1=m64b, op=ALU.mult), reads=[PS(BATT), "consts"], writes=[KH("g_attm")])
                            for c in range(8):
                                tt, half = c // 2, c % 2
                                csl = slice(c * 64, (c + 1) * 64)
                                tsl = slice(tt * 128, (tt + 1) * 128)
                                cur_par = par_state[hh]
                                if half == 0:
                                    p.op("pe", lambda e, tt=tt, tsl=tsl: e.matmul(banks[BO][:, tsl], lhsT=vtm[tt][:, hh * 128:(hh + 1) * 128], rhs=attm[:, tt, :], start=True, stop=False),
                                         reads=[("g_vtm", tt), KH("g_attm")], writes=[PS(BO)])
                                p.op("pe", lambda e, csl=csl, cur_par=cur_par, half=half: e.matmul(banks[BO][:, csl], lhsT=Sbfs[hh][cur_par], rhs=qdz[hh][:, csl], start=False, stop=(half == 1)),
                                     reads=[("Sbf", hh, cur_par), ("g_qdz", hh)], writes=[PS(BO)])
                                p.op("pe", lambda e, tt=tt, half=half: e.matmul(banks[BKV][:, 0:128], lhsT=kendz[tt][half], rhs=vtm[tt][:, hh * 128:(hh + 1) * 128], start=True, stop=True),
                                     reads=[("g_kendz", tt, half), ("g_vtm", tt)], writes=[PS(BKV)])
                                col = c * 64 + 63
                                p.op("dve", lambda e, col=col: e.scalar_tensor_tensor(out=S32[b0:b0 + 64, :], in0=S32[b0:b0 + 64, :], scalar=ecp[b0:b0 + 64, col:col + 1], in1=banks[BKV][b0:b0 + 64, 0:128], op0=ALU.mult, op1=ALU.add),
                                     reads=[("S32", hh), "g_ecp", PS(BKV)], writes=[("S32", hh)])
                                nxt = 1 - cur_par
                                p.op("act", lambda e, nxt=nxt: e.copy(out=Sbfs[hh][nxt][b0:b0 + 64, :], in_=S32[b0:b0 + 64, :]), reads=[("S32", hh)], writes=[("Sbf", hh, nxt)])
                                par_state[hh] = nxt
                            p.op("act", lambda e: e.activation(out=sqb, in_=banks[BO][:, :], func=AF.Square), reads=[PS(BO)], writes=[KH("g_sqb")])
                            p.op("pe", lambda e: e.matmul(banks[BATT][:, :], lhsT=ones128b, rhs=sqb, start=True, stop=True), reads=["ones128b", KH("g_sqb")], writes=[PS(BATT)])
                            p.op("act", lambda e: e.activation(out=rs, in_=banks[BATT][:, :], func=AF.Ln, bias=LNEPS[:, 1:2]), reads=[PS(BATT), "lneps"], writes=[KH("g_rs")])
                            p.op("act", lambda e: e.activation(out=rs, in_=rs, func=AF.Exp, scale=-0.5), reads=[KH("g_rs")], writes=[KH("g_rs")])
                            p.op("dve", lambda e: e.tensor_tensor(out=t1, in0=banks[BO][:, :], in1=rs, op=ALU.mult), reads=[PS(BO), KH("g_rs")], writes=[KH("g_t1")])
                            p.op("dve", lambda e: e.scalar_tensor_tensor(out=yblk[:, hh, :], in0=t1, scalar=PPc(l, PP_GNORM), in1=sg[hh], op0=ALU.mult, op1=ALU.mult), reads=[KH("g_t1"), ("g_sg", hh), "pp"], writes=[("yblk", hh)])

                        replay_ops(interleave([cap_ops(head_ops, tb, 0), cap_ops(head_ops, tb, 1)]))
                        if ada_next is not None:
                            slot = hp * NTB + tb
                            for blk in range(slot * 3, slot * 3 + 3):
                                adaln_block(ada_next, blk, ada_bufs_m, 2 + (blk % 2) * 3)
                            if slot == 2 * NTB - 1:
                                adaln_finish(ada_next, 0)
                        out_proj(tb, 2, [0, 1])

            if do_ssd:
                ssd_units(l, cv, mark, load_win, load_wout, proj_fm, proj_tm, out_proj, conv_silu, yblk, identb, ones512b)

        cv = Carve()
        ada_bufs = ada_bufs_alloc(cv)
        for blk in range(N_ADA):
            adaln_block(0, blk, ada_bufs, blk % 4)
        adaln_finish(0, 1)

        for tb in range(NTB):
            scale_alpha(tb, engs=("pool", "dve", "act"))
        for l in range(depth):
            p.new_epoch()
            for tb in range(NTB):
                modulate(l, 1, tb)
            barrier()
            ada_in_mixer = do_gla and (l + 1 < depth)
            if do_lru or do_gla or do_ssd:
                mixer(l, ada_next=(l + 1 if ada_in_mixer else None))
            barrier()
            cv = Carve()
            if do_moe:
                rgroup, rfinish = router(l, cv, 2, 3)

                def after_tb(tb):
                    modulate(l, 2, tb)
                    rgroup(tb)
                layernorm(l, PP_LN1G, PP_LN1B, cv, 0, 1, after_tb=after_tb)
                rfinish()
            else:
                layernorm(l, PP_LN1G, PP_LN1B, cv, 0, 1, after_tb=lambda tb: modulate(l, 2, tb))
            barrier()
            cv = Carve()
            hooks = {}
            if l + 1 < depth and not ada_in_mixer:
                ada_bufs2 = ada_bufs_alloc(cv)
                for blk in range(N_ADA):
                    hooks[2 + blk] = (lambda blk=blk: adaln_block(l + 1, blk, ada_bufs2, 4 + blk % 4))
                hooks[2 + N_ADA] = (lambda: adaln_finish(l + 1, 4))
            if do_moe:
                moe(l, cv, hooks)
            else:
                for k in sorted(hooks):
                    hooks[k]()
            barrier()
            cv = Carve()
            if l == depth - 1:
                yv = yT_d.rearrange("(c p) t -> p c t", p=128)

                def out_tb(tb):
                    p.dma(("out", tb), yv[:, :, tb * TB:(tb + 1) * TB], xT[:, :, tb * TB:(tb + 1) * TB], reads=[("x", c, tb) for c in range(8)])
                layernorm(l, PP_LN2G, PP_LN2B, cv, 0, 1, final=True, after_tb=out_tb)
            else:
                layernorm(l, PP_LN2G, PP_LN2B, cv, 0, 1)
            barrier()

        p.emit()
    return nc


def prep_inputs(inputs):
    f = lambda a: np.ascontiguousarray(np.asarray(a, dtype=np.float32))
    L = DEPTH
    shared = {}
    shared["consts"] = build_consts()
    pc = lambda v, n: np.asarray(v, np.float32).reshape(n, 128).T
    pp = np.zeros((L, 128, NPP), np.float32)
    prow = np.zeros((L, NPR), np.float32)
    wabd = np.zeros((L, 4, 128, 128), np.float32)
    wxbd = np.zeros((L, 4, 128, 128), np.float32)
    wal = np.zeros((L, 32, 256), np.float32)
    for l in range(L):
        pp[l, :, PP_LN1G:PP_LN1G + 8] = pc(inputs["ln1_g"][l], 8)
        pp[l, :, PP_LN1B:PP_LN1B + 8] = pc(inputs["ln1_b"][l], 8)
        pp[l, :, PP_LN2G:PP_LN2G + 8] = pc(inputs["ln2_g"][l], 8)
        pp[l, :, PP_LN2B:PP_LN2B + 8] = pc(inputs["ln2_b"][l], 8)
        for m in range(4):
            for k in range(4):
                pp[l, :, PP_LCW + m * 4 + k] = inputs["lru_conv_w"][l, k, m * 128:(m + 1) * 128]
        pp[l, :, PP_LCB:PP_LCB + 4] = pc(inputs["lru_conv_b"][l], 4)
        pp[l, :, PP_LBA:PP_LBA + 4] = pc(inputs["lru_b_a"][l], 4)
        pp[l, :, PP_LBX:PP_LBX + 4] = pc(inputs["lru_b_x"][l], 4)
        pp[l, :, PP_LLAM:PP_LLAM + 4] = pc(inputs["lru_lambda"][l], 4)
        for j in range(12):
            for k in range(4):
                pp[l, :, PP_SCW + j * 4 + k] = inputs["ssd_conv_w"][l, k, j * 128:(j + 1) * 128]
        pp[l, :, PP_SCB:PP_SCB + 12] = pc(inputs["ssd_conv_b"][l], 12)
        pp[l, :, PP_SNORM:PP_SNORM + 8] = pc(inputs["ssd_norm"][l], 8)
        pp[l, :, PP_GNORM] = inputs["gla_norm"][l]
        pp[l, :, PP_SD:PP_SD + 8] = pc(np.repeat(np.asarray(inputs["ssd_d"][l]), 64), 8)
        prow[l, PR_DTB:PR_DTB + 16] = inputs["ssd_dt_bias"][l]
        prow[l, PR_ALOG:PR_ALOG + 16] = inputs["ssd_a_log"][l]
        prow[l, PR_RB:PR_RB + NE] = inputs["router_bias"][l]
        for m in range(4):
            for q in range(2):
                wabd[l, m, q * 64:(q + 1) * 64, q * 64:(q + 1) * 64] = inputs["lru_w_a"][l, 2 * m + q]
                wxbd[l, m, q * 64:(q + 1) * 64, q * 64:(q + 1) * 64] = inputs["lru_w_x"][l, 2 * m + q]
        wal[l, 0:16] = inputs["gla_w_alpha"][l]
        wal[l, 16] = inputs["gla_b_alpha"][l]
    shared.update(pp=pp, prow=prow, lru_wa_bd=wabd, lru_wx_bd=wxbd, walpha_ext=wal)
    for k in ("w_ada", "b_ada", "w_in", "w_out", "router_w", "exp_w1", "exp_w3", "exp_w2", "shared_w1", "shared_w3", "shared_w2"):
        shared[k] = f(inputs[k])
    x = np.asarray(inputs["x"], np.float32)
    c = np.asarray(inputs["c"], np.float32)
    maps = []
    for b in range(x.shape[0]):
        m = dict(shared)
        m["xT"] = np.ascontiguousarray(x[b].T)
        m["cpc"] = np.ascontiguousarray(c[b].reshape(8, 128).T)
        maps.append(m)
    return maps


_NC_CACHE = {}


def kernel(**inputs):
    maps = prep_inputs(inputs)
    if "nc" not in _NC_CACHE:
        _NC_CACHE["nc"] = build_program()
    nc = _NC_CACHE["nc"]
    res = run_bass_kernel_spmd(nc, maps, core_ids=list(range(len(maps))))
    out = np.stack([np.ascontiguousarray(r["yT"].T) for r in res.results], axis=0)
    return out.astype(np.float32)
```
